# Optimizing a Trainium2 kernel written in Bass

```python
import jax, jax.numpy as jnp
from jax import lax
import numpy as np

D_MODEL = 1024
BATCH = 4
SEQ = 4096
DEPTH = 1

D_MIX = D_MODEL
ATTN_WIDTH = D_MIX // 2
HGRN_WIDTH = D_MIX - ATTN_WIDTH
ATTN_HEAD_DIM = 64
ATTN_HEADS = ATTN_WIDTH // ATTN_HEAD_DIM
DILATED_BRANCHES = ((128, 1), (512, 4), (2048, 16))
ROPE_THETA = 500000.0
ROPE_DIM = ATTN_HEAD_DIM // 4
HGRN_EXPAND = 128
HGRN_HEADS = HGRN_WIDTH // HGRN_EXPAND
HGRN_CHUNK = 64
IN_COLS = 3 * ATTN_WIDTH + 4 * HGRN_WIDTH
N_EXPERTS = 32
TOP_K = 4
D_EXPERT = D_MODEL
SWIGLU_LIMIT = 7.0
SWIGLU_ALPHA = 1.702
PLE_DIM = 256
RMS_EPS = 1e-6

kernel_name = "hymba_dilated_hgrn2_moe_ple"


def rms_norm(x, g):
    xf = x.astype(jnp.float32)
    y = xf * lax.rsqrt(jnp.mean(xf * xf, axis=-1, keepdims=True) + RMS_EPS)
    return (y * g.astype(jnp.float32)).astype(x.dtype)


def partial_rope(x, positions):
    half = ROPE_DIM // 2
    inv_freq = ROPE_THETA ** (-jnp.arange(half, dtype=jnp.float32) / half)
    ang = positions.astype(jnp.float32)[..., None] * inv_freq
    cos = jnp.cos(ang)[:, :, None, :]
    sin = jnp.sin(ang)[:, :, None, :]
    xr = x[..., :ROPE_DIM].astype(jnp.float32)
    x1, x2 = xr[..., :half], xr[..., half:]
    rot = jnp.concatenate([x1 * cos - x2 * sin, x2 * cos + x1 * sin], axis=-1).astype(x.dtype)
    return jnp.concatenate([rot, x[..., ROPE_DIM:]], axis=-1)


def dilated_branch(q, k, v, window, dilation):
    B, S, H, hd = q.shape
    L = S // dilation
    W = window // dilation
    nblk = -(-L // W)
    Lp = nblk * W

    def to_blocks(t):
        t = t.reshape(B, L, dilation, H, hd).transpose(0, 2, 3, 1, 4)
        t = jnp.pad(t, ((0, 0), (0, 0), (0, 0), (0, Lp - L), (0, 0)))
        return t.reshape(B, dilation, H, nblk, W, hd)

    qb, kb, vb = to_blocks(q), to_blocks(k), to_blocks(v)
    pad_prev = ((0, 0), (0, 0), (0, 0), (1, 0), (0, 0), (0, 0))
    kk = jnp.concatenate([jnp.pad(kb, pad_prev)[:, :, :, :-1], kb], axis=4)
    vv = jnp.concatenate([jnp.pad(vb, pad_prev)[:, :, :, :-1], vb], axis=4)
    scores = jnp.einsum('bdhnqc,bdhnkc->bdhnqk', qb, kk).astype(jnp.float32)
    blk = jnp.arange(nblk)[:, None] * W
    qpos = blk + jnp.arange(W)[None, :]
    kpos = blk - W + jnp.arange(2 * W)[None, :]
    rel = qpos[:, :, None] - kpos[:, None, :]
    mask = (rel >= 0) & (rel <= W) & (kpos[:, None, :] >= 0)
    scores = jnp.where(mask, scores, -jnp.inf)
    lse = jax.nn.logsumexp(scores, axis=-1)
    probs = jnp.exp(scores - lse[..., None]).astype(v.dtype)
    out = jnp.einsum('bdhnqk,bdhnkc->bdhnqc', probs, vv)
    out = out.reshape(B, dilation, H, Lp, hd)[:, :, :, :L]
    out = out.transpose(0, 3, 1, 2, 4).reshape(B, S, H, hd)
    lse = lse.reshape(B, dilation, H, Lp)[:, :, :, :L].transpose(0, 3, 1, 2).reshape(B, S, H)
    return out, lse


def dilated_attention(q, k, v):
    outs, lses = [], []
    for window, dilation in DILATED_BRANCHES:
        o, l = dilated_branch(q, k, v, window, dilation)
        outs.append(o)
        lses.append(l)
    w = jax.nn.softmax(jnp.stack(lses, axis=0), axis=0)
    out = jnp.sum(w[..., None] * jnp.stack(outs, axis=0).astype(jnp.float32), axis=0)
    return out.astype(q.dtype)


def hgrn2_mixer(q_raw, f_raw, i_raw, g_raw, lower_bound, norm_g):
    B, S, _ = q_raw.shape
    H, E, C = HGRN_HEADS, HGRN_EXPAND, HGRN_CHUNK
    N = S // C
    f32 = jnp.float32
    q = jax.nn.silu(q_raw.astype(f32))
    lb = lower_bound.astype(f32)
    f = lb + (1.0 - lb) * jax.nn.sigmoid(f_raw.astype(f32))
    k = 1.0 - f
    log_f = jnp.log(f)
    v = i_raw.astype(f32)

    def to_chunks(t):
        return t.reshape(B, N, C, H, E).transpose(1, 0, 3, 2, 4)

    causal = jnp.tril(jnp.ones((C, C), dtype=bool))[:, :, None]

    def chunk_step(state, inp):
        qc, kc, vc, gc = inp
        b = jnp.cumsum(gc, axis=2)
        o_inter = jnp.einsum('bhtc,bhcv->bhtv', qc * jnp.exp(b), state)
        diff = b[:, :, :, None, :] - b[:, :, None, :, :]
        decay = jnp.exp(jnp.where(causal, diff, -jnp.inf))
        A = jnp.einsum('bhtc,bhsc,bhtsc->bhts', qc, kc, decay)
        o = o_inter + jnp.einsum('bhts,bhsv->bhtv', A, vc)
        b_last = b[:, :, -1, :]
        new_state = jnp.exp(b_last)[..., None] * state + jnp.einsum(
            'bhsc,bhsv->bhcv', kc * jnp.exp(b_last[:, :, None, :] - b), vc)
        return new_state, o

    state0 = jnp.zeros((B, H, E, E), f32)
    _, o = lax.scan(chunk_step, state0, (to_chunks(q), to_chunks(k), to_chunks(v), to_chunks(log_f)))
    o = o.transpose(1, 0, 3, 2, 4).reshape(B, S, H, E)
    gate = g_raw.astype(f32).reshape(B, S, H, E)
    o = rms_norm(o, norm_g) * jax.nn.silu(gate)
    return o.reshape(B, S, H * E).astype(q_raw.dtype)


def moe_ffn(x, router_w, router_b, w_gu, b_gu, w_down, b_down):
    B, S, D = x.shape
    t = x.reshape(B * S, D)
    logits = (t @ router_w + router_b).astype(jnp.float32)
    top_val, top_idx = lax.top_k(logits, TOP_K)
    top_w = jax.nn.softmax(top_val, axis=-1)
    gates = jnp.sum(jax.nn.one_hot(top_idx, N_EXPERTS, dtype=jnp.float32) * top_w[..., None], axis=1)
    gates = gates.astype(x.dtype)
    out = jnp.zeros_like(t)
    for e in range(N_EXPERTS):
        gu = t @ w_gu[e] + b_gu[e]
        gate = jnp.minimum(gu[:, :D_EXPERT], SWIGLU_LIMIT)
        up = jnp.clip(gu[:, D_EXPERT:], -SWIGLU_LIMIT, SWIGLU_LIMIT)
        glu = gate * jax.nn.sigmoid(SWIGLU_ALPHA * gate)
        y = ((up + 1.0) * glu) @ w_down[e] + b_down[e]
        out = out + gates[:, e:e + 1] * y
    return out.reshape(B, S, D)


def setup_inputs(seed: int = 0) -> dict:
    key = jax.random.key(seed)
    ks = jax.random.split(key, 20)
    f32 = jnp.float32
    nrm = lambda k, shape, scale: jax.random.normal(k, shape, f32) * scale
    return {
        "x": nrm(ks[0], (BATCH, SEQ, D_MODEL), 1.0),
        "p": nrm(ks[1], (DEPTH, BATCH, SEQ, PLE_DIM), 1.0),
        "positions": jnp.broadcast_to(jnp.arange(SEQ, dtype=jnp.int32), (BATCH, SEQ)),
        "mix_norm_g": 1.0 + nrm(ks[2], (DEPTH, D_MODEL), 0.02),
        "w_in": nrm(ks[3], (DEPTH, D_MODEL, IN_COLS), D_MODEL ** -0.5),
        "q_norm_g": 1.0 + nrm(ks[4], (DEPTH, ATTN_HEAD_DIM), 0.02),
        "k_norm_g": 1.0 + nrm(ks[5], (DEPTH, ATTN_HEAD_DIM), 0.02),
        "hgrn_lb_logits": nrm(ks[6], (DEPTH + 1, HGRN_WIDTH), 0.5),
        "hgrn_norm_g": 1.0 + nrm(ks[7], (DEPTH, HGRN_EXPAND), 0.02),
        "w_out": nrm(ks[8], (DEPTH, D_MIX, D_MODEL), D_MIX ** -0.5),
        "ffn_norm_g": 1.0 + nrm(ks[9], (DEPTH, D_MODEL), 0.02),
        "router_w": nrm(ks[10], (DEPTH, D_MODEL, N_EXPERTS), D_MODEL ** -0.5),
        "router_b": nrm(ks[11], (DEPTH, N_EXPERTS), 0.01),
        "expert_w_gate_up": nrm(ks[12], (DEPTH, N_EXPERTS, D_MODEL, 2 * D_EXPERT), D_MODEL ** -0.5),
        "expert_b_gate_up": nrm(ks[13], (DEPTH, N_EXPERTS, 2 * D_EXPERT), 0.02),
        "expert_w_down": nrm(ks[14], (DEPTH, N_EXPERTS, D_EXPERT, D_MODEL), D_EXPERT ** -0.5),
        "expert_b_down": nrm(ks[15], (DEPTH, N_EXPERTS, D_MODEL), 0.02),
        "ple_proj": nrm(ks[16], (DEPTH, PLE_DIM, D_MODEL), PLE_DIM ** -0.5),
        "ple_gate": nrm(ks[17], (DEPTH, D_MODEL, D_MODEL), D_MODEL ** -0.5),
    }


def reference(x, p, positions, mix_norm_g, w_in, q_norm_g, k_norm_g, hgrn_lb_logits,
              hgrn_norm_g, w_out, ffn_norm_g, router_w, router_b, expert_w_gate_up,
              expert_b_gate_up, expert_w_down, expert_b_down, ple_proj, ple_gate):
    B, S, _ = x.shape
    lower_bounds = jnp.cumsum(jax.nn.softmax(hgrn_lb_logits.astype(jnp.float32), axis=0), axis=0)
    splits = [ATTN_WIDTH * j for j in (1, 2, 3)] + [3 * ATTN_WIDTH + HGRN_WIDTH * j for j in (1, 2, 3)]
    h = x
    for i in range(DEPTH):
        a = rms_norm(h, mix_norm_g[i])
        z = a @ w_in[i]
        q_a, k_a, v_a, q_h, f_h, i_h, g_h = jnp.split(z, splits, axis=-1)
        q = q_a.reshape(B, S, ATTN_HEADS, ATTN_HEAD_DIM)
        k = k_a.reshape(B, S, ATTN_HEADS, ATTN_HEAD_DIM)
        v = v_a.reshape(B, S, ATTN_HEADS, ATTN_HEAD_DIM)
        q = partial_rope(rms_norm(q, q_norm_g[i]), positions) * (ATTN_HEAD_DIM ** -0.5)
        k = partial_rope(rms_norm(k, k_norm_g[i]), positions)
        attn = dilated_attention(q, k, v).reshape(B, S, ATTN_WIDTH)
        rec = hgrn2_mixer(q_h, f_h, i_h, g_h, lower_bounds[i], hgrn_norm_g[i])
        h = h + jnp.concatenate([attn, rec], axis=-1) @ w_out[i]
        m = rms_norm(h, ffn_norm_g[i])
        h = h + moe_ffn(m, router_w[i], router_b[i], expert_w_gate_up[i], expert_b_gate_up[i],
                        expert_w_down[i], expert_b_down[i])
        h = h + (p[i] @ ple_proj[i]) * jax.nn.sigmoid(h @ ple_gate[i])
    return h
```

```python
import math
from contextlib import ExitStack

import numpy as np
import ml_dtypes
import concourse.bass as bass
import concourse.mybir as mybir
from concourse.bass_utils import run_bass_kernel_spmd

F32 = mybir.dt.float32
BF16 = mybir.dt.bfloat16
I32 = mybir.dt.int32
AF = mybir.ActivationFunctionType
ALU = mybir.AluOpType
AX = mybir.AxisListType

D = 1024
TOK = 2048
NE = 32
EPS = 1e-6
NCORES = 8
SPARSE = True
XROWS = NE * TOK


class _Op:
    __slots__ = ("sig_aux", "eng", "fn", "deps", "signal", "sig", "dma", "emitted", "idx", "region")


class Prog:
    COMPUTE = ("pe", "act", "dve", "pool")

    def __init__(self, nc, n_dma_sems=36):
        self.nc = nc
        self.ops = []
        self.last_w = {}
        self.readers = {}
        self.sems = {}
        self.cnt = {e: 0 for e in self.COMPUTE}
        self.dma_sems = []
        self.dma_tot = []
        self.dma_rr = 0
        self.n_dma_sems = n_dma_sems
        self.waited = {}
        self.stack = None
        self.cur_region = None
        self.regs = {}
        self.wstream_ops = set()

    def setup(self, stack):
        nc = self.nc
        for e in self.COMPUTE:
            self.sems[e] = stack.enter_context(nc.semaphore("s_" + e))
        for i in range(self.n_dma_sems):
            self.dma_sems.append(stack.enter_context(nc.semaphore("s_dma%d" % i)))
            self.dma_tot.append(0)
        self.wsems = [stack.enter_context(nc.semaphore("s_wdma%d" % i)) for i in range(32)]
        self.wtot = [0] * 32
        self.wrr = 0
        self.gsems = [stack.enter_context(nc.semaphore("s_gdma%d" % i)) for i in range(20)]
        self.gtot = [0] * 20
        self.grr = 0

    def add(self, eng, fn, reads=(), writes=(), dma=False, region=None):
        op = _Op()
        op.region = self.cur_region if region is None else (region or None)
        op.eng = eng
        op.fn = fn
        op.dma = dma
        op.signal = dma
        op.sig = None
        op.emitted = False
        op.idx = len(self.ops)
        deps = []
        for k in reads:
            w = self.last_w.get(k)
            if w is not None:
                deps.append(w)
        for k in writes:
            w = self.last_w.get(k)
            if w is not None:
                deps.append(w)
            for r in self.readers.get(k, ()):
                deps.append(r)
        dd = []
        seen = set()
        for d in deps:
            if d is op or id(d) in seen:
                continue
            seen.add(id(d))
            if d.eng == "pe" and eng == "pe" and not d.dma and not dma:
                continue
            if d.emitted and not d.dma:
                continue
            if d.eng == "sp" and not d.dma:
                continue
            dd.append(d)
            d.signal = True
        op.deps = dd
        for k in reads:
            self.readers.setdefault(k, []).append(op)
        for k in writes:
            self.last_w[k] = op
            self.readers[k] = []
        self.ops.append(op)
        return op

    def pe(self, fn, reads=(), writes=()):
        return self.add("pe", fn, reads, writes)

    def act(self, fn, reads=(), writes=()):
        return self.add("act", fn, reads, writes)

    def dve(self, fn, reads=(), writes=()):
        return self.add("dve", fn, reads, writes)

    def pool(self, fn, reads=(), writes=()):
        return self.add("pool", fn, reads, writes)

    def dma(self, q, fn, reads=(), writes=(), wstream=False):
        op = self.add(q, fn, reads, writes, dma=True)
        if wstream:
            self.wstream_ops.add(id(op))
        return op

    def regload(self, engname, key, ap, reads=()):
        op = self.add(engname, "regload", reads, (), region=False)
        op.region = None
        op.sig_aux = (key, ap)
        return op

    def emit(self):
        nc = self.nc
        todo = [o for o in self.ops if not o.emitted]
        for o in todo:
            if o.dma and id(o) in self.wstream_ops:
                j = self.wrr
                self.wrr = (self.wrr + 1) % len(self.wsems)
                prev = self.wtot[j]
                self.wtot[j] += 16
                o.sig = (self.wsems[j], self.wtot[j], ("w", j), prev)
            elif o.dma and o.eng == "pool":
                j = self.grr
                self.grr = (self.grr + 1) % len(self.gsems)
                prev = self.gtot[j]
                self.gtot[j] += 16
                o.sig = (self.gsems[j], self.gtot[j], ("g", j), prev)
            elif o.dma:
                j = self.dma_rr
                self.dma_rr = (self.dma_rr + 1) % self.n_dma_sems
                prev = self.dma_tot[j]
                self.dma_tot[j] += 16
                o.sig = (self.dma_sems[j], self.dma_tot[j], ("d", j), prev)
            elif o.signal:
                self.cnt[o.eng] += 1
                o.sig = (self.sems[o.eng], self.cnt[o.eng], ("c", o.eng), None)
        by = {}
        for o in todo:
            by.setdefault(o.eng, []).append(o)
        qmap = {"pe": "tensor", "act": "scalar", "dve": "vector", "pool": "gpsimd", "sp": "sync"}

        def run(engname, ops):
            def body(eng):
                waited = self.waited.setdefault(engname, {})
                regs = self.regs.setdefault(engname, {})
                rstack = ExitStack()

                def getreg(key):
                    if key not in regs:
                        regs[key] = rstack.enter_context(eng.register("r_%s_%s" % (engname, key)))
                    return regs[key]

                def emit_op(o):
                    ws = {}
                    for d in o.deps:
                        if d.sig[2] not in ws or ws[d.sig[2]][1] < d.sig[1]:
                            ws[d.sig[2]] = (d.sig[0], d.sig[1])
                    if o.dma and o.sig[3] > 0:
                        if o.sig[2] not in ws or ws[o.sig[2]][1] < o.sig[3]:
                            ws[o.sig[2]] = (o.sig[0], o.sig[3])
                    for key, (sem, val) in ws.items():
                        if waited.get(key, 0) >= val:
                            continue
                        waited[key] = val
                        eng.wait_ge(sem, val)
                    if o.fn == "regload":
                        eng.reg_load(getreg(o.sig_aux[0]), o.sig_aux[1])
                    else:
                        inst = o.fn(eng)
                        if o.sig is not None:
                            inst.then_inc(o.sig[0], 16 if o.dma else 1)
                    o.emitted = True

                def comp_for(groups):
                    ncomp = sum(1 for grp in groups for g in grp if g.sig is not None and not g.dma)
                    dmas = [g for grp in groups for g in grp if g.dma]
                    if ncomp:
                        eng.drain()
                        eng.sem_inc(self.sems[engname], ncomp)
                    for g in dmas:
                        if g.sig[3] > 0:
                            eng.wait_ge(g.sig[0], g.sig[3])
                        eng.sem_inc(g.sig[0], 16)
                    if not ncomp and not dmas:
                        eng.nop()

                def emit_chain(groups, gi):
                    grp = groups[gi]
                    with eng.If_lt(getreg(grp[0].region[0]), grp[0].region[1] + 1):
                        comp_for(groups[gi:])
                    with eng.Else():
                        for g in grp:
                            emit_op(g)
                        if gi + 1 < len(groups):
                            emit_chain(groups, gi + 1)

                i = 0
                while i < len(ops):
                    o = ops[i]
                    if o.region is None:
                        emit_op(o)
                        i += 1
                        continue
                    groups = []
                    j = i
                    while j < len(ops) and ops[j].region is not None and ops[j].region[0] == o.region[0] and \
                            (not groups or ops[j].region[1] >= groups[-1][0].region[1]):
                        if groups and ops[j].region == groups[-1][0].region:
                            groups[-1].append(ops[j])
                        else:
                            groups.append([ops[j]])
                        j += 1
                    saved = dict(waited)
                    emit_chain(groups, 0)
                    waited.clear()
                    waited.update(saved)
                    i = j
                rstack.close()
            return body

        with nc.Block() as block:
            for engname, ops in by.items():
                getattr(block, qmap[engname])(run(engname, ops))
        for o in todo:
            o.fn = None


_UID = [0]


def U(name):
    _UID[0] += 1
    return "%s_%d" % (name, _UID[0])


class Rot:
    def __init__(self, nc, stack, name, shape, dtype, n, psum=False):
        self.bufs = []
        for i in range(n):
            alloc = nc.psum_tensor if psum else nc.sbuf_tensor
            self.bufs.append(stack.enter_context(alloc(U("%s%d" % (name, i)), list(shape), dtype)))
        self.name = name
        self.i = 0
        self.gen = 0

    @classmethod
    def from_aps(cls, name, aps):
        r = cls.__new__(cls)
        r.bufs = list(aps)
        r.name = name
        r.i = 0
        r.gen = 0
        return r

    def next(self):
        b = self.bufs[self.i]
        key = (self.name, self.i)
        self.i = (self.i + 1) % len(self.bufs)
        return b, key


def build(dbg=None):
    nc = bass.Bass("TRN2", target_bir_lowering=False)

    def din(name, shape, dt=F32):
        return nc.dram_tensor(name, list(shape), dt, kind="ExternalInput").ap()

    xo = din("xo", [TOK, D])
    xc = din("xc", [TOK, D])
    pos = din("pos", [1, 4096], I32)
    onesctx = din("onesctx", [128, 64])
    g_mix = din("g_mix", [128, 8])
    w_in = din("w_in", [D, 3584])
    gq = din("gq", [128, 1])
    gk = din("gk", [128, 1])
    lb0 = din("lb0", [128, 4])
    lb1 = din("lb1", [128, 4])
    gnorm = din("gnorm", [128, 1])
    w_out = din("w_out", [D, D])
    g_ffn = din("g_ffn", [128, 8])
    router_w = din("router_w", [D, NE])
    router_b = din("router_b", [1, NE])
    if SPARSE:
        w_gu_nat = din("w_gu_nat", [NE, D, 2 * D])
        b_gu_nat = din("b_gu_nat", [NE, 2 * D])
    else:
        wgu = din("wgu", [NE, 4, 128, 4096])
        bgu = din("bgu", [128, NE, 16])
    w_down = din("w_down", [NE, D, D])
    b_down = din("b_down", [NE, D])
    ple_proj = din("ple_proj", [256, D])
    ple_gate = din("ple_gate", [D, D])
    p_own = din("p_own", [TOK, 256])
    c_ident_bf = din("c_ident_bf", [128, 128], BF16)
    c_ident_f = din("c_ident_f", [128, 128])
    c_band = din("c_band", [128, 256], BF16)
    c_hmask = din("c_hmask", [128, 64])
    c_blockones = din("c_blockones", [128, 128], BF16)
    c_ones_f = din("c_ones_f", [128, 128])
    c_pm = din("c_pm", [128, 128], BF16)
    c_invf = din("c_invf", [128, 1])
    c_reset = din("c_reset", [128, 512])
    if dbg == "moe":
        d_mixT = din("d_mixT", [128, 8, TOK], BF16)
    out = nc.dram_tensor("out", [TOK, D], F32, kind="ExternalOutput").ap()
    gffn_row = din("gffn_row", [1, D])
    c_tri = din("c_tri", [128, 128], BF16)
    c_iota = din("c_iota", [128, NE])
    c_zbase = din("c_zbase", [128, 2 * NE])
    c_zlim = din("c_zlim", [128, 2 * NE])
    c_ztrash = din("c_ztrash", [128, 2 * NE])
    Xd = nc.dram_tensor("Xd", [NE * TOK + 128, D], BF16, kind="Internal").ap()
    Yd = nc.dram_tensor("Yd", [NE * TOK, D], F32, kind="Internal").ap()
    if dbg and dbg.startswith("mix"):
        d_out_mixT = nc.dram_tensor("d_out_mixT", [128, 8, TOK], BF16, kind="ExternalOutput").ap()

    P = Prog(nc)
    with ExitStack() as top:
        P.setup(top)
        sb = lambda name, shape, dt, st=top: st.enter_context(nc.sbuf_tensor(U(name), list(shape), dt))
        ident_bf = sb("ident_bf", [128, 128], BF16)
        ident_f = sb("ident_f", [128, 128], F32)
        acc = sb("acc", [128, 16, D], F32)
        accb = acc[:].rearrange("p a b -> p (a b)").bitcast(BF16)
        gw = sb("gw", [128, 16, 4], F32)
        ridx = sb("ridx", [128, 16, 4], I32)
        cnt_i = sb("cnt_i", [1, NE], I32)
        smix = ExitStack()
        mixT = smix.enter_context(nc.sbuf_tensor(U("mixT"), [128, 8, TOK], BF16))
        P.dma("sp", lambda e: e.dma_start(out=ident_bf[:], in_=c_ident_bf), writes=["ident_bf"])
        P.dma("sp", lambda e: e.dma_start(out=ident_f[:], in_=c_ident_f), writes=["ident_f"])

        if dbg == "moe":
            P.dma("sp", lambda e: e.dma_start(out=mixT[:], in_=d_mixT), writes=["mixT"])
        else:
            emit_mixer(nc, P, locals(), upto=(dbg[3:] if dbg and dbg.startswith("mix") and len(dbg) > 3 else "T"))
        if dbg and dbg.startswith("mix"):
            P.dma("sp", lambda e: e.dma_start(out=d_out_mixT, in_=mixT[:]), reads=["mixT"], writes=["d_out"])
            P.add("sp", lambda e: e.nop(), reads=["d_out"])
            P.emit()
            smix.close()
            return nc

        if SPARSE:
            emit_sparse(nc, P, locals(), smix)
        else:
            for half in range(2):
                emit_half(nc, P, locals(), half)
            smix.close()
    return nc


def a1_block(nc, P, env, pools, blk, gmix):
    xt_r, junk_r, xn_r, st_r, tp_r, aT_r = pools
    ident_bf = env["ident_bf"]
    aT, aTk = aT_r.next()
    for t in range(4):
        gt = blk * 4 + t
        src = env["xc"][gt * 128:(gt + 1) * 128, :] if gt < 16 else env["xo"][(gt - 16) * 128:(gt - 15) * 128, :]
        xt, xk = xt_r.next()
        P.dma("sp", lambda e, xt=xt, src=src: e.dma_start(out=xt[:], in_=src), writes=[xk])
        junk, jk = junk_r.next()
        st, stk = st_r.next()
        P.act(lambda e, junk=junk, xt=xt, st=st: e.activation(out=junk[:], in_=xt[:], func=AF.Square, accum_out=st[:, 0:1]),
              reads=[xk], writes=[jk, stk])
        P.act(lambda e, st=st: e.activation(out=st[:, 1:2], in_=st[:, 0:1], func=AF.Sqrt, scale=1.0 / D, bias=EPS), reads=[stk], writes=[stk])
        P.dve(lambda e, st=st: e.reciprocal(out=st[:, 2:3], in_=st[:, 1:2]), reads=[stk], writes=[stk])
        xn, xnk = xn_r.next()
        P.dve(lambda e, xn=xn, xt=xt, st=st: e.tensor_scalar(out=xn[:], in0=xt[:], scalar1=st[:, 2:3], scalar2=None, op0=ALU.mult),
              reads=[xk, stk], writes=[xnk])
        tp, tpk = tp_r.next()
        for kc in range(8):
            P.pe(lambda e, tp=tp, xn=xn, kc=kc: e.transpose(out=tp[:, kc, :], in_=xn[:, kc * 128:(kc + 1) * 128], identity=ident_bf[:]),
                 reads=[xnk, "ident_bf"], writes=[tpk])
        P.dve(lambda e, aT=aT, tp=tp, t=t: e.tensor_tensor(out=aT[:, :, t * 128:(t + 1) * 128], in0=tp[:],
                                                            in1=gmix[:].unsqueeze(2).broadcast_to([128, 8, 128]), op=ALU.mult),
              reads=[tpk, "gmix"], writes=[(aTk, t)])
    return aT, [(aTk, t) for t in range(4)]


import os as _os


def emit_mixer(nc, P, env, upto="T"):
    mixT = env["mixT"]
    ident_bf = env["ident_bf"]
    with ExitStack() as sm_:
        sbm = lambda name, shape, dt: sm_.enter_context(nc.sbuf_tensor(U(name), list(shape), dt))
        gmix = sbm("gmix", [128, 8], F32)
        P.dma("sp", lambda e: e.dma_start(out=gmix[:], in_=env["g_mix"]), writes=["gmix"])

        with ExitStack() as sh:
            sbh = lambda name, shape, dt: sh.enter_context(nc.sbuf_tensor(U(name), list(shape), dt))
            accb = env["accb"]
            w_h = accb[:, 0:16384].rearrange("p (k c) -> p k c", k=8)
            for kc in range(8):
                P.dma("pool", lambda e, kc=kc: e.dma_start(out=w_h[:, kc, :], in_=env["w_in"][kc * 128:(kc + 1) * 128, 1536:3584]),
                      writes=[("w_h", kc)])
            hmask = sbh("hmask", [128, 64], F32)
            ones_f = sbh("ones_f", [128, 128], F32)
            reset = sbh("reset", [128, 512], F32)
            gnorm = sbh("gnorm", [128, 1], F32)
            l0 = sbh("l0", [128, 4], F32)
            l1 = sbh("l1", [128, 4], F32)
            oml = sbh("oml", [128, 4], F32)
            noml = sbh("noml", [128, 4], F32)
            for (t_, src, key) in ((hmask, "c_hmask", "hmask"), (ones_f, "c_ones_f", "ones_f"), (reset, "c_reset", "reset"),
                                   (gnorm, "gnorm", "gnorm"), (l0, "lb0", "l0"), (l1, "lb1", "l1")):
                P.dma("sp", lambda e, t_=t_, src=src: e.dma_start(out=t_[:], in_=env[src]), writes=[key])
            P.dve(lambda e: e.tensor_tensor(out=oml[:], in0=l1[:], in1=l0[:], op=ALU.subtract), reads=["l0", "l1"], writes=["oml"])
            P.act(lambda e: e.activation(out=oml[:], in_=oml[:], func=AF.Sigmoid), reads=["oml"], writes=["oml"])
            P.dve(lambda e: e.tensor_scalar(out=noml[:], in0=oml[:], scalar1=-1.0, scalar2=None, op0=ALU.mult), reads=["oml"], writes=["oml2"])
            tpb_r = Rot(nc, sh, "tpb", [128, 8, 128], BF16, 2, psum=True)
            pools = (Rot(nc, sh, "xt", [128, D], F32, 2), Rot(nc, sh, "junk", [128, D], BF16, 1), Rot(nc, sh, "xn", [128, D], BF16, 2),
                     Rot(nc, sh, "st", [128, 4], F32, 4), tpb_r,
                     Rot.from_aps("aTh", [accb[:, 16384 + i * 4096:16384 + (i + 1) * 4096].rearrange("p (k c) -> p k c", k=8) for i in range(2)]))
            vt_r = Rot.from_aps("vth", [accb[0:64, 24576 + i * 4096:24576 + (i + 1) * 4096].rearrange("p (k c) -> p k c", k=8) for i in range(2)])
            At_r = Rot(nc, sh, "At", [128, 64], BF16, 4)
            eb = [sbh("eb%d" % i, [128, 512], F32) for i in range(4)]
            kt = [sbh("kt%d" % i, [128, 512], BF16) for i in range(4)]
            qt = [sbh("qt%d" % i, [128, 512], BF16) for i in range(4)]
            ktok = [sbh("ktok%d" % i, [64, 8, 128], BF16) for i in range(4)]
            sgate = [sbh("sgate%d" % i, [128, 512], F32) for i in range(4)]
            oT = [sbh("oT%d" % i, [128, 512], F32) for i in range(4)]
            S = sbh("S", [128, 4, 128], F32)
            S_bf = sbh("S_bf", [128, 4, 128], BF16)
            pb_r = Rot(nc, sh, "pb", [128, 512], F32, 6, psum=True)
            mm_r = pb_r
            P.dve(lambda e: e.memset(S[:], 0.0), writes=[("S", i) for i in range(4)])
            P.dve(lambda e: e.memset(S_bf[:], 0.0), writes=[("S_bf", i) for i in range(4)])

            def mmgroup(col0, aT, aTkeys):
                mm, mmk = mm_r.next()
                for kc in range(8):
                    P.pe(lambda e, mm=mm, kc=kc, col0=col0, aT=aT: e.matmul(mm[:], w_h[:, kc, col0:col0 + 128], aT[:, kc, :],
                                                                          start=(kc == 0), stop=(kc == 7)),
                         reads=[("w_h", kc)] + aTkeys, writes=[mmk])
                return mm, mmk

            snegs = [sbh("snegh%d" % i, [128, 512], F32) for i in range(4)]
            ffs = [sbh("ffh%d" % i, [128, 512], F32) for i in range(4)]
            bbs = [sbh("bbh%d" % i, [128, 512], F32) for i in range(4)]
            enbs = [sbh("enbh%d" % i, [128, 512], F32) for i in range(4)]
            khs = [sbh("khh%d" % i, [128, 512], BF16) for i in range(4)]
            qss = [sbh("qsh%d" % i, [128, 512], F32) for i in range(4)]
            H4 = range(4)
            USE_STT = False
            for blk in range(8):
                own = blk >= 4
                ob = blk - 4
                aT, aTkeys = a1_block(nc, P, env, pools, blk, gmix)
                vt, vtk = vt_r.next()
                for n in range(8):
                    mm, mmk = pb_r.next()
                    for kc in range(8):
                        P.pe(lambda e, mm=mm, kc=kc, n=n, aT=aT: e.matmul(mm[0:64, :], aT[:, kc, n * 64:(n + 1) * 64], w_h[:, kc, 1024:1536],
                                                                         start=(kc == 0), stop=(kc == 7)),
                             reads=[("w_h", kc), aTkeys[n // 2]], writes=[mmk])
                    P.act(lambda e, vt=vt, mm=mm, n=n: e.activation(out=vt[0:64, n, :], in_=mm[0:64, :], func=AF.Copy), reads=[mmk], writes=[(vtk, n)])
                mf = [mmgroup(512 + hd * 128, aT, aTkeys) for hd in H4]
                for hd in H4:
                    P.act(lambda e, hd=hd, mf=mf: e.activation(out=snegs[hd][:], in_=mf[hd][0][:], func=AF.Sigmoid, scale=-1.0), reads=[mf[hd][1]], writes=[("sneg", hd)])
                if own:
                    mq = [mmgroup(hd * 128, aT, aTkeys) for hd in H4]
                    for hd in H4:
                        P.act(lambda e, hd=hd, mq=mq: e.activation(out=qss[hd][:], in_=mq[hd][0][:], func=AF.Silu), reads=[mq[hd][1]], writes=[("qs", hd)])
                for hd in H4:
                    P.dve(lambda e, hd=hd: e.tensor_scalar(out=ffs[hd][:], in0=snegs[hd][:], scalar1=noml[:, hd:hd + 1], scalar2=1.0, op0=ALU.mult, op1=ALU.add),
                          reads=[("sneg", hd), "oml2"], writes=[("ff", hd)])
                for hd in H4:
                    P.act(lambda e, hd=hd: e.activation(out=ffs[hd][:], in_=ffs[hd][:], func=AF.Ln), reads=[("ff", hd)], writes=[("ff", hd)])
                if own:
                    mg = [mmgroup(1536 + hd * 128, aT, aTkeys) for hd in H4]
                    for hd in H4:
                        P.act(lambda e, hd=hd, mg=mg: e.activation(out=sgate[hd][:], in_=mg[hd][0][:], func=AF.Silu), reads=[mg[hd][1]], writes=[("sgate", hd)])
                for hd in H4:
                    P.dve(lambda e, hd=hd: e.tensor_tensor_scan(out=bbs[hd][:], data0=reset[:], data1=ffs[hd][:], initial=0.0, op0=ALU.mult, op1=ALU.add),
                          reads=[("ff", hd), "reset"], writes=[("bb", hd)])
                for hd in H4:
                    P.act(lambda e, hd=hd: e.activation(out=eb[hd][:], in_=bbs[hd][:], func=AF.Exp), reads=[("bb", hd)], writes=[("eb", hd)])
                    P.act(lambda e, hd=hd: e.activation(out=enbs[hd][:], in_=bbs[hd][:], func=AF.Exp, scale=-1.0), reads=[("bb", hd)], writes=[("enb", hd)])
                for hd in H4:
                    P.dve(lambda e, hd=hd: e.scalar_tensor_tensor(out=kt[hd][:], in0=snegs[hd][:], scalar=oml[:, hd:hd + 1], in1=enbs[hd][:],
                                                                  op0=ALU.mult, op1=ALU.mult), reads=[("sneg", hd), ("enb", hd), "oml"], writes=[("kt", hd)])
                    if own:
                        P.pool(lambda e, hd=hd: e.tensor_tensor(out=qt[hd][:], in0=qss[hd][:], in1=eb[hd][:], op=ALU.mult),
                               reads=[("qs", hd), ("eb", hd)], writes=[("qt", hd)])
                for hd in H4:
                    P.pool(lambda e, hd=hd: e.tensor_tensor(
                        out=khs[hd][:].rearrange("p (n c) -> p n c", c=64), in0=kt[hd][:].rearrange("p (n c) -> p n c", c=64),
                        in1=eb[hd][:].rearrange("p (n c) -> p n c", c=64)[:, :, 63:64].broadcast_to([128, 8, 64]), op=ALU.mult),
                        reads=[("kt", hd), ("eb", hd)], writes=[("kh", hd)])
                for hd in H4:
                    tpk_, tpkk = tpb_r.next()
                    for n in range(8):
                        P.pe(lambda e, hd=hd, n=n, tpk_=tpk_: e.transpose(out=tpk_[0:64, n, :], in_=khs[hd][:, n * 64:(n + 1) * 64], identity=ident_bf[:]),
                             reads=[("kh", hd), "ident_bf"], writes=[tpkk])
                    P.dve(lambda e, hd=hd, tpk_=tpk_: e.tensor_copy(out=ktok[hd][:], in_=tpk_[0:64, :, :]), reads=[tpkk], writes=[("ktok", hd)])
                for n in range(8):
                    t = n
                    cs = slice(n * 64, n * 64 + 64)
                    for hd in H4:
                        if own:
                            pA, pAk = pb_r.next()
                            P.pe(lambda e, hd=hd, cs=cs, pA=pA: e.matmul(pA[0:64, 0:64], kt[hd][:, cs], qt[hd][:, cs], start=True, stop=True),
                                 reads=[("kt", hd), ("qt", hd)], writes=[pAk])
                            At, Atk = At_r.next()
                            P.dve(lambda e, At=At, pA=pA: e.tensor_tensor(out=At[0:64, :], in0=pA[0:64, 0:64], in1=hmask[0:64, :], op=ALU.mult),
                                  reads=[pAk, "hmask"], writes=[Atk])
                            pO, pOk = pb_r.next()
                            P.pe(lambda e, hd=hd, cs=cs, pO=pO: e.matmul(pO[:, 0:64], S_bf[:, hd, :], qt[hd][:, cs], start=True, stop=False),
                                 reads=[("S_bf", hd), ("qt", hd)], writes=[pOk])
                            P.pe(lambda e, hd=hd, t=t, At=At, vt=vt, pO=pO: e.matmul(pO[:, 0:64], vt[0:64, t, hd * 128:(hd + 1) * 128], At[0:64, :],
                                                                                 start=False, stop=True),
                                 reads=[(vtk, t), Atk], writes=[pOk])
                            P.act(lambda e, hd=hd, cs=cs, pO=pO: e.activation(out=oT[hd][:, cs], in_=pO[:, 0:64], func=AF.Copy),
                                  reads=[pOk], writes=[("oT", hd, n)])
                        pS, pSk = pb_r.next()
                        P.pe(lambda e, hd=hd, t=t, vt=vt, pS=pS: e.matmul(pS[:, 0:128], ktok[hd][0:64, t, :], vt[0:64, t, hd * 128:(hd + 1) * 128],
                                                                        start=True, stop=True),
                             reads=[("ktok", hd), (vtk, t)], writes=[pSk])
                        if USE_STT:
                            P.dve(lambda e, hd=hd, n=n, pS=pS: e.scalar_tensor_tensor(out=S[:, hd, :], in0=S[:, hd, :], scalar=eb[hd][:, n * 64 + 63:n * 64 + 64],
                                                                                    in1=pS[:, 0:128], op0=ALU.mult, op1=ALU.add),
                                  reads=[("S", hd), ("eb", hd), pSk], writes=[("S", hd)])
                        else:
                            if own and hd < 2:
                                P.pool(lambda e, hd=hd, n=n: e.tensor_scalar(out=S[:, hd, :], in0=S[:, hd, :], scalar1=eb[hd][:, n * 64 + 63:n * 64 + 64], scalar2=None, op0=ALU.mult),
                                       reads=[("S", hd), ("eb", hd)], writes=[("S", hd)])
                            else:
                                P.act(lambda e, hd=hd, n=n: e.activation(out=S[:, hd, :], in_=S[:, hd, :], func=AF.Copy, scale=eb[hd][:, n * 64 + 63:n * 64 + 64]),
                                      reads=[("S", hd), ("eb", hd)], writes=[("S", hd)])
                            P.dve(lambda e, hd=hd, pS=pS: e.tensor_tensor(out=S[:, hd, :], in0=pS[:, 0:128], in1=S[:, hd, :], op=ALU.add),
                                  reads=[("S", hd), pSk], writes=[("S", hd)])
                        if own or (blk == 3 and n == 7):
                            P.act(lambda e, hd=hd: e.activation(out=S_bf[:, hd, :], in_=S[:, hd, :], func=AF.Copy), reads=[("S", hd)], writes=[("S_bf", hd)])
                if own:
                    okeys = [[("oT", hd, n) for n in range(8)] for hd in H4]
                    for hd in H4:
                        P.act(lambda e, hd=hd: e.activation(out=snegs[hd][:], in_=oT[hd][:], func=AF.Square), reads=okeys[hd], writes=[("sneg", hd)])
                    mr = []
                    for hd in H4:
                        mm, mmk = pb_r.next()
                        P.pe(lambda e, mm=mm, hd=hd: e.matmul(mm[:], ones_f[:], snegs[hd][:], start=True, stop=True), reads=[("sneg", hd), "ones_f"], writes=[mmk])
                        mr.append((mm, mmk))
                    for hd in H4:
                        P.act(lambda e, hd=hd, mr=mr: e.activation(out=ffs[hd][:], in_=mr[hd][0][:], func=AF.Sqrt, scale=1.0 / 128, bias=EPS), reads=[mr[hd][1]], writes=[("ff", hd)])
                    for hd in H4:
                        P.dve(lambda e, hd=hd: e.reciprocal(out=ffs[hd][:], in_=ffs[hd][:]), reads=[("ff", hd)], writes=[("ff", hd)])
                    for hd in H4:
                        P.dve(lambda e, hd=hd: e.scalar_tensor_tensor(out=bbs[hd][:], in0=oT[hd][:], scalar=gnorm[:, 0:1], in1=ffs[hd][:],
                                                                      op0=ALU.mult, op1=ALU.mult), reads=okeys[hd] + [("ff", hd), "gnorm"], writes=[("bb", hd)])
                    for hd in H4:
                        P.pool(lambda e, hd=hd, ob=ob: e.tensor_tensor(out=mixT[:, 4 + hd, ob * 512:(ob + 1) * 512], in0=bbs[hd][:], in1=sgate[hd][:], op=ALU.mult),
                               reads=[("bb", hd), ("sgate", hd)], writes=[("mixT", 4 + hd, ob)])
            P.emit()

        if upto == "H":
            return
        accb = env["accb"]
        KT = [accb[:, i * 4096:(i + 1) * 4096] for i in range(4)]
        VT = [accb[:, 16384 + i * 4096:16384 + (i + 1) * 4096] for i in range(4)]
        QT = [sbm("QT%d" % i, [128, TOK], BF16) for i in range(4)]
        with ExitStack() as sp_:
            sbp = lambda name, shape, dt: sp_.enter_context(nc.sbuf_tensor(U(name), list(shape), dt))
            w_a = sbp("w_a", [128, 8, 1536], BF16)
            for kc in range(8):
                P.dma("pool", lambda e, kc=kc: e.dma_start(out=w_a[:, kc, :], in_=env["w_in"][kc * 128:(kc + 1) * 128, 0:1536]),
                      writes=[("w_a", kc)])
            Ct = sbp("Ct", [128, 4096], BF16)
            St = sbp("St", [128, 4096], BF16)
            blockones = sbp("blockones", [128, 128], BF16)
            pm = sbp("pm", [128, 128], BF16)
            invf = sbp("invf", [128, 1], F32)
            gq = sbp("gq", [128, 1], F32)
            gk = sbp("gk", [128, 1], F32)
            for (t_, src, key) in ((blockones, "c_blockones", "blockones"), (pm, "c_pm", "pm"), (invf, "c_invf", "invf"),
                                   (gq, "gq", "gq"), (gk, "gk", "gk")):
                P.dma("sp", lambda e, t_=t_, src=src: e.dma_start(out=t_[:], in_=env[src]), writes=[key])
            srope = ExitStack()
            posi_r = Rot(nc, srope, "posi", [128, 1024], I32, 2)
            rf_r = Rot(nc, srope, "rf", [128, 1024], F32, 4)
            ri_r = Rot(nc, srope, "ri", [128, 1024], I32, 2)
            for ch in range(4):
                csl = slice(ch * 1024, (ch + 1) * 1024)
                posi, pik = posi_r.next()
                P.dma("sp", lambda e, posi=posi, csl=csl: e.dma_start(out=posi[:], in_=env["pos"][0:1, csl].partition_broadcast(128)), writes=[pik])
                posf, pfk = rf_r.next()
                P.dve(lambda e, posf=posf, posi=posi: e.tensor_copy(out=posf[:], in_=posi[:]), reads=[pik], writes=[pfk])
                for (off, tab, tkey) in ((0.5, St, "St"), (0.75, Ct, "Ct")):
                    y, yk = rf_r.next()
                    P.dve(lambda e, y=y, posf=posf, off=off: e.tensor_scalar(out=y[:], in0=posf[:], scalar1=invf[:, 0:1], scalar2=off, op0=ALU.mult, op1=ALU.add),
                          reads=[pfk, "invf"], writes=[yk])
                    ni, nik = ri_r.next()
                    P.dve(lambda e, ni=ni, y=y: e.tensor_copy(out=ni[:], in_=y[:]), reads=[yk], writes=[nik])
                    nf, nfk = rf_r.next()
                    P.dve(lambda e, nf=nf, ni=ni: e.tensor_copy(out=nf[:], in_=ni[:]), reads=[nik], writes=[nfk])
                    P.dve(lambda e, y=y, nf=nf: e.tensor_tensor(out=y[:], in0=y[:], in1=nf[:], op=ALU.subtract), reads=[yk, nfk], writes=[yk])
                    P.dve(lambda e, y=y, nf=nf: e.tensor_single_scalar(out=nf[:], in_=y[:], scalar=0.0, op=ALU.is_lt), reads=[yk], writes=[nfk])
                    P.dve(lambda e, y=y, nf=nf: e.tensor_tensor(out=y[:], in0=y[:], in1=nf[:], op=ALU.add), reads=[yk, nfk], writes=[yk])
                    P.act(lambda e, y=y, tab=tab, csl=csl: e.activation(out=tab[:, csl], in_=y[:], func=AF.Sin, scale=6.2831845, bias=-3.1415922),
                          reads=[yk], writes=[(tkey, ch)])
            P.emit()
            srope.close()
            pools = (Rot(nc, sp_, "xt", [128, D], F32, 2), Rot(nc, sp_, "junk", [128, D], BF16, 1), Rot(nc, sp_, "xn", [128, D], BF16, 2),
                     Rot(nc, sp_, "st", [128, 4], F32, 4), Rot(nc, sp_, "tp", [128, 8, 128], BF16, 1, psum=True),
                     Rot(nc, sp_, "aT", [128, 8, 512], BF16, 2))
            mm_r = Rot(nc, sp_, "mm", [128, 512], F32, 3, psum=True)
            pn_r = Rot(nc, sp_, "pn", [128, 512], F32, 2, psum=True)
            pq_r = Rot(nc, sp_, "pq", [128, 512], F32, 2, psum=True)
            sq_r = Rot(nc, sp_, "sq", [128, 512], BF16, 2)
            rt_r = Rot(nc, sp_, "rt", [128, 512], F32, 2)
            qn_r = Rot(nc, sp_, "qn", [128, 512], BF16, 2)
            t1_r = Rot(nc, sp_, "t1", [128, 512], F32, 2)
            t2_r = Rot(nc, sp_, "t2", [128, 512], F32, 2)
            q1 = []
            q2 = []

            def qk_stage0(nm, coff, dest, dsl, gg, sc, bi, hp, blk, aT, aTkeys, tsl, tch):
                mm, mmk = mm_r.next()
                for kc in range(8):
                    P.pe(lambda e, mm=mm, kc=kc: e.matmul(mm[:], w_a[:, kc, coff + hp * 128:coff + (hp + 1) * 128], aT[:, kc, :],
                                                          start=(kc == 0), stop=(kc == 7)),
                         reads=[("w_a", kc)] + aTkeys, writes=[mmk])
                sq, sqk = sq_r.next()
                P.act(lambda e: e.activation(out=sq[:], in_=mm[:], func=AF.Square), reads=[mmk], writes=[sqk])

                def stage1():
                    pn, pnk = pn_r.next()
                    P.pe(lambda e: e.matmul(pn[:], blockones[:], sq[:], start=True, stop=True), reads=[sqk, "blockones"], writes=[pnk])
                    rt, rtk = rt_r.next()
                    P.act(lambda e: e.activation(out=rt[:], in_=pn[:], func=AF.Sqrt, scale=sc, bias=bi), reads=[pnk], writes=[rtk])
                    P.dve(lambda e: e.reciprocal(out=rt[:], in_=rt[:]), reads=[rtk], writes=[rtk])
                    qn, qnk = qn_r.next()
                    P.dve(lambda e: e.scalar_tensor_tensor(out=qn[:], in0=mm[:], scalar=gg[:, 0:1], in1=rt[:], op0=ALU.mult, op1=ALU.mult),
                          reads=[mmk, rtk, nm == "k" and "gk" or "gq"], writes=[qnk])

                    def stage2():
                        pq, pqk = pq_r.next()
                        P.pe(lambda e: e.matmul(pq[:], pm[:], qn[:], start=True, stop=True), reads=[qnk, "pm"], writes=[pqk])
                        t1, t1k = t1_r.next()
                        P.pool(lambda e: e.tensor_tensor(out=t1[:], in0=qn[:], in1=Ct[:, tsl], op=ALU.mult), reads=[qnk, ("Ct", tch)], writes=[t1k])
                        t2, t2k = t2_r.next()
                        P.dve(lambda e: e.tensor_tensor(out=t2[:], in0=pq[:], in1=St[:, tsl], op=ALU.mult), reads=[pqk, ("St", tch)], writes=[t2k])
                        P.pool(lambda e: e.tensor_tensor(out=dest[:, dsl], in0=t1[:], in1=t2[:], op=ALU.add), reads=[t1k, t2k], writes=[(nm + "T", hp, blk)])
                    return stage2
                return stage1

            def v_stage0(hp, blk, aT, aTkeys, tsl):
                mm, mmk = mm_r.next()
                for kc in range(8):
                    P.pe(lambda e, mm=mm, kc=kc: e.matmul(mm[:], w_a[:, kc, 1024 + hp * 128:1024 + (hp + 1) * 128], aT[:, kc, :],
                                                          start=(kc == 0), stop=(kc == 7)),
                         reads=[("w_a", kc)] + aTkeys, writes=[mmk])
                P.act(lambda e: e.activation(out=VT[hp][:, tsl], in_=mm[:], func=AF.Copy), reads=[mmk], writes=[("VT", hp, blk)])
                return None

            def step(s1):
                if q2:
                    q2.pop(0)()
                if q1:
                    q2.append(q1.pop(0)())
                if s1 is not None:
                    q1.append(s1)

            nxt = a1_block(nc, P, env, pools, 0, gmix)
            for blk in range(8):
                own = blk >= 4
                aT, aTkeys = nxt
                tsl = slice(blk * 512, (blk + 1) * 512)
                tch = blk // 2
                for hp in range(4):
                    step(qk_stage0("k", 512, KT[hp], tsl, gk, 1.0 / 64, EPS, hp, blk, aT, aTkeys, tsl, tch))
                    if own:
                        step(qk_stage0("q", 0, QT[hp], slice((blk - 4) * 512, (blk - 3) * 512), gq, 1.0, 64 * EPS, hp, blk, aT, aTkeys, tsl, tch))
                    v_stage0(hp, blk, aT, aTkeys, tsl)
                    if hp == 1 and blk + 1 < 8:
                        nxt = a1_block(nc, P, env, pools, blk + 1, gmix)
            step(None)
            step(None)
            step(None)
            P.emit()

        if upto == "P":
            return
        with ExitStack() as st_:
            sbt = lambda name, shape, dt: st_.enter_context(nc.sbuf_tensor(U(name), list(shape), dt))
            band = sbt("band", [128, 256], BF16)
            onesb = sbt("onesb", [128, 64], BF16)
            onesc32 = sbt("onesc32", [128, 64], F32)
            onesc = sbt("onesc", [128, 64], BF16)
            P.dma("sp", lambda e: e.dma_start(out=band[:], in_=env["c_band"]), writes=["band"])
            P.dma("sp", lambda e: e.dma_start(out=onesc32[:], in_=env["onesctx"]), writes=["onesc32"])
            P.dve(lambda e: e.tensor_copy(out=onesc[:], in_=onesc32[:]), reads=["onesc32"], writes=["onesc"])
            P.dve(lambda e: e.memset(onesb[:], 1.0), writes=["onesb"])
            Vt_r = Rot(nc, st_, "Vt", [128, 32, 192], BF16, 2)
            acc = sbt("acca", [128, 2, TOK], F32)
            den = sbt("den", [128, TOK], F32)
            E_r = Rot(nc, st_, "E", [128, 256], BF16, 3)
            Pm_r = Rot(nc, st_, "Pm", [128, 256], BF16, 3)
            tpv_r = Rot(nc, st_, "tpv", [128, 8, 128], BF16, 2, psum=True)
            ps_r = Rot(nc, st_, "psS", [128, 512], F32, 3, psum=True)
            po_r = Rot(nc, st_, "psO", [128, 512], F32, 3, psum=True)
            for hp in range(4):
                for bi_, d in enumerate((1, 4, 16)):
                    n_lo = 2048 // (128 * d)
                    n_hi = 4096 // (128 * d) - 1
                    Vt, Vtk = Vt_r.next()
                    tiles = {}
                    for r in range(d):
                        for m in range(n_lo - 1, n_hi + 1):
                            idx = len(tiles)
                            tiles[(r, m)] = idx
                            u0 = r + d * 128 * m
                            ksl = slice(u0, u0 + d * 127 + 1, d)
                            tpv, tpvk = tpv_r.next()
                            P.pe(lambda e, tpv=tpv, hp=hp, ksl=ksl: e.transpose(out=tpv[:, 0, :], in_=VT[hp][:, ksl], identity=ident_bf[:]),
                                 reads=[("VT", hp), "ident_bf"], writes=[tpvk])
                            P.act(lambda e, Vt=Vt, idx=idx, tpv=tpv: e.activation(
                                out=Vt[:, idx, :].rearrange("p (s c) -> p s c", c=64)[:, 0:3:2, :],
                                in_=tpv[:, 0, :].rearrange("p (s c) -> p s c", c=64), func=AF.Copy),
                                reads=[tpvk], writes=[(Vtk, idx)])
                            osrc = onesc if m < n_lo else onesb
                            P.pool(lambda e, Vt=Vt, idx=idx, osrc=osrc: e.tensor_copy(out=Vt[:, idx, 64:128], in_=osrc[:]),
                                   reads=["onesc", "onesb"], writes=[(Vtk, idx, "o")])
                    pend = []
                    LA = 2
                    for hh in range(2):
                        prow = slice(hh * 64, hh * 64 + 64)
                        vsl = slice(hh * 64, hh * 64 + 128)
                        for r in range(d):
                            for n in range(n_lo, n_hi + 1):
                                q0 = r + d * 128 * n - 2048
                                qsl = slice(q0, q0 + d * 127 + 1, d)
                                ps, psk = ps_r.next()
                                for w_, m in enumerate((n - 1, n)):
                                    u0 = r + d * 128 * m
                                    ksl = slice(u0, u0 + d * 127 + 1, d)
                                    P.pe(lambda e, ps=ps, w_=w_, hp=hp, prow=prow, ksl=ksl, qsl=qsl: e.matmul(
                                        ps[:, w_ * 128:(w_ + 1) * 128], KT[hp][prow, ksl], QT[hp][prow, qsl], start=True, stop=True),
                                        reads=[("KT", hp), ("QT", hp)], writes=[psk])
                                E, Ek = E_r.next()
                                P.act(lambda e, E=E, ps=ps: e.activation(out=E[:], in_=ps[:, 0:256], func=AF.Exp), reads=[psk], writes=[Ek])
                                Pm_, Pmk = Pm_r.next()
                                P.pool(lambda e, Pm_=Pm_, E=E: e.tensor_tensor(out=Pm_[:], in0=E[:], in1=band[:], op=ALU.mult), reads=[Ek, "band"], writes=[Pmk])
                                ai = r * (n_hi - n_lo + 1) + (n - n_lo)

                                def pv(Pm_=Pm_, Pmk=Pmk, r=r, n=n, hh=hh, qsl=qsl, vsl=vsl, ai=ai, Vt=Vt, Vtk=Vtk, tiles=tiles, bi_=bi_):
                                    po, pok = po_r.next()
                                    for w_, m in enumerate((n - 1, n)):
                                        idx = tiles[(r, m)]
                                        P.pe(lambda e, po=po, Vt=Vt, idx=idx, vsl=vsl, Pm_=Pm_, w_=w_: e.matmul(
                                            po[:, 0:128], Vt[:, idx, vsl], Pm_[:, w_ * 128:(w_ + 1) * 128], start=(w_ == 0), stop=(w_ == 1)),
                                            reads=[(Vtk, idx), (Vtk, idx, "o"), Pmk], writes=[pok])
                                    if bi_ == 0:
                                        P.dve(lambda e, po=po, hh=hh, qsl=qsl: e.tensor_copy(out=acc[:, hh, qsl], in_=po[:, 0:128]),
                                              reads=[pok], writes=[("acca", hh, 0, ai)])
                                    else:
                                        P.dve(lambda e, po=po, hh=hh, qsl=qsl: e.tensor_tensor(out=acc[:, hh, qsl], in0=po[:, 0:128], in1=acc[:, hh, qsl], op=ALU.add),
                                              reads=[pok] + [("acca", hh, bi_ - 1, i_) for i_ in range(16)], writes=[("acca", hh, bi_, ai)])
                                pend.append(pv)
                                if len(pend) > LA:
                                    pend.pop(0)()
                    while pend:
                        pend.pop(0)()
                for hh in range(2):
                    nrow = slice(hh * 64, hh * 64 + 64)
                    drow = slice(64 - hh * 64, 128 - hh * 64)
                    akeys = [("acca", hh, b_, i_) for b_ in range(3) for i_ in range(16)]
                    P.act(lambda e, hh=hh, nrow=nrow, drow=drow: e.activation(out=den[nrow, :], in_=acc[drow, hh, :], func=AF.Copy),
                          reads=akeys, writes=[("den", hh)])
                    P.dve(lambda e, nrow=nrow: e.reciprocal(out=den[nrow, :], in_=den[nrow, :]), reads=[("den", hh)], writes=[("den", hh)])
                    P.dve(lambda e, hh=hh, hp=hp, nrow=nrow: e.tensor_tensor(out=mixT[nrow, hp, :], in0=acc[nrow, hh, :], in1=den[nrow, :], op=ALU.mult),
                          reads=akeys + [("den", hh)], writes=[("mixT", hp, hh)])
            P.emit()


def run_skewed(n, stages):
    K = len(stages)
    for step in range(n + K - 1):
        for k, f in enumerate(stages):
            t = step - k
            if 0 <= t < n:
                f(t)


def emit_sparse(nc, P, env, smix):
    xo = env["xo"]; out = env["out"]; mixT = env["mixT"]
    ident_bf = env["ident_bf"]; ident_f = env["ident_f"]
    Xd = env["Xd"]; Yd = env["Yd"]
    NT = 16
    with ExitStack() as st:
        sb = lambda name, shape, dt: st.enter_context(nc.sbuf_tensor(U(name), list(shape), dt))
        acc = env["acc"]; gw = env["gw"]; ridx = env["ridx"]; cnt_i = env["cnt_i"]
        with ExitStack() as sw:
            sbw = lambda name, shape, dt: sw.enter_context(nc.sbuf_tensor(U(name), list(shape), dt))
            wo = sbw("wo", [128, 8, D], BF16)
            rw = sbw("rw", [128, 8, NE], F32)
            rb = sbw("rb", [128, NE], F32)
            gffn = sbw("gffn", [128, 8], F32)
            grow = sbw("grow", [128, D], F32)
            tri = sbw("tri", [128, 128], BF16)
            ones_bf = sbw("ones_bf", [128, 128], BF16)
            iota = sbw("iota", [128, NE], F32)
            run = sbw("run", [128, NE], F32)
            xt_r = Rot(nc, sw, "xt", [128, D], F32, 2)
            junk_r = Rot(nc, sw, "junk", [128, D], BF16, 1)
            hn_r = Rot(nc, sw, "hn", [128, D], F32, 3)
            mrow_r = Rot(nc, sw, "mrow", [128, D], BF16, 5)
            m32_r = Rot(nc, sw, "m32", [128, 8, 128], F32, 3)
            sm_r = Rot(nc, sw, "sm", [128, 8], F32, 12)
            lg_r = Rot(nc, sw, "lg", [128, 4, NE], F32, 3)
            selb_r = Rot(nc, sw, "selb", [128, NE], BF16, 3)
            ix_r = Rot(nc, sw, "ix", [128, 8], mybir.dt.uint32, 3)
            ef_r = Rot(nc, sw, "ef", [128, 12], F32, 3)
            pw_r = Rot(nc, sw, "pw", [128, 2, 512], F32, 2, psum=True)
            pt_r = Rot(nc, sw, "pt", [128, 8, 128], F32, 1, psum=True)
            pl_r = Rot(nc, sw, "pl", [128, 512], F32, 2, psum=True)
            for kc in range(8):
                P.dma("pool", lambda e, kc=kc: e.dma_start(out=wo[:, kc, :], in_=env["w_out"][kc * 128:(kc + 1) * 128, :]), writes=[("wo", kc)])
            P.dma("sp", lambda e: e.dma_start(out=rw[:], in_=env["router_w"].rearrange("(kc p) n -> p kc n", p=128)), writes=["rw"])
            P.dma("sp", lambda e: e.dma_start(out=rb[:], in_=env["router_b"].partition_broadcast(128)), writes=["rb"])
            P.dma("sp", lambda e: e.dma_start(out=gffn[:], in_=env["g_ffn"]), writes=["gffn"])
            P.dma("sp", lambda e: e.dma_start(out=grow[:], in_=env["gffn_row"].partition_broadcast(128)), writes=["grow"])
            P.dma("sp", lambda e: e.dma_start(out=tri[:], in_=env["c_tri"]), writes=["tri"])
            P.dma("sp", lambda e: e.dma_start(out=iota[:], in_=env["c_iota"]), writes=["iota"])
            P.dve(lambda e: e.memset(ones_bf[:], 1.0), writes=["ones_bf"])
            P.dve(lambda e: e.memset(run[:], 0.0), writes=["run"])
            C = [dict() for _ in range(NT)]

            def W0(tt):
                c = C[tt]
                xt, xk = xt_r.next()
                P.dma("sp", lambda e: e.dma_start(out=xt[:], in_=xo[tt * 128:(tt + 1) * 128, :]), writes=[xk])
                pw, pwk = pw_r.next()
                for dh in range(2):
                    for kc in range(8):
                        P.pe(lambda e, dh=dh, kc=kc: e.matmul(pw[:, dh, :], mixT[:, kc, tt * 128:(tt + 1) * 128], wo[:, kc, dh * 512:(dh + 1) * 512],
                                                              start=(kc == 0), stop=(kc == 7)), reads=["mixT", ("wo", kc)], writes=[pwk])
                c.update(xt=xt, xk=xk, pw=pw, pwk=pwk)

            def W1(tt):
                c = C[tt]
                xt, xk, pw, pwk = c["xt"], c["xk"], c["pw"], c["pwk"]
                hk = ("acc", tt)
                P.dve(lambda e: e.tensor_tensor(out=acc[:, tt, :], in0=pw[:].rearrange("p a b -> p (a b)"), in1=xt[:], op=ALU.add), reads=[pwk, xk], writes=[hk])
                junk, jk = junk_r.next()
                sm, smk = sm_r.next()
                P.act(lambda e: e.activation(out=junk[:], in_=acc[:, tt, :], func=AF.Square, accum_out=sm[:, 0:1]), reads=[hk], writes=[jk, smk])
                P.act(lambda e: e.activation(out=sm[:, 1:2], in_=sm[:, 0:1], func=AF.Sqrt, scale=1.0 / D, bias=EPS), reads=[smk], writes=[smk])
                P.dve(lambda e: e.reciprocal(out=sm[:, 2:3], in_=sm[:, 1:2]), reads=[smk], writes=[smk])
                hn, hnk = hn_r.next()
                P.dve(lambda e: e.tensor_scalar(out=hn[:], in0=acc[:, tt, :], scalar1=sm[:, 2:3], scalar2=None, op0=ALU.mult), reads=[hk, smk], writes=[hnk])
                mrow, mrk = mrow_r.next()
                P.pool(lambda e: e.tensor_tensor(out=mrow[:], in0=hn[:], in1=grow[:], op=ALU.mult), reads=[hnk, "grow"], writes=[mrk])
                c.update(hn=hn, hnk=hnk, mrow=mrow, mrk=mrk)

            def W2(tt):
                c = C[tt]
                hn, hnk = c["hn"], c["hnk"]
                pt, ptk = pt_r.next()
                for kc in range(8):
                    P.pe(lambda e, kc=kc: e.transpose(out=pt[:, kc, :], in_=hn[:, kc * 128:(kc + 1) * 128], identity=ident_f[:]), reads=[hnk, "ident_f"], writes=[ptk])
                m32, m32k = m32_r.next()
                P.dve(lambda e: e.tensor_tensor(out=m32[:], in0=pt[:], in1=gffn[:].unsqueeze(2).broadcast_to([128, 8, 128]), op=ALU.mult), reads=[ptk, "gffn"], writes=[m32k])
                c.update(m32=m32, m32k=m32k)

            def W3(tt):
                c = C[tt]
                m32, m32k = c["m32"], c["m32k"]
                pl, plk = pl_r.next()
                for kc in range(8):
                    P.pe(lambda e, kc=kc: e.matmul(pl[:, 0:NE], m32[:, kc, :], rw[:, kc, :], start=(kc == 0), stop=(kc == 7)), reads=[m32k, "rw"], writes=[plk])
                lg, lgk = lg_r.next()
                P.dve(lambda e: e.tensor_tensor(out=lg[:, 0, :], in0=pl[:, 0:NE], in1=rb[:], op=ALU.add), reads=[plk, "rb"], writes=[lgk])
                sm2, sm2k = sm_r.next()
                P.dve(lambda e: e.max(out=sm2[:, 0:8], in_=lg[:, 0, :]), reads=[lgk], writes=[sm2k])
                ix, ixk = ix_r.next()
                P.dve(lambda e: e.max_index(out=ix[:], in_max=sm2[:, 0:8], in_values=lg[:, 0, :]), reads=[lgk, sm2k], writes=[ixk])
                ef, efk = ef_r.next()
                P.dve(lambda e: e.tensor_copy(out=ef[:, 0:4], in_=ix[:, 0:4]), reads=[ixk], writes=[efk])
                selb, selk = selb_r.next()
                P.dve(lambda e: e.tensor_scalar(out=selb[:], in0=lg[:, 0, :], scalar1=sm2[:, 3:4], scalar2=None, op0=ALU.is_ge), reads=[lgk, sm2k], writes=[selk])
                sm3, sm3k = sm_r.next()
                P.dve(lambda e: e.tensor_scalar(out=sm3[:, 0:1], in0=sm2[:, 0:1], scalar1=-1.0, scalar2=None, op0=ALU.mult), reads=[sm2k], writes=[sm3k])
                P.act(lambda e: e.activation(out=sm3[:, 4:8], in_=sm2[:, 0:4], func=AF.Exp, bias=sm3[:, 0:1], scale=1.0), reads=[sm2k, sm3k], writes=[sm3k])
                P.dve(lambda e: e.tensor_reduce(out=sm3[:, 1:2], in_=sm3[:, 4:8], axis=AX.X, op=ALU.add), reads=[sm3k], writes=[sm3k])
                P.dve(lambda e: e.reciprocal(out=sm3[:, 2:3], in_=sm3[:, 1:2]), reads=[sm3k], writes=[sm3k])
                P.dve(lambda e: e.tensor_scalar(out=gw[:, tt, :], in0=sm3[:, 4:8], scalar1=sm3[:, 2:3], scalar2=None, op0=ALU.mult), reads=[sm3k], writes=[("gw", tt)])
                c.update(lg=lg, lgk=lgk, ef=ef, efk=efk, selb=selb, selk=selk)

            def W4(tt):
                c = C[tt]
                lg, lgk, ef, efk, selb, selk, mrow, mrk = c["lg"], c["lgk"], c["ef"], c["efk"], c["selb"], c["selk"], c["mrow"], c["mrk"]
                pl2, pl2k = pl_r.next()
                P.pe(lambda e: e.matmul(pl2[:, 0:NE], tri[:], selb[:], start=True, stop=True), reads=[selk, "tri"], writes=[pl2k])
                P.pe(lambda e: e.matmul(pl2[:, NE:2 * NE], ones_bf[:], selb[:], start=True, stop=True), reads=[selk, "ones_bf"], writes=[pl2k])
                P.dve(lambda e: e.tensor_tensor(out=lg[:, 1, :], in0=pl2[:, 0:NE], in1=run[:], op=ALU.add), reads=[pl2k, "run", lgk], writes=[lgk])
                P.dve(lambda e: e.tensor_tensor(out=run[:], in0=pl2[:, NE:2 * NE], in1=run[:], op=ALU.add), reads=[pl2k, "run", lgk], writes=["run"])
                for k in range(4):
                    P.dve(lambda e, k=k: e.scalar_tensor_tensor(out=lg[:, 2, :], in0=iota[:], scalar=ef[:, k:k + 1], in1=lg[:, 1, :],
                                                                 op0=ALU.is_equal, op1=ALU.mult, accum_out=ef[:, 4 + k:5 + k]),
                          reads=[lgk, efk, "iota"], writes=[lgk, efk])
                P.dve(lambda e: e.scalar_tensor_tensor(out=ef[:, 8:12], in0=ef[:, 0:4], scalar=float(TOK), in1=ef[:, 4:8], op0=ALU.mult, op1=ALU.add),
                      reads=[efk], writes=[efk])
                P.dve(lambda e: e.tensor_scalar(out=ridx[:, tt, :], in0=ef[:, 8:12], scalar1=-1.0, scalar2=None, op0=ALU.add), reads=[efk], writes=[("ridx", tt)])
                for k in range(4):
                    P.dma("pool", lambda e, k=k: e.indirect_dma_start(
                        out=Xd[:, :], out_offset=bass.IndirectOffsetOnAxis(ap=ridx[:, tt, k:k + 1], axis=0), in_=mrow[:, :], in_offset=None),
                        reads=[mrk, ("ridx", tt)], writes=[("Xd", tt, k)])

            run_skewed(NT, [W0, W1, W2, W3, W4])
            P.dve(lambda e: e.tensor_copy(out=cnt_i[:], in_=run[0:1, :]), reads=["run"], writes=["cnt_i"])
            zb = sbw("zb", [128, 2 * NE], F32)
            zl = sbw("zl", [128, 2 * NE], F32)
            zf = sbw("zf", [128, 2 * NE], F32)
            zm = sbw("zm", [128, 2 * NE], F32)
            zi = sbw("zi", [128, 2 * NE], I32)
            zrow = sbw("zrow", [128, D], BF16)
            P.dma("sp", lambda e: e.dma_start(out=zb[:], in_=env["c_zbase"]), writes=["zb"])
            P.dma("sp", lambda e: e.dma_start(out=zl[:], in_=env["c_zlim"]), writes=["zl"])
            P.dve(lambda e: e.memset(zrow[:], 0.0), writes=["zrow"])
            P.dve(lambda e: e.tensor_tensor(out=zf[:].rearrange("p (a b) -> p a b", b=2), in0=zb[:].rearrange("p (a b) -> p a b", b=2),
                                            in1=run[:].unsqueeze(2).broadcast_to([128, NE, 2]), op=ALU.add), reads=["zb", "run"], writes=["zf"])
            P.dve(lambda e: e.tensor_tensor(out=zm[:], in0=zf[:], in1=zl[:], op=ALU.is_ge), reads=["zf", "zl"], writes=["zm"])
            zt = sbw("zt", [128, 2 * NE], F32)
            P.dma("sp", lambda e: e.dma_start(out=zt[:], in_=env["c_ztrash"]), writes=["zt"])
            P.dve(lambda e: e.tensor_tensor(out=zt[:], in0=zt[:], in1=zf[:], op=ALU.subtract), reads=["zt", "zf"], writes=["zt"])
            P.dve(lambda e: e.tensor_tensor(out=zt[:], in0=zt[:], in1=zm[:], op=ALU.mult), reads=["zt", "zm"], writes=["zt"])
            P.dve(lambda e: e.tensor_tensor(out=zf[:], in0=zf[:], in1=zt[:], op=ALU.add), reads=["zt", "zf"], writes=["zf"])
            P.dve(lambda e: e.tensor_copy(out=zi[:], in_=zf[:]), reads=["zf"], writes=["zi"])
            for c_ in range(2 * NE):
                P.dma("pool", lambda e, c_=c_: e.indirect_dma_start(
                    out=Xd[:, :], out_offset=bass.IndirectOffsetOnAxis(ap=zi[:, c_:c_ + 1], axis=0), in_=zrow[:, :], in_offset=None),
                    reads=["zi", "zrow"], writes=[("Xz", c_)])
            P.emit()
        smix.close()

        with ExitStack() as se:
            sbe = lambda name, shape, dt: se.enter_context(nc.sbuf_tensor(U(name), list(shape), dt))
            wg = [sbe("wg%d" % i, [128, 8, 2 * D], BF16) for i in range(2)]
            wd = [sbe("wd%d" % i, [128, 8, D], BF16) for i in range(2)]
            bgr = sbe("bgr", [33, 2 * D], BF16)
            bdr = sbe("bdr", [33, D], BF16)
            ones_row = sbe("ones_row", [33, 128], BF16)
            xb_r = Rot(nc, se, "xb", [128, D], BF16, 2)
            xT_pre = [sbe("xTp%d" % i, [128, 8, 128], BF16) for i in range(2)]
            xT_r = Rot(nc, se, "xT", [128, 8, 128], BF16, 2)
            g_r = Rot(nc, se, "g", [128, 512], BF16, 2)
            sg_r = Rot(nc, se, "sg", [128, 512], BF16, 2)
            u_r = Rot(nc, se, "u", [128, 512], BF16, 2)
            actb_r = Rot(nc, se, "actb", [128, D], BF16, 1)
            actT_r = Rot(nc, se, "actT", [128, 8, 128], BF16, 1)
            ysb_r = Rot(nc, se, "ysb", [128, D], F32, 1)
            tpx_r = Rot(nc, se, "tpx", [128, 8, 128], BF16, 1, psum=True)
            pg_r = Rot(nc, se, "pgg", [128, 512], F32, 2, psum=True)
            pu_r = Rot(nc, se, "pgu", [128, 512], F32, 2, psum=True)
            tpa_r = Rot(nc, se, "tpa", [128, 8, 128], BF16, 1, psum=True)
            pd_r = Rot(nc, se, "pd", [128, 2, 512], F32, 1, psum=True)
            P.dve(lambda e: e.memset(ones_row[:], 1.0), writes=["ones_row"])
            xd_keys = [("Xd", tt, k) for tt in range(NT) for k in range(4)] + [("Xz", c_) for c_ in range(2 * NE)]
            P.add("sp", lambda e: e.nop(), reads=xd_keys, writes=["Xall"])
            engs = ("pe", "act", "dve", "sp")

            def load_weights(e_):
                par = e_ % 2
                pp = 32 * par
                for kc in range(8):
                    P.dma("pool", lambda e, kc=kc, par=par, e_=e_: e.dma_start(out=wg[par][:, kc, :], in_=env["w_gu_nat"][e_, kc * 128:(kc + 1) * 128, :]),
                          writes=[("wg", par, kc)], wstream=True)
                for fc in range(8):
                    P.dma("pool", lambda e, fc=fc, par=par, e_=e_: e.dma_start(out=wd[par][:, fc, :], in_=env["w_down"][e_, fc * 128:(fc + 1) * 128, :]),
                          writes=[("wd", par, fc)], wstream=True)
                P.dma("pool", lambda e, pp=pp, e_=e_: e.dma_start(out=bgr[pp:pp + 1, :], in_=env["b_gu_nat"][e_:e_ + 1, :]), writes=[("bgr", par)], wstream=True)
                P.dma("pool", lambda e, pp=pp, e_=e_: e.dma_start(out=bdr[pp:pp + 1, :], in_=env["b_down"][e_:e_ + 1, :]), writes=[("bdr", par)], wstream=True)

            xb_pre = [sbe("xbp%d" % i, [128, D], BF16) for i in range(2)]

            def xload(e_, k, xb, xbk):
                r0 = e_ * TOK + k * 128
                P.dma("sp", lambda e, xb=xb, r0=r0: e.dma_start(out=xb[:], in_=Xd[r0:r0 + 128, :]), writes=[xbk])

            def xtrans(xb, xbk, xT, xTk):
                tpx, tpxk = tpx_r.next()
                for kc in range(8):
                    P.pe(lambda e, tpx=tpx, xb=xb, kc=kc: e.transpose(out=tpx[:, kc, :], in_=xb[:, kc * 128:(kc + 1) * 128], identity=ident_bf[:]),
                         reads=[xbk, "ident_bf"], writes=[tpxk])
                P.dve(lambda e, xT=xT, tpx=tpx: e.tensor_copy(out=xT[:], in_=tpx[:]), reads=[tpxk], writes=[xTk])

            def xprep(e_, k, xT, xTk):
                xb, xbk = xb_r.next()
                xload(e_, k, xb, xbk)
                xtrans(xb, xbk, xT, xTk)

            load_weights(0)
            xload(0, 0, xb_pre[0], ("xbp", 0))
            ykeys = []
            for e_ in range(NE):
                par = e_ % 2
                pp = 32 * par
                if e_ + 1 < NE:
                    load_weights(e_ + 1)
                    xload(e_ + 1, 0, xb_pre[1 - par], ("xbp", 1 - par))
                xtrans(xb_pre[par], ("xbp", par), xT_pre[par], ("xTp", par))
                for en in engs:
                    P.regload(en, "n", cnt_i[0:1, e_:e_ + 1], reads=["cnt_i"])
                cur = (xT_pre[par], ("xTp", par))
                for k in range(TOK // 128):
                    P.cur_region = ("n", 128 * k)
                    r0 = e_ * TOK + k * 128
                    xT, xTk = cur
                    actb, actbk = actb_r.next()
                    for hf in range(2):
                        pg, pgk = pg_r.next()
                        pu, puk = pu_r.next()
                        for (pp_, ppk, c0) in ((pg, pgk, hf * 512), (pu, puk, D + hf * 512)):
                            for kc in range(8):
                                P.pe(lambda e, pp_=pp_, xT=xT, kc=kc, c0=c0, par=par: e.matmul(pp_[:], xT[:, kc, :], wg[par][:, kc, c0:c0 + 512],
                                                                                             start=(kc == 0), stop=False),
                                     reads=[xTk, ("wg", par, kc)], writes=[ppk])
                            P.pe(lambda e, pp_=pp_, c0=c0, pp=pp: e.matmul(pp_[:], ones_row[pp:pp + 1, :], bgr[pp:pp + 1, c0:c0 + 512], start=False, stop=True),
                                 reads=["ones_row", ("bgr", par)], writes=[ppk])
                        g, gk_ = g_r.next()
                        sg, sgk = sg_r.next()
                        u, uk = u_r.next()
                        P.dve(lambda e, g=g, pg=pg: e.tensor_scalar(out=g[:], in0=pg[:], scalar1=7.0, scalar2=None, op0=ALU.min), reads=[pgk], writes=[gk_])
                        P.act(lambda e, g=g, sg=sg: e.activation(out=sg[:], in_=g[:], func=AF.Sigmoid, scale=1.702), reads=[gk_], writes=[sgk])
                        P.dve(lambda e, u=u, pu=pu: e.tensor_scalar(out=u[:], in0=pu[:], scalar1=7.0, scalar2=-7.0, op0=ALU.min, op1=ALU.max), reads=[puk], writes=[uk])
                        P.dve(lambda e, g=g, sg=sg: e.tensor_tensor(out=g[:], in0=g[:], in1=sg[:], op=ALU.mult), reads=[gk_, sgk], writes=[gk_])
                        P.dve(lambda e, g=g, u=u, actb=actb, hf=hf: e.scalar_tensor_tensor(out=actb[:, hf * 512:(hf + 1) * 512], in0=u[:], scalar=1.0, in1=g[:],
                                                                                          op0=ALU.add, op1=ALU.mult), reads=[gk_, uk], writes=[(actbk, hf)])
                    if k + 1 < TOK // 128:
                        nxt = xT_r.next()
                        xprep(e_, k + 1, nxt[0], nxt[1])
                        cur = nxt
                    tpa, tpak = tpa_r.next()
                    for fc in range(8):
                        P.pe(lambda e, tpa=tpa, actb=actb, fc=fc: e.transpose(out=tpa[:, fc, :], in_=actb[:, fc * 128:(fc + 1) * 128], identity=ident_bf[:]),
                             reads=[(actbk, fc // 4), "ident_bf"], writes=[tpak])
                    actT, actTk = actT_r.next()
                    P.dve(lambda e, actT=actT, tpa=tpa: e.tensor_copy(out=actT[:], in_=tpa[:]), reads=[tpak], writes=[actTk])
                    pd, pdk = pd_r.next()
                    for dh in range(2):
                        for fc in range(8):
                            P.pe(lambda e, pd=pd, actT=actT, fc=fc, dh=dh, par=par: e.matmul(pd[:, dh, :], actT[:, fc, :], wd[par][:, fc, dh * 512:(dh + 1) * 512],
                                                                                            start=(fc == 0), stop=False),
                                 reads=[actTk, ("wd", par, fc)], writes=[pdk])
                        P.pe(lambda e, pd=pd, dh=dh, pp=pp: e.matmul(pd[:, dh, :], ones_row[pp:pp + 1, :], bdr[pp:pp + 1, dh * 512:(dh + 1) * 512], start=False, stop=True),
                             reads=["ones_row", ("bdr", par)], writes=[pdk])
                    ysb, ysbk = ysb_r.next()
                    P.dve(lambda e, ysb=ysb, pd=pd: e.tensor_copy(out=ysb[:], in_=pd[:].rearrange("p a b -> p (a b)")), reads=[pdk], writes=[ysbk])
                    yk = ("Yd", e_, k)
                    P.dma("sp", lambda e, ysb=ysb, r0=r0: e.dma_start(out=Yd[r0:r0 + 128, :], in_=ysb[:]), reads=[ysbk], writes=[yk])
                    ykeys.append(yk)
                    P.cur_region = None
            P.add("pool", lambda e: e.nop(), reads=ykeys, writes=["Yall"])
            P.emit()

        def combine(sf):
            yg_r = Rot(nc, sf, "yg", [128, D], F32, 8)

            def tile(tt):
                for k in range(4):
                    yg, ygk = yg_r.next()
                    P.dma("pool", lambda e, yg=yg, k=k: e.indirect_dma_start(
                        out=yg[:, :], out_offset=None, in_=Yd[:, :], in_offset=bass.IndirectOffsetOnAxis(ap=ridx[:, tt, k:k + 1], axis=0)),
                        reads=["Yall", ("ridx", tt)], writes=[ygk])
                    P.dve(lambda e, yg=yg, k=k: e.scalar_tensor_tensor(out=acc[:, tt, :], in0=yg[:], scalar=gw[:, tt, k:k + 1], in1=acc[:, tt, :],
                                                                       op0=ALU.mult, op1=ALU.add), reads=[ygk, ("gw", tt), ("acc", tt)], writes=[("acc", tt)])
            return tile
        emit_ple(nc, P, env, acc, 0, NT, pre=combine)


def emit_half(nc, P, env, half):
    xo = env["xo"]; out = env["out"]; mixT = env["mixT"]
    ident_bf = env["ident_bf"]; ident_f = env["ident_f"]
    HT = 8
    t0 = half * HT
    with ExitStack() as st:
        sb = lambda name, shape, dt: st.enter_context(nc.sbuf_tensor(U(name), list(shape), dt))
        acc = sb("acc", [128, HT, D], F32)
        mT = sb("mT", [128, 8, 1024], BF16)
        gates = sb("gates", [128, HT, NE], F32)
        gT = sb("gT", [NE, 1024], F32)
        bd = sb("bd", [NE, D], F32)
        bg = sb("bg", [128, NE, 16], F32)
        rb = sb("rb", [128, NE], F32)
        gffn = sb("gffn", [128, 8], F32)
        with ExitStack() as sw:
            sbw = lambda name, shape, dt: sw.enter_context(nc.sbuf_tensor(U(name), list(shape), dt))
            wo = sbw("wo", [128, 8, D], BF16)
            rw = sbw("rw", [128, 8, NE], F32)
            xt_r = Rot(nc, sw, "xt", [128, D], F32, 2)
            junk_r = Rot(nc, sw, "junk", [128, D], BF16, 2)
            hn_r = Rot(nc, sw, "hn", [128, D], F32, 2)
            m32_r = Rot(nc, sw, "m32", [128, 8, 128], F32, 2)
            sm_r = Rot(nc, sw, "sm", [128, 8], F32, 4)
            lg_r = Rot(nc, sw, "lg", [128, 3, NE], F32, 2)
            pw_r = Rot(nc, sw, "pw", [128, 2, 512], F32, 2, psum=True)
            pt_r = Rot(nc, sw, "pt", [128, 8, 128], F32, 1, psum=True)
            pl_r = Rot(nc, sw, "pl", [128, 512], F32, 2, psum=True)
            for kc in range(8):
                P.dma("pool", lambda e, kc=kc: e.dma_start(out=wo[:, kc, :], in_=env["w_out"][kc * 128:(kc + 1) * 128, :]),
                      writes=[("wo", kc)])
            P.dma("sp", lambda e: e.dma_start(out=rw[:], in_=env["router_w"].rearrange("(kc p) n -> p kc n", p=128)), writes=["rw"])
            P.dma("sp", lambda e: e.dma_start(out=rb[:], in_=env["router_b"].partition_broadcast(128)), writes=["rb"])
            P.dma("sp", lambda e: e.dma_start(out=gffn[:], in_=env["g_ffn"]), writes=["gffn"])
            P.dma("sp", lambda e: e.dma_start(out=bd[:], in_=env["b_down"]), writes=["bd"])
            P.dma("sp", lambda e: e.dma_start(out=bg[:], in_=env["bgu"]), writes=["bg"])
            P.dve(lambda e: e.tensor_scalar(out=bg[:, :, 8:16], in0=bg[:, :, 8:16], scalar1=1.0, scalar2=None, op0=ALU.add),
                  reads=["bg"], writes=["bg"])
            for tt in range(HT):
                gt = t0 + tt
                xt, xk = xt_r.next()
                P.dma("sp", lambda e, xt=xt, gt=gt: e.dma_start(out=xt[:], in_=xo[gt * 128:(gt + 1) * 128, :]), writes=[xk])
                pw, pwk = pw_r.next()
                for dh in range(2):
                    for kc in range(8):
                        P.pe(lambda e, pw=pw, dh=dh, kc=kc, gt=gt: e.matmul(
                            pw[:, dh, :], mixT[:, kc, gt * 128:(gt + 1) * 128], wo[:, kc, dh * 512:(dh + 1) * 512],
                            start=(kc == 0), stop=(kc == 7)), reads=["mixT", ("wo", kc)], writes=[pwk])
                hk = ("acc", tt)
                P.dve(lambda e, pw=pw, xt=xt, tt=tt: e.tensor_tensor(
                    out=acc[:, tt, :], in0=pw[:].rearrange("p a b -> p (a b)"), in1=xt[:], op=ALU.add),
                    reads=[pwk, xk], writes=[hk])
                junk, jk = junk_r.next()
                sm, smk = sm_r.next()
                P.act(lambda e, junk=junk, sm=sm, tt=tt: e.activation(out=junk[:], in_=acc[:, tt, :], func=AF.Square, accum_out=sm[:, 0:1]),
                      reads=[hk], writes=[jk, smk])
                P.act(lambda e, sm=sm: e.activation(out=sm[:, 1:2], in_=sm[:, 0:1], func=AF.Sqrt, scale=1.0 / D, bias=EPS),
                      reads=[smk], writes=[smk])
                P.dve(lambda e, sm=sm: e.reciprocal(out=sm[:, 2:3], in_=sm[:, 1:2]), reads=[smk], writes=[smk])
                hn, hnk = hn_r.next()
                P.dve(lambda e, hn=hn, sm=sm, tt=tt: e.tensor_scalar(out=hn[:], in0=acc[:, tt, :], scalar1=sm[:, 2:3], scalar2=None, op0=ALU.mult),
                      reads=[hk, smk], writes=[hnk])
                pt, ptk = pt_r.next()
                for kc in range(8):
                    P.pe(lambda e, pt=pt, hn=hn, kc=kc: e.transpose(out=pt[:, kc, :], in_=hn[:, kc * 128:(kc + 1) * 128], identity=ident_f[:]),
                         reads=[hnk, "ident_f"], writes=[ptk])
                m32, m32k = m32_r.next()
                P.dve(lambda e, pt=pt, m32=m32: e.tensor_tensor(out=m32[:], in0=pt[:], in1=gffn[:].unsqueeze(2).broadcast_to([128, 8, 128]), op=ALU.mult),
                      reads=[ptk, "gffn"], writes=[m32k])
                P.act(lambda e, m32=m32, tt=tt: e.activation(out=mT[:, :, tt * 128:(tt + 1) * 128], in_=m32[:], func=AF.Copy),
                      reads=[m32k], writes=[("mT", tt)])
                pl, plk = pl_r.next()
                for kc in range(8):
                    P.pe(lambda e, pl=pl, m32=m32, kc=kc: e.matmul(pl[:, 0:NE], m32[:, kc, :], rw[:, kc, :], start=(kc == 0), stop=(kc == 7)),
                         reads=[m32k, "rw"], writes=[plk])
                lg, lgk = lg_r.next()
                P.dve(lambda e, pl=pl, lg=lg: e.tensor_tensor(out=lg[:, 0, :], in0=pl[:, 0:NE], in1=rb[:], op=ALU.add),
                      reads=[plk, "rb"], writes=[lgk])
                sm2, sm2k = sm_r.next()
                P.dve(lambda e, lg=lg, sm2=sm2: e.max(out=sm2[:, 0:8], in_=lg[:, 0, :]), reads=[lgk], writes=[sm2k])
                P.dve(lambda e, lg=lg, sm2=sm2: e.tensor_scalar(out=lg[:, 1, :], in0=lg[:, 0, :], scalar1=sm2[:, 3:4], scalar2=None, op0=ALU.is_ge),
                      reads=[lgk, sm2k], writes=[lgk])
                sm3, sm3k = sm_r.next()
                P.dve(lambda e, sm2=sm2, sm3=sm3: e.tensor_scalar(out=sm3[:, 0:1], in0=sm2[:, 0:1], scalar1=-1.0, scalar2=None, op0=ALU.mult),
                      reads=[sm2k], writes=[sm3k])
                P.act(lambda e, lg=lg, sm3=sm3: e.activation(out=lg[:, 2, :], in_=lg[:, 0, :], func=AF.Exp, bias=sm3[:, 0:1], scale=1.0),
                      reads=[lgk, sm3k], writes=[lgk])
                P.dve(lambda e, lg=lg: e.tensor_tensor(out=lg[:, 2, :], in0=lg[:, 2, :], in1=lg[:, 1, :], op=ALU.mult), reads=[lgk], writes=[lgk])
                P.dve(lambda e, lg=lg, sm3=sm3: e.tensor_reduce(out=sm3[:, 1:2], in_=lg[:, 2, :], axis=AX.X, op=ALU.add), reads=[lgk, sm3k], writes=[sm3k])
                P.dve(lambda e, sm3=sm3: e.reciprocal(out=sm3[:, 2:3], in_=sm3[:, 1:2]), reads=[sm3k], writes=[sm3k])
                P.dve(lambda e, lg=lg, sm3=sm3, tt=tt: e.tensor_scalar(out=gates[:, tt, :], in0=lg[:, 2, :], scalar1=sm3[:, 2:3], scalar2=None, op0=ALU.mult),
                      reads=[lgk, sm3k], writes=[("gates", tt)])
                pl2, pl2k = pl_r.next()
                P.pe(lambda e, pl2=pl2, tt=tt: e.transpose(out=pl2[0:NE, 0:128], in_=gates[:, tt, :], identity=ident_f[:]),
                     reads=[("gates", tt), "ident_f"], writes=[pl2k])
                P.act(lambda e, pl2=pl2, tt=tt: e.activation(out=gT[:, tt * 128:(tt + 1) * 128], in_=pl2[0:NE, 0:128], func=AF.Copy),
                      reads=[pl2k], writes=[("gT", tt)])
                pw2, pw2k = pw_r.next()
                for dh in range(2):
                    P.pe(lambda e, pw2=pw2, dh=dh, tt=tt: e.matmul(pw2[:, dh, :], gT[:, tt * 128:(tt + 1) * 128], bd[:, dh * 512:(dh + 1) * 512],
                                                                     start=True, stop=True), reads=[("gT", tt), "bd"], writes=[pw2k])
                P.dve(lambda e, pw2=pw2, tt=tt: e.tensor_tensor(out=acc[:, tt, :], in0=pw2[:].rearrange("p a b -> p (a b)"), in1=acc[:, tt, :], op=ALU.add),
                      reads=[pw2k, hk], writes=[hk])
            P.emit()

        with ExitStack() as se:
            sbe = lambda name, shape, dt: se.enter_context(nc.sbuf_tensor(U(name), list(shape), dt))
            NSLOT = 9
            ring = [sbe("ring%d" % i, [128, 4096], BF16) for i in range(NSLOT)]
            actT = sbe("actT", [128, 8, 1024], BF16)
            g_r = Rot(nc, se, "g", [128, 512], F32, 2)
            sg_r = Rot(nc, se, "sg", [128, 512], F32, 2)
            u_r = Rot(nc, se, "u", [128, 512], F32, 2)
            pg_r = Rot(nc, se, "pg", [128, 512], F32, 2, psum=True)
            pu_r = Rot(nc, se, "pu", [128, 512], F32, 2, psum=True)
            pd_r = Rot(nc, se, "pd", [128, 2, 512], F32, 2, psum=True)
            pieces = []
            for e_ in range(NE):
                for j in range(4):
                    pieces.append(("gu", e_, j))
                for j in range(2):
                    pieces.append(("dn", e_, j))
            state = {"next": 0, "mark": -1}
            last_reader = {}
            finished = set()

            def issue_loads():
                while state["next"] < len(pieces):
                    i = state["next"]
                    if i >= NSLOT:
                        prev = i - NSLOT
                        if prev not in finished or last_reader[prev] > state["mark"]:
                            return
                    kind, e_, j = pieces[i]
                    slot = ring[i % NSLOT]
                    key = ("ring", i % NSLOT)
                    if kind == "gu":
                        src = env["wgu"][e_, j]
                        P.dma("pool", lambda e, slot=slot, src=src: e.dma_start(out=slot[:], in_=src, max_dma_last_dim=8192), writes=[key])
                    else:
                        src = env["w_down"][e_].rearrange("(fc p) d -> p fc d", p=128)[:, 4 * j:4 * j + 4, :]
                        P.dma("pool", lambda e, slot=slot, src=src: e.dma_start(out=slot[:].rearrange("p (fc d) -> p fc d", fc=4), in_=src), writes=[key])
                    state["next"] += 1

            issue_loads()
            for e_ in range(NE):
                base = e_ * 6
                for fc in range(8):
                    j = fc // 2
                    pi = base + j
                    assert pi < state["next"]
                    slot = ring[pi % NSLOT]
                    skey = ("ring", pi % NSLOT)
                    goff = (fc % 2) * 128
                    uoff = 256 + (fc % 2) * 128
                    for tb in range(2):
                        pg, pgk = pg_r.next()
                        pu, puk = pu_r.next()
                        lastop = None
                        for (pp, ppk, off) in ((pg, pgk, goff), (pu, puk, uoff)):
                            for kc in range(8):
                                lastop = P.pe(lambda e, pp=pp, slot=slot, kc=kc, off=off, tb=tb: e.matmul(
                                    pp[:], slot[:, kc * 512 + off: kc * 512 + off + 128], mT[:, kc, tb * 512:(tb + 1) * 512],
                                    start=(kc == 0), stop=(kc == 7)),
                                    reads=[skey] + [("mT", tb * 4 + q) for q in range(4)], writes=[ppk])
                        last_reader[pi] = lastop.idx
                        if fc % 2 == 1 and tb == 1:
                            finished.add(pi)
                        g, gk_ = g_r.next()
                        sg, sgk = sg_r.next()
                        u, uk = u_r.next()
                        P.dve(lambda e, g=g, pg=pg, e_=e_, fc=fc: e.tensor_scalar(out=g[:], in0=pg[:], scalar1=bg[:, e_, fc:fc + 1], scalar2=7.0,
                                                                                    op0=ALU.add, op1=ALU.min), reads=[pgk, "bg"], writes=[gk_])
                        P.act(lambda e, g=g, sg=sg: e.activation(out=sg[:], in_=g[:], func=AF.Sigmoid, scale=1.702), reads=[gk_], writes=[sgk])
                        P.act(lambda e, u=u, pu=pu, e_=e_, fc=fc: e.activation(out=u[:], in_=pu[:], func=AF.Identity, bias=bg[:, e_, 8 + fc:9 + fc], scale=1.0),
                              reads=[puk, "bg"], writes=[uk])
                        P.pool(lambda e, u=u: e.tensor_scalar(out=u[:], in0=u[:], scalar1=8.0, scalar2=-6.0, op0=ALU.min, op1=ALU.max),
                               reads=[uk], writes=[uk])
                        P.pool(lambda e, g=g, sg=sg: e.tensor_tensor(out=g[:], in0=g[:], in1=sg[:], op=ALU.mult), reads=[gk_, sgk], writes=[gk_])
                        P.pool(lambda e, g=g, u=u, fc=fc, tb=tb: e.tensor_tensor(out=actT[:, fc, tb * 512:(tb + 1) * 512], in0=g[:], in1=u[:], op=ALU.mult),
                               reads=[gk_, uk], writes=[("actT", fc, tb)])
                        state["mark"] = lastop.idx
                        issue_loads()
                p0 = base + 4
                for tt in range(HT):
                    pd, pdk = pd_r.next()
                    lastop = None
                    for dh in range(2):
                        for fc in range(8):
                            pj = p0 + fc // 4
                            slot = ring[pj % NSLOT]
                            lastop = P.pe(lambda e, pd=pd, dh=dh, fc=fc, tt=tt, slot=slot: e.matmul(
                                pd[:, dh, :], actT[:, fc, tt * 128:(tt + 1) * 128],
                                slot[:, (fc % 4) * 1024 + dh * 512:(fc % 4) * 1024 + (dh + 1) * 512],
                                start=(fc == 0), stop=(fc == 7)),
                                reads=[("ring", pj % NSLOT), ("actT", fc, tt // 4)], writes=[pdk])
                            last_reader[pj] = lastop.idx
                    P.dve(lambda e, pd=pd, tt=tt, e_=e_: e.scalar_tensor_tensor(
                        out=acc[:, tt, :], in0=pd[:].rearrange("p a b -> p (a b)"), scalar=gates[:, tt, e_:e_ + 1], in1=acc[:, tt, :],
                        op0=ALU.mult, op1=ALU.add), reads=[pdk, ("gates", tt), ("acc", tt)], writes=[("acc", tt)])
                finished.add(p0)
                finished.add(p0 + 1)
            P.emit()

        emit_ple(nc, P, env, acc, t0, HT)


def emit_ple(nc, P, env, acc, t0, HT, pre=None):
    out = env["out"]; ident_bf = env["ident_bf"]
    with ExitStack() as sf:
        pre_tile = pre(sf) if pre is not None else None
        sbf = lambda name, shape, dt: sf.enter_context(nc.sbuf_tensor(U(name), list(shape), dt))
        wg = sbf("wg", [128, 8, D], BF16)
        wp = sbf("wp", [128, 2, D], BF16)
        hb_r = Rot(nc, sf, "hb", [128, D], BF16, 3)
        hT_r = Rot(nc, sf, "hT", [128, 8, 128], BF16, 3)
        pt_r = Rot(nc, sf, "ptile", [128, 256], F32, 3)
        pb_r = Rot(nc, sf, "pb", [128, 256], BF16, 3)
        pT_r = Rot(nc, sf, "pT", [128, 2, 128], BF16, 3)
        sgo_r = Rot(nc, sf, "sgo", [128, D], F32, 2)
        o_r = Rot(nc, sf, "o", [128, D], F32, 2)
        ptp_r = Rot(nc, sf, "ptp", [128, 8, 128], BF16, 2, psum=True)
        ptq_r = Rot(nc, sf, "ptq", [128, 8, 128], BF16, 1, psum=True)
        pgp_r = Rot(nc, sf, "pgp", [128, 2, 512], F32, 1, psum=True)
        ppp_r = Rot(nc, sf, "ppp", [128, 2, 512], F32, 1, psum=True)
        for kc in range(8):
            P.dma("pool", lambda e, kc=kc: e.dma_start(out=wg[:, kc, :], in_=env["ple_gate"][kc * 128:(kc + 1) * 128, :]), writes=[("wg", kc)])
        for kc in range(2):
            P.dma("pool", lambda e, kc=kc: e.dma_start(out=wp[:, kc, :], in_=env["ple_proj"][kc * 128:(kc + 1) * 128, :]), writes=[("wp", kc)])
        C = [dict() for _ in range(HT)]

        def F0(tt):
            c = C[tt]
            if pre_tile is not None:
                pre_tile(tt)
            ptile, ptk = pt_r.next()
            P.dma("sp", lambda e: e.dma_start(out=ptile[:], in_=env["p_own"][(t0 + tt) * 128:(t0 + tt + 1) * 128, :]), writes=[ptk])
            c.update(ptile=ptile, ptk=ptk)

        def F1(tt):
            c = C[tt]
            hb, hbk = hb_r.next()
            P.act(lambda e: e.activation(out=hb[:], in_=acc[:, tt, :], func=AF.Copy), reads=[("acc", tt)], writes=[hbk])
            pb, pbk = pb_r.next()
            P.act(lambda e: e.activation(out=pb[:], in_=c["ptile"][:], func=AF.Copy), reads=[c["ptk"]], writes=[pbk])
            c.update(hb=hb, hbk=hbk, pb=pb, pbk=pbk)

        def F2(tt):
            c = C[tt]
            hb, hbk, pb, pbk = c["hb"], c["hbk"], c["pb"], c["pbk"]
            ptp, ptpk = ptp_r.next()
            for kc in range(8):
                P.pe(lambda e, kc=kc: e.transpose(out=ptp[:, kc, :], in_=hb[:, kc * 128:(kc + 1) * 128], identity=ident_bf[:]), reads=[hbk, "ident_bf"], writes=[ptpk])
            ptq, ptqk = ptq_r.next()
            for kc in range(2):
                P.pe(lambda e, kc=kc: e.transpose(out=ptq[:, kc, :], in_=pb[:, kc * 128:(kc + 1) * 128], identity=ident_bf[:]), reads=[pbk, "ident_bf"], writes=[ptqk])
            hT, hTk = hT_r.next()
            P.dve(lambda e: e.tensor_copy(out=hT[:], in_=ptp[:]), reads=[ptpk], writes=[hTk])
            pT, pTk = pT_r.next()
            P.dve(lambda e: e.tensor_copy(out=pT[:], in_=ptq[:, 0:2, :]), reads=[ptqk], writes=[pTk])
            c.update(hT=hT, hTk=hTk, pT=pT, pTk=pTk)

        def F3(tt):
            c = C[tt]
            hT, hTk, pT, pTk = c["hT"], c["hTk"], c["pT"], c["pTk"]
            pgp, pgpk = pgp_r.next()
            ppp, pppk = ppp_r.next()
            for dh in range(2):
                for kc in range(8):
                    P.pe(lambda e, kc=kc, dh=dh: e.matmul(pgp[:, dh, :], hT[:, kc, :], wg[:, kc, dh * 512:(dh + 1) * 512], start=(kc == 0), stop=(kc == 7)),
                         reads=[hTk, ("wg", kc)], writes=[pgpk])
                for kc in range(2):
                    P.pe(lambda e, kc=kc, dh=dh: e.matmul(ppp[:, dh, :], pT[:, kc, :], wp[:, kc, dh * 512:(dh + 1) * 512], start=(kc == 0), stop=(kc == 1)),
                         reads=[pTk, ("wp", kc)], writes=[pppk])
            sgo, sgok = sgo_r.next()
            P.act(lambda e: e.activation(out=sgo[:], in_=pgp[:].rearrange("p a b -> p (a b)"), func=AF.Sigmoid), reads=[pgpk], writes=[sgok])
            P.dve(lambda e: e.tensor_tensor(out=sgo[:], in0=ppp[:].rearrange("p a b -> p (a b)"), in1=sgo[:], op=ALU.mult), reads=[pppk, sgok], writes=[sgok])
            o, ok = o_r.next()
            P.pool(lambda e: e.tensor_tensor(out=o[:], in0=sgo[:], in1=acc[:, tt, :], op=ALU.add), reads=[sgok, ("acc", tt)], writes=[ok])
            P.dma("sp", lambda e: e.dma_start(out=out[(t0 + tt) * 128:(t0 + tt + 1) * 128, :], in_=o[:]), reads=[ok], writes=[("out", t0 + tt)])

        run_skewed(HT, [F0, F1, F2, F3])
        P.add("sp", lambda e: e.nop(), reads=[("out", t0 + tt) for tt in range(HT)])
        P.emit()


def _consts():
    c = {}
    c["c_ident_bf"] = np.eye(128, dtype=np.float32).astype(ml_dtypes.bfloat16)
    c["c_ident_f"] = np.eye(128, dtype=np.float32)
    k = np.arange(128)[:, None]
    q = np.arange(128)[None, :]
    band = np.concatenate([(q <= k), (q >= k)], axis=1).astype(np.float32)
    c["c_band"] = band.astype(ml_dtypes.bfloat16)
    s = np.arange(64)[:, None]
    t = np.arange(64)[None, :]
    hm = (s <= t).astype(np.float32)
    c["c_hmask"] = np.concatenate([hm, hm], axis=0).astype(np.float32)
    bo = np.zeros((128, 128), np.float32)
    bo[:64, :64] = 1
    bo[64:, 64:] = 1
    c["c_blockones"] = bo.astype(ml_dtypes.bfloat16)
    c["c_ones_f"] = np.ones((128, 128), np.float32)
    pm = np.zeros((128, 128), np.float32)
    for hh in range(2):
        for m in range(8):
            pm[hh * 64 + m + 8, hh * 64 + m] = -1.0
            pm[hh * 64 + m, hh * 64 + m + 8] = 1.0
    c["c_pm"] = pm.astype(ml_dtypes.bfloat16)
    invf = np.zeros((128, 1), np.float64)
    for p in range(128):
        cc = p % 64
        if cc < 16:
            invf[p, 0] = (500000.0 ** (-(cc % 8) / 8.0)) / (2 * math.pi)
    c["c_invf"] = invf.astype(np.float32)
    rs = np.ones((128, 512), np.float32)
    rs[:, 0::64] = 0
    c["c_reset"] = rs
    c["c_tri"] = (np.arange(128)[:, None] <= np.arange(128)[None, :]).astype(np.float32).astype(ml_dtypes.bfloat16)
    c["c_iota"] = np.tile(np.arange(NE, dtype=np.float32)[None, :], (128, 1))
    p = np.arange(128, dtype=np.float32)[:, None, None]
    e_ = np.arange(NE, dtype=np.float32)[None, :, None]
    j2 = np.arange(2, dtype=np.float32)[None, None, :]
    c["c_zbase"] = np.ascontiguousarray((e_ * TOK + j2 * 128 + p).reshape(128, 2 * NE).astype(np.float32))
    c["c_ztrash"] = np.ascontiguousarray(np.broadcast_to(XROWS + p, (128, NE, 2)).reshape(128, 2 * NE).astype(np.float32))
    c["c_zlim"] = np.ascontiguousarray(np.broadcast_to((e_ + 1) * TOK, (128, NE, 2)).reshape(128, 2 * NE).astype(np.float32))
    return c


def _fm(v, n):
    return np.ascontiguousarray(np.asarray(v, np.float32).reshape(n, 128).T)


def make_in_maps(inp, dbg=None, extra=None):
    x = np.asarray(inp["x"], np.float32)
    p = np.asarray(inp["p"], np.float32)[0]
    positions = np.asarray(inp["positions"]).astype(np.int32)
    consts = _consts()
    shared = dict(consts)
    shared["g_mix"] = _fm(inp["mix_norm_g"][0], 8)
    shared["w_in"] = np.ascontiguousarray(inp["w_in"][0], np.float32)
    shared["gq"] = np.tile(np.asarray(inp["q_norm_g"][0], np.float32), 2).reshape(128, 1)
    shared["gk"] = np.tile(np.asarray(inp["k_norm_g"][0], np.float32), 2).reshape(128, 1)
    shared["lb0"] = _fm(inp["hgrn_lb_logits"][0], 4)
    shared["lb1"] = _fm(inp["hgrn_lb_logits"][1], 4)
    shared["gnorm"] = np.asarray(inp["hgrn_norm_g"][0], np.float32).reshape(128, 1)
    shared["w_out"] = np.ascontiguousarray(inp["w_out"][0], np.float32)
    shared["g_ffn"] = _fm(inp["ffn_norm_g"][0], 8)
    shared["router_w"] = np.ascontiguousarray(inp["router_w"][0], np.float32)
    shared["router_b"] = np.asarray(inp["router_b"][0], np.float32).reshape(1, NE)
    shared["gffn_row"] = np.asarray(inp["ffn_norm_g"][0], np.float32).reshape(1, D)
    if SPARSE:
        shared["w_gu_nat"] = np.ascontiguousarray(inp["expert_w_gate_up"][0], np.float32)
        shared["b_gu_nat"] = np.ascontiguousarray(inp["expert_b_gate_up"][0], np.float32)
    wg = np.asarray(inp["expert_w_gate_up"][0], np.float32)
    wg5 = wg.reshape(NE, 8, 128, 2, 4, 256)
    if not SPARSE:
        shared["wgu"] = np.ascontiguousarray(wg5.transpose(0, 4, 2, 1, 3, 5)).reshape(NE, 4, 128, 4096)
        bgu = np.asarray(inp["expert_b_gate_up"][0], np.float32)
        shared["bgu"] = np.ascontiguousarray(bgu.reshape(NE, 16, 128).transpose(2, 0, 1))
    shared["w_down"] = np.ascontiguousarray(inp["expert_w_down"][0], np.float32)
    shared["b_down"] = np.ascontiguousarray(inp["expert_b_down"][0], np.float32)
    shared["ple_proj"] = np.ascontiguousarray(inp["ple_proj"][0], np.float32)
    shared["ple_gate"] = np.ascontiguousarray(inp["ple_gate"][0], np.float32)
    maps = []
    for c in range(NCORES):
        b, h = divmod(c, 2)
        m = dict(shared)
        m["xo"] = np.ascontiguousarray(x[b, h * TOK:(h + 1) * TOK])
        m["p_own"] = np.ascontiguousarray(p[b, h * TOK:(h + 1) * TOK])
        if h == 1:
            m["xc"] = np.ascontiguousarray(x[b, 0:TOK])
            pc = positions[b, 0:TOK]
            m["onesctx"] = np.ones((128, 64), np.float32)
        else:
            m["xc"] = np.zeros((TOK, D), np.float32)
            pc = np.zeros((TOK,), np.int32)
            m["onesctx"] = np.zeros((128, 64), np.float32)
        m["pos"] = np.concatenate([pc, positions[b, h * TOK:(h + 1) * TOK]]).reshape(1, 4096).astype(np.int32)
        if extra is not None:
            m.update(extra(c))
        maps.append(m)
    return maps


_NC_CACHE = {}


def kernel(**inputs):
    if "nc" not in _NC_CACHE:
        _NC_CACHE["nc"] = build()
    nc = _NC_CACHE["nc"]
    maps = make_in_maps(inputs)
    res = run_bass_kernel_spmd(nc, maps, core_ids=list(range(NCORES)))
    outp = np.zeros((4, 4096, D), np.float32)
    for c in range(NCORES):
        b, h = divmod(c, 2)
        outp[b, h * TOK:(h + 1) * TOK] = res.results[c]["out"]
    return outp
```

```python
import math
from contextlib import ExitStack

import numpy as np
import ml_dtypes
import concourse.bass as bass
import concourse.mybir as mybir
from concourse.bass_utils import run_bass_kernel_spmd

F32 = mybir.dt.float32
BF16 = mybir.dt.bfloat16
I32 = mybir.dt.int32
AF = mybir.ActivationFunctionType
ALU = mybir.AluOpType
AX = mybir.AxisListType

D = 1024
TOK = 2048
NE = 32
EPS = 1e-6
NCORES = 8
SPARSE = True
XROWS = NE * TOK


class _Op:
    __slots__ = ("sig_aux", "eng", "fn", "deps", "signal", "sig", "dma", "emitted", "idx", "region")


class Prog:
    COMPUTE = ("pe", "act", "dve", "pool")

    def __init__(self, nc, n_dma_sems=36):
        self.nc = nc
        self.ops = []
        self.last_w = {}
        self.readers = {}
        self.sems = {}
        self.cnt = {e: 0 for e in self.COMPUTE}
        self.dma_sems = []
        self.dma_tot = []
        self.dma_rr = 0
        self.n_dma_sems = n_dma_sems
        self.waited = {}
        self.stack = None
        self.cur_region = None
        self.regs = {}
        self.wstream_ops = set()

    def setup(self, stack):
        nc = self.nc
        for e in self.COMPUTE:
            self.sems[e] = stack.enter_context(nc.semaphore("s_" + e))
        for i in range(self.n_dma_sems):
            self.dma_sems.append(stack.enter_context(nc.semaphore("s_dma%d" % i)))
            self.dma_tot.append(0)
        self.wsems = [stack.enter_context(nc.semaphore("s_wdma%d" % i)) for i in range(32)]
        self.wtot = [0] * 32
        self.wrr = 0
        self.gsems = [stack.enter_context(nc.semaphore("s_gdma%d" % i)) for i in range(20)]
        self.gtot = [0] * 20
        self.grr = 0

    def add(self, eng, fn, reads=(), writes=(), dma=False, region=None):
        op = _Op()
        op.region = self.cur_region if region is None else (region or None)
        op.eng = eng
        op.fn = fn
        op.dma = dma
        op.signal = dma
        op.sig = None
        op.emitted = False
        op.idx = len(self.ops)
        deps = []
        for k in reads:
            w = self.last_w.get(k)
            if w is not None:
                deps.append(w)
        for k in writes:
            w = self.last_w.get(k)
            if w is not None:
                deps.append(w)
            for r in self.readers.get(k, ()):
                deps.append(r)
        dd = []
        seen = set()
        for d in deps:
            if d is op or id(d) in seen:
                continue
            seen.add(id(d))
            if d.eng == "pe" and eng == "pe" and not d.dma and not dma:
                continue
            if d.emitted and not d.dma:
                continue
            if d.eng == "sp" and not d.dma:
                continue
            dd.append(d)
            d.signal = True
        op.deps = dd
        for k in reads:
            self.readers.setdefault(k, []).append(op)
        for k in writes:
            self.last_w[k] = op
            self.readers[k] = []
        self.ops.append(op)
        return op

    def pe(self, fn, reads=(), writes=()):
        return self.add("pe", fn, reads, writes)

    def act(self, fn, reads=(), writes=()):
        return self.add("act", fn, reads, writes)

    def dve(self, fn, reads=(), writes=()):
        return self.add("dve", fn, reads, writes)

    def pool(self, fn, reads=(), writes=()):
        return self.add("pool", fn, reads, writes)

    def dma(self, q, fn, reads=(), writes=(), wstream=False):
        op = self.add(q, fn, reads, writes, dma=True)
        if wstream:
            self.wstream_ops.add(id(op))
        return op

    def regload(self, engname, key, ap, reads=()):
        op = self.add(engname, "regload", reads, (), region=False)
        op.region = None
        op.sig_aux = (key, ap)
        return op

    def emit(self):
        nc = self.nc
        todo = [o for o in self.ops if not o.emitted]
        for o in todo:
            if o.dma and id(o) in self.wstream_ops:
                j = self.wrr
                self.wrr = (self.wrr + 1) % len(self.wsems)
                prev = self.wtot[j]
                self.wtot[j] += 16
                o.sig = (self.wsems[j], self.wtot[j], ("w", j), prev)
            elif o.dma and o.eng == "pool":
                j = self.grr
                self.grr = (self.grr + 1) % len(self.gsems)
                prev = self.gtot[j]
                self.gtot[j] += 16
                o.sig = (self.gsems[j], self.gtot[j], ("g", j), prev)
            elif o.dma:
                j = self.dma_rr
                self.dma_rr = (self.dma_rr + 1) % self.n_dma_sems
                prev = self.dma_tot[j]
                self.dma_tot[j] += 16
                o.sig = (self.dma_sems[j], self.dma_tot[j], ("d", j), prev)
            elif o.signal:
                self.cnt[o.eng] += 1
                o.sig = (self.sems[o.eng], self.cnt[o.eng], ("c", o.eng), None)
        by = {}
        for o in todo:
            by.setdefault(o.eng, []).append(o)
        qmap = {"pe": "tensor", "act": "scalar", "dve": "vector", "pool": "gpsimd", "sp": "sync"}

        def run(engname, ops):
            def body(eng):
                waited = self.waited.setdefault(engname, {})
                regs = self.regs.setdefault(engname, {})
                rstack = ExitStack()

                def getreg(key):
                    if key not in regs:
                        regs[key] = rstack.enter_context(eng.register("r_%s_%s" % (engname, key)))
                    return regs[key]

                def emit_op(o):
                    ws = {}
                    for d in o.deps:
                        if d.sig[2] not in ws or ws[d.sig[2]][1] < d.sig[1]:
                            ws[d.sig[2]] = (d.sig[0], d.sig[1])
                    if o.dma and o.sig[3] > 0:
                        if o.sig[2] not in ws or ws[o.sig[2]][1] < o.sig[3]:
                            ws[o.sig[2]] = (o.sig[0], o.sig[3])
                    for key, (sem, val) in ws.items():
                        if waited.get(key, 0) >= val:
                            continue
                        waited[key] = val
                        eng.wait_ge(sem, val)
                    if o.fn == "regload":
                        eng.reg_load(getreg(o.sig_aux[0]), o.sig_aux[1])
                    else:
                        inst = o.fn(eng)
                        if o.sig is not None:
                            inst.then_inc(o.sig[0], 16 if o.dma else 1)
                    o.emitted = True

                def comp_for(groups):
                    ncomp = sum(1 for grp in groups for g in grp if g.sig is not None and not g.dma)
                    dmas = [g for grp in groups for g in grp if g.dma]
                    if ncomp:
                        eng.drain()
                        eng.sem_inc(self.sems[engname], ncomp)
                    for g in dmas:
                        if g.sig[3] > 0:
                            eng.wait_ge(g.sig[0], g.sig[3])
                        eng.sem_inc(g.sig[0], 16)
                    if not ncomp and not dmas:
                        eng.nop()

                def emit_chain(groups, gi):
                    grp = groups[gi]
                    with eng.If_lt(getreg(grp[0].region[0]), grp[0].region[1] + 1):
                        comp_for(groups[gi:])
                    with eng.Else():
                        for g in grp:
                            emit_op(g)
                        if gi + 1 < len(groups):
                            emit_chain(groups, gi + 1)

                i = 0
                while i < len(ops):
                    o = ops[i]
                    if o.region is None:
                        emit_op(o)
                        i += 1
                        continue
                    groups = []
                    j = i
                    while j < len(ops) and ops[j].region is not None and ops[j].region[0] == o.region[0] and \
                            (not groups or ops[j].region[1] >= groups[-1][0].region[1]):
                        if groups and ops[j].region == groups[-1][0].region:
                            groups[-1].append(ops[j])
                        else:
                            groups.append([ops[j]])
                        j += 1
                    saved = dict(waited)
                    emit_chain(groups, 0)
                    waited.clear()
                    waited.update(saved)
                    i = j
                rstack.close()
            return body

        with nc.Block() as block:
            for engname, ops in by.items():
                getattr(block, qmap[engname])(run(engname, ops))
        for o in todo:
            o.fn = None


_UID = [0]


def U(name):
    _UID[0] += 1
    return "%s_%d" % (name, _UID[0])


class Rot:
    def __init__(self, nc, stack, name, shape, dtype, n, psum=False):
        self.bufs = []
        for i in range(n):
            alloc = nc.psum_tensor if psum else nc.sbuf_tensor
            self.bufs.append(stack.enter_context(alloc(U("%s%d" % (name, i)), list(shape), dtype)))
        self.name = name
        self.i = 0
        self.gen = 0

    @classmethod
    def from_aps(cls, name, aps):
        r = cls.__new__(cls)
        r.bufs = list(aps)
        r.name = name
        r.i = 0
        r.gen = 0
        return r

    def next(self):
        b = self.bufs[self.i]
        key = (self.name, self.i)
        self.i = (self.i + 1) % len(self.bufs)
        return b, key


def build(dbg=None):
    nc = bass.Bass("TRN2", target_bir_lowering=False)

    def din(name, shape, dt=F32):
        return nc.dram_tensor(name, list(shape), dt, kind="ExternalInput").ap()

    xo = din("xo", [TOK, D])
    xc = din("xc", [TOK, D])
    pos = din("pos", [1, 4096], I32)
    onesctx = din("onesctx", [128, 64])
    g_mix = din("g_mix", [128, 8])
    w_in = din("w_in", [D, 3584])
    gq = din("gq", [128, 1])
    gk = din("gk", [128, 1])
    lb0 = din("lb0", [128, 4])
    lb1 = din("lb1", [128, 4])
    gnorm = din("gnorm", [128, 1])
    w_out = din("w_out", [D, D])
    g_ffn = din("g_ffn", [128, 8])
    router_w = din("router_w", [D, NE])
    router_b = din("router_b", [1, NE])
    if SPARSE:
        w_gu_nat = din("w_gu_nat", [NE, D, 2 * D])
        b_gu_nat = din("b_gu_nat", [NE, 2 * D])
    else:
        wgu = din("wgu", [NE, 4, 128, 4096])
        bgu = din("bgu", [128, NE, 16])
    w_down = din("w_down", [NE, D, D])
    b_down = din("b_down", [NE, D])
    ple_proj = din("ple_proj", [256, D])
    ple_gate = din("ple_gate", [D, D])
    p_own = din("p_own", [TOK, 256])
    c_ident_bf = din("c_ident_bf", [128, 128], BF16)
    c_ident_f = din("c_ident_f", [128, 128])
    c_band = din("c_band", [128, 256], BF16)
    c_hmask = din("c_hmask", [128, 64])
    c_blockones = din("c_blockones", [128, 128], BF16)
    c_ones_f = din("c_ones_f", [128, 128])
    c_pm = din("c_pm", [128, 128], BF16)
    c_invf = din("c_invf", [128, 1])
    c_reset = din("c_reset", [128, 512])
    if dbg == "moe":
        d_mixT = din("d_mixT", [128, 8, TOK], BF16)
    out = nc.dram_tensor("out", [TOK, D], F32, kind="ExternalOutput").ap()
    gffn_row = din("gffn_row", [1, D])
    c_tri = din("c_tri", [128, 128], BF16)
    c_iota = din("c_iota", [128, NE])
    c_zbase = din("c_zbase", [128, 2 * NE])
    c_zlim = din("c_zlim", [128, 2 * NE])
    c_ztrash = din("c_ztrash", [128, 2 * NE])
    Xd = nc.dram_tensor("Xd", [NE * TOK + 128, D], BF16, kind="Internal").ap()
    Yd = nc.dram_tensor("Yd", [NE * TOK, D], F32, kind="Internal").ap()
    if dbg and dbg.startswith("mix"):
        d_out_mixT = nc.dram_tensor("d_out_mixT", [128, 8, TOK], BF16, kind="ExternalOutput").ap()

    P = Prog(nc)
    with ExitStack() as top:
        P.setup(top)
        sb = lambda name, shape, dt, st=top: st.enter_context(nc.sbuf_tensor(U(name), list(shape), dt))
        ident_bf = sb("ident_bf", [128, 128], BF16)
        ident_f = sb("ident_f", [128, 128], F32)
        acc = sb("acc", [128, 16, D], F32)
        accb = acc[:].rearrange("p a b -> p (a b)").bitcast(BF16)
        gw = sb("gw", [128, 16, 4], F32)
        ridx = sb("ridx", [128, 16, 4], I32)
        cnt_i = sb("cnt_i", [1, NE], I32)
        zi = sb("zi", [128, 2 * NE], I32)
        smix = ExitStack()
        mixT = smix.enter_context(nc.sbuf_tensor(U("mixT"), [128, 8, TOK], BF16))
        P.dma("sp", lambda e: e.dma_start(out=ident_bf[:], in_=c_ident_bf), writes=["ident_bf"])
        P.dma("sp", lambda e: e.dma_start(out=ident_f[:], in_=c_ident_f), writes=["ident_f"])

        if dbg == "moe":
            P.dma("sp", lambda e: e.dma_start(out=mixT[:], in_=d_mixT), writes=["mixT"])
        else:
            emit_mixer(nc, P, locals(), upto=(dbg[3:] if dbg and dbg.startswith("mix") and len(dbg) > 3 else "T"))
        if dbg and dbg.startswith("mix"):
            P.dma("sp", lambda e: e.dma_start(out=d_out_mixT, in_=mixT[:]), reads=["mixT"], writes=["d_out"])
            P.add("sp", lambda e: e.nop(), reads=["d_out"])
            P.emit()
            smix.close()
            return nc

        if SPARSE:
            emit_sparse(nc, P, locals(), smix)
        else:
            for half in range(2):
                emit_half(nc, P, locals(), half)
            smix.close()
    return nc


def a1_block(nc, P, env, pools, blk, gmix):
    xt_r, junk_r, xn_r, st_r, tp_r, aT_r = pools
    ident_bf = env["ident_bf"]
    aT, aTk = aT_r.next()
    for t in range(4):
        gt = blk * 4 + t
        src = env["xc"][gt * 128:(gt + 1) * 128, :] if gt < 16 else env["xo"][(gt - 16) * 128:(gt - 15) * 128, :]
        xt, xk = xt_r.next()
        P.dma("sp", lambda e, xt=xt, src=src: e.dma_start(out=xt[:], in_=src), writes=[xk])
        junk, jk = junk_r.next()
        st, stk = st_r.next()
        P.act(lambda e, junk=junk, xt=xt, st=st: e.activation(out=junk[:], in_=xt[:], func=AF.Square, accum_out=st[:, 0:1]),
              reads=[xk], writes=[jk, stk])
        P.act(lambda e, st=st: e.activation(out=st[:, 1:2], in_=st[:, 0:1], func=AF.Sqrt, scale=1.0 / D, bias=EPS), reads=[stk], writes=[stk])
        P.dve(lambda e, st=st: e.reciprocal(out=st[:, 2:3], in_=st[:, 1:2]), reads=[stk], writes=[stk])
        xn, xnk = xn_r.next()
        P.dve(lambda e, xn=xn, xt=xt, st=st: e.tensor_scalar(out=xn[:], in0=xt[:], scalar1=st[:, 2:3], scalar2=None, op0=ALU.mult),
              reads=[xk, stk], writes=[xnk])
        tp, tpk = tp_r.next()
        for kc in range(8):
            P.pe(lambda e, tp=tp, xn=xn, kc=kc: e.transpose(out=tp[:, kc, :], in_=xn[:, kc * 128:(kc + 1) * 128], identity=ident_bf[:]),
                 reads=[xnk, "ident_bf"], writes=[tpk])
        P.dve(lambda e, aT=aT, tp=tp, t=t: e.tensor_tensor(out=aT[:, :, t * 128:(t + 1) * 128], in0=tp[:],
                                                            in1=gmix[:].unsqueeze(2).broadcast_to([128, 8, 128]), op=ALU.mult),
              reads=[tpk, "gmix"], writes=[(aTk, t)])
    return aT, [(aTk, t) for t in range(4)]


import os as _os


def emit_mixer(nc, P, env, upto="T"):
    mixT = env["mixT"]
    ident_bf = env["ident_bf"]
    with ExitStack() as sm_:
        sbm = lambda name, shape, dt: sm_.enter_context(nc.sbuf_tensor(U(name), list(shape), dt))
        gmix = sbm("gmix", [128, 8], F32)
        P.dma("sp", lambda e: e.dma_start(out=gmix[:], in_=env["g_mix"]), writes=["gmix"])

        with ExitStack() as sh:
            sbh = lambda name, shape, dt: sh.enter_context(nc.sbuf_tensor(U(name), list(shape), dt))
            accb = env["accb"]
            w_h = accb[:, 0:16384].rearrange("p (k c) -> p k c", k=8)
            for kc in range(8):
                P.dma("pool", lambda e, kc=kc: e.dma_start(out=w_h[:, kc, :], in_=env["w_in"][kc * 128:(kc + 1) * 128, 1536:3584]),
                      writes=[("w_h", kc)])
            hmask = sbh("hmask", [128, 64], F32)
            ones_f = sbh("ones_f", [128, 128], F32)
            reset = sbh("reset", [128, 512], F32)
            gnorm = sbh("gnorm", [128, 1], F32)
            l0 = sbh("l0", [128, 4], F32)
            l1 = sbh("l1", [128, 4], F32)
            oml = sbh("oml", [128, 4], F32)
            noml = sbh("noml", [128, 4], F32)
            for (t_, src, key) in ((hmask, "c_hmask", "hmask"), (ones_f, "c_ones_f", "ones_f"), (reset, "c_reset", "reset"),
                                   (gnorm, "gnorm", "gnorm"), (l0, "lb0", "l0"), (l1, "lb1", "l1")):
                P.dma("sp", lambda e, t_=t_, src=src: e.dma_start(out=t_[:], in_=env[src]), writes=[key])
            P.dve(lambda e: e.tensor_tensor(out=oml[:], in0=l1[:], in1=l0[:], op=ALU.subtract), reads=["l0", "l1"], writes=["oml"])
            P.act(lambda e: e.activation(out=oml[:], in_=oml[:], func=AF.Sigmoid), reads=["oml"], writes=["oml"])
            P.dve(lambda e: e.tensor_scalar(out=noml[:], in0=oml[:], scalar1=-1.0, scalar2=None, op0=ALU.mult), reads=["oml"], writes=["oml2"])
            tpb_r = Rot(nc, sh, "tpb", [128, 8, 128], BF16, 2, psum=True)
            pools = (Rot(nc, sh, "xt", [128, D], F32, 2), Rot(nc, sh, "junk", [128, D], BF16, 1), Rot(nc, sh, "xn", [128, D], BF16, 2),
                     Rot(nc, sh, "st", [128, 4], F32, 4), tpb_r,
                     Rot.from_aps("aTh", [accb[:, 16384 + i * 4096:16384 + (i + 1) * 4096].rearrange("p (k c) -> p k c", k=8) for i in range(2)]))
            vt_r = Rot.from_aps("vth", [accb[0:64, 24576 + i * 4096:24576 + (i + 1) * 4096].rearrange("p (k c) -> p k c", k=8) for i in range(2)])
            At_r = Rot(nc, sh, "At", [128, 64], BF16, 4)
            eb = [sbh("eb%d" % i, [128, 512], F32) for i in range(4)]
            kt = [sbh("kt%d" % i, [128, 512], BF16) for i in range(4)]
            qt = [sbh("qt%d" % i, [128, 512], BF16) for i in range(4)]
            ktok = [sbh("ktok%d" % i, [64, 8, 128], BF16) for i in range(4)]
            sgate = [sbh("sgate%d" % i, [128, 512], F32) for i in range(4)]
            oT = [sbh("oT%d" % i, [128, 512], F32) for i in range(4)]
            S = sbh("S", [128, 4, 128], F32)
            S_bf = sbh("S_bf", [128, 4, 128], BF16)
            pb_r = Rot(nc, sh, "pb", [128, 512], F32, 6, psum=True)
            mm_r = pb_r
            P.dve(lambda e: e.memset(S[:], 0.0), writes=[("S", i) for i in range(4)])
            P.dve(lambda e: e.memset(S_bf[:], 0.0), writes=[("S_bf", i) for i in range(4)])

            def mmgroup(col0, aT, aTkeys):
                mm, mmk = mm_r.next()
                for kc in range(8):
                    P.pe(lambda e, mm=mm, kc=kc, col0=col0, aT=aT: e.matmul(mm[:], w_h[:, kc, col0:col0 + 128], aT[:, kc, :],
                                                                          start=(kc == 0), stop=(kc == 7)),
                         reads=[("w_h", kc)] + aTkeys, writes=[mmk])
                return mm, mmk

            snegs = [sbh("snegh%d" % i, [128, 512], F32) for i in range(4)]
            ffs = [sbh("ffh%d" % i, [128, 512], F32) for i in range(4)]
            bbs = [sbh("bbh%d" % i, [128, 512], F32) for i in range(4)]
            enbs = [sbh("enbh%d" % i, [128, 512], F32) for i in range(4)]
            khs = [sbh("khh%d" % i, [128, 512], BF16) for i in range(4)]
            qss = [sbh("qsh%d" % i, [128, 512], F32) for i in range(4)]
            H4 = range(4)
            USE_STT = False
            for blk in range(8):
                own = blk >= 4
                ob = blk - 4
                aT, aTkeys = a1_block(nc, P, env, pools, blk, gmix)
                vt, vtk = vt_r.next()
                for n in range(8):
                    mm, mmk = pb_r.next()
                    for kc in range(8):
                        P.pe(lambda e, mm=mm, kc=kc, n=n, aT=aT: e.matmul(mm[0:64, :], aT[:, kc, n * 64:(n + 1) * 64], w_h[:, kc, 1024:1536],
                                                                         start=(kc == 0), stop=(kc == 7)),
                             reads=[("w_h", kc), aTkeys[n // 2]], writes=[mmk])
                    P.act(lambda e, vt=vt, mm=mm, n=n: e.activation(out=vt[0:64, n, :], in_=mm[0:64, :], func=AF.Copy), reads=[mmk], writes=[(vtk, n)])
                mf = [mmgroup(512 + hd * 128, aT, aTkeys) for hd in H4]
                for hd in H4:
                    P.act(lambda e, hd=hd, mf=mf: e.activation(out=snegs[hd][:], in_=mf[hd][0][:], func=AF.Sigmoid, scale=-1.0), reads=[mf[hd][1]], writes=[("sneg", hd)])
                if own:
                    mq = [mmgroup(hd * 128, aT, aTkeys) for hd in H4]
                    for hd in H4:
                        P.act(lambda e, hd=hd, mq=mq: e.activation(out=qss[hd][:], in_=mq[hd][0][:], func=AF.Silu), reads=[mq[hd][1]], writes=[("qs", hd)])
                for hd in H4:
                    P.dve(lambda e, hd=hd: e.tensor_scalar(out=ffs[hd][:], in0=snegs[hd][:], scalar1=noml[:, hd:hd + 1], scalar2=1.0, op0=ALU.mult, op1=ALU.add),
                          reads=[("sneg", hd), "oml2"], writes=[("ff", hd)])
                for hd in H4:
                    P.act(lambda e, hd=hd: e.activation(out=ffs[hd][:], in_=ffs[hd][:], func=AF.Ln), reads=[("ff", hd)], writes=[("ff", hd)])
                if own:
                    mg = [mmgroup(1536 + hd * 128, aT, aTkeys) for hd in H4]
                    for hd in H4:
                        P.act(lambda e, hd=hd, mg=mg: e.activation(out=sgate[hd][:], in_=mg[hd][0][:], func=AF.Silu), reads=[mg[hd][1]], writes=[("sgate", hd)])
                for hd in H4:
                    P.dve(lambda e, hd=hd: e.tensor_tensor_scan(out=bbs[hd][:], data0=reset[:], data1=ffs[hd][:], initial=0.0, op0=ALU.mult, op1=ALU.add),
                          reads=[("ff", hd), "reset"], writes=[("bb", hd)])
                for hd in H4:
                    P.act(lambda e, hd=hd: e.activation(out=eb[hd][:], in_=bbs[hd][:], func=AF.Exp), reads=[("bb", hd)], writes=[("eb", hd)])
                    P.act(lambda e, hd=hd: e.activation(out=enbs[hd][:], in_=bbs[hd][:], func=AF.Exp, scale=-1.0), reads=[("bb", hd)], writes=[("enb", hd)])
                for hd in H4:
                    P.dve(lambda e, hd=hd: e.scalar_tensor_tensor(out=kt[hd][:], in0=snegs[hd][:], scalar=oml[:, hd:hd + 1], in1=enbs[hd][:],
                                                                  op0=ALU.mult, op1=ALU.mult), reads=[("sneg", hd), ("enb", hd), "oml"], writes=[("kt", hd)])
                    if own:
                        P.pool(lambda e, hd=hd: e.tensor_tensor(out=qt[hd][:], in0=qss[hd][:], in1=eb[hd][:], op=ALU.mult),
                               reads=[("qs", hd), ("eb", hd)], writes=[("qt", hd)])
                for hd in H4:
                    P.pool(lambda e, hd=hd: e.tensor_tensor(
                        out=khs[hd][:].rearrange("p (n c) -> p n c", c=64), in0=kt[hd][:].rearrange("p (n c) -> p n c", c=64),
                        in1=eb[hd][:].rearrange("p (n c) -> p n c", c=64)[:, :, 63:64].broadcast_to([128, 8, 64]), op=ALU.mult),
                        reads=[("kt", hd), ("eb", hd)], writes=[("kh", hd)])
                for hd in H4:
                    tpk_, tpkk = tpb_r.next()
                    for n in range(8):
                        P.pe(lambda e, hd=hd, n=n, tpk_=tpk_: e.transpose(out=tpk_[0:64, n, :], in_=khs[hd][:, n * 64:(n + 1) * 64], identity=ident_bf[:]),
                             reads=[("kh", hd), "ident_bf"], writes=[tpkk])
                    P.dve(lambda e, hd=hd, tpk_=tpk_: e.tensor_copy(out=ktok[hd][:], in_=tpk_[0:64, :, :]), reads=[tpkk], writes=[("ktok", hd)])
                for n in range(8):
                    t = n
                    cs = slice(n * 64, n * 64 + 64)
                    for hd in H4:
                        if own:
                            pA, pAk = pb_r.next()
                            P.pe(lambda e, hd=hd, cs=cs, pA=pA: e.matmul(pA[0:64, 0:64], kt[hd][:, cs], qt[hd][:, cs], start=True, stop=True),
                                 reads=[("kt", hd), ("qt", hd)], writes=[pAk])
                            At, Atk = At_r.next()
                            P.dve(lambda e, At=At, pA=pA: e.tensor_tensor(out=At[0:64, :], in0=pA[0:64, 0:64], in1=hmask[0:64, :], op=ALU.mult),
                                  reads=[pAk, "hmask"], writes=[Atk])
                            pO, pOk = pb_r.next()
                            P.pe(lambda e, hd=hd, cs=cs, pO=pO: e.matmul(pO[:, 0:64], S_bf[:, hd, :], qt[hd][:, cs], start=True, stop=False),
                                 reads=[("S_bf", hd), ("qt", hd)], writes=[pOk])
                            P.pe(lambda e, hd=hd, t=t, At=At, vt=vt, pO=pO: e.matmul(pO[:, 0:64], vt[0:64, t, hd * 128:(hd + 1) * 128], At[0:64, :],
                                                                                 start=False, stop=True),
                                 reads=[(vtk, t), Atk], writes=[pOk])
                            P.act(lambda e, hd=hd, cs=cs, pO=pO: e.activation(out=oT[hd][:, cs], in_=pO[:, 0:64], func=AF.Copy),
                                  reads=[pOk], writes=[("oT", hd, n)])
                        pS, pSk = pb_r.next()
                        P.pe(lambda e, hd=hd, t=t, vt=vt, pS=pS: e.matmul(pS[:, 0:128], ktok[hd][0:64, t, :], vt[0:64, t, hd * 128:(hd + 1) * 128],
                                                                        start=True, stop=True),
                             reads=[("ktok", hd), (vtk, t)], writes=[pSk])
                        if USE_STT:
                            P.dve(lambda e, hd=hd, n=n, pS=pS: e.scalar_tensor_tensor(out=S[:, hd, :], in0=S[:, hd, :], scalar=eb[hd][:, n * 64 + 63:n * 64 + 64],
                                                                                    in1=pS[:, 0:128], op0=ALU.mult, op1=ALU.add),
                                  reads=[("S", hd), ("eb", hd), pSk], writes=[("S", hd)])
                        else:
                            if own and hd < 2:
                                P.pool(lambda e, hd=hd, n=n: e.tensor_scalar(out=S[:, hd, :], in0=S[:, hd, :], scalar1=eb[hd][:, n * 64 + 63:n * 64 + 64], scalar2=None, op0=ALU.mult),
                                       reads=[("S", hd), ("eb", hd)], writes=[("S", hd)])
                            else:
                                P.act(lambda e, hd=hd, n=n: e.activation(out=S[:, hd, :], in_=S[:, hd, :], func=AF.Copy, scale=eb[hd][:, n * 64 + 63:n * 64 + 64]),
                                      reads=[("S", hd), ("eb", hd)], writes=[("S", hd)])
                            P.dve(lambda e, hd=hd, pS=pS: e.tensor_tensor(out=S[:, hd, :], in0=pS[:, 0:128], in1=S[:, hd, :], op=ALU.add),
                                  reads=[("S", hd), pSk], writes=[("S", hd)])
                        if own or (blk == 3 and n == 7):
                            P.act(lambda e, hd=hd: e.activation(out=S_bf[:, hd, :], in_=S[:, hd, :], func=AF.Copy), reads=[("S", hd)], writes=[("S_bf", hd)])
                if own:
                    okeys = [[("oT", hd, n) for n in range(8)] for hd in H4]
                    for hd in H4:
                        P.act(lambda e, hd=hd: e.activation(out=snegs[hd][:], in_=oT[hd][:], func=AF.Square), reads=okeys[hd], writes=[("sneg", hd)])
                    mr = []
                    for hd in H4:
                        mm, mmk = pb_r.next()
                        P.pe(lambda e, mm=mm, hd=hd: e.matmul(mm[:], ones_f[:], snegs[hd][:], start=True, stop=True), reads=[("sneg", hd), "ones_f"], writes=[mmk])
                        mr.append((mm, mmk))
                    for hd in H4:
                        P.act(lambda e, hd=hd, mr=mr: e.activation(out=ffs[hd][:], in_=mr[hd][0][:], func=AF.Sqrt, scale=1.0 / 128, bias=EPS), reads=[mr[hd][1]], writes=[("ff", hd)])
                    for hd in H4:
                        P.dve(lambda e, hd=hd: e.reciprocal(out=ffs[hd][:], in_=ffs[hd][:]), reads=[("ff", hd)], writes=[("ff", hd)])
                    for hd in H4:
                        P.dve(lambda e, hd=hd: e.scalar_tensor_tensor(out=bbs[hd][:], in0=oT[hd][:], scalar=gnorm[:, 0:1], in1=ffs[hd][:],
                                                                      op0=ALU.mult, op1=ALU.mult), reads=okeys[hd] + [("ff", hd), "gnorm"], writes=[("bb", hd)])
                    for hd in H4:
                        P.pool(lambda e, hd=hd, ob=ob: e.tensor_tensor(out=mixT[:, 4 + hd, ob * 512:(ob + 1) * 512], in0=bbs[hd][:], in1=sgate[hd][:], op=ALU.mult),
                               reads=[("bb", hd), ("sgate", hd)], writes=[("mixT", 4 + hd, ob)])
            P.emit()

        if upto == "H":
            return
        accb = env["accb"]
        KT = [accb[:, i * 4096:(i + 1) * 4096] for i in range(4)]
        VT = [accb[:, 16384 + i * 4096:16384 + (i + 1) * 4096] for i in range(4)]
        QT = [sbm("QT%d" % i, [128, TOK], BF16) for i in range(4)]
        with ExitStack() as sp_:
            sbp = lambda name, shape, dt: sp_.enter_context(nc.sbuf_tensor(U(name), list(shape), dt))
            w_a = sbp("w_a", [128, 8, 1536], BF16)
            for kc in range(8):
                P.dma("pool", lambda e, kc=kc: e.dma_start(out=w_a[:, kc, :], in_=env["w_in"][kc * 128:(kc + 1) * 128, 0:1536]),
                      writes=[("w_a", kc)])
            Ct = sbp("Ct", [128, 4096], BF16)
            St = sbp("St", [128, 4096], BF16)
            blockones = sbp("blockones", [128, 128], BF16)
            pm = sbp("pm", [128, 128], BF16)
            invf = sbp("invf", [128, 1], F32)
            gq = sbp("gq", [128, 1], F32)
            gk = sbp("gk", [128, 1], F32)
            for (t_, src, key) in ((blockones, "c_blockones", "blockones"), (pm, "c_pm", "pm"), (invf, "c_invf", "invf"),
                                   (gq, "gq", "gq"), (gk, "gk", "gk")):
                P.dma("sp", lambda e, t_=t_, src=src: e.dma_start(out=t_[:], in_=env[src]), writes=[key])
            srope = ExitStack()
            posi_r = Rot(nc, srope, "posi", [128, 1024], I32, 2)
            rf_r = Rot(nc, srope, "rf", [128, 1024], F32, 4)
            ri_r = Rot(nc, srope, "ri", [128, 1024], I32, 2)
            for ch in range(4):
                csl = slice(ch * 1024, (ch + 1) * 1024)
                posi, pik = posi_r.next()
                P.dma("sp", lambda e, posi=posi, csl=csl: e.dma_start(out=posi[:], in_=env["pos"][0:1, csl].partition_broadcast(128)), writes=[pik])
                posf, pfk = rf_r.next()
                P.dve(lambda e, posf=posf, posi=posi: e.tensor_copy(out=posf[:], in_=posi[:]), reads=[pik], writes=[pfk])
                for (off, tab, tkey) in ((0.5, St, "St"), (0.75, Ct, "Ct")):
                    y, yk = rf_r.next()
                    P.dve(lambda e, y=y, posf=posf, off=off: e.tensor_scalar(out=y[:], in0=posf[:], scalar1=invf[:, 0:1], scalar2=off, op0=ALU.mult, op1=ALU.add),
                          reads=[pfk, "invf"], writes=[yk])
                    ni, nik = ri_r.next()
                    P.dve(lambda e, ni=ni, y=y: e.tensor_copy(out=ni[:], in_=y[:]), reads=[yk], writes=[nik])
                    nf, nfk = rf_r.next()
                    P.dve(lambda e, nf=nf, ni=ni: e.tensor_copy(out=nf[:], in_=ni[:]), reads=[nik], writes=[nfk])
                    P.dve(lambda e, y=y, nf=nf: e.tensor_tensor(out=y[:], in0=y[:], in1=nf[:], op=ALU.subtract), reads=[yk, nfk], writes=[yk])
                    P.dve(lambda e, y=y, nf=nf: e.tensor_single_scalar(out=nf[:], in_=y[:], scalar=0.0, op=ALU.is_lt), reads=[yk], writes=[nfk])
                    P.dve(lambda e, y=y, nf=nf: e.tensor_tensor(out=y[:], in0=y[:], in1=nf[:], op=ALU.add), reads=[yk, nfk], writes=[yk])
                    P.act(lambda e, y=y, tab=tab, csl=csl: e.activation(out=tab[:, csl], in_=y[:], func=AF.Sin, scale=6.2831845, bias=-3.1415922),
                          reads=[yk], writes=[(tkey, ch)])
            P.emit()
            srope.close()
            pools = (Rot(nc, sp_, "xt", [128, D], F32, 2), Rot(nc, sp_, "junk", [128, D], BF16, 1), Rot(nc, sp_, "xn", [128, D], BF16, 2),
                     Rot(nc, sp_, "st", [128, 4], F32, 4), Rot(nc, sp_, "tp", [128, 8, 128], BF16, 1, psum=True),
                     Rot(nc, sp_, "aT", [128, 8, 512], BF16, 2))
            mm_r = Rot(nc, sp_, "mm", [128, 512], F32, 3, psum=True)
            pn_r = Rot(nc, sp_, "pn", [128, 512], F32, 2, psum=True)
            pq_r = Rot(nc, sp_, "pq", [128, 512], F32, 2, psum=True)
            sq_r = Rot(nc, sp_, "sq", [128, 512], BF16, 2)
            rt_r = Rot(nc, sp_, "rt", [128, 512], F32, 2)
            qn_r = Rot(nc, sp_, "qn", [128, 512], BF16, 2)
            t1_r = Rot(nc, sp_, "t1", [128, 512], F32, 2)
            t2_r = Rot(nc, sp_, "t2", [128, 512], F32, 2)
            q1 = []
            q2 = []

            def qk_stage0(nm, coff, dest, dsl, gg, sc, bi, hp, blk, aT, aTkeys, tsl, tch):
                mm, mmk = mm_r.next()
                for kc in range(8):
                    P.pe(lambda e, mm=mm, kc=kc: e.matmul(mm[:], w_a[:, kc, coff + hp * 128:coff + (hp + 1) * 128], aT[:, kc, :],
                                                          start=(kc == 0), stop=(kc == 7)),
                         reads=[("w_a", kc)] + aTkeys, writes=[mmk])
                sq, sqk = sq_r.next()
                P.act(lambda e: e.activation(out=sq[:], in_=mm[:], func=AF.Square), reads=[mmk], writes=[sqk])

                def stage1():
                    pn, pnk = pn_r.next()
                    P.pe(lambda e: e.matmul(pn[:], blockones[:], sq[:], start=True, stop=True), reads=[sqk, "blockones"], writes=[pnk])
                    rt, rtk = rt_r.next()
                    P.act(lambda e: e.activation(out=rt[:], in_=pn[:], func=AF.Sqrt, scale=sc, bias=bi), reads=[pnk], writes=[rtk])
                    P.dve(lambda e: e.reciprocal(out=rt[:], in_=rt[:]), reads=[rtk], writes=[rtk])
                    qn, qnk = qn_r.next()
                    P.dve(lambda e: e.scalar_tensor_tensor(out=qn[:], in0=mm[:], scalar=gg[:, 0:1], in1=rt[:], op0=ALU.mult, op1=ALU.mult),
                          reads=[mmk, rtk, nm == "k" and "gk" or "gq"], writes=[qnk])

                    def stage2():
                        pq, pqk = pq_r.next()
                        P.pe(lambda e: e.matmul(pq[:], pm[:], qn[:], start=True, stop=True), reads=[qnk, "pm"], writes=[pqk])
                        t1, t1k = t1_r.next()
                        P.pool(lambda e: e.tensor_tensor(out=t1[:], in0=qn[:], in1=Ct[:, tsl], op=ALU.mult), reads=[qnk, ("Ct", tch)], writes=[t1k])
                        t2, t2k = t2_r.next()
                        P.dve(lambda e: e.tensor_tensor(out=t2[:], in0=pq[:], in1=St[:, tsl], op=ALU.mult), reads=[pqk, ("St", tch)], writes=[t2k])
                        P.pool(lambda e: e.tensor_tensor(out=dest[:, dsl], in0=t1[:], in1=t2[:], op=ALU.add), reads=[t1k, t2k], writes=[(nm + "T", hp, blk)])
                    return stage2
                return stage1

            def v_stage0(hp, blk, aT, aTkeys, tsl):
                mm, mmk = mm_r.next()
                for kc in range(8):
                    P.pe(lambda e, mm=mm, kc=kc: e.matmul(mm[:], w_a[:, kc, 1024 + hp * 128:1024 + (hp + 1) * 128], aT[:, kc, :],
                                                          start=(kc == 0), stop=(kc == 7)),
                         reads=[("w_a", kc)] + aTkeys, writes=[mmk])
                P.act(lambda e: e.activation(out=VT[hp][:, tsl], in_=mm[:], func=AF.Copy), reads=[mmk], writes=[("VT", hp, blk)])
                return None

            def step(s1):
                if q2:
                    q2.pop(0)()
                if q1:
                    q2.append(q1.pop(0)())
                if s1 is not None:
                    q1.append(s1)

            nxt = a1_block(nc, P, env, pools, 0, gmix)
            for blk in range(8):
                own = blk >= 4
                aT, aTkeys = nxt
                tsl = slice(blk * 512, (blk + 1) * 512)
                tch = blk // 2
                for hp in range(4):
                    step(qk_stage0("k", 512, KT[hp], tsl, gk, 1.0 / 64, EPS, hp, blk, aT, aTkeys, tsl, tch))
                    if own:
                        step(qk_stage0("q", 0, QT[hp], slice((blk - 4) * 512, (blk - 3) * 512), gq, 1.0, 64 * EPS, hp, blk, aT, aTkeys, tsl, tch))
                    v_stage0(hp, blk, aT, aTkeys, tsl)
                    if hp == 1 and blk + 1 < 8:
                        nxt = a1_block(nc, P, env, pools, blk + 1, gmix)
            step(None)
            step(None)
            step(None)
            P.emit()

        if upto == "P":
            return
        with ExitStack() as st_:
            sbt = lambda name, shape, dt: st_.enter_context(nc.sbuf_tensor(U(name), list(shape), dt))
            band = sbt("band", [128, 256], BF16)
            onesb = sbt("onesb", [128, 64], BF16)
            onesc32 = sbt("onesc32", [128, 64], F32)
            onesc = sbt("onesc", [128, 64], BF16)
            P.dma("sp", lambda e: e.dma_start(out=band[:], in_=env["c_band"]), writes=["band"])
            P.dma("sp", lambda e: e.dma_start(out=onesc32[:], in_=env["onesctx"]), writes=["onesc32"])
            P.dve(lambda e: e.tensor_copy(out=onesc[:], in_=onesc32[:]), reads=["onesc32"], writes=["onesc"])
            P.dve(lambda e: e.memset(onesb[:], 1.0), writes=["onesb"])
            Vt_r = Rot(nc, st_, "Vt", [128, 32, 192], BF16, 2)
            acc = sbt("acca", [128, 2, TOK], F32)
            den = sbt("den", [128, TOK], F32)
            E_r = Rot(nc, st_, "E", [128, 256], BF16, 3)
            Pm_r = Rot(nc, st_, "Pm", [128, 256], BF16, 3)
            tpv_r = Rot(nc, st_, "tpv", [128, 8, 128], BF16, 2, psum=True)
            ps_r = Rot(nc, st_, "psS", [128, 512], F32, 3, psum=True)
            po_r = Rot(nc, st_, "psO", [128, 512], F32, 3, psum=True)
            for hp in range(4):
                for bi_, d in enumerate((1, 4, 16)):
                    n_lo = 2048 // (128 * d)
                    n_hi = 4096 // (128 * d) - 1
                    Vt, Vtk = Vt_r.next()
                    tiles = {}
                    for r in range(d):
                        for m in range(n_lo - 1, n_hi + 1):
                            idx = len(tiles)
                            tiles[(r, m)] = idx
                            u0 = r + d * 128 * m
                            ksl = slice(u0, u0 + d * 127 + 1, d)
                            tpv, tpvk = tpv_r.next()
                            P.pe(lambda e, tpv=tpv, hp=hp, ksl=ksl: e.transpose(out=tpv[:, 0, :], in_=VT[hp][:, ksl], identity=ident_bf[:]),
                                 reads=[("VT", hp), "ident_bf"], writes=[tpvk])
                            P.act(lambda e, Vt=Vt, idx=idx, tpv=tpv: e.activation(
                                out=Vt[:, idx, :].rearrange("p (s c) -> p s c", c=64)[:, 0:3:2, :],
                                in_=tpv[:, 0, :].rearrange("p (s c) -> p s c", c=64), func=AF.Copy),
                                reads=[tpvk], writes=[(Vtk, idx)])
                            osrc = onesc if m < n_lo else onesb
                            P.pool(lambda e, Vt=Vt, idx=idx, osrc=osrc: e.tensor_copy(out=Vt[:, idx, 64:128], in_=osrc[:]),
                                   reads=["onesc", "onesb"], writes=[(Vtk, idx, "o")])
                    pend = []
                    LA = 2
                    for hh in range(2):
                        prow = slice(hh * 64, hh * 64 + 64)
                        vsl = slice(hh * 64, hh * 64 + 128)
                        for r in range(d):
                            for n in range(n_lo, n_hi + 1):
                                q0 = r + d * 128 * n - 2048
                                qsl = slice(q0, q0 + d * 127 + 1, d)
                                ps, psk = ps_r.next()
                                for w_, m in enumerate((n - 1, n)):
                                    u0 = r + d * 128 * m
                                    ksl = slice(u0, u0 + d * 127 + 1, d)
                                    P.pe(lambda e, ps=ps, w_=w_, hp=hp, prow=prow, ksl=ksl, qsl=qsl: e.matmul(
                                        ps[:, w_ * 128:(w_ + 1) * 128], KT[hp][prow, ksl], QT[hp][prow, qsl], start=True, stop=True),
                                        reads=[("KT", hp), ("QT", hp)], writes=[psk])
                                E, Ek = E_r.next()
                                P.act(lambda e, E=E, ps=ps: e.activation(out=E[:], in_=ps[:, 0:256], func=AF.Exp), reads=[psk], writes=[Ek])
                                Pm_, Pmk = Pm_r.next()
                                P.pool(lambda e, Pm_=Pm_, E=E: e.tensor_tensor(out=Pm_[:], in0=E[:], in1=band[:], op=ALU.mult), reads=[Ek, "band"], writes=[Pmk])
                                ai = r * (n_hi - n_lo + 1) + (n - n_lo)

                                def pv(Pm_=Pm_, Pmk=Pmk, r=r, n=n, hh=hh, qsl=qsl, vsl=vsl, ai=ai, Vt=Vt, Vtk=Vtk, tiles=tiles, bi_=bi_):
                                    po, pok = po_r.next()
                                    for w_, m in enumerate((n - 1, n)):
                                        idx = tiles[(r, m)]
                                        P.pe(lambda e, po=po, Vt=Vt, idx=idx, vsl=vsl, Pm_=Pm_, w_=w_: e.matmul(
                                            po[:, 0:128], Vt[:, idx, vsl], Pm_[:, w_ * 128:(w_ + 1) * 128], start=(w_ == 0), stop=(w_ == 1)),
                                            reads=[(Vtk, idx), (Vtk, idx, "o"), Pmk], writes=[pok])
                                    if bi_ == 0:
                                        P.dve(lambda e, po=po, hh=hh, qsl=qsl: e.tensor_copy(out=acc[:, hh, qsl], in_=po[:, 0:128]),
                                              reads=[pok], writes=[("acca", hh, 0, ai)])
                                    else:
                                        P.dve(lambda e, po=po, hh=hh, qsl=qsl: e.tensor_tensor(out=acc[:, hh, qsl], in0=po[:, 0:128], in1=acc[:, hh, qsl], op=ALU.add),
                                              reads=[pok] + [("acca", hh, bi_ - 1, i_) for i_ in range(16)], writes=[("acca", hh, bi_, ai)])
                                pend.append(pv)
                                if len(pend) > LA:
                                    pend.pop(0)()
                    while pend:
                        pend.pop(0)()
                for hh in range(2):
                    nrow = slice(hh * 64, hh * 64 + 64)
                    drow = slice(64 - hh * 64, 128 - hh * 64)
                    akeys = [("acca", hh, b_, i_) for b_ in range(3) for i_ in range(16)]
                    P.act(lambda e, hh=hh, nrow=nrow, drow=drow: e.activation(out=den[nrow, :], in_=acc[drow, hh, :], func=AF.Copy),
                          reads=akeys, writes=[("den", hh)])
                    P.dve(lambda e, nrow=nrow: e.reciprocal(out=den[nrow, :], in_=den[nrow, :]), reads=[("den", hh)], writes=[("den", hh)])
                    P.dve(lambda e, hh=hh, hp=hp, nrow=nrow: e.tensor_tensor(out=mixT[nrow, hp, :], in0=acc[nrow, hh, :], in1=den[nrow, :], op=ALU.mult),
                          reads=akeys + [("den", hh)], writes=[("mixT", hp, hh)])
            P.emit()


def run_skewed(n, stages):
    K = len(stages)
    for step in range(n + K - 1):
        for k, f in enumerate(stages):
            t = step - k
            if 0 <= t < n:
                f(t)


def emit_sparse(nc, P, env, smix):
    xo = env["xo"]; out = env["out"]; mixT = env["mixT"]
    ident_bf = env["ident_bf"]; ident_f = env["ident_f"]
    Xd = env["Xd"]; Yd = env["Yd"]
    NT = 16
    with ExitStack() as st:
        sb = lambda name, shape, dt: st.enter_context(nc.sbuf_tensor(U(name), list(shape), dt))
        acc = env["acc"]; gw = env["gw"]; ridx = env["ridx"]; cnt_i = env["cnt_i"]
        with ExitStack() as sw:
            sbw = lambda name, shape, dt: sw.enter_context(nc.sbuf_tensor(U(name), list(shape), dt))
            wo = sbw("wo", [128, 8, D], BF16)
            rw = sbw("rw", [128, 8, NE], F32)
            rb = sbw("rb", [128, NE], F32)
            gffn = sbw("gffn", [128, 8], F32)
            grow = sbw("grow", [128, D], F32)
            tri = sbw("tri", [128, 128], BF16)
            ones_bf = sbw("ones_bf", [128, 128], BF16)
            iota = sbw("iota", [128, NE], F32)
            run = sbw("run", [128, NE], F32)
            xt_r = Rot(nc, sw, "xt", [128, D], F32, 2)
            junk_r = Rot(nc, sw, "junk", [128, D], BF16, 1)
            hn_r = Rot(nc, sw, "hn", [128, D], F32, 3)
            mrow_r = Rot(nc, sw, "mrow", [128, D], BF16, 5)
            m32_r = Rot(nc, sw, "m32", [128, 8, 128], F32, 3)
            sm_r = Rot(nc, sw, "sm", [128, 8], F32, 12)
            lg_r = Rot(nc, sw, "lg", [128, 4, NE], F32, 3)
            selb_r = Rot(nc, sw, "selb", [128, NE], BF16, 3)
            ix_r = Rot(nc, sw, "ix", [128, 8], mybir.dt.uint32, 3)
            ef_r = Rot(nc, sw, "ef", [128, 12], F32, 3)
            pw_r = Rot(nc, sw, "pw", [128, 2, 512], F32, 2, psum=True)
            pt_r = Rot(nc, sw, "pt", [128, 8, 128], F32, 1, psum=True)
            pl_r = Rot(nc, sw, "pl", [128, 512], F32, 2, psum=True)
            for kc in range(8):
                P.dma("pool", lambda e, kc=kc: e.dma_start(out=wo[:, kc, :], in_=env["w_out"][kc * 128:(kc + 1) * 128, :]), writes=[("wo", kc)])
            P.dma("sp", lambda e: e.dma_start(out=rw[:], in_=env["router_w"].rearrange("(kc p) n -> p kc n", p=128)), writes=["rw"])
            P.dma("sp", lambda e: e.dma_start(out=rb[:], in_=env["router_b"].partition_broadcast(128)), writes=["rb"])
            P.dma("sp", lambda e: e.dma_start(out=gffn[:], in_=env["g_ffn"]), writes=["gffn"])
            P.dma("sp", lambda e: e.dma_start(out=grow[:], in_=env["gffn_row"].partition_broadcast(128)), writes=["grow"])
            P.dma("sp", lambda e: e.dma_start(out=tri[:], in_=env["c_tri"]), writes=["tri"])
            P.dma("sp", lambda e: e.dma_start(out=iota[:], in_=env["c_iota"]), writes=["iota"])
            P.dve(lambda e: e.memset(ones_bf[:], 1.0), writes=["ones_bf"])
            P.dve(lambda e: e.memset(run[:], 0.0), writes=["run"])
            C = [dict() for _ in range(NT)]

            def W0(tt):
                c = C[tt]
                xt, xk = xt_r.next()
                P.dma("sp", lambda e: e.dma_start(out=xt[:], in_=xo[tt * 128:(tt + 1) * 128, :]), writes=[xk])
                pw, pwk = pw_r.next()
                for dh in range(2):
                    for kc in range(8):
                        P.pe(lambda e, dh=dh, kc=kc: e.matmul(pw[:, dh, :], mixT[:, kc, tt * 128:(tt + 1) * 128], wo[:, kc, dh * 512:(dh + 1) * 512],
                                                              start=(kc == 0), stop=(kc == 7)), reads=["mixT", ("wo", kc)], writes=[pwk])
                c.update(xt=xt, xk=xk, pw=pw, pwk=pwk)

            def W1(tt):
                c = C[tt]
                xt, xk, pw, pwk = c["xt"], c["xk"], c["pw"], c["pwk"]
                hk = ("acc", tt)
                P.dve(lambda e: e.tensor_tensor(out=acc[:, tt, :], in0=pw[:].rearrange("p a b -> p (a b)"), in1=xt[:], op=ALU.add), reads=[pwk, xk], writes=[hk])
                junk, jk = junk_r.next()
                sm, smk = sm_r.next()
                P.act(lambda e: e.activation(out=junk[:], in_=acc[:, tt, :], func=AF.Square, accum_out=sm[:, 0:1]), reads=[hk], writes=[jk, smk])
                P.act(lambda e: e.activation(out=sm[:, 1:2], in_=sm[:, 0:1], func=AF.Sqrt, scale=1.0 / D, bias=EPS), reads=[smk], writes=[smk])
                P.dve(lambda e: e.reciprocal(out=sm[:, 2:3], in_=sm[:, 1:2]), reads=[smk], writes=[smk])
                hn, hnk = hn_r.next()
                P.dve(lambda e: e.tensor_scalar(out=hn[:], in0=acc[:, tt, :], scalar1=sm[:, 2:3], scalar2=None, op0=ALU.mult), reads=[hk, smk], writes=[hnk])
                mrow, mrk = mrow_r.next()
                P.pool(lambda e: e.tensor_tensor(out=mrow[:], in0=hn[:], in1=grow[:], op=ALU.mult), reads=[hnk, "grow"], writes=[mrk])
                c.update(hn=hn, hnk=hnk, mrow=mrow, mrk=mrk)

            def W2(tt):
                c = C[tt]
                hn, hnk = c["hn"], c["hnk"]
                pt, ptk = pt_r.next()
                for kc in range(8):
                    P.pe(lambda e, kc=kc: e.transpose(out=pt[:, kc, :], in_=hn[:, kc * 128:(kc + 1) * 128], identity=ident_f[:]), reads=[hnk, "ident_f"], writes=[ptk])
                m32, m32k = m32_r.next()
                P.dve(lambda e: e.tensor_tensor(out=m32[:], in0=pt[:], in1=gffn[:].unsqueeze(2).broadcast_to([128, 8, 128]), op=ALU.mult), reads=[ptk, "gffn"], writes=[m32k])
                c.update(m32=m32, m32k=m32k)

            def W3(tt):
                c = C[tt]
                m32, m32k = c["m32"], c["m32k"]
                pl, plk = pl_r.next()
                for kc in range(8):
                    P.pe(lambda e, kc=kc: e.matmul(pl[:, 0:NE], m32[:, kc, :], rw[:, kc, :], start=(kc == 0), stop=(kc == 7)), reads=[m32k, "rw"], writes=[plk])
                lg, lgk = lg_r.next()
                P.dve(lambda e: e.tensor_tensor(out=lg[:, 0, :], in0=pl[:, 0:NE], in1=rb[:], op=ALU.add), reads=[plk, "rb"], writes=[lgk])
                sm2, sm2k = sm_r.next()
                P.dve(lambda e: e.max(out=sm2[:, 0:8], in_=lg[:, 0, :]), reads=[lgk], writes=[sm2k])
                ix, ixk = ix_r.next()
                P.dve(lambda e: e.max_index(out=ix[:], in_max=sm2[:, 0:8], in_values=lg[:, 0, :]), reads=[lgk, sm2k], writes=[ixk])
                ef, efk = ef_r.next()
                P.dve(lambda e: e.tensor_copy(out=ef[:, 0:4], in_=ix[:, 0:4]), reads=[ixk], writes=[efk])
                selb, selk = selb_r.next()
                P.dve(lambda e: e.tensor_scalar(out=selb[:], in0=lg[:, 0, :], scalar1=sm2[:, 3:4], scalar2=None, op0=ALU.is_ge), reads=[lgk, sm2k], writes=[selk])
                sm3, sm3k = sm_r.next()
                P.dve(lambda e: e.tensor_scalar(out=sm3[:, 0:1], in0=sm2[:, 0:1], scalar1=-1.0, scalar2=None, op0=ALU.mult), reads=[sm2k], writes=[sm3k])
                P.act(lambda e: e.activation(out=sm3[:, 4:8], in_=sm2[:, 0:4], func=AF.Exp, bias=sm3[:, 0:1], scale=1.0), reads=[sm2k, sm3k], writes=[sm3k])
                P.dve(lambda e: e.tensor_reduce(out=sm3[:, 1:2], in_=sm3[:, 4:8], axis=AX.X, op=ALU.add), reads=[sm3k], writes=[sm3k])
                P.dve(lambda e: e.reciprocal(out=sm3[:, 2:3], in_=sm3[:, 1:2]), reads=[sm3k], writes=[sm3k])
                P.dve(lambda e: e.tensor_scalar(out=gw[:, tt, :], in0=sm3[:, 4:8], scalar1=sm3[:, 2:3], scalar2=None, op0=ALU.mult), reads=[sm3k], writes=[("gw", tt)])
                c.update(lg=lg, lgk=lgk, ef=ef, efk=efk, selb=selb, selk=selk)

            def W4(tt):
                c = C[tt]
                lg, lgk, ef, efk, selb, selk, mrow, mrk = c["lg"], c["lgk"], c["ef"], c["efk"], c["selb"], c["selk"], c["mrow"], c["mrk"]
                pl2, pl2k = pl_r.next()
                P.pe(lambda e: e.matmul(pl2[:, 0:NE], tri[:], selb[:], start=True, stop=True), reads=[selk, "tri"], writes=[pl2k])
                P.pe(lambda e: e.matmul(pl2[:, NE:2 * NE], ones_bf[:], selb[:], start=True, stop=True), reads=[selk, "ones_bf"], writes=[pl2k])
                P.dve(lambda e: e.tensor_tensor(out=lg[:, 1, :], in0=pl2[:, 0:NE], in1=run[:], op=ALU.add), reads=[pl2k, "run", lgk], writes=[lgk])
                P.dve(lambda e: e.tensor_tensor(out=run[:], in0=pl2[:, NE:2 * NE], in1=run[:], op=ALU.add), reads=[pl2k, "run", lgk], writes=["run"])
                for k in range(4):
                    P.dve(lambda e, k=k: e.scalar_tensor_tensor(out=lg[:, 2, :], in0=iota[:], scalar=ef[:, k:k + 1], in1=lg[:, 1, :],
                                                                 op0=ALU.is_equal, op1=ALU.mult, accum_out=ef[:, 4 + k:5 + k]),
                          reads=[lgk, efk, "iota"], writes=[lgk, efk])
                P.dve(lambda e: e.scalar_tensor_tensor(out=ef[:, 8:12], in0=ef[:, 0:4], scalar=float(TOK), in1=ef[:, 4:8], op0=ALU.mult, op1=ALU.add),
                      reads=[efk], writes=[efk])
                P.dve(lambda e: e.tensor_scalar(out=ridx[:, tt, :], in0=ef[:, 8:12], scalar1=-1.0, scalar2=None, op0=ALU.add), reads=[efk], writes=[("ridx", tt)])
                for k in range(4):
                    P.dma("pool", lambda e, k=k: e.indirect_dma_start(
                        out=Xd[:, :], out_offset=bass.IndirectOffsetOnAxis(ap=ridx[:, tt, k:k + 1], axis=0), in_=mrow[:, :], in_offset=None),
                        reads=[mrk, ("ridx", tt)], writes=[("Xd", tt, k)])

            run_skewed(NT, [W0, W1, W2, W3, W4])
            P.dve(lambda e: e.tensor_copy(out=cnt_i[:], in_=run[0:1, :]), reads=["run"], writes=["cnt_i"])
            zb = sbw("zb", [128, 2 * NE], F32)
            zl = sbw("zl", [128, 2 * NE], F32)
            zf = sbw("zf", [128, 2 * NE], F32)
            zm = sbw("zm", [128, 2 * NE], F32)
            zi = env["zi"]
            P.dma("sp", lambda e: e.dma_start(out=zb[:], in_=env["c_zbase"]), writes=["zb"])
            P.dma("sp", lambda e: e.dma_start(out=zl[:], in_=env["c_zlim"]), writes=["zl"])
            P.dve(lambda e: e.tensor_tensor(out=zf[:].rearrange("p (a b) -> p a b", b=2), in0=zb[:].rearrange("p (a b) -> p a b", b=2),
                                            in1=run[:].unsqueeze(2).broadcast_to([128, NE, 2]), op=ALU.add), reads=["zb", "run"], writes=["zf"])
            P.dve(lambda e: e.tensor_tensor(out=zm[:], in0=zf[:], in1=zl[:], op=ALU.is_ge), reads=["zf", "zl"], writes=["zm"])
            zt = sbw("zt", [128, 2 * NE], F32)
            P.dma("sp", lambda e: e.dma_start(out=zt[:], in_=env["c_ztrash"]), writes=["zt"])
            P.dve(lambda e: e.tensor_tensor(out=zt[:], in0=zt[:], in1=zf[:], op=ALU.subtract), reads=["zt", "zf"], writes=["zt"])
            P.dve(lambda e: e.tensor_tensor(out=zt[:], in0=zt[:], in1=zm[:], op=ALU.mult), reads=["zt", "zm"], writes=["zt"])
            P.dve(lambda e: e.tensor_tensor(out=zf[:], in0=zf[:], in1=zt[:], op=ALU.add), reads=["zt", "zf"], writes=["zf"])
            P.dve(lambda e: e.tensor_copy(out=zi[:], in_=zf[:]), reads=["zf"], writes=["zi"])
            P.emit()
        smix.close()

        with ExitStack() as se:
            sbe = lambda name, shape, dt: se.enter_context(nc.sbuf_tensor(U(name), list(shape), dt))
            wg = [sbe("wg%d" % i, [128, 8, 2 * D], BF16) for i in range(2)]
            wd = [sbe("wd%d" % i, [128, 8, D], BF16) for i in range(2)]
            bgr = sbe("bgr", [33, 2 * D], BF16)
            bdr = sbe("bdr", [33, D], BF16)
            ones_row = sbe("ones_row", [33, 128], BF16)
            xb_r = Rot(nc, se, "xb", [128, D], BF16, 2)
            xT_pre = [sbe("xTp%d" % i, [128, 8, 128], BF16) for i in range(2)]
            xT_r = Rot(nc, se, "xT", [128, 8, 128], BF16, 2)
            g_r = Rot(nc, se, "g", [128, 512], BF16, 2)
            sg_r = Rot(nc, se, "sg", [128, 512], BF16, 2)
            u_r = Rot(nc, se, "u", [128, 512], BF16, 2)
            actb_r = Rot(nc, se, "actb", [128, D], BF16, 1)
            actT_r = Rot(nc, se, "actT", [128, 8, 128], BF16, 1)
            ysb_r = Rot(nc, se, "ysb", [128, D], F32, 1)
            tpx_r = Rot(nc, se, "tpx", [128, 8, 128], BF16, 1, psum=True)
            pg_r = Rot(nc, se, "pgg", [128, 512], F32, 2, psum=True)
            pu_r = Rot(nc, se, "pgu", [128, 512], F32, 2, psum=True)
            tpa_r = Rot(nc, se, "tpa", [128, 8, 128], BF16, 1, psum=True)
            pd_r = Rot(nc, se, "pd", [128, 2, 512], F32, 1, psum=True)
            P.dve(lambda e: e.memset(ones_row[:], 1.0), writes=["ones_row"])
            xd_keys = [("Xd", tt, k) for tt in range(NT) for k in range(4)]
            zrow = sbe("zrow", [128, D], BF16)
            P.dve(lambda e: e.memset(zrow[:], 0.0), writes=["zrow"])

            def zero_tail(e_):
                for c_ in (2 * e_, 2 * e_ + 1):
                    P.dma("pool", lambda e, c_=c_: e.indirect_dma_start(
                        out=Xd[:, :], out_offset=bass.IndirectOffsetOnAxis(ap=env["zi"][:, c_:c_ + 1], axis=0), in_=zrow[:, :], in_offset=None),
                        reads=["zi", "zrow"], writes=[("Xz", e_)])
            P.add("sp", lambda e: e.nop(), reads=xd_keys, writes=["Xall"])
            engs = ("pe", "act", "dve", "sp")

            def load_weights(e_):
                par = e_ % 2
                pp = 32 * par
                for kc in range(8):
                    P.dma("pool", lambda e, kc=kc, par=par, e_=e_: e.dma_start(out=wg[par][:, kc, :], in_=env["w_gu_nat"][e_, kc * 128:(kc + 1) * 128, :]),
                          writes=[("wg", par, kc)], wstream=True)
                for fc in range(8):
                    P.dma("pool", lambda e, fc=fc, par=par, e_=e_: e.dma_start(out=wd[par][:, fc, :], in_=env["w_down"][e_, fc * 128:(fc + 1) * 128, :]),
                          writes=[("wd", par, fc)], wstream=True)
                P.dma("pool", lambda e, pp=pp, e_=e_: e.dma_start(out=bgr[pp:pp + 1, :], in_=env["b_gu_nat"][e_:e_ + 1, :]), writes=[("bgr", par)], wstream=True)
                P.dma("pool", lambda e, pp=pp, e_=e_: e.dma_start(out=bdr[pp:pp + 1, :], in_=env["b_down"][e_:e_ + 1, :]), writes=[("bdr", par)], wstream=True)

            xb_pre = [sbe("xbp%d" % i, [128, D], BF16) for i in range(2)]

            def xload(e_, k, xb, xbk):
                r0 = e_ * TOK + k * 128
                P.dma("sp", lambda e, xb=xb, r0=r0: e.dma_start(out=xb[:], in_=Xd[r0:r0 + 128, :]), reads=[("Xz", e_)], writes=[xbk])

            def xtrans(xb, xbk, xT, xTk):
                tpx, tpxk = tpx_r.next()
                for kc in range(8):
                    P.pe(lambda e, tpx=tpx, xb=xb, kc=kc: e.transpose(out=tpx[:, kc, :], in_=xb[:, kc * 128:(kc + 1) * 128], identity=ident_bf[:]),
                         reads=[xbk, "ident_bf"], writes=[tpxk])
                P.dve(lambda e, xT=xT, tpx=tpx: e.tensor_copy(out=xT[:], in_=tpx[:]), reads=[tpxk], writes=[xTk])

            def xprep(e_, k, xT, xTk):
                xb, xbk = xb_r.next()
                xload(e_, k, xb, xbk)
                xtrans(xb, xbk, xT, xTk)

            zero_tail(0)
            zero_tail(1)
            load_weights(0)
            xload(0, 0, xb_pre[0], ("xbp", 0))
            ykeys = []
            for e_ in range(NE):
                par = e_ % 2
                pp = 32 * par
                if e_ + 1 < NE:
                    load_weights(e_ + 1)
                    if e_ + 2 < NE:
                        zero_tail(e_ + 2)
                    xload(e_ + 1, 0, xb_pre[1 - par], ("xbp", 1 - par))
                xtrans(xb_pre[par], ("xbp", par), xT_pre[par], ("xTp", par))
                for en in engs:
                    P.regload(en, "n", cnt_i[0:1, e_:e_ + 1], reads=["cnt_i"])
                cur = (xT_pre[par], ("xTp", par))
                for k in range(TOK // 128):
                    P.cur_region = ("n", 128 * k)
                    r0 = e_ * TOK + k * 128
                    xT, xTk = cur
                    actb, actbk = actb_r.next()
                    for hf in range(2):
                        pg, pgk = pg_r.next()
                        pu, puk = pu_r.next()
                        for (pp_, ppk, c0) in ((pg, pgk, hf * 512), (pu, puk, D + hf * 512)):
                            for kc in range(8):
                                P.pe(lambda e, pp_=pp_, xT=xT, kc=kc, c0=c0, par=par: e.matmul(pp_[:], xT[:, kc, :], wg[par][:, kc, c0:c0 + 512],
                                                                                             start=(kc == 0), stop=False),
                                     reads=[xTk, ("wg", par, kc)], writes=[ppk])
                            P.pe(lambda e, pp_=pp_, c0=c0, pp=pp: e.matmul(pp_[:], ones_row[pp:pp + 1, :], bgr[pp:pp + 1, c0:c0 + 512], start=False, stop=True),
                                 reads=["ones_row", ("bgr", par)], writes=[ppk])
                        g, gk_ = g_r.next()
                        sg, sgk = sg_r.next()
                        u, uk = u_r.next()
                        P.dve(lambda e, g=g, pg=pg: e.tensor_scalar(out=g[:], in0=pg[:], scalar1=7.0, scalar2=None, op0=ALU.min), reads=[pgk], writes=[gk_])
                        P.act(lambda e, g=g, sg=sg: e.activation(out=sg[:], in_=g[:], func=AF.Sigmoid, scale=1.702), reads=[gk_], writes=[sgk])
                        P.dve(lambda e, u=u, pu=pu: e.tensor_scalar(out=u[:], in0=pu[:], scalar1=7.0, scalar2=-7.0, op0=ALU.min, op1=ALU.max), reads=[puk], writes=[uk])
                        P.dve(lambda e, g=g, sg=sg: e.tensor_tensor(out=g[:], in0=g[:], in1=sg[:], op=ALU.mult), reads=[gk_, sgk], writes=[gk_])
                        P.dve(lambda e, g=g, u=u, actb=actb, hf=hf: e.scalar_tensor_tensor(out=actb[:, hf * 512:(hf + 1) * 512], in0=u[:], scalar=1.0, in1=g[:],
                                                                                          op0=ALU.add, op1=ALU.mult), reads=[gk_, uk], writes=[(actbk, hf)])
                    if k + 1 < TOK // 128:
                        nxt = xT_r.next()
                        xprep(e_, k + 1, nxt[0], nxt[1])
                        cur = nxt
                    tpa, tpak = tpa_r.next()
                    for fc in range(8):
                        P.pe(lambda e, tpa=tpa, actb=actb, fc=fc: e.transpose(out=tpa[:, fc, :], in_=actb[:, fc * 128:(fc + 1) * 128], identity=ident_bf[:]),
                             reads=[(actbk, fc // 4), "ident_bf"], writes=[tpak])
                    actT, actTk = actT_r.next()
                    P.dve(lambda e, actT=actT, tpa=tpa: e.tensor_copy(out=actT[:], in_=tpa[:]), reads=[tpak], writes=[actTk])
                    pd, pdk = pd_r.next()
                    for dh in range(2):
                        for fc in range(8):
                            P.pe(lambda e, pd=pd, actT=actT, fc=fc, dh=dh, par=par: e.matmul(pd[:, dh, :], actT[:, fc, :], wd[par][:, fc, dh * 512:(dh + 1) * 512],
                                                                                            start=(fc == 0), stop=False),
                                 reads=[actTk, ("wd", par, fc)], writes=[pdk])
                        P.pe(lambda e, pd=pd, dh=dh, pp=pp: e.matmul(pd[:, dh, :], ones_row[pp:pp + 1, :], bdr[pp:pp + 1, dh * 512:(dh + 1) * 512], start=False, stop=True),
                             reads=["ones_row", ("bdr", par)], writes=[pdk])
                    ysb, ysbk = ysb_r.next()
                    P.dve(lambda e, ysb=ysb, pd=pd: e.tensor_copy(out=ysb[:], in_=pd[:].rearrange("p a b -> p (a b)")), reads=[pdk], writes=[ysbk])
                    yk = ("Yd", e_, k)
                    P.dma("sp", lambda e, ysb=ysb, r0=r0: e.dma_start(out=Yd[r0:r0 + 128, :], in_=ysb[:]), reads=[ysbk], writes=[yk])
                    ykeys.append(yk)
                    P.cur_region = None
            P.add("pool", lambda e: e.nop(), reads=ykeys, writes=["Yall"])
            P.emit()

        def combine(sf):
            yg_r = Rot(nc, sf, "yg", [128, D], F32, 8)

            def tile(tt):
                for k in range(4):
                    yg, ygk = yg_r.next()
                    P.dma("pool", lambda e, yg=yg, k=k: e.indirect_dma_start(
                        out=yg[:, :], out_offset=None, in_=Yd[:, :], in_offset=bass.IndirectOffsetOnAxis(ap=ridx[:, tt, k:k + 1], axis=0)),
                        reads=["Yall", ("ridx", tt)], writes=[ygk])
                    P.dve(lambda e, yg=yg, k=k: e.scalar_tensor_tensor(out=acc[:, tt, :], in0=yg[:], scalar=gw[:, tt, k:k + 1], in1=acc[:, tt, :],
                                                                       op0=ALU.mult, op1=ALU.add), reads=[ygk, ("gw", tt), ("acc", tt)], writes=[("acc", tt)])
            return tile
        emit_ple(nc, P, env, acc, 0, NT, pre=combine)


def emit_half(nc, P, env, half):
    xo = env["xo"]; out = env["out"]; mixT = env["mixT"]
    ident_bf = env["ident_bf"]; ident_f = env["ident_f"]
    HT = 8
    t0 = half * HT
    with ExitStack() as st:
        sb = lambda name, shape, dt: st.enter_context(nc.sbuf_tensor(U(name), list(shape), dt))
        acc = sb("acc", [128, HT, D], F32)
        mT = sb("mT", [128, 8, 1024], BF16)
        gates = sb("gates", [128, HT, NE], F32)
        gT = sb("gT", [NE, 1024], F32)
        bd = sb("bd", [NE, D], F32)
        bg = sb("bg", [128, NE, 16], F32)
        rb = sb("rb", [128, NE], F32)
        gffn = sb("gffn", [128, 8], F32)
        with ExitStack() as sw:
            sbw = lambda name, shape, dt: sw.enter_context(nc.sbuf_tensor(U(name), list(shape), dt))
            wo = sbw("wo", [128, 8, D], BF16)
            rw = sbw("rw", [128, 8, NE], F32)
            xt_r = Rot(nc, sw, "xt", [128, D], F32, 2)
            junk_r = Rot(nc, sw, "junk", [128, D], BF16, 2)
            hn_r = Rot(nc, sw, "hn", [128, D], F32, 2)
            m32_r = Rot(nc, sw, "m32", [128, 8, 128], F32, 2)
            sm_r = Rot(nc, sw, "sm", [128, 8], F32, 4)
            lg_r = Rot(nc, sw, "lg", [128, 3, NE], F32, 2)
            pw_r = Rot(nc, sw, "pw", [128, 2, 512], F32, 2, psum=True)
            pt_r = Rot(nc, sw, "pt", [128, 8, 128], F32, 1, psum=True)
            pl_r = Rot(nc, sw, "pl", [128, 512], F32, 2, psum=True)
            for kc in range(8):
                P.dma("pool", lambda e, kc=kc: e.dma_start(out=wo[:, kc, :], in_=env["w_out"][kc * 128:(kc + 1) * 128, :]),
                      writes=[("wo", kc)])
            P.dma("sp", lambda e: e.dma_start(out=rw[:], in_=env["router_w"].rearrange("(kc p) n -> p kc n", p=128)), writes=["rw"])
            P.dma("sp", lambda e: e.dma_start(out=rb[:], in_=env["router_b"].partition_broadcast(128)), writes=["rb"])
            P.dma("sp", lambda e: e.dma_start(out=gffn[:], in_=env["g_ffn"]), writes=["gffn"])
            P.dma("sp", lambda e: e.dma_start(out=bd[:], in_=env["b_down"]), writes=["bd"])
            P.dma("sp", lambda e: e.dma_start(out=bg[:], in_=env["bgu"]), writes=["bg"])
            P.dve(lambda e: e.tensor_scalar(out=bg[:, :, 8:16], in0=bg[:, :, 8:16], scalar1=1.0, scalar2=None, op0=ALU.add),
                  reads=["bg"], writes=["bg"])
            for tt in range(HT):
                gt = t0 + tt
                xt, xk = xt_r.next()
                P.dma("sp", lambda e, xt=xt, gt=gt: e.dma_start(out=xt[:], in_=xo[gt * 128:(gt + 1) * 128, :]), writes=[xk])
                pw, pwk = pw_r.next()
                for dh in range(2):
                    for kc in range(8):
                        P.pe(lambda e, pw=pw, dh=dh, kc=kc, gt=gt: e.matmul(
                            pw[:, dh, :], mixT[:, kc, gt * 128:(gt + 1) * 128], wo[:, kc, dh * 512:(dh + 1) * 512],
                            start=(kc == 0), stop=(kc == 7)), reads=["mixT", ("wo", kc)], writes=[pwk])
                hk = ("acc", tt)
                P.dve(lambda e, pw=pw, xt=xt, tt=tt: e.tensor_tensor(
                    out=acc[:, tt, :], in0=pw[:].rearrange("p a b -> p (a b)"), in1=xt[:], op=ALU.add),
                    reads=[pwk, xk], writes=[hk])
                junk, jk = junk_r.next()
                sm, smk = sm_r.next()
                P.act(lambda e, junk=junk, sm=sm, tt=tt: e.activation(out=junk[:], in_=acc[:, tt, :], func=AF.Square, accum_out=sm[:, 0:1]),
                      reads=[hk], writes=[jk, smk])
                P.act(lambda e, sm=sm: e.activation(out=sm[:, 1:2], in_=sm[:, 0:1], func=AF.Sqrt, scale=1.0 / D, bias=EPS),
                      reads=[smk], writes=[smk])
                P.dve(lambda e, sm=sm: e.reciprocal(out=sm[:, 2:3], in_=sm[:, 1:2]), reads=[smk], writes=[smk])
                hn, hnk = hn_r.next()
                P.dve(lambda e, hn=hn, sm=sm, tt=tt: e.tensor_scalar(out=hn[:], in0=acc[:, tt, :], scalar1=sm[:, 2:3], scalar2=None, op0=ALU.mult),
                      reads=[hk, smk], writes=[hnk])
                pt, ptk = pt_r.next()
                for kc in range(8):
                    P.pe(lambda e, pt=pt, hn=hn, kc=kc: e.transpose(out=pt[:, kc, :], in_=hn[:, kc * 128:(kc + 1) * 128], identity=ident_f[:]),
                         reads=[hnk, "ident_f"], writes=[ptk])
                m32, m32k = m32_r.next()
                P.dve(lambda e, pt=pt, m32=m32: e.tensor_tensor(out=m32[:], in0=pt[:], in1=gffn[:].unsqueeze(2).broadcast_to([128, 8, 128]), op=ALU.mult),
                      reads=[ptk, "gffn"], writes=[m32k])
                P.act(lambda e, m32=m32, tt=tt: e.activation(out=mT[:, :, tt * 128:(tt + 1) * 128], in_=m32[:], func=AF.Copy),
                      reads=[m32k], writes=[("mT", tt)])
                pl, plk = pl_r.next()
                for kc in range(8):
                    P.pe(lambda e, pl=pl, m32=m32, kc=kc: e.matmul(pl[:, 0:NE], m32[:, kc, :], rw[:, kc, :], start=(kc == 0), stop=(kc == 7)),
                         reads=[m32k, "rw"], writes=[plk])
                lg, lgk = lg_r.next()
                P.dve(lambda e, pl=pl, lg=lg: e.tensor_tensor(out=lg[:, 0, :], in0=pl[:, 0:NE], in1=rb[:], op=ALU.add),
                      reads=[plk, "rb"], writes=[lgk])
                sm2, sm2k = sm_r.next()
                P.dve(lambda e, lg=lg, sm2=sm2: e.max(out=sm2[:, 0:8], in_=lg[:, 0, :]), reads=[lgk], writes=[sm2k])
                P.dve(lambda e, lg=lg, sm2=sm2: e.tensor_scalar(out=lg[:, 1, :], in0=lg[:, 0, :], scalar1=sm2[:, 3:4], scalar2=None, op0=ALU.is_ge),
                      reads=[lgk, sm2k], writes=[lgk])
                sm3, sm3k = sm_r.next()
                P.dve(lambda e, sm2=sm2, sm3=sm3: e.tensor_scalar(out=sm3[:, 0:1], in0=sm2[:, 0:1], scalar1=-1.0, scalar2=None, op0=ALU.mult),
                      reads=[sm2k], writes=[sm3k])
                P.act(lambda e, lg=lg, sm3=sm3: e.activation(out=lg[:, 2, :], in_=lg[:, 0, :], func=AF.Exp, bias=sm3[:, 0:1], scale=1.0),
                      reads=[lgk, sm3k], writes=[lgk])
                P.dve(lambda e, lg=lg: e.tensor_tensor(out=lg[:, 2, :], in0=lg[:, 2, :], in1=lg[:, 1, :], op=ALU.mult), reads=[lgk], writes=[lgk])
                P.dve(lambda e, lg=lg, sm3=sm3: e.tensor_reduce(out=sm3[:, 1:2], in_=lg[:, 2, :], axis=AX.X, op=ALU.add), reads=[lgk, sm3k], writes=[sm3k])
                P.dve(lambda e, sm3=sm3: e.reciprocal(out=sm3[:, 2:3], in_=sm3[:, 1:2]), reads=[sm3k], writes=[sm3k])
                P.dve(lambda e, lg=lg, sm3=sm3, tt=tt: e.tensor_scalar(out=gates[:, tt, :], in0=lg[:, 2, :], scalar1=sm3[:, 2:3], scalar2=None, op0=ALU.mult),
                      reads=[lgk, sm3k], writes=[("gates", tt)])
                pl2, pl2k = pl_r.next()
                P.pe(lambda e, pl2=pl2, tt=tt: e.transpose(out=pl2[0:NE, 0:128], in_=gates[:, tt, :], identity=ident_f[:]),
                     reads=[("gates", tt), "ident_f"], writes=[pl2k])
                P.act(lambda e, pl2=pl2, tt=tt: e.activation(out=gT[:, tt * 128:(tt + 1) * 128], in_=pl2[0:NE, 0:128], func=AF.Copy),
                      reads=[pl2k], writes=[("gT", tt)])
                pw2, pw2k = pw_r.next()
                for dh in range(2):
                    P.pe(lambda e, pw2=pw2, dh=dh, tt=tt: e.matmul(pw2[:, dh, :], gT[:, tt * 128:(tt + 1) * 128], bd[:, dh * 512:(dh + 1) * 512],
                                                                     start=True, stop=True), reads=[("gT", tt), "bd"], writes=[pw2k])
                P.dve(lambda e, pw2=pw2, tt=tt: e.tensor_tensor(out=acc[:, tt, :], in0=pw2[:].rearrange("p a b -> p (a b)"), in1=acc[:, tt, :], op=ALU.add),
                      reads=[pw2k, hk], writes=[hk])
            P.emit()

        with ExitStack() as se:
            sbe = lambda name, shape, dt: se.enter_context(nc.sbuf_tensor(U(name), list(shape), dt))
            NSLOT = 9
            ring = [sbe("ring%d" % i, [128, 4096], BF16) for i in range(NSLOT)]
            actT = sbe("actT", [128, 8, 1024], BF16)
            g_r = Rot(nc, se, "g", [128, 512], F32, 2)
            sg_r = Rot(nc, se, "sg", [128, 512], F32, 2)
            u_r = Rot(nc, se, "u", [128, 512], F32, 2)
            pg_r = Rot(nc, se, "pg", [128, 512], F32, 2, psum=True)
            pu_r = Rot(nc, se, "pu", [128, 512], F32, 2, psum=True)
            pd_r = Rot(nc, se, "pd", [128, 2, 512], F32, 2, psum=True)
            pieces = []
            for e_ in range(NE):
                for j in range(4):
                    pieces.append(("gu", e_, j))
                for j in range(2):
                    pieces.append(("dn", e_, j))
            state = {"next": 0, "mark": -1}
            last_reader = {}
            finished = set()

            def issue_loads():
                while state["next"] < len(pieces):
                    i = state["next"]
                    if i >= NSLOT:
                        prev = i - NSLOT
                        if prev not in finished or last_reader[prev] > state["mark"]:
                            return
                    kind, e_, j = pieces[i]
                    slot = ring[i % NSLOT]
                    key = ("ring", i % NSLOT)
                    if kind == "gu":
                        src = env["wgu"][e_, j]
                        P.dma("pool", lambda e, slot=slot, src=src: e.dma_start(out=slot[:], in_=src, max_dma_last_dim=8192), writes=[key])
                    else:
                        src = env["w_down"][e_].rearrange("(fc p) d -> p fc d", p=128)[:, 4 * j:4 * j + 4, :]
                        P.dma("pool", lambda e, slot=slot, src=src: e.dma_start(out=slot[:].rearrange("p (fc d) -> p fc d", fc=4), in_=src), writes=[key])
                    state["next"] += 1

            issue_loads()
            for e_ in range(NE):
                base = e_ * 6
                for fc in range(8):
                    j = fc // 2
                    pi = base + j
                    assert pi < state["next"]
                    slot = ring[pi % NSLOT]
                    skey = ("ring", pi % NSLOT)
                    goff = (fc % 2) * 128
                    uoff = 256 + (fc % 2) * 128
                    for tb in range(2):
                        pg, pgk = pg_r.next()
                        pu, puk = pu_r.next()
                        lastop = None
                        for (pp, ppk, off) in ((pg, pgk, goff), (pu, puk, uoff)):
                            for kc in range(8):
                                lastop = P.pe(lambda e, pp=pp, slot=slot, kc=kc, off=off, tb=tb: e.matmul(
                                    pp[:], slot[:, kc * 512 + off: kc * 512 + off + 128], mT[:, kc, tb * 512:(tb + 1) * 512],
                                    start=(kc == 0), stop=(kc == 7)),
                                    reads=[skey] + [("mT", tb * 4 + q) for q in range(4)], writes=[ppk])
                        last_reader[pi] = lastop.idx
                        if fc % 2 == 1 and tb == 1:
                            finished.add(pi)
                        g, gk_ = g_r.next()
                        sg, sgk = sg_r.next()
                        u, uk = u_r.next()
                        P.dve(lambda e, g=g, pg=pg, e_=e_, fc=fc: e.tensor_scalar(out=g[:], in0=pg[:], scalar1=bg[:, e_, fc:fc + 1], scalar2=7.0,
                                                                                    op0=ALU.add, op1=ALU.min), reads=[pgk, "bg"], writes=[gk_])
                        P.act(lambda e, g=g, sg=sg: e.activation(out=sg[:], in_=g[:], func=AF.Sigmoid, scale=1.702), reads=[gk_], writes=[sgk])
                        P.act(lambda e, u=u, pu=pu, e_=e_, fc=fc: e.activation(out=u[:], in_=pu[:], func=AF.Identity, bias=bg[:, e_, 8 + fc:9 + fc], scale=1.0),
                              reads=[puk, "bg"], writes=[uk])
                        P.pool(lambda e, u=u: e.tensor_scalar(out=u[:], in0=u[:], scalar1=8.0, scalar2=-6.0, op0=ALU.min, op1=ALU.max),
                               reads=[uk], writes=[uk])
                        P.pool(lambda e, g=g, sg=sg: e.tensor_tensor(out=g[:], in0=g[:], in1=sg[:], op=ALU.mult), reads=[gk_, sgk], writes=[gk_])
                        P.pool(lambda e, g=g, u=u, fc=fc, tb=tb: e.tensor_tensor(out=actT[:, fc, tb * 512:(tb + 1) * 512], in0=g[:], in1=u[:], op=ALU.mult),
                               reads=[gk_, uk], writes=[("actT", fc, tb)])
                        state["mark"] = lastop.idx
                        issue_loads()
                p0 = base + 4
                for tt in range(HT):
                    pd, pdk = pd_r.next()
                    lastop = None
                    for dh in range(2):
                        for fc in range(8):
                            pj = p0 + fc // 4
                            slot = ring[pj % NSLOT]
                            lastop = P.pe(lambda e, pd=pd, dh=dh, fc=fc, tt=tt, slot=slot: e.matmul(
                                pd[:, dh, :], actT[:, fc, tt * 128:(tt + 1) * 128],
                                slot[:, (fc % 4) * 1024 + dh * 512:(fc % 4) * 1024 + (dh + 1) * 512],
                                start=(fc == 0), stop=(fc == 7)),
                                reads=[("ring", pj % NSLOT), ("actT", fc, tt // 4)], writes=[pdk])
                            last_reader[pj] = lastop.idx
                    P.dve(lambda e, pd=pd, tt=tt, e_=e_: e.scalar_tensor_tensor(
                        out=acc[:, tt, :], in0=pd[:].rearrange("p a b -> p (a b)"), scalar=gates[:, tt, e_:e_ + 1], in1=acc[:, tt, :],
                        op0=ALU.mult, op1=ALU.add), reads=[pdk, ("gates", tt), ("acc", tt)], writes=[("acc", tt)])
                finished.add(p0)
                finished.add(p0 + 1)
            P.emit()

        emit_ple(nc, P, env, acc, t0, HT)


def emit_ple(nc, P, env, acc, t0, HT, pre=None):
    out = env["out"]; ident_bf = env["ident_bf"]
    with ExitStack() as sf:
        pre_tile = pre(sf) if pre is not None else None
        sbf = lambda name, shape, dt: sf.enter_context(nc.sbuf_tensor(U(name), list(shape), dt))
        wg = sbf("wg", [128, 8, D], BF16)
        wp = sbf("wp", [128, 2, D], BF16)
        hb_r = Rot(nc, sf, "hb", [128, D], BF16, 3)
        hT_r = Rot(nc, sf, "hT", [128, 8, 128], BF16, 3)
        pt_r = Rot(nc, sf, "ptile", [128, 256], F32, 3)
        pb_r = Rot(nc, sf, "pb", [128, 256], BF16, 3)
        pT_r = Rot(nc, sf, "pT", [128, 2, 128], BF16, 3)
        sgo_r = Rot(nc, sf, "sgo", [128, D], F32, 2)
        o_r = Rot(nc, sf, "o", [128, D], F32, 2)
        ptp_r = Rot(nc, sf, "ptp", [128, 8, 128], BF16, 2, psum=True)
        ptq_r = Rot(nc, sf, "ptq", [128, 8, 128], BF16, 1, psum=True)
        pgp_r = Rot(nc, sf, "pgp", [128, 2, 512], F32, 1, psum=True)
        ppp_r = Rot(nc, sf, "ppp", [128, 2, 512], F32, 1, psum=True)
        for kc in range(8):
            P.dma("pool", lambda e, kc=kc: e.dma_start(out=wg[:, kc, :], in_=env["ple_gate"][kc * 128:(kc + 1) * 128, :]), writes=[("wg", kc)])
        for kc in range(2):
            P.dma("pool", lambda e, kc=kc: e.dma_start(out=wp[:, kc, :], in_=env["ple_proj"][kc * 128:(kc + 1) * 128, :]), writes=[("wp", kc)])
        C = [dict() for _ in range(HT)]

        def F0(tt):
            c = C[tt]
            if pre_tile is not None:
                pre_tile(tt)
            ptile, ptk = pt_r.next()
            P.dma("sp", lambda e: e.dma_start(out=ptile[:], in_=env["p_own"][(t0 + tt) * 128:(t0 + tt + 1) * 128, :]), writes=[ptk])
            c.update(ptile=ptile, ptk=ptk)

        def F1(tt):
            c = C[tt]
            hb, hbk = hb_r.next()
            P.act(lambda e: e.activation(out=hb[:], in_=acc[:, tt, :], func=AF.Copy), reads=[("acc", tt)], writes=[hbk])
            pb, pbk = pb_r.next()
            P.act(lambda e: e.activation(out=pb[:], in_=c["ptile"][:], func=AF.Copy), reads=[c["ptk"]], writes=[pbk])
            c.update(hb=hb, hbk=hbk, pb=pb, pbk=pbk)

        def F2(tt):
            c = C[tt]
            hb, hbk, pb, pbk = c["hb"], c["hbk"], c["pb"], c["pbk"]
            ptp, ptpk = ptp_r.next()
            for kc in range(8):
                P.pe(lambda e, kc=kc: e.transpose(out=ptp[:, kc, :], in_=hb[:, kc * 128:(kc + 1) * 128], identity=ident_bf[:]), reads=[hbk, "ident_bf"], writes=[ptpk])
            ptq, ptqk = ptq_r.next()
            for kc in range(2):
                P.pe(lambda e, kc=kc: e.transpose(out=ptq[:, kc, :], in_=pb[:, kc * 128:(kc + 1) * 128], identity=ident_bf[:]), reads=[pbk, "ident_bf"], writes=[ptqk])
            hT, hTk = hT_r.next()
            P.dve(lambda e: e.tensor_copy(out=hT[:], in_=ptp[:]), reads=[ptpk], writes=[hTk])
            pT, pTk = pT_r.next()
            P.dve(lambda e: e.tensor_copy(out=pT[:], in_=ptq[:, 0:2, :]), reads=[ptqk], writes=[pTk])
            c.update(hT=hT, hTk=hTk, pT=pT, pTk=pTk)

        def F3(tt):
            c = C[tt]
            hT, hTk, pT, pTk = c["hT"], c["hTk"], c["pT"], c["pTk"]
            pgp, pgpk = pgp_r.next()
            ppp, pppk = ppp_r.next()
            for dh in range(2):
                for kc in range(8):
                    P.pe(lambda e, kc=kc, dh=dh: e.matmul(pgp[:, dh, :], hT[:, kc, :], wg[:, kc, dh * 512:(dh + 1) * 512], start=(kc == 0), stop=(kc == 7)),
                         reads=[hTk, ("wg", kc)], writes=[pgpk])
                for kc in range(2):
                    P.pe(lambda e, kc=kc, dh=dh: e.matmul(ppp[:, dh, :], pT[:, kc, :], wp[:, kc, dh * 512:(dh + 1) * 512], start=(kc == 0), stop=(kc == 1)),
                         reads=[pTk, ("wp", kc)], writes=[pppk])
            sgo, sgok = sgo_r.next()
            P.act(lambda e: e.activation(out=sgo[:], in_=pgp[:].rearrange("p a b -> p (a b)"), func=AF.Sigmoid), reads=[pgpk], writes=[sgok])
            P.dve(lambda e: e.tensor_tensor(out=sgo[:], in0=ppp[:].rearrange("p a b -> p (a b)"), in1=sgo[:], op=ALU.mult), reads=[pppk, sgok], writes=[sgok])
            o, ok = o_r.next()
            P.pool(lambda e: e.tensor_tensor(out=o[:], in0=sgo[:], in1=acc[:, tt, :], op=ALU.add), reads=[sgok, ("acc", tt)], writes=[ok])
            P.dma("sp", lambda e: e.dma_start(out=out[(t0 + tt) * 128:(t0 + tt + 1) * 128, :], in_=o[:]), reads=[ok], writes=[("out", t0 + tt)])

        run_skewed(HT, [F0, F1, F2, F3])
        P.add("sp", lambda e: e.nop(), reads=[("out", t0 + tt) for tt in range(HT)])
        P.emit()


def _consts():
    c = {}
    c["c_ident_bf"] = np.eye(128, dtype=np.float32).astype(ml_dtypes.bfloat16)
    c["c_ident_f"] = np.eye(128, dtype=np.float32)
    k = np.arange(128)[:, None]
    q = np.arange(128)[None, :]
    band = np.concatenate([(q <= k), (q >= k)], axis=1).astype(np.float32)
    c["c_band"] = band.astype(ml_dtypes.bfloat16)
    s = np.arange(64)[:, None]
    t = np.arange(64)[None, :]
    hm = (s <= t).astype(np.float32)
    c["c_hmask"] = np.concatenate([hm, hm], axis=0).astype(np.float32)
    bo = np.zeros((128, 128), np.float32)
    bo[:64, :64] = 1
    bo[64:, 64:] = 1
    c["c_blockones"] = bo.astype(ml_dtypes.bfloat16)
    c["c_ones_f"] = np.ones((128, 128), np.float32)
    pm = np.zeros((128, 128), np.float32)
    for hh in range(2):
        for m in range(8):
            pm[hh * 64 + m + 8, hh * 64 + m] = -1.0
            pm[hh * 64 + m, hh * 64 + m + 8] = 1.0
    c["c_pm"] = pm.astype(ml_dtypes.bfloat16)
    invf = np.zeros((128, 1), np.float64)
    for p in range(128):
        cc = p % 64
        if cc < 16:
            invf[p, 0] = (500000.0 ** (-(cc % 8) / 8.0)) / (2 * math.pi)
    c["c_invf"] = invf.astype(np.float32)
    rs = np.ones((128, 512), np.float32)
    rs[:, 0::64] = 0
    c["c_reset"] = rs
    c["c_tri"] = (np.arange(128)[:, None] <= np.arange(128)[None, :]).astype(np.float32).astype(ml_dtypes.bfloat16)
    c["c_iota"] = np.tile(np.arange(NE, dtype=np.float32)[None, :], (128, 1))
    p = np.arange(128, dtype=np.float32)[:, None, None]
    e_ = np.arange(NE, dtype=np.float32)[None, :, None]
    j2 = np.arange(2, dtype=np.float32)[None, None, :]
    c["c_zbase"] = np.ascontiguousarray((e_ * TOK + j2 * 128 + p).reshape(128, 2 * NE).astype(np.float32))
    c["c_ztrash"] = np.ascontiguousarray(np.broadcast_to(XROWS + p, (128, NE, 2)).reshape(128, 2 * NE).astype(np.float32))
    c["c_zlim"] = np.ascontiguousarray(np.broadcast_to((e_ + 1) * TOK, (128, NE, 2)).reshape(128, 2 * NE).astype(np.float32))
    return c


def _fm(v, n):
    return np.ascontiguousarray(np.asarray(v, np.float32).reshape(n, 128).T)


def make_in_maps(inp, dbg=None, extra=None):
    x = np.asarray(inp["x"], np.float32)
    p = np.asarray(inp["p"], np.float32)[0]
    positions = np.asarray(inp["positions"]).astype(np.int32)
    consts = _consts()
    shared = dict(consts)
    shared["g_mix"] = _fm(inp["mix_norm_g"][0], 8)
    shared["w_in"] = np.ascontiguousarray(inp["w_in"][0], np.float32)
    shared["gq"] = np.tile(np.asarray(inp["q_norm_g"][0], np.float32), 2).reshape(128, 1)
    shared["gk"] = np.tile(np.asarray(inp["k_norm_g"][0], np.float32), 2).reshape(128, 1)
    shared["lb0"] = _fm(inp["hgrn_lb_logits"][0], 4)
    shared["lb1"] = _fm(inp["hgrn_lb_logits"][1], 4)
    shared["gnorm"] = np.asarray(inp["hgrn_norm_g"][0], np.float32).reshape(128, 1)
    shared["w_out"] = np.ascontiguousarray(inp["w_out"][0], np.float32)
    shared["g_ffn"] = _fm(inp["ffn_norm_g"][0], 8)
    shared["router_w"] = np.ascontiguousarray(inp["router_w"][0], np.float32)
    shared["router_b"] = np.asarray(inp["router_b"][0], np.float32).reshape(1, NE)
    shared["gffn_row"] = np.asarray(inp["ffn_norm_g"][0], np.float32).reshape(1, D)
    if SPARSE:
        shared["w_gu_nat"] = np.ascontiguousarray(inp["expert_w_gate_up"][0], np.float32)
        shared["b_gu_nat"] = np.ascontiguousarray(inp["expert_b_gate_up"][0], np.float32)
    wg = np.asarray(inp["expert_w_gate_up"][0], np.float32)
    wg5 = wg.reshape(NE, 8, 128, 2, 4, 256)
    if not SPARSE:
        shared["wgu"] = np.ascontiguousarray(wg5.transpose(0, 4, 2, 1, 3, 5)).reshape(NE, 4, 128, 4096)
        bgu = np.asarray(inp["expert_b_gate_up"][0], np.float32)
        shared["bgu"] = np.ascontiguousarray(bgu.reshape(NE, 16, 128).transpose(2, 0, 1))
    shared["w_down"] = np.ascontiguousarray(inp["expert_w_down"][0], np.float32)
    shared["b_down"] = np.ascontiguousarray(inp["expert_b_down"][0], np.float32)
    shared["ple_proj"] = np.ascontiguousarray(inp["ple_proj"][0], np.float32)
    shared["ple_gate"] = np.ascontiguousarray(inp["ple_gate"][0], np.float32)
    maps = []
    for c in range(NCORES):
        b, h = divmod(c, 2)
        m = dict(shared)
        m["xo"] = np.ascontiguousarray(x[b, h * TOK:(h + 1) * TOK])
        m["p_own"] = np.ascontiguousarray(p[b, h * TOK:(h + 1) * TOK])
        if h == 1:
            m["xc"] = np.ascontiguousarray(x[b, 0:TOK])
            pc = positions[b, 0:TOK]
            m["onesctx"] = np.ones((128, 64), np.float32)
        else:
            m["xc"] = np.zeros((TOK, D), np.float32)
            pc = np.zeros((TOK,), np.int32)
            m["onesctx"] = np.zeros((128, 64), np.float32)
        m["pos"] = np.concatenate([pc, positions[b, h * TOK:(h + 1) * TOK]]).reshape(1, 4096).astype(np.int32)
        if extra is not None:
            m.update(extra(c))
        maps.append(m)
    return maps


_NC_CACHE = {}


def kernel(**inputs):
    if "nc" not in _NC_CACHE:
        _NC_CACHE["nc"] = build()
    nc = _NC_CACHE["nc"]
    maps = make_in_maps(inputs)
    res = run_bass_kernel_spmd(nc, maps, core_ids=list(range(NCORES)))
    outp = np.zeros((4, 4096, D), np.float32)
    for c in range(NCORES):
        b, h = divmod(c, 2)
        outp[b, h * TOK:(h + 1) * TOK] = res.results[c]["out"]
    return outp
```

```python
import math
from contextlib import ExitStack

import numpy as np
import ml_dtypes
import concourse.bass as bass
import concourse.mybir as mybir
from concourse.bass_utils import run_bass_kernel_spmd

F32 = mybir.dt.float32
BF16 = mybir.dt.bfloat16
I32 = mybir.dt.int32
AF = mybir.ActivationFunctionType
ALU = mybir.AluOpType
AX = mybir.AxisListType

D = 1024
TOK = 2048
NE = 32
EPS = 1e-6
NCORES = 8
SPARSE = True
XROWS = NE * TOK


class _Op:
    __slots__ = ("sig_aux", "eng", "fn", "deps", "signal", "sig", "dma", "emitted", "idx", "region")


class Prog:
    COMPUTE = ("pe", "act", "dve", "pool")

    def __init__(self, nc, n_dma_sems=36):
        self.nc = nc
        self.ops = []
        self.last_w = {}
        self.readers = {}
        self.sems = {}
        self.cnt = {e: 0 for e in self.COMPUTE}
        self.dma_sems = []
        self.dma_tot = []
        self.dma_rr = 0
        self.n_dma_sems = n_dma_sems
        self.waited = {}
        self.stack = None
        self.cur_region = None
        self.regs = {}
        self.wstream_ops = set()

    def setup(self, stack):
        nc = self.nc
        for e in self.COMPUTE:
            self.sems[e] = stack.enter_context(nc.semaphore("s_" + e))
        for i in range(self.n_dma_sems):
            self.dma_sems.append(stack.enter_context(nc.semaphore("s_dma%d" % i)))
            self.dma_tot.append(0)
        self.wsems = [stack.enter_context(nc.semaphore("s_wdma%d" % i)) for i in range(32)]
        self.wtot = [0] * 32
        self.wrr = 0
        self.gsems = [stack.enter_context(nc.semaphore("s_gdma%d" % i)) for i in range(20)]
        self.gtot = [0] * 20
        self.grr = 0

    def add(self, eng, fn, reads=(), writes=(), dma=False, region=None):
        op = _Op()
        op.region = self.cur_region if region is None else (region or None)
        op.eng = eng
        op.fn = fn
        op.dma = dma
        op.signal = dma
        op.sig = None
        op.emitted = False
        op.idx = len(self.ops)
        deps = []
        for k in reads:
            w = self.last_w.get(k)
            if w is not None:
                deps.append(w)
        for k in writes:
            w = self.last_w.get(k)
            if w is not None:
                deps.append(w)
            for r in self.readers.get(k, ()):
                deps.append(r)
        dd = []
        seen = set()
        for d in deps:
            if d is op or id(d) in seen:
                continue
            seen.add(id(d))
            if d.eng == "pe" and eng == "pe" and not d.dma and not dma:
                continue
            if d.emitted and not d.dma:
                continue
            if d.eng == "sp" and not d.dma:
                continue
            dd.append(d)
            d.signal = True
        op.deps = dd
        for k in reads:
            self.readers.setdefault(k, []).append(op)
        for k in writes:
            self.last_w[k] = op
            self.readers[k] = []
        self.ops.append(op)
        return op

    def pe(self, fn, reads=(), writes=()):
        return self.add("pe", fn, reads, writes)

    def act(self, fn, reads=(), writes=()):
        return self.add("act", fn, reads, writes)

    def dve(self, fn, reads=(), writes=()):
        return self.add("dve", fn, reads, writes)

    def pool(self, fn, reads=(), writes=()):
        return self.add("pool", fn, reads, writes)

    def dma(self, q, fn, reads=(), writes=(), wstream=False):
        op = self.add(q, fn, reads, writes, dma=True)
        if wstream:
            self.wstream_ops.add(id(op))
        return op

    def regload(self, engname, key, ap, reads=()):
        op = self.add(engname, "regload", reads, (), region=False)
        op.region = None
        op.sig_aux = (key, ap)
        return op

    def emit(self):
        nc = self.nc
        todo = [o for o in self.ops if not o.emitted]
        for o in todo:
            if o.dma and id(o) in self.wstream_ops:
                j = self.wrr
                self.wrr = (self.wrr + 1) % len(self.wsems)
                prev = self.wtot[j]
                self.wtot[j] += 16
                o.sig = (self.wsems[j], self.wtot[j], ("w", j), prev)
            elif o.dma and o.eng == "pool":
                j = self.grr
                self.grr = (self.grr + 1) % len(self.gsems)
                prev = self.gtot[j]
                self.gtot[j] += 16
                o.sig = (self.gsems[j], self.gtot[j], ("g", j), prev)
            elif o.dma:
                j = self.dma_rr
                self.dma_rr = (self.dma_rr + 1) % self.n_dma_sems
                prev = self.dma_tot[j]
                self.dma_tot[j] += 16
                o.sig = (self.dma_sems[j], self.dma_tot[j], ("d", j), prev)
            elif o.signal:
                self.cnt[o.eng] += 1
                o.sig = (self.sems[o.eng], self.cnt[o.eng], ("c", o.eng), None)
        by = {}
        for o in todo:
            by.setdefault(o.eng, []).append(o)
        qmap = {"pe": "tensor", "act": "scalar", "dve": "vector", "pool": "gpsimd", "sp": "sync"}

        def run(engname, ops):
            def body(eng):
                waited = self.waited.setdefault(engname, {})
                regs = self.regs.setdefault(engname, {})
                rstack = ExitStack()

                def getreg(key):
                    if key not in regs:
                        regs[key] = rstack.enter_context(eng.register("r_%s_%s" % (engname, key)))
                    return regs[key]

                def emit_op(o):
                    ws = {}
                    for d in o.deps:
                        if d.sig[2] not in ws or ws[d.sig[2]][1] < d.sig[1]:
                            ws[d.sig[2]] = (d.sig[0], d.sig[1])
                    if o.dma and o.sig[3] > 0:
                        if o.sig[2] not in ws or ws[o.sig[2]][1] < o.sig[3]:
                            ws[o.sig[2]] = (o.sig[0], o.sig[3])
                    for key, (sem, val) in ws.items():
                        if waited.get(key, 0) >= val:
                            continue
                        waited[key] = val
                        eng.wait_ge(sem, val)
                    if o.fn == "regload":
                        eng.reg_load(getreg(o.sig_aux[0]), o.sig_aux[1])
                    else:
                        inst = o.fn(eng)
                        if o.sig is not None:
                            inst.then_inc(o.sig[0], 16 if o.dma else 1)
                    o.emitted = True

                def comp_for(groups):
                    ncomp = sum(1 for grp in groups for g in grp if g.sig is not None and not g.dma)
                    dmas = [g for grp in groups for g in grp if g.dma]
                    if ncomp:
                        eng.drain()
                        eng.sem_inc(self.sems[engname], ncomp)
                    for g in dmas:
                        if g.sig[3] > 0:
                            eng.wait_ge(g.sig[0], g.sig[3])
                        eng.sem_inc(g.sig[0], 16)
                    if not ncomp and not dmas:
                        eng.nop()

                def emit_chain(groups, gi):
                    grp = groups[gi]
                    with eng.If_lt(getreg(grp[0].region[0]), grp[0].region[1] + 1):
                        comp_for(groups[gi:])
                    with eng.Else():
                        for g in grp:
                            emit_op(g)
                        if gi + 1 < len(groups):
                            emit_chain(groups, gi + 1)

                i = 0
                while i < len(ops):
                    o = ops[i]
                    if o.region is None:
                        emit_op(o)
                        i += 1
                        continue
                    groups = []
                    j = i
                    while j < len(ops) and ops[j].region is not None and ops[j].region[0] == o.region[0] and \
                            (not groups or ops[j].region[1] >= groups[-1][0].region[1]):
                        if groups and ops[j].region == groups[-1][0].region:
                            groups[-1].append(ops[j])
                        else:
                            groups.append([ops[j]])
                        j += 1
                    saved = dict(waited)
                    emit_chain(groups, 0)
                    waited.clear()
                    waited.update(saved)
                    i = j
                rstack.close()
            return body

        with nc.Block() as block:
            for engname, ops in by.items():
                getattr(block, qmap[engname])(run(engname, ops))
        for o in todo:
            o.fn = None


_UID = [0]


def U(name):
    _UID[0] += 1
    return "%s_%d" % (name, _UID[0])


class Rot:
    def __init__(self, nc, stack, name, shape, dtype, n, psum=False):
        self.bufs = []
        for i in range(n):
            alloc = nc.psum_tensor if psum else nc.sbuf_tensor
            self.bufs.append(stack.enter_context(alloc(U("%s%d" % (name, i)), list(shape), dtype)))
        self.name = name
        self.i = 0
        self.gen = 0

    @classmethod
    def from_aps(cls, name, aps):
        r = cls.__new__(cls)
        r.bufs = list(aps)
        r.name = name
        r.i = 0
        r.gen = 0
        return r

    def next(self):
        b = self.bufs[self.i]
        key = (self.name, self.i)
        self.i = (self.i + 1) % len(self.bufs)
        return b, key


def build(dbg=None):
    nc = bass.Bass("TRN2", target_bir_lowering=False)

    def din(name, shape, dt=F32):
        return nc.dram_tensor(name, list(shape), dt, kind="ExternalInput").ap()

    xo = din("xo", [TOK, D])
    xc = din("xc", [TOK, D])
    pos = din("pos", [1, 4096], I32)
    onesctx = din("onesctx", [128, 64])
    g_mix = din("g_mix", [128, 8])
    w_in = din("w_in", [D, 3584])
    gq = din("gq", [128, 1])
    gk = din("gk", [128, 1])
    lb0 = din("lb0", [128, 4])
    lb1 = din("lb1", [128, 4])
    gnorm = din("gnorm", [128, 1])
    w_out = din("w_out", [D, D])
    g_ffn = din("g_ffn", [128, 8])
    router_w = din("router_w", [D, NE])
    router_b = din("router_b", [1, NE])
    if SPARSE:
        w_gu_nat = din("w_gu_nat", [NE, D, 2 * D])
        b_gu_nat = din("b_gu_nat", [NE, 2 * D])
    else:
        wgu = din("wgu", [NE, 4, 128, 4096])
        bgu = din("bgu", [128, NE, 16])
    w_down = din("w_down", [NE, D, D])
    b_down = din("b_down", [NE, D])
    ple_proj = din("ple_proj", [256, D])
    ple_gate = din("ple_gate", [D, D])
    p_own = din("p_own", [TOK, 256])
    c_ident_bf = din("c_ident_bf", [128, 128], BF16)
    c_ident_f = din("c_ident_f", [128, 128])
    c_band = din("c_band", [128, 256], BF16)
    c_hmask = din("c_hmask", [128, 64])
    c_blockones = din("c_blockones", [128, 128], BF16)
    c_ones_f = din("c_ones_f", [128, 128])
    c_pm = din("c_pm", [128, 128], BF16)
    c_invf = din("c_invf", [128, 1])
    c_reset = din("c_reset", [128, 512])
    if dbg == "moe":
        d_mixT = din("d_mixT", [128, 8, TOK], BF16)
    out = nc.dram_tensor("out", [TOK, D], F32, kind="ExternalOutput").ap()
    gffn_row = din("gffn_row", [1, D])
    c_tri = din("c_tri", [128, 128], BF16)
    c_iota = din("c_iota", [128, NE])
    c_zbase = din("c_zbase", [128, 2 * NE])
    c_zlim = din("c_zlim", [128, 2 * NE])
    c_ztrash = din("c_ztrash", [128, 2 * NE])
    Xd = nc.dram_tensor("Xd", [NE * TOK + 128, D], BF16, kind="Internal").ap()
    Yd = nc.dram_tensor("Yd", [NE * TOK, D], F32, kind="Internal").ap()
    if dbg and dbg.startswith("mix"):
        d_out_mixT = nc.dram_tensor("d_out_mixT", [128, 8, TOK], BF16, kind="ExternalOutput").ap()

    P = Prog(nc)
    with ExitStack() as top:
        P.setup(top)
        sb = lambda name, shape, dt, st=top: st.enter_context(nc.sbuf_tensor(U(name), list(shape), dt))
        ident_bf = sb("ident_bf", [128, 128], BF16)
        ident_f = sb("ident_f", [128, 128], F32)
        acc = sb("acc", [128, 16, D], F32)
        accb = acc[:].rearrange("p a b -> p (a b)").bitcast(BF16)
        gw = sb("gw", [128, 16, 4], F32)
        ridx = sb("ridx", [128, 16, 4], I32)
        cnt_i = sb("cnt_i", [1, NE], I32)
        zi = sb("zi", [128, 2 * NE], I32)
        smix = ExitStack()
        mixT = smix.enter_context(nc.sbuf_tensor(U("mixT"), [128, 8, TOK], BF16))
        P.dma("sp", lambda e: e.dma_start(out=ident_bf[:], in_=c_ident_bf), writes=["ident_bf"])
        P.dma("sp", lambda e: e.dma_start(out=ident_f[:], in_=c_ident_f), writes=["ident_f"])

        if dbg == "moe":
            P.dma("sp", lambda e: e.dma_start(out=mixT[:], in_=d_mixT), writes=["mixT"])
        else:
            emit_mixer(nc, P, locals(), upto=(dbg[3:] if dbg and dbg.startswith("mix") and len(dbg) > 3 else "T"))
        if dbg and dbg.startswith("mix"):
            P.dma("sp", lambda e: e.dma_start(out=d_out_mixT, in_=mixT[:]), reads=["mixT"], writes=["d_out"])
            P.add("sp", lambda e: e.nop(), reads=["d_out"])
            P.emit()
            smix.close()
            return nc

        if SPARSE:
            emit_sparse(nc, P, locals(), smix)
        else:
            for half in range(2):
                emit_half(nc, P, locals(), half)
            smix.close()
    return nc


def a1_block(nc, P, env, pools, blk, gmix):
    xt_r, junk_r, xn_r, st_r, tp_r, aT_r = pools
    ident_bf = env["ident_bf"]
    aT, aTk = aT_r.next()
    for t in range(4):
        gt = blk * 4 + t
        src = env["xc"][gt * 128:(gt + 1) * 128, :] if gt < 16 else env["xo"][(gt - 16) * 128:(gt - 15) * 128, :]
        xt, xk = xt_r.next()
        P.dma("sp", lambda e, xt=xt, src=src: e.dma_start(out=xt[:], in_=src), writes=[xk])
        junk, jk = junk_r.next()
        st, stk = st_r.next()
        P.act(lambda e, junk=junk, xt=xt, st=st: e.activation(out=junk[:], in_=xt[:], func=AF.Square, accum_out=st[:, 0:1]),
              reads=[xk], writes=[jk, stk])
        P.act(lambda e, st=st: e.activation(out=st[:, 1:2], in_=st[:, 0:1], func=AF.Sqrt, scale=1.0 / D, bias=EPS), reads=[stk], writes=[stk])
        P.dve(lambda e, st=st: e.reciprocal(out=st[:, 2:3], in_=st[:, 1:2]), reads=[stk], writes=[stk])
        xn, xnk = xn_r.next()
        P.dve(lambda e, xn=xn, xt=xt, st=st: e.tensor_scalar(out=xn[:], in0=xt[:], scalar1=st[:, 2:3], scalar2=None, op0=ALU.mult),
              reads=[xk, stk], writes=[xnk])
        tp, tpk = tp_r.next()
        for kc in range(8):
            P.pe(lambda e, tp=tp, xn=xn, kc=kc: e.transpose(out=tp[:, kc, :], in_=xn[:, kc * 128:(kc + 1) * 128], identity=ident_bf[:]),
                 reads=[xnk, "ident_bf"], writes=[tpk])
        P.dve(lambda e, aT=aT, tp=tp, t=t: e.tensor_tensor(out=aT[:, :, t * 128:(t + 1) * 128], in0=tp[:],
                                                            in1=gmix[:].unsqueeze(2).broadcast_to([128, 8, 128]), op=ALU.mult),
              reads=[tpk, "gmix"], writes=[(aTk, t)])
    return aT, [(aTk, t) for t in range(4)]


import os as _os


def emit_mixer(nc, P, env, upto="T"):
    mixT = env["mixT"]
    ident_bf = env["ident_bf"]
    with ExitStack() as sm_:
        sbm = lambda name, shape, dt: sm_.enter_context(nc.sbuf_tensor(U(name), list(shape), dt))
        gmix = sbm("gmix", [128, 8], F32)
        P.dma("sp", lambda e: e.dma_start(out=gmix[:], in_=env["g_mix"]), writes=["gmix"])

        with ExitStack() as sh:
            sbh = lambda name, shape, dt: sh.enter_context(nc.sbuf_tensor(U(name), list(shape), dt))
            accb = env["accb"]
            w_h = accb[:, 0:16384].rearrange("p (k c) -> p k c", k=8)
            for kc in range(8):
                P.dma("pool", lambda e, kc=kc: e.dma_start(out=w_h[:, kc, :], in_=env["w_in"][kc * 128:(kc + 1) * 128, 1536:3584]),
                      writes=[("w_h", kc)])
            hmask = sbh("hmask", [128, 64], F32)
            ones_f = sbh("ones_f", [128, 128], F32)
            reset = sbh("reset", [128, 512], F32)
            gnorm = sbh("gnorm", [128, 1], F32)
            l0 = sbh("l0", [128, 4], F32)
            l1 = sbh("l1", [128, 4], F32)
            oml = sbh("oml", [128, 4], F32)
            noml = sbh("noml", [128, 4], F32)
            for (t_, src, key) in ((hmask, "c_hmask", "hmask"), (ones_f, "c_ones_f", "ones_f"), (reset, "c_reset", "reset"),
                                   (gnorm, "gnorm", "gnorm"), (l0, "lb0", "l0"), (l1, "lb1", "l1")):
                P.dma("sp", lambda e, t_=t_, src=src: e.dma_start(out=t_[:], in_=env[src]), writes=[key])
            P.dve(lambda e: e.tensor_tensor(out=oml[:], in0=l1[:], in1=l0[:], op=ALU.subtract), reads=["l0", "l1"], writes=["oml"])
            P.act(lambda e: e.activation(out=oml[:], in_=oml[:], func=AF.Sigmoid), reads=["oml"], writes=["oml"])
            P.dve(lambda e: e.tensor_scalar(out=noml[:], in0=oml[:], scalar1=-1.0, scalar2=None, op0=ALU.mult), reads=["oml"], writes=["oml2"])
            tpb_r = Rot(nc, sh, "tpb", [128, 8, 128], BF16, 2, psum=True)
            pools = (Rot(nc, sh, "xt", [128, D], F32, 2), Rot(nc, sh, "junk", [128, D], BF16, 1), Rot(nc, sh, "xn", [128, D], BF16, 2),
                     Rot(nc, sh, "st", [128, 4], F32, 4), tpb_r,
                     Rot.from_aps("aTh", [accb[:, 16384 + i * 4096:16384 + (i + 1) * 4096].rearrange("p (k c) -> p k c", k=8) for i in range(2)]))
            vt_r = Rot.from_aps("vth", [accb[0:64, 24576 + i * 4096:24576 + (i + 1) * 4096].rearrange("p (k c) -> p k c", k=8) for i in range(2)])
            At_r = Rot(nc, sh, "At", [128, 64], BF16, 4)
            eb = [sbh("eb%d" % i, [128, 512], F32) for i in range(4)]
            kt = [sbh("kt%d" % i, [128, 512], BF16) for i in range(4)]
            qt = [sbh("qt%d" % i, [128, 512], BF16) for i in range(4)]
            ktok = [sbh("ktok%d" % i, [64, 8, 128], BF16) for i in range(4)]
            sgate = [sbh("sgate%d" % i, [128, 512], F32) for i in range(4)]
            oT = [sbh("oT%d" % i, [128, 512], F32) for i in range(4)]
            S = sbh("S", [128, 4, 128], F32)
            S_bf = sbh("S_bf", [128, 4, 128], BF16)
            pb_r = Rot(nc, sh, "pb", [128, 512], F32, 6, psum=True)
            mm_r = pb_r
            P.dve(lambda e: e.memset(S[:], 0.0), writes=[("S", i) for i in range(4)])
            P.dve(lambda e: e.memset(S_bf[:], 0.0), writes=[("S_bf", i) for i in range(4)])

            def mmgroup(col0, aT, aTkeys):
                mm, mmk = mm_r.next()
                for kc in range(8):
                    P.pe(lambda e, mm=mm, kc=kc, col0=col0, aT=aT: e.matmul(mm[:], w_h[:, kc, col0:col0 + 128], aT[:, kc, :],
                                                                          start=(kc == 0), stop=(kc == 7)),
                         reads=[("w_h", kc)] + aTkeys, writes=[mmk])
                return mm, mmk

            snegs = [sbh("snegh%d" % i, [128, 512], F32) for i in range(4)]
            ffs = [sbh("ffh%d" % i, [128, 512], F32) for i in range(4)]
            bbs = [sbh("bbh%d" % i, [128, 512], F32) for i in range(4)]
            enbs = [sbh("enbh%d" % i, [128, 512], F32) for i in range(4)]
            khs = [sbh("khh%d" % i, [128, 512], BF16) for i in range(4)]
            qss = [sbh("qsh%d" % i, [128, 512], F32) for i in range(4)]
            H4 = range(4)
            USE_STT = False
            for blk in range(8):
                own = blk >= 4
                ob = blk - 4
                aT, aTkeys = a1_block(nc, P, env, pools, blk, gmix)
                vt, vtk = vt_r.next()
                for n in range(8):
                    mm, mmk = pb_r.next()
                    for kc in range(8):
                        P.pe(lambda e, mm=mm, kc=kc, n=n, aT=aT: e.matmul(mm[0:64, :], aT[:, kc, n * 64:(n + 1) * 64], w_h[:, kc, 1024:1536],
                                                                         start=(kc == 0), stop=(kc == 7)),
                             reads=[("w_h", kc), aTkeys[n // 2]], writes=[mmk])
                    P.act(lambda e, vt=vt, mm=mm, n=n: e.activation(out=vt[0:64, n, :], in_=mm[0:64, :], func=AF.Copy), reads=[mmk], writes=[(vtk, n)])
                mf = [mmgroup(512 + hd * 128, aT, aTkeys) for hd in H4]
                for hd in H4:
                    P.act(lambda e, hd=hd, mf=mf: e.activation(out=snegs[hd][:], in_=mf[hd][0][:], func=AF.Sigmoid, scale=-1.0), reads=[mf[hd][1]], writes=[("sneg", hd)])
                if own:
                    mq = [mmgroup(hd * 128, aT, aTkeys) for hd in H4]
                    for hd in H4:
                        P.act(lambda e, hd=hd, mq=mq: e.activation(out=qss[hd][:], in_=mq[hd][0][:], func=AF.Silu), reads=[mq[hd][1]], writes=[("qs", hd)])
                for hd in H4:
                    P.dve(lambda e, hd=hd: e.tensor_scalar(out=ffs[hd][:], in0=snegs[hd][:], scalar1=noml[:, hd:hd + 1], scalar2=1.0, op0=ALU.mult, op1=ALU.add),
                          reads=[("sneg", hd), "oml2"], writes=[("ff", hd)])
                for hd in H4:
                    P.act(lambda e, hd=hd: e.activation(out=ffs[hd][:], in_=ffs[hd][:], func=AF.Ln), reads=[("ff", hd)], writes=[("ff", hd)])
                if own:
                    mg = [mmgroup(1536 + hd * 128, aT, aTkeys) for hd in H4]
                    for hd in H4:
                        P.act(lambda e, hd=hd, mg=mg: e.activation(out=sgate[hd][:], in_=mg[hd][0][:], func=AF.Silu), reads=[mg[hd][1]], writes=[("sgate", hd)])
                for hd in H4:
                    P.dve(lambda e, hd=hd: e.tensor_tensor_scan(out=bbs[hd][:], data0=reset[:], data1=ffs[hd][:], initial=0.0, op0=ALU.mult, op1=ALU.add),
                          reads=[("ff", hd), "reset"], writes=[("bb", hd)])
                for hd in H4:
                    P.act(lambda e, hd=hd: e.activation(out=eb[hd][:], in_=bbs[hd][:], func=AF.Exp), reads=[("bb", hd)], writes=[("eb", hd)])
                    P.act(lambda e, hd=hd: e.activation(out=enbs[hd][:], in_=bbs[hd][:], func=AF.Exp, scale=-1.0), reads=[("bb", hd)], writes=[("enb", hd)])
                for hd in H4:
                    P.dve(lambda e, hd=hd: e.scalar_tensor_tensor(out=kt[hd][:], in0=snegs[hd][:], scalar=oml[:, hd:hd + 1], in1=enbs[hd][:],
                                                                  op0=ALU.mult, op1=ALU.mult), reads=[("sneg", hd), ("enb", hd), "oml"], writes=[("kt", hd)])
                    if own:
                        P.pool(lambda e, hd=hd: e.tensor_tensor(out=qt[hd][:], in0=qss[hd][:], in1=eb[hd][:], op=ALU.mult),
                               reads=[("qs", hd), ("eb", hd)], writes=[("qt", hd)])
                for hd in H4:
                    P.pool(lambda e, hd=hd: e.tensor_tensor(
                        out=khs[hd][:].rearrange("p (n c) -> p n c", c=64), in0=kt[hd][:].rearrange("p (n c) -> p n c", c=64),
                        in1=eb[hd][:].rearrange("p (n c) -> p n c", c=64)[:, :, 63:64].broadcast_to([128, 8, 64]), op=ALU.mult),
                        reads=[("kt", hd), ("eb", hd)], writes=[("kh", hd)])
                for hd in H4:
                    tpk_, tpkk = tpb_r.next()
                    for n in range(8):
                        P.pe(lambda e, hd=hd, n=n, tpk_=tpk_: e.transpose(out=tpk_[0:64, n, :], in_=khs[hd][:, n * 64:(n + 1) * 64], identity=ident_bf[:]),
                             reads=[("kh", hd), "ident_bf"], writes=[tpkk])
                    P.dve(lambda e, hd=hd, tpk_=tpk_: e.tensor_copy(out=ktok[hd][:], in_=tpk_[0:64, :, :]), reads=[tpkk], writes=[("ktok", hd)])
                for n in range(8):
                    t = n
                    cs = slice(n * 64, n * 64 + 64)
                    for hd in H4:
                        if own:
                            pA, pAk = pb_r.next()
                            P.pe(lambda e, hd=hd, cs=cs, pA=pA: e.matmul(pA[0:64, 0:64], kt[hd][:, cs], qt[hd][:, cs], start=True, stop=True),
                                 reads=[("kt", hd), ("qt", hd)], writes=[pAk])
                            At, Atk = At_r.next()
                            P.dve(lambda e, At=At, pA=pA: e.tensor_tensor(out=At[0:64, :], in0=pA[0:64, 0:64], in1=hmask[0:64, :], op=ALU.mult),
                                  reads=[pAk, "hmask"], writes=[Atk])
                            pO, pOk = pb_r.next()
                            P.pe(lambda e, hd=hd, cs=cs, pO=pO: e.matmul(pO[:, 0:64], S_bf[:, hd, :], qt[hd][:, cs], start=True, stop=False),
                                 reads=[("S_bf", hd), ("qt", hd)], writes=[pOk])
                            P.pe(lambda e, hd=hd, t=t, At=At, vt=vt, pO=pO: e.matmul(pO[:, 0:64], vt[0:64, t, hd * 128:(hd + 1) * 128], At[0:64, :],
                                                                                 start=False, stop=True),
                                 reads=[(vtk, t), Atk], writes=[pOk])
                            P.act(lambda e, hd=hd, cs=cs, pO=pO: e.activation(out=oT[hd][:, cs], in_=pO[:, 0:64], func=AF.Copy),
                                  reads=[pOk], writes=[("oT", hd, n)])
                        pS, pSk = pb_r.next()
                        P.pe(lambda e, hd=hd, t=t, vt=vt, pS=pS: e.matmul(pS[:, 0:128], ktok[hd][0:64, t, :], vt[0:64, t, hd * 128:(hd + 1) * 128],
                                                                        start=True, stop=True),
                             reads=[("ktok", hd), (vtk, t)], writes=[pSk])
                        if USE_STT:
                            P.dve(lambda e, hd=hd, n=n, pS=pS: e.scalar_tensor_tensor(out=S[:, hd, :], in0=S[:, hd, :], scalar=eb[hd][:, n * 64 + 63:n * 64 + 64],
                                                                                    in1=pS[:, 0:128], op0=ALU.mult, op1=ALU.add),
                                  reads=[("S", hd), ("eb", hd), pSk], writes=[("S", hd)])
                        else:
                            if own and hd < 2:
                                P.pool(lambda e, hd=hd, n=n: e.tensor_scalar(out=S[:, hd, :], in0=S[:, hd, :], scalar1=eb[hd][:, n * 64 + 63:n * 64 + 64], scalar2=None, op0=ALU.mult),
                                       reads=[("S", hd), ("eb", hd)], writes=[("S", hd)])
                            else:
                                P.act(lambda e, hd=hd, n=n: e.activation(out=S[:, hd, :], in_=S[:, hd, :], func=AF.Copy, scale=eb[hd][:, n * 64 + 63:n * 64 + 64]),
                                      reads=[("S", hd), ("eb", hd)], writes=[("S", hd)])
                            P.dve(lambda e, hd=hd, pS=pS: e.tensor_tensor(out=S[:, hd, :], in0=pS[:, 0:128], in1=S[:, hd, :], op=ALU.add),
                                  reads=[("S", hd), pSk], writes=[("S", hd)])
                        if own or (blk == 3 and n == 7):
                            P.act(lambda e, hd=hd: e.activation(out=S_bf[:, hd, :], in_=S[:, hd, :], func=AF.Copy), reads=[("S", hd)], writes=[("S_bf", hd)])
                if own:
                    okeys = [[("oT", hd, n) for n in range(8)] for hd in H4]
                    for hd in H4:
                        P.act(lambda e, hd=hd: e.activation(out=snegs[hd][:], in_=oT[hd][:], func=AF.Square), reads=okeys[hd], writes=[("sneg", hd)])
                    mr = []
                    for hd in H4:
                        mm, mmk = pb_r.next()
                        P.pe(lambda e, mm=mm, hd=hd: e.matmul(mm[:], ones_f[:], snegs[hd][:], start=True, stop=True), reads=[("sneg", hd), "ones_f"], writes=[mmk])
                        mr.append((mm, mmk))
                    for hd in H4:
                        P.act(lambda e, hd=hd, mr=mr: e.activation(out=ffs[hd][:], in_=mr[hd][0][:], func=AF.Sqrt, scale=1.0 / 128, bias=EPS), reads=[mr[hd][1]], writes=[("ff", hd)])
                    for hd in H4:
                        P.dve(lambda e, hd=hd: e.reciprocal(out=ffs[hd][:], in_=ffs[hd][:]), reads=[("ff", hd)], writes=[("ff", hd)])
                    for hd in H4:
                        P.dve(lambda e, hd=hd: e.scalar_tensor_tensor(out=bbs[hd][:], in0=oT[hd][:], scalar=gnorm[:, 0:1], in1=ffs[hd][:],
                                                                      op0=ALU.mult, op1=ALU.mult), reads=okeys[hd] + [("ff", hd), "gnorm"], writes=[("bb", hd)])
                    for hd in H4:
                        P.pool(lambda e, hd=hd, ob=ob: e.tensor_tensor(out=mixT[:, 4 + hd, ob * 512:(ob + 1) * 512], in0=bbs[hd][:], in1=sgate[hd][:], op=ALU.mult),
                               reads=[("bb", hd), ("sgate", hd)], writes=[("mixT", 4 + hd, ob)])
            P.emit()

        if upto == "H":
            return
        accb = env["accb"]
        KT = [accb[:, i * 4096:(i + 1) * 4096] for i in range(4)]
        VT = [accb[:, 16384 + i * 4096:16384 + (i + 1) * 4096] for i in range(4)]
        QT = [sbm("QT%d" % i, [128, TOK], BF16) for i in range(4)]
        with ExitStack() as sp_:
            sbp = lambda name, shape, dt: sp_.enter_context(nc.sbuf_tensor(U(name), list(shape), dt))
            w_a = sbp("w_a", [128, 8, 1536], BF16)
            for kc in range(8):
                P.dma("pool", lambda e, kc=kc: e.dma_start(out=w_a[:, kc, :], in_=env["w_in"][kc * 128:(kc + 1) * 128, 0:1536]),
                      writes=[("w_a", kc)])
            Ct = sbp("Ct", [128, 4096], BF16)
            St = sbp("St", [128, 4096], BF16)
            blockones = sbp("blockones", [128, 128], BF16)
            pm = sbp("pm", [128, 128], BF16)
            invf = sbp("invf", [128, 1], F32)
            gq = sbp("gq", [128, 1], F32)
            gk = sbp("gk", [128, 1], F32)
            for (t_, src, key) in ((blockones, "c_blockones", "blockones"), (pm, "c_pm", "pm"), (invf, "c_invf", "invf"),
                                   (gq, "gq", "gq"), (gk, "gk", "gk")):
                P.dma("sp", lambda e, t_=t_, src=src: e.dma_start(out=t_[:], in_=env[src]), writes=[key])
            srope = ExitStack()
            posi_r = Rot(nc, srope, "posi", [128, 1024], I32, 2)
            rf_r = Rot(nc, srope, "rf", [128, 1024], F32, 4)
            ri_r = Rot(nc, srope, "ri", [128, 1024], I32, 2)
            for ch in range(4):
                csl = slice(ch * 1024, (ch + 1) * 1024)
                posi, pik = posi_r.next()
                P.dma("sp", lambda e, posi=posi, csl=csl: e.dma_start(out=posi[:], in_=env["pos"][0:1, csl].partition_broadcast(128)), writes=[pik])
                posf, pfk = rf_r.next()
                P.dve(lambda e, posf=posf, posi=posi: e.tensor_copy(out=posf[:], in_=posi[:]), reads=[pik], writes=[pfk])
                for (off, tab, tkey) in ((0.5, St, "St"), (0.75, Ct, "Ct")):
                    y, yk = rf_r.next()
                    P.dve(lambda e, y=y, posf=posf, off=off: e.tensor_scalar(out=y[:], in0=posf[:], scalar1=invf[:, 0:1], scalar2=off, op0=ALU.mult, op1=ALU.add),
                          reads=[pfk, "invf"], writes=[yk])
                    ni, nik = ri_r.next()
                    P.dve(lambda e, ni=ni, y=y: e.tensor_copy(out=ni[:], in_=y[:]), reads=[yk], writes=[nik])
                    nf, nfk = rf_r.next()
                    P.dve(lambda e, nf=nf, ni=ni: e.tensor_copy(out=nf[:], in_=ni[:]), reads=[nik], writes=[nfk])
                    P.dve(lambda e, y=y, nf=nf: e.tensor_tensor(out=y[:], in0=y[:], in1=nf[:], op=ALU.subtract), reads=[yk, nfk], writes=[yk])
                    P.dve(lambda e, y=y, nf=nf: e.tensor_single_scalar(out=nf[:], in_=y[:], scalar=0.0, op=ALU.is_lt), reads=[yk], writes=[nfk])
                    P.dve(lambda e, y=y, nf=nf: e.tensor_tensor(out=y[:], in0=y[:], in1=nf[:], op=ALU.add), reads=[yk, nfk], writes=[yk])
                    P.act(lambda e, y=y, tab=tab, csl=csl: e.activation(out=tab[:, csl], in_=y[:], func=AF.Sin, scale=6.2831845, bias=-3.1415922),
                          reads=[yk], writes=[(tkey, ch)])
            P.emit()
            srope.close()
            pools = (Rot(nc, sp_, "xt", [128, D], F32, 2), Rot(nc, sp_, "junk", [128, D], BF16, 1), Rot(nc, sp_, "xn", [128, D], BF16, 2),
                     Rot(nc, sp_, "st", [128, 4], F32, 4), Rot(nc, sp_, "tp", [128, 8, 128], BF16, 1, psum=True),
                     Rot(nc, sp_, "aT", [128, 8, 512], BF16, 2))
            mm_r = Rot(nc, sp_, "mm", [128, 512], F32, 3, psum=True)
            pn_r = Rot(nc, sp_, "pn", [128, 512], F32, 2, psum=True)
            pq_r = Rot(nc, sp_, "pq", [128, 512], F32, 2, psum=True)
            sq_r = Rot(nc, sp_, "sq", [128, 512], BF16, 2)
            rt_r = Rot(nc, sp_, "rt", [128, 512], F32, 2)
            qn_r = Rot(nc, sp_, "qn", [128, 512], BF16, 2)
            t1_r = Rot(nc, sp_, "t1", [128, 512], F32, 2)
            t2_r = Rot(nc, sp_, "t2", [128, 512], F32, 2)
            q1 = []
            q2 = []

            def qk_stage0(nm, coff, dest, dsl, gg, sc, bi, hp, blk, aT, aTkeys, tsl, tch):
                mm, mmk = mm_r.next()
                for kc in range(8):
                    P.pe(lambda e, mm=mm, kc=kc: e.matmul(mm[:], w_a[:, kc, coff + hp * 128:coff + (hp + 1) * 128], aT[:, kc, :],
                                                          start=(kc == 0), stop=(kc == 7)),
                         reads=[("w_a", kc)] + aTkeys, writes=[mmk])
                sq, sqk = sq_r.next()
                P.act(lambda e: e.activation(out=sq[:], in_=mm[:], func=AF.Square), reads=[mmk], writes=[sqk])

                def stage1():
                    pn, pnk = pn_r.next()
                    P.pe(lambda e: e.matmul(pn[:], blockones[:], sq[:], start=True, stop=True), reads=[sqk, "blockones"], writes=[pnk])
                    rt, rtk = rt_r.next()
                    P.act(lambda e: e.activation(out=rt[:], in_=pn[:], func=AF.Sqrt, scale=sc, bias=bi), reads=[pnk], writes=[rtk])
                    P.dve(lambda e: e.reciprocal(out=rt[:], in_=rt[:]), reads=[rtk], writes=[rtk])
                    qn, qnk = qn_r.next()
                    P.dve(lambda e: e.scalar_tensor_tensor(out=qn[:], in0=mm[:], scalar=gg[:, 0:1], in1=rt[:], op0=ALU.mult, op1=ALU.mult),
                          reads=[mmk, rtk, nm == "k" and "gk" or "gq"], writes=[qnk])

                    def stage2():
                        pq, pqk = pq_r.next()
                        P.pe(lambda e: e.matmul(pq[:], pm[:], qn[:], start=True, stop=True), reads=[qnk, "pm"], writes=[pqk])
                        t1, t1k = t1_r.next()
                        P.pool(lambda e: e.tensor_tensor(out=t1[:], in0=qn[:], in1=Ct[:, tsl], op=ALU.mult), reads=[qnk, ("Ct", tch)], writes=[t1k])
                        t2, t2k = t2_r.next()
                        P.dve(lambda e: e.tensor_tensor(out=t2[:], in0=pq[:], in1=St[:, tsl], op=ALU.mult), reads=[pqk, ("St", tch)], writes=[t2k])
                        P.pool(lambda e: e.tensor_tensor(out=dest[:, dsl], in0=t1[:], in1=t2[:], op=ALU.add), reads=[t1k, t2k], writes=[(nm + "T", hp, blk)])
                    return stage2
                return stage1

            def v_stage0(hp, blk, aT, aTkeys, tsl):
                mm, mmk = mm_r.next()
                for kc in range(8):
                    P.pe(lambda e, mm=mm, kc=kc: e.matmul(mm[:], w_a[:, kc, 1024 + hp * 128:1024 + (hp + 1) * 128], aT[:, kc, :],
                                                          start=(kc == 0), stop=(kc == 7)),
                         reads=[("w_a", kc)] + aTkeys, writes=[mmk])
                P.act(lambda e: e.activation(out=VT[hp][:, tsl], in_=mm[:], func=AF.Copy), reads=[mmk], writes=[("VT", hp, blk)])
                return None

            def step(s1):
                if q2:
                    q2.pop(0)()
                if q1:
                    q2.append(q1.pop(0)())
                if s1 is not None:
                    q1.append(s1)

            nxt = a1_block(nc, P, env, pools, 0, gmix)
            for blk in range(8):
                own = blk >= 4
                aT, aTkeys = nxt
                tsl = slice(blk * 512, (blk + 1) * 512)
                tch = blk // 2
                for hp in range(4):
                    step(qk_stage0("k", 512, KT[hp], tsl, gk, 1.0 / 64, EPS, hp, blk, aT, aTkeys, tsl, tch))
                    if own:
                        step(qk_stage0("q", 0, QT[hp], slice((blk - 4) * 512, (blk - 3) * 512), gq, 1.0, 64 * EPS, hp, blk, aT, aTkeys, tsl, tch))
                    v_stage0(hp, blk, aT, aTkeys, tsl)
                    if hp == 1 and blk + 1 < 8:
                        nxt = a1_block(nc, P, env, pools, blk + 1, gmix)
            step(None)
            step(None)
            step(None)
            P.emit()

        if upto == "P":
            return
        with ExitStack() as st_:
            sbt = lambda name, shape, dt: st_.enter_context(nc.sbuf_tensor(U(name), list(shape), dt))
            band = sbt("band", [128, 256], BF16)
            onesb = sbt("onesb", [128, 64], BF16)
            onesc32 = sbt("onesc32", [128, 64], F32)
            onesc = sbt("onesc", [128, 64], BF16)
            P.dma("sp", lambda e: e.dma_start(out=band[:], in_=env["c_band"]), writes=["band"])
            P.dma("sp", lambda e: e.dma_start(out=onesc32[:], in_=env["onesctx"]), writes=["onesc32"])
            P.dve(lambda e: e.tensor_copy(out=onesc[:], in_=onesc32[:]), reads=["onesc32"], writes=["onesc"])
            P.dve(lambda e: e.memset(onesb[:], 1.0), writes=["onesb"])
            Vt_r = Rot(nc, st_, "Vt", [128, 32, 192], BF16, 2)
            acc = sbt("acca", [128, 2, TOK], F32)
            den = sbt("den", [128, TOK], F32)
            E_r = Rot(nc, st_, "E", [128, 256], BF16, 4)
            Pm_r = Rot(nc, st_, "Pm", [128, 256], BF16, 4)
            tpv_r = Rot(nc, st_, "tpv", [128, 8, 128], BF16, 1, psum=True)
            ps_r = Rot(nc, st_, "psS", [128, 512], F32, 4, psum=True)
            po_r = Rot(nc, st_, "psO", [128, 512], F32, 3, psum=True)
            for hp in range(4):
                for bi_, d in enumerate((1, 4, 16)):
                    n_lo = 2048 // (128 * d)
                    n_hi = 4096 // (128 * d) - 1
                    Vt, Vtk = Vt_r.next()
                    tiles = {}
                    for r in range(d):
                        for m in range(n_lo - 1, n_hi + 1):
                            idx = len(tiles)
                            tiles[(r, m)] = idx
                            u0 = r + d * 128 * m
                            ksl = slice(u0, u0 + d * 127 + 1, d)
                            tpv, tpvk = tpv_r.next()
                            P.pe(lambda e, tpv=tpv, hp=hp, ksl=ksl: e.transpose(out=tpv[:, 0, :], in_=VT[hp][:, ksl], identity=ident_bf[:]),
                                 reads=[("VT", hp), "ident_bf"], writes=[tpvk])
                            P.act(lambda e, Vt=Vt, idx=idx, tpv=tpv: e.activation(
                                out=Vt[:, idx, :].rearrange("p (s c) -> p s c", c=64)[:, 0:3:2, :],
                                in_=tpv[:, 0, :].rearrange("p (s c) -> p s c", c=64), func=AF.Copy),
                                reads=[tpvk], writes=[(Vtk, idx)])
                            osrc = onesc if m < n_lo else onesb
                            P.pool(lambda e, Vt=Vt, idx=idx, osrc=osrc: e.tensor_copy(out=Vt[:, idx, 64:128], in_=osrc[:]),
                                   reads=["onesc", "onesb"], writes=[(Vtk, idx, "o")])
                    pend = []
                    LA = 3
                    for hh in range(2):
                        prow = slice(hh * 64, hh * 64 + 64)
                        vsl = slice(hh * 64, hh * 64 + 128)
                        for r in range(d):
                            for n in range(n_lo, n_hi + 1):
                                q0 = r + d * 128 * n - 2048
                                qsl = slice(q0, q0 + d * 127 + 1, d)
                                ps, psk = ps_r.next()
                                for w_, m in enumerate((n - 1, n)):
                                    u0 = r + d * 128 * m
                                    ksl = slice(u0, u0 + d * 127 + 1, d)
                                    P.pe(lambda e, ps=ps, w_=w_, hp=hp, prow=prow, ksl=ksl, qsl=qsl: e.matmul(
                                        ps[:, w_ * 128:(w_ + 1) * 128], KT[hp][prow, ksl], QT[hp][prow, qsl], start=True, stop=True),
                                        reads=[("KT", hp), ("QT", hp)], writes=[psk])
                                E, Ek = E_r.next()
                                P.act(lambda e, E=E, ps=ps: e.activation(out=E[:], in_=ps[:, 0:256], func=AF.Exp), reads=[psk], writes=[Ek])
                                Pm_, Pmk = Pm_r.next()
                                P.pool(lambda e, Pm_=Pm_, E=E: e.tensor_tensor(out=Pm_[:], in0=E[:], in1=band[:], op=ALU.mult), reads=[Ek, "band"], writes=[Pmk])
                                ai = r * (n_hi - n_lo + 1) + (n - n_lo)

                                def pv(Pm_=Pm_, Pmk=Pmk, r=r, n=n, hh=hh, qsl=qsl, vsl=vsl, ai=ai, Vt=Vt, Vtk=Vtk, tiles=tiles, bi_=bi_):
                                    po, pok = po_r.next()
                                    for w_, m in enumerate((n - 1, n)):
                                        idx = tiles[(r, m)]
                                        P.pe(lambda e, po=po, Vt=Vt, idx=idx, vsl=vsl, Pm_=Pm_, w_=w_: e.matmul(
                                            po[:, 0:128], Vt[:, idx, vsl], Pm_[:, w_ * 128:(w_ + 1) * 128], start=(w_ == 0), stop=(w_ == 1)),
                                            reads=[(Vtk, idx), (Vtk, idx, "o"), Pmk], writes=[pok])
                                    if bi_ == 0:
                                        P.dve(lambda e, po=po, hh=hh, qsl=qsl: e.tensor_copy(out=acc[:, hh, qsl], in_=po[:, 0:128]),
                                              reads=[pok], writes=[("acca", hh, 0, ai)])
                                    else:
                                        P.dve(lambda e, po=po, hh=hh, qsl=qsl: e.tensor_tensor(out=acc[:, hh, qsl], in0=po[:, 0:128], in1=acc[:, hh, qsl], op=ALU.add),
                                              reads=[pok] + [("acca", hh, bi_ - 1, i_) for i_ in range(16)], writes=[("acca", hh, bi_, ai)])
                                pend.append(pv)
                                if len(pend) > LA:
                                    pend.pop(0)()
                    while pend:
                        pend.pop(0)()
                for hh in range(2):
                    nrow = slice(hh * 64, hh * 64 + 64)
                    drow = slice(64 - hh * 64, 128 - hh * 64)
                    akeys = [("acca", hh, b_, i_) for b_ in range(3) for i_ in range(16)]
                    P.act(lambda e, hh=hh, nrow=nrow, drow=drow: e.activation(out=den[nrow, :], in_=acc[drow, hh, :], func=AF.Copy),
                          reads=akeys, writes=[("den", hh)])
                    P.dve(lambda e, nrow=nrow: e.reciprocal(out=den[nrow, :], in_=den[nrow, :]), reads=[("den", hh)], writes=[("den", hh)])
                    P.dve(lambda e, hh=hh, hp=hp, nrow=nrow: e.tensor_tensor(out=mixT[nrow, hp, :], in0=acc[nrow, hh, :], in1=den[nrow, :], op=ALU.mult),
                          reads=akeys + [("den", hh)], writes=[("mixT", hp, hh)])
            P.emit()


def run_skewed(n, stages):
    K = len(stages)
    for step in range(n + K - 1):
        for k, f in enumerate(stages):
            t = step - k
            if 0 <= t < n:
                f(t)


def emit_sparse(nc, P, env, smix):
    xo = env["xo"]; out = env["out"]; mixT = env["mixT"]
    ident_bf = env["ident_bf"]; ident_f = env["ident_f"]
    Xd = env["Xd"]; Yd = env["Yd"]
    NT = 16
    with ExitStack() as st:
        sb = lambda name, shape, dt: st.enter_context(nc.sbuf_tensor(U(name), list(shape), dt))
        acc = env["acc"]; gw = env["gw"]; ridx = env["ridx"]; cnt_i = env["cnt_i"]
        with ExitStack() as sw:
            sbw = lambda name, shape, dt: sw.enter_context(nc.sbuf_tensor(U(name), list(shape), dt))
            wo = sbw("wo", [128, 8, D], BF16)
            rw = sbw("rw", [128, 8, NE], F32)
            rb = sbw("rb", [128, NE], F32)
            gffn = sbw("gffn", [128, 8], F32)
            grow = sbw("grow", [128, D], F32)
            tri = sbw("tri", [128, 128], BF16)
            ones_bf = sbw("ones_bf", [128, 128], BF16)
            iota = sbw("iota", [128, NE], F32)
            run = sbw("run", [128, NE], F32)
            xt_r = Rot(nc, sw, "xt", [128, D], F32, 2)
            junk_r = Rot(nc, sw, "junk", [128, D], BF16, 1)
            hn_r = Rot(nc, sw, "hn", [128, D], F32, 3)
            mrow_r = Rot(nc, sw, "mrow", [128, D], BF16, 5)
            m32_r = Rot(nc, sw, "m32", [128, 8, 128], F32, 3)
            sm_r = Rot(nc, sw, "sm", [128, 8], F32, 12)
            lg_r = Rot(nc, sw, "lg", [128, 4, NE], F32, 3)
            selb_r = Rot(nc, sw, "selb", [128, NE], BF16, 3)
            ix_r = Rot(nc, sw, "ix", [128, 8], mybir.dt.uint32, 3)
            ef_r = Rot(nc, sw, "ef", [128, 12], F32, 3)
            pw_r = Rot(nc, sw, "pw", [128, 2, 512], F32, 2, psum=True)
            pt_r = Rot(nc, sw, "pt", [128, 8, 128], F32, 1, psum=True)
            pl_r = Rot(nc, sw, "pl", [128, 512], F32, 2, psum=True)
            for kc in range(8):
                P.dma("pool", lambda e, kc=kc: e.dma_start(out=wo[:, kc, :], in_=env["w_out"][kc * 128:(kc + 1) * 128, :]), writes=[("wo", kc)])
            P.dma("sp", lambda e: e.dma_start(out=rw[:], in_=env["router_w"].rearrange("(kc p) n -> p kc n", p=128)), writes=["rw"])
            P.dma("sp", lambda e: e.dma_start(out=rb[:], in_=env["router_b"].partition_broadcast(128)), writes=["rb"])
            P.dma("sp", lambda e: e.dma_start(out=gffn[:], in_=env["g_ffn"]), writes=["gffn"])
            P.dma("sp", lambda e: e.dma_start(out=grow[:], in_=env["gffn_row"].partition_broadcast(128)), writes=["grow"])
            P.dma("sp", lambda e: e.dma_start(out=tri[:], in_=env["c_tri"]), writes=["tri"])
            P.dma("sp", lambda e: e.dma_start(out=iota[:], in_=env["c_iota"]), writes=["iota"])
            P.dve(lambda e: e.memset(ones_bf[:], 1.0), writes=["ones_bf"])
            P.dve(lambda e: e.memset(run[:], 0.0), writes=["run"])
            C = [dict() for _ in range(NT)]

            def W0(tt):
                c = C[tt]
                xt, xk = xt_r.next()
                P.dma("sp", lambda e: e.dma_start(out=xt[:], in_=xo[tt * 128:(tt + 1) * 128, :]), writes=[xk])
                pw, pwk = pw_r.next()
                for dh in range(2):
                    for kc in range(8):
                        P.pe(lambda e, dh=dh, kc=kc: e.matmul(pw[:, dh, :], mixT[:, kc, tt * 128:(tt + 1) * 128], wo[:, kc, dh * 512:(dh + 1) * 512],
                                                              start=(kc == 0), stop=(kc == 7)), reads=["mixT", ("wo", kc)], writes=[pwk])
                c.update(xt=xt, xk=xk, pw=pw, pwk=pwk)

            def W1(tt):
                c = C[tt]
                xt, xk, pw, pwk = c["xt"], c["xk"], c["pw"], c["pwk"]
                hk = ("acc", tt)
                P.dve(lambda e: e.tensor_tensor(out=acc[:, tt, :], in0=pw[:].rearrange("p a b -> p (a b)"), in1=xt[:], op=ALU.add), reads=[pwk, xk], writes=[hk])
                junk, jk = junk_r.next()
                sm, smk = sm_r.next()
                P.act(lambda e: e.activation(out=junk[:], in_=acc[:, tt, :], func=AF.Square, accum_out=sm[:, 0:1]), reads=[hk], writes=[jk, smk])
                P.act(lambda e: e.activation(out=sm[:, 1:2], in_=sm[:, 0:1], func=AF.Sqrt, scale=1.0 / D, bias=EPS), reads=[smk], writes=[smk])
                P.dve(lambda e: e.reciprocal(out=sm[:, 2:3], in_=sm[:, 1:2]), reads=[smk], writes=[smk])
                hn, hnk = hn_r.next()
                P.dve(lambda e: e.tensor_scalar(out=hn[:], in0=acc[:, tt, :], scalar1=sm[:, 2:3], scalar2=None, op0=ALU.mult), reads=[hk, smk], writes=[hnk])
                mrow, mrk = mrow_r.next()
                P.pool(lambda e: e.tensor_tensor(out=mrow[:], in0=hn[:], in1=grow[:], op=ALU.mult), reads=[hnk, "grow"], writes=[mrk])
                c.update(hn=hn, hnk=hnk, mrow=mrow, mrk=mrk)

            def W2(tt):
                c = C[tt]
                hn, hnk = c["hn"], c["hnk"]
                pt, ptk = pt_r.next()
                for kc in range(8):
                    P.pe(lambda e, kc=kc: e.transpose(out=pt[:, kc, :], in_=hn[:, kc * 128:(kc + 1) * 128], identity=ident_f[:]), reads=[hnk, "ident_f"], writes=[ptk])
                m32, m32k = m32_r.next()
                P.dve(lambda e: e.tensor_tensor(out=m32[:], in0=pt[:], in1=gffn[:].unsqueeze(2).broadcast_to([128, 8, 128]), op=ALU.mult), reads=[ptk, "gffn"], writes=[m32k])
                c.update(m32=m32, m32k=m32k)

            def W3(tt):
                c = C[tt]
                m32, m32k = c["m32"], c["m32k"]
                pl, plk = pl_r.next()
                for kc in range(8):
                    P.pe(lambda e, kc=kc: e.matmul(pl[:, 0:NE], m32[:, kc, :], rw[:, kc, :], start=(kc == 0), stop=(kc == 7)), reads=[m32k, "rw"], writes=[plk])
                lg, lgk = lg_r.next()
                P.dve(lambda e: e.tensor_tensor(out=lg[:, 0, :], in0=pl[:, 0:NE], in1=rb[:], op=ALU.add), reads=[plk, "rb"], writes=[lgk])
                sm2, sm2k = sm_r.next()
                P.dve(lambda e: e.max(out=sm2[:, 0:8], in_=lg[:, 0, :]), reads=[lgk], writes=[sm2k])
                ix, ixk = ix_r.next()
                P.dve(lambda e: e.max_index(out=ix[:], in_max=sm2[:, 0:8], in_values=lg[:, 0, :]), reads=[lgk, sm2k], writes=[ixk])
                ef, efk = ef_r.next()
                P.dve(lambda e: e.tensor_copy(out=ef[:, 0:4], in_=ix[:, 0:4]), reads=[ixk], writes=[efk])
                selb, selk = selb_r.next()
                P.dve(lambda e: e.tensor_scalar(out=selb[:], in0=lg[:, 0, :], scalar1=sm2[:, 3:4], scalar2=None, op0=ALU.is_ge), reads=[lgk, sm2k], writes=[selk])
                sm3, sm3k = sm_r.next()
                P.dve(lambda e: e.tensor_scalar(out=sm3[:, 0:1], in0=sm2[:, 0:1], scalar1=-1.0, scalar2=None, op0=ALU.mult), reads=[sm2k], writes=[sm3k])
                P.act(lambda e: e.activation(out=sm3[:, 4:8], in_=sm2[:, 0:4], func=AF.Exp, bias=sm3[:, 0:1], scale=1.0), reads=[sm2k, sm3k], writes=[sm3k])
                P.dve(lambda e: e.tensor_reduce(out=sm3[:, 1:2], in_=sm3[:, 4:8], axis=AX.X, op=ALU.add), reads=[sm3k], writes=[sm3k])
                P.dve(lambda e: e.reciprocal(out=sm3[:, 2:3], in_=sm3[:, 1:2]), reads=[sm3k], writes=[sm3k])
                P.dve(lambda e: e.tensor_scalar(out=gw[:, tt, :], in0=sm3[:, 4:8], scalar1=sm3[:, 2:3], scalar2=None, op0=ALU.mult), reads=[sm3k], writes=[("gw", tt)])
                c.update(lg=lg, lgk=lgk, ef=ef, efk=efk, selb=selb, selk=selk)

            def W4(tt):
                c = C[tt]
                lg, lgk, ef, efk, selb, selk, mrow, mrk = c["lg"], c["lgk"], c["ef"], c["efk"], c["selb"], c["selk"], c["mrow"], c["mrk"]
                pl2, pl2k = pl_r.next()
                P.pe(lambda e: e.matmul(pl2[:, 0:NE], tri[:], selb[:], start=True, stop=True), reads=[selk, "tri"], writes=[pl2k])
                P.pe(lambda e: e.matmul(pl2[:, NE:2 * NE], ones_bf[:], selb[:], start=True, stop=True), reads=[selk, "ones_bf"], writes=[pl2k])
                P.dve(lambda e: e.tensor_tensor(out=lg[:, 1, :], in0=pl2[:, 0:NE], in1=run[:], op=ALU.add), reads=[pl2k, "run", lgk], writes=[lgk])
                P.dve(lambda e: e.tensor_tensor(out=run[:], in0=pl2[:, NE:2 * NE], in1=run[:], op=ALU.add), reads=[pl2k, "run", lgk], writes=["run"])
                for k in range(4):
                    P.dve(lambda e, k=k: e.scalar_tensor_tensor(out=lg[:, 2, :], in0=iota[:], scalar=ef[:, k:k + 1], in1=lg[:, 1, :],
                                                                 op0=ALU.is_equal, op1=ALU.mult, accum_out=ef[:, 4 + k:5 + k]),
                          reads=[lgk, efk, "iota"], writes=[lgk, efk])
                P.dve(lambda e: e.scalar_tensor_tensor(out=ef[:, 8:12], in0=ef[:, 0:4], scalar=float(TOK), in1=ef[:, 4:8], op0=ALU.mult, op1=ALU.add),
                      reads=[efk], writes=[efk])
                P.dve(lambda e: e.tensor_scalar(out=ridx[:, tt, :], in0=ef[:, 8:12], scalar1=-1.0, scalar2=None, op0=ALU.add), reads=[efk], writes=[("ridx", tt)])
                for k in range(4):
                    P.dma("pool", lambda e, k=k: e.indirect_dma_start(
                        out=Xd[:, :], out_offset=bass.IndirectOffsetOnAxis(ap=ridx[:, tt, k:k + 1], axis=0), in_=mrow[:, :], in_offset=None),
                        reads=[mrk, ("ridx", tt)], writes=[("Xd", tt, k)])

            run_skewed(NT, [W0, W1, W2, W3, W4])
            P.dve(lambda e: e.tensor_copy(out=cnt_i[:], in_=run[0:1, :]), reads=["run"], writes=["cnt_i"])
            zb = sbw("zb", [128, 2 * NE], F32)
            zl = sbw("zl", [128, 2 * NE], F32)
            zf = sbw("zf", [128, 2 * NE], F32)
            zm = sbw("zm", [128, 2 * NE], F32)
            zi = env["zi"]
            P.dma("sp", lambda e: e.dma_start(out=zb[:], in_=env["c_zbase"]), writes=["zb"])
            P.dma("sp", lambda e: e.dma_start(out=zl[:], in_=env["c_zlim"]), writes=["zl"])
            P.dve(lambda e: e.tensor_tensor(out=zf[:].rearrange("p (a b) -> p a b", b=2), in0=zb[:].rearrange("p (a b) -> p a b", b=2),
                                            in1=run[:].unsqueeze(2).broadcast_to([128, NE, 2]), op=ALU.add), reads=["zb", "run"], writes=["zf"])
            P.dve(lambda e: e.tensor_tensor(out=zm[:], in0=zf[:], in1=zl[:], op=ALU.is_ge), reads=["zf", "zl"], writes=["zm"])
            zt = sbw("zt", [128, 2 * NE], F32)
            P.dma("sp", lambda e: e.dma_start(out=zt[:], in_=env["c_ztrash"]), writes=["zt"])
            P.dve(lambda e: e.tensor_tensor(out=zt[:], in0=zt[:], in1=zf[:], op=ALU.subtract), reads=["zt", "zf"], writes=["zt"])
            P.dve(lambda e: e.tensor_tensor(out=zt[:], in0=zt[:], in1=zm[:], op=ALU.mult), reads=["zt", "zm"], writes=["zt"])
            P.dve(lambda e: e.tensor_tensor(out=zf[:], in0=zf[:], in1=zt[:], op=ALU.add), reads=["zt", "zf"], writes=["zf"])
            P.dve(lambda e: e.tensor_copy(out=zi[:], in_=zf[:]), reads=["zf"], writes=["zi"])
            P.emit()
        smix.close()

        with ExitStack() as se:
            sbe = lambda name, shape, dt: se.enter_context(nc.sbuf_tensor(U(name), list(shape), dt))
            wg = [sbe("wg%d" % i, [128, 8, 2 * D], BF16) for i in range(2)]
            wd = [sbe("wd%d" % i, [128, 8, D], BF16) for i in range(2)]
            bgr = sbe("bgr", [33, 2 * D], BF16)
            bdr = sbe("bdr", [33, D], BF16)
            ones_row = sbe("ones_row", [33, 128], BF16)
            xb_r = Rot(nc, se, "xb", [128, D], BF16, 2)
            xT_pre = [sbe("xTp%d" % i, [128, 8, 128], BF16) for i in range(2)]
            xT_r = Rot(nc, se, "xT", [128, 8, 128], BF16, 2)
            g_r = Rot(nc, se, "g", [128, 512], BF16, 2)
            sg_r = Rot(nc, se, "sg", [128, 512], BF16, 2)
            u_r = Rot(nc, se, "u", [128, 512], BF16, 2)
            actb_r = Rot(nc, se, "actb", [128, D], BF16, 1)
            actT_r = Rot(nc, se, "actT", [128, 8, 128], BF16, 1)
            ysb_r = Rot(nc, se, "ysb", [128, D], F32, 1)
            tpx_r = Rot(nc, se, "tpx", [128, 8, 128], BF16, 1, psum=True)
            pg_r = Rot(nc, se, "pgg", [128, 512], F32, 2, psum=True)
            pu_r = Rot(nc, se, "pgu", [128, 512], F32, 2, psum=True)
            tpa_r = Rot(nc, se, "tpa", [128, 8, 128], BF16, 1, psum=True)
            pd_r = Rot(nc, se, "pd", [128, 2, 512], F32, 1, psum=True)
            P.dve(lambda e: e.memset(ones_row[:], 1.0), writes=["ones_row"])
            xd_keys = [("Xd", tt, k) for tt in range(NT) for k in range(4)]
            zrow = sbe("zrow", [128, D], BF16)
            P.dve(lambda e: e.memset(zrow[:], 0.0), writes=["zrow"])

            def zero_tail(e_):
                for c_ in (2 * e_, 2 * e_ + 1):
                    P.dma("pool", lambda e, c_=c_: e.indirect_dma_start(
                        out=Xd[:, :], out_offset=bass.IndirectOffsetOnAxis(ap=env["zi"][:, c_:c_ + 1], axis=0), in_=zrow[:, :], in_offset=None),
                        reads=["zi", "zrow"], writes=[("Xz", e_)])
            P.add("sp", lambda e: e.nop(), reads=xd_keys, writes=["Xall"])
            engs = ("pe", "act", "dve", "sp")

            def load_weights(e_):
                par = e_ % 2
                pp = 32 * par
                for kc in range(8):
                    P.dma("pool", lambda e, kc=kc, par=par, e_=e_: e.dma_start(out=wg[par][:, kc, :], in_=env["w_gu_nat"][e_, kc * 128:(kc + 1) * 128, :]),
                          writes=[("wg", par, kc)], wstream=True)
                for fc in range(8):
                    P.dma("pool", lambda e, fc=fc, par=par, e_=e_: e.dma_start(out=wd[par][:, fc, :], in_=env["w_down"][e_, fc * 128:(fc + 1) * 128, :]),
                          writes=[("wd", par, fc)], wstream=True)
                P.dma("pool", lambda e, pp=pp, e_=e_: e.dma_start(out=bgr[pp:pp + 1, :], in_=env["b_gu_nat"][e_:e_ + 1, :]), writes=[("bgr", par)], wstream=True)
                P.dma("pool", lambda e, pp=pp, e_=e_: e.dma_start(out=bdr[pp:pp + 1, :], in_=env["b_down"][e_:e_ + 1, :]), writes=[("bdr", par)], wstream=True)

            xb_pre = [sbe("xbp%d" % i, [128, D], BF16) for i in range(2)]

            def xload(e_, k, xb, xbk):
                r0 = e_ * TOK + k * 128
                P.dma("sp", lambda e, xb=xb, r0=r0: e.dma_start(out=xb[:], in_=Xd[r0:r0 + 128, :]), reads=[("Xz", e_)], writes=[xbk])

            def xtrans(xb, xbk, xT, xTk):
                tpx, tpxk = tpx_r.next()
                for kc in range(8):
                    P.pe(lambda e, tpx=tpx, xb=xb, kc=kc: e.transpose(out=tpx[:, kc, :], in_=xb[:, kc * 128:(kc + 1) * 128], identity=ident_bf[:]),
                         reads=[xbk, "ident_bf"], writes=[tpxk])
                P.dve(lambda e, xT=xT, tpx=tpx: e.tensor_copy(out=xT[:], in_=tpx[:]), reads=[tpxk], writes=[xTk])

            def xprep(e_, k, xT, xTk):
                xb, xbk = xb_r.next()
                xload(e_, k, xb, xbk)
                xtrans(xb, xbk, xT, xTk)

            zero_tail(0)
            zero_tail(1)
            load_weights(0)
            xload(0, 0, xb_pre[0], ("xbp", 0))
            ykeys = []
            for e_ in range(NE):
                par = e_ % 2
                pp = 32 * par
                if e_ + 1 < NE:
                    load_weights(e_ + 1)
                    if e_ + 2 < NE:
                        zero_tail(e_ + 2)
                    xload(e_ + 1, 0, xb_pre[1 - par], ("xbp", 1 - par))
                xtrans(xb_pre[par], ("xbp", par), xT_pre[par], ("xTp", par))
                for en in engs:
                    P.regload(en, "n", cnt_i[0:1, e_:e_ + 1], reads=["cnt_i"])
                cur = (xT_pre[par], ("xTp", par))
                for k in range(TOK // 128):
                    P.cur_region = ("n", 128 * k)
                    r0 = e_ * TOK + k * 128
                    xT, xTk = cur
                    actb, actbk = actb_r.next()
                    for hf in range(2):
                        pg, pgk = pg_r.next()
                        pu, puk = pu_r.next()
                        for (pp_, ppk, c0) in ((pg, pgk, hf * 512), (pu, puk, D + hf * 512)):
                            for kc in range(8):
                                P.pe(lambda e, pp_=pp_, xT=xT, kc=kc, c0=c0, par=par: e.matmul(pp_[:], xT[:, kc, :], wg[par][:, kc, c0:c0 + 512],
                                                                                             start=(kc == 0), stop=False),
                                     reads=[xTk, ("wg", par, kc)], writes=[ppk])
                            P.pe(lambda e, pp_=pp_, c0=c0, pp=pp: e.matmul(pp_[:], ones_row[pp:pp + 1, :], bgr[pp:pp + 1, c0:c0 + 512], start=False, stop=True),
                                 reads=["ones_row", ("bgr", par)], writes=[ppk])
                        g, gk_ = g_r.next()
                        sg, sgk = sg_r.next()
                        u, uk = u_r.next()
                        P.dve(lambda e, g=g, pg=pg: e.tensor_scalar(out=g[:], in0=pg[:], scalar1=7.0, scalar2=None, op0=ALU.min), reads=[pgk], writes=[gk_])
                        P.act(lambda e, g=g, sg=sg: e.activation(out=sg[:], in_=g[:], func=AF.Sigmoid, scale=1.702), reads=[gk_], writes=[sgk])
                        P.dve(lambda e, u=u, pu=pu: e.tensor_scalar(out=u[:], in0=pu[:], scalar1=7.0, scalar2=-7.0, op0=ALU.min, op1=ALU.max), reads=[puk], writes=[uk])
                        P.dve(lambda e, g=g, sg=sg: e.tensor_tensor(out=g[:], in0=g[:], in1=sg[:], op=ALU.mult), reads=[gk_, sgk], writes=[gk_])
                        P.dve(lambda e, g=g, u=u, actb=actb, hf=hf: e.scalar_tensor_tensor(out=actb[:, hf * 512:(hf + 1) * 512], in0=u[:], scalar=1.0, in1=g[:],
                                                                                          op0=ALU.add, op1=ALU.mult), reads=[gk_, uk], writes=[(actbk, hf)])
                    if k + 1 < TOK // 128:
                        nxt = xT_r.next()
                        xprep(e_, k + 1, nxt[0], nxt[1])
                        cur = nxt
                    tpa, tpak = tpa_r.next()
                    for fc in range(8):
                        P.pe(lambda e, tpa=tpa, actb=actb, fc=fc: e.transpose(out=tpa[:, fc, :], in_=actb[:, fc * 128:(fc + 1) * 128], identity=ident_bf[:]),
                             reads=[(actbk, fc // 4), "ident_bf"], writes=[tpak])
                    actT, actTk = actT_r.next()
                    P.dve(lambda e, actT=actT, tpa=tpa: e.tensor_copy(out=actT[:], in_=tpa[:]), reads=[tpak], writes=[actTk])
                    pd, pdk = pd_r.next()
                    for dh in range(2):
                        for fc in range(8):
                            P.pe(lambda e, pd=pd, actT=actT, fc=fc, dh=dh, par=par: e.matmul(pd[:, dh, :], actT[:, fc, :], wd[par][:, fc, dh * 512:(dh + 1) * 512],
                                                                                            start=(fc == 0), stop=False),
                                 reads=[actTk, ("wd", par, fc)], writes=[pdk])
                        P.pe(lambda e, pd=pd, dh=dh, pp=pp: e.matmul(pd[:, dh, :], ones_row[pp:pp + 1, :], bdr[pp:pp + 1, dh * 512:(dh + 1) * 512], start=False, stop=True),
                             reads=["ones_row", ("bdr", par)], writes=[pdk])
                    ysb, ysbk = ysb_r.next()
                    P.dve(lambda e, ysb=ysb, pd=pd: e.tensor_copy(out=ysb[:], in_=pd[:].rearrange("p a b -> p (a b)")), reads=[pdk], writes=[ysbk])
                    yk = ("Yd", e_, k)
                    P.dma("sp", lambda e, ysb=ysb, r0=r0: e.dma_start(out=Yd[r0:r0 + 128, :], in_=ysb[:]), reads=[ysbk], writes=[yk])
                    ykeys.append(yk)
                    P.cur_region = None
            P.add("pool", lambda e: e.nop(), reads=ykeys, writes=["Yall"])
            P.emit()

        def combine(sf):
            yg_r = Rot(nc, sf, "yg", [128, D], F32, 8)

            def tile(tt):
                for k in range(4):
                    yg, ygk = yg_r.next()
                    P.dma("pool", lambda e, yg=yg, k=k: e.indirect_dma_start(
                        out=yg[:, :], out_offset=None, in_=Yd[:, :], in_offset=bass.IndirectOffsetOnAxis(ap=ridx[:, tt, k:k + 1], axis=0)),
                        reads=["Yall", ("ridx", tt)], writes=[ygk])
                    P.dve(lambda e, yg=yg, k=k: e.scalar_tensor_tensor(out=acc[:, tt, :], in0=yg[:], scalar=gw[:, tt, k:k + 1], in1=acc[:, tt, :],
                                                                       op0=ALU.mult, op1=ALU.add), reads=[ygk, ("gw", tt), ("acc", tt)], writes=[("acc", tt)])
            return tile
        emit_ple(nc, P, env, acc, 0, NT, pre=combine)


def emit_half(nc, P, env, half):
    xo = env["xo"]; out = env["out"]; mixT = env["mixT"]
    ident_bf = env["ident_bf"]; ident_f = env["ident_f"]
    HT = 8
    t0 = half * HT
    with ExitStack() as st:
        sb = lambda name, shape, dt: st.enter_context(nc.sbuf_tensor(U(name), list(shape), dt))
        acc = sb("acc", [128, HT, D], F32)
        mT = sb("mT", [128, 8, 1024], BF16)
        gates = sb("gates", [128, HT, NE], F32)
        gT = sb("gT", [NE, 1024], F32)
        bd = sb("bd", [NE, D], F32)
        bg = sb("bg", [128, NE, 16], F32)
        rb = sb("rb", [128, NE], F32)
        gffn = sb("gffn", [128, 8], F32)
        with ExitStack() as sw:
            sbw = lambda name, shape, dt: sw.enter_context(nc.sbuf_tensor(U(name), list(shape), dt))
            wo = sbw("wo", [128, 8, D], BF16)
            rw = sbw("rw", [128, 8, NE], F32)
            xt_r = Rot(nc, sw, "xt", [128, D], F32, 2)
            junk_r = Rot(nc, sw, "junk", [128, D], BF16, 2)
            hn_r = Rot(nc, sw, "hn", [128, D], F32, 2)
            m32_r = Rot(nc, sw, "m32", [128, 8, 128], F32, 2)
            sm_r = Rot(nc, sw, "sm", [128, 8], F32, 4)
            lg_r = Rot(nc, sw, "lg", [128, 3, NE], F32, 2)
            pw_r = Rot(nc, sw, "pw", [128, 2, 512], F32, 2, psum=True)
            pt_r = Rot(nc, sw, "pt", [128, 8, 128], F32, 1, psum=True)
            pl_r = Rot(nc, sw, "pl", [128, 512], F32, 2, psum=True)
            for kc in range(8):
                P.dma("pool", lambda e, kc=kc: e.dma_start(out=wo[:, kc, :], in_=env["w_out"][kc * 128:(kc + 1) * 128, :]),
                      writes=[("wo", kc)])
            P.dma("sp", lambda e: e.dma_start(out=rw[:], in_=env["router_w"].rearrange("(kc p) n -> p kc n", p=128)), writes=["rw"])
            P.dma("sp", lambda e: e.dma_start(out=rb[:], in_=env["router_b"].partition_broadcast(128)), writes=["rb"])
            P.dma("sp", lambda e: e.dma_start(out=gffn[:], in_=env["g_ffn"]), writes=["gffn"])
            P.dma("sp", lambda e: e.dma_start(out=bd[:], in_=env["b_down"]), writes=["bd"])
            P.dma("sp", lambda e: e.dma_start(out=bg[:], in_=env["bgu"]), writes=["bg"])
            P.dve(lambda e: e.tensor_scalar(out=bg[:, :, 8:16], in0=bg[:, :, 8:16], scalar1=1.0, scalar2=None, op0=ALU.add),
                  reads=["bg"], writes=["bg"])
            for tt in range(HT):
                gt = t0 + tt
                xt, xk = xt_r.next()
                P.dma("sp", lambda e, xt=xt, gt=gt: e.dma_start(out=xt[:], in_=xo[gt * 128:(gt + 1) * 128, :]), writes=[xk])
                pw, pwk = pw_r.next()
                for dh in range(2):
                    for kc in range(8):
                        P.pe(lambda e, pw=pw, dh=dh, kc=kc, gt=gt: e.matmul(
                            pw[:, dh, :], mixT[:, kc, gt * 128:(gt + 1) * 128], wo[:, kc, dh * 512:(dh + 1) * 512],
                            start=(kc == 0), stop=(kc == 7)), reads=["mixT", ("wo", kc)], writes=[pwk])
                hk = ("acc", tt)
                P.dve(lambda e, pw=pw, xt=xt, tt=tt: e.tensor_tensor(
                    out=acc[:, tt, :], in0=pw[:].rearrange("p a b -> p (a b)"), in1=xt[:], op=ALU.add),
                    reads=[pwk, xk], writes=[hk])
                junk, jk = junk_r.next()
                sm, smk = sm_r.next()
                P.act(lambda e, junk=junk, sm=sm, tt=tt: e.activation(out=junk[:], in_=acc[:, tt, :], func=AF.Square, accum_out=sm[:, 0:1]),
                      reads=[hk], writes=[jk, smk])
                P.act(lambda e, sm=sm: e.activation(out=sm[:, 1:2], in_=sm[:, 0:1], func=AF.Sqrt, scale=1.0 / D, bias=EPS),
                      reads=[smk], writes=[smk])
                P.dve(lambda e, sm=sm: e.reciprocal(out=sm[:, 2:3], in_=sm[:, 1:2]), reads=[smk], writes=[smk])
                hn, hnk = hn_r.next()
                P.dve(lambda e, hn=hn, sm=sm, tt=tt: e.tensor_scalar(out=hn[:], in0=acc[:, tt, :], scalar1=sm[:, 2:3], scalar2=None, op0=ALU.mult),
                      reads=[hk, smk], writes=[hnk])
                pt, ptk = pt_r.next()
                for kc in range(8):
                    P.pe(lambda e, pt=pt, hn=hn, kc=kc: e.transpose(out=pt[:, kc, :], in_=hn[:, kc * 128:(kc + 1) * 128], identity=ident_f[:]),
                         reads=[hnk, "ident_f"], writes=[ptk])
                m32, m32k = m32_r.next()
                P.dve(lambda e, pt=pt, m32=m32: e.tensor_tensor(out=m32[:], in0=pt[:], in1=gffn[:].unsqueeze(2).broadcast_to([128, 8, 128]), op=ALU.mult),
                      reads=[ptk, "gffn"], writes=[m32k])
                P.act(lambda e, m32=m32, tt=tt: e.activation(out=mT[:, :, tt * 128:(tt + 1) * 128], in_=m32[:], func=AF.Copy),
                      reads=[m32k], writes=[("mT", tt)])
                pl, plk = pl_r.next()
                for kc in range(8):
                    P.pe(lambda e, pl=pl, m32=m32, kc=kc: e.matmul(pl[:, 0:NE], m32[:, kc, :], rw[:, kc, :], start=(kc == 0), stop=(kc == 7)),
                         reads=[m32k, "rw"], writes=[plk])
                lg, lgk = lg_r.next()
                P.dve(lambda e, pl=pl, lg=lg: e.tensor_tensor(out=lg[:, 0, :], in0=pl[:, 0:NE], in1=rb[:], op=ALU.add),
                      reads=[plk, "rb"], writes=[lgk])
                sm2, sm2k = sm_r.next()
                P.dve(lambda e, lg=lg, sm2=sm2: e.max(out=sm2[:, 0:8], in_=lg[:, 0, :]), reads=[lgk], writes=[sm2k])
                P.dve(lambda e, lg=lg, sm2=sm2: e.tensor_scalar(out=lg[:, 1, :], in0=lg[:, 0, :], scalar1=sm2[:, 3:4], scalar2=None, op0=ALU.is_ge),
                      reads=[lgk, sm2k], writes=[lgk])
                sm3, sm3k = sm_r.next()
                P.dve(lambda e, sm2=sm2, sm3=sm3: e.tensor_scalar(out=sm3[:, 0:1], in0=sm2[:, 0:1], scalar1=-1.0, scalar2=None, op0=ALU.mult),
                      reads=[sm2k], writes=[sm3k])
                P.act(lambda e, lg=lg, sm3=sm3: e.activation(out=lg[:, 2, :], in_=lg[:, 0, :], func=AF.Exp, bias=sm3[:, 0:1], scale=1.0),
                      reads=[lgk, sm3k], writes=[lgk])
                P.dve(lambda e, lg=lg: e.tensor_tensor(out=lg[:, 2, :], in0=lg[:, 2, :], in1=lg[:, 1, :], op=ALU.mult), reads=[lgk], writes=[lgk])
                P.dve(lambda e, lg=lg, sm3=sm3: e.tensor_reduce(out=sm3[:, 1:2], in_=lg[:, 2, :], axis=AX.X, op=ALU.add), reads=[lgk, sm3k], writes=[sm3k])
                P.dve(lambda e, sm3=sm3: e.reciprocal(out=sm3[:, 2:3], in_=sm3[:, 1:2]), reads=[sm3k], writes=[sm3k])
                P.dve(lambda e, lg=lg, sm3=sm3, tt=tt: e.tensor_scalar(out=gates[:, tt, :], in0=lg[:, 2, :], scalar1=sm3[:, 2:3], scalar2=None, op0=ALU.mult),
                      reads=[lgk, sm3k], writes=[("gates", tt)])
                pl2, pl2k = pl_r.next()
                P.pe(lambda e, pl2=pl2, tt=tt: e.transpose(out=pl2[0:NE, 0:128], in_=gates[:, tt, :], identity=ident_f[:]),
                     reads=[("gates", tt), "ident_f"], writes=[pl2k])
                P.act(lambda e, pl2=pl2, tt=tt: e.activation(out=gT[:, tt * 128:(tt + 1) * 128], in_=pl2[0:NE, 0:128], func=AF.Copy),
                      reads=[pl2k], writes=[("gT", tt)])
                pw2, pw2k = pw_r.next()
                for dh in range(2):
                    P.pe(lambda e, pw2=pw2, dh=dh, tt=tt: e.matmul(pw2[:, dh, :], gT[:, tt * 128:(tt + 1) * 128], bd[:, dh * 512:(dh + 1) * 512],
                                                                     start=True, stop=True), reads=[("gT", tt), "bd"], writes=[pw2k])
                P.dve(lambda e, pw2=pw2, tt=tt: e.tensor_tensor(out=acc[:, tt, :], in0=pw2[:].rearrange("p a b -> p (a b)"), in1=acc[:, tt, :], op=ALU.add),
                      reads=[pw2k, hk], writes=[hk])
            P.emit()

        with ExitStack() as se:
            sbe = lambda name, shape, dt: se.enter_context(nc.sbuf_tensor(U(name), list(shape), dt))
            NSLOT = 9
            ring = [sbe("ring%d" % i, [128, 4096], BF16) for i in range(NSLOT)]
            actT = sbe("actT", [128, 8, 1024], BF16)
            g_r = Rot(nc, se, "g", [128, 512], F32, 2)
            sg_r = Rot(nc, se, "sg", [128, 512], F32, 2)
            u_r = Rot(nc, se, "u", [128, 512], F32, 2)
            pg_r = Rot(nc, se, "pg", [128, 512], F32, 2, psum=True)
            pu_r = Rot(nc, se, "pu", [128, 512], F32, 2, psum=True)
            pd_r = Rot(nc, se, "pd", [128, 2, 512], F32, 2, psum=True)
            pieces = []
            for e_ in range(NE):
                for j in range(4):
                    pieces.append(("gu", e_, j))
                for j in range(2):
                    pieces.append(("dn", e_, j))
            state = {"next": 0, "mark": -1}
            last_reader = {}
            finished = set()

            def issue_loads():
                while state["next"] < len(pieces):
                    i = state["next"]
                    if i >= NSLOT:
                        prev = i - NSLOT
                        if prev not in finished or last_reader[prev] > state["mark"]:
                            return
                    kind, e_, j = pieces[i]
                    slot = ring[i % NSLOT]
                    key = ("ring", i % NSLOT)
                    if kind == "gu":
                        src = env["wgu"][e_, j]
                        P.dma("pool", lambda e, slot=slot, src=src: e.dma_start(out=slot[:], in_=src, max_dma_last_dim=8192), writes=[key])
                    else:
                        src = env["w_down"][e_].rearrange("(fc p) d -> p fc d", p=128)[:, 4 * j:4 * j + 4, :]
                        P.dma("pool", lambda e, slot=slot, src=src: e.dma_start(out=slot[:].rearrange("p (fc d) -> p fc d", fc=4), in_=src), writes=[key])
                    state["next"] += 1

            issue_loads()
            for e_ in range(NE):
                base = e_ * 6
                for fc in range(8):
                    j = fc // 2
                    pi = base + j
                    assert pi < state["next"]
                    slot = ring[pi % NSLOT]
                    skey = ("ring", pi % NSLOT)
                    goff = (fc % 2) * 128
                    uoff = 256 + (fc % 2) * 128
                    for tb in range(2):
                        pg, pgk = pg_r.next()
                        pu, puk = pu_r.next()
                        lastop = None
                        for (pp, ppk, off) in ((pg, pgk, goff), (pu, puk, uoff)):
                            for kc in range(8):
                                lastop = P.pe(lambda e, pp=pp, slot=slot, kc=kc, off=off, tb=tb: e.matmul(
                                    pp[:], slot[:, kc * 512 + off: kc * 512 + off + 128], mT[:, kc, tb * 512:(tb + 1) * 512],
                                    start=(kc == 0), stop=(kc == 7)),
                                    reads=[skey] + [("mT", tb * 4 + q) for q in range(4)], writes=[ppk])
                        last_reader[pi] = lastop.idx
                        if fc % 2 == 1 and tb == 1:
                            finished.add(pi)
                        g, gk_ = g_r.next()
                        sg, sgk = sg_r.next()
                        u, uk = u_r.next()
                        P.dve(lambda e, g=g, pg=pg, e_=e_, fc=fc: e.tensor_scalar(out=g[:], in0=pg[:], scalar1=bg[:, e_, fc:fc + 1], scalar2=7.0,
                                                                                    op0=ALU.add, op1=ALU.min), reads=[pgk, "bg"], writes=[gk_])
                        P.act(lambda e, g=g, sg=sg: e.activation(out=sg[:], in_=g[:], func=AF.Sigmoid, scale=1.702), reads=[gk_], writes=[sgk])
                        P.act(lambda e, u=u, pu=pu, e_=e_, fc=fc: e.activation(out=u[:], in_=pu[:], func=AF.Identity, bias=bg[:, e_, 8 + fc:9 + fc], scale=1.0),
                              reads=[puk, "bg"], writes=[uk])
                        P.pool(lambda e, u=u: e.tensor_scalar(out=u[:], in0=u[:], scalar1=8.0, scalar2=-6.0, op0=ALU.min, op1=ALU.max),
                               reads=[uk], writes=[uk])
                        P.pool(lambda e, g=g, sg=sg: e.tensor_tensor(out=g[:], in0=g[:], in1=sg[:], op=ALU.mult), reads=[gk_, sgk], writes=[gk_])
                        P.pool(lambda e, g=g, u=u, fc=fc, tb=tb: e.tensor_tensor(out=actT[:, fc, tb * 512:(tb + 1) * 512], in0=g[:], in1=u[:], op=ALU.mult),
                               reads=[gk_, uk], writes=[("actT", fc, tb)])
                        state["mark"] = lastop.idx
                        issue_loads()
                p0 = base + 4
                for tt in range(HT):
                    pd, pdk = pd_r.next()
                    lastop = None
                    for dh in range(2):
                        for fc in range(8):
                            pj = p0 + fc // 4
                            slot = ring[pj % NSLOT]
                            lastop = P.pe(lambda e, pd=pd, dh=dh, fc=fc, tt=tt, slot=slot: e.matmul(
                                pd[:, dh, :], actT[:, fc, tt * 128:(tt + 1) * 128],
                                slot[:, (fc % 4) * 1024 + dh * 512:(fc % 4) * 1024 + (dh + 1) * 512],
                                start=(fc == 0), stop=(fc == 7)),
                                reads=[("ring", pj % NSLOT), ("actT", fc, tt // 4)], writes=[pdk])
                            last_reader[pj] = lastop.idx
                    P.dve(lambda e, pd=pd, tt=tt, e_=e_: e.scalar_tensor_tensor(
                        out=acc[:, tt, :], in0=pd[:].rearrange("p a b -> p (a b)"), scalar=gates[:, tt, e_:e_ + 1], in1=acc[:, tt, :],
                        op0=ALU.mult, op1=ALU.add), reads=[pdk, ("gates", tt), ("acc", tt)], writes=[("acc", tt)])
                finished.add(p0)
                finished.add(p0 + 1)
            P.emit()

        emit_ple(nc, P, env, acc, t0, HT)


def emit_ple(nc, P, env, acc, t0, HT, pre=None):
    out = env["out"]; ident_bf = env["ident_bf"]
    with ExitStack() as sf:
        pre_tile = pre(sf) if pre is not None else None
        sbf = lambda name, shape, dt: sf.enter_context(nc.sbuf_tensor(U(name), list(shape), dt))
        wg = sbf("wg", [128, 8, D], BF16)
        wp = sbf("wp", [128, 2, D], BF16)
        hb_r = Rot(nc, sf, "hb", [128, D], BF16, 3)
        hT_r = Rot(nc, sf, "hT", [128, 8, 128], BF16, 3)
        pt_r = Rot(nc, sf, "ptile", [128, 256], F32, 3)
        pb_r = Rot(nc, sf, "pb", [128, 256], BF16, 3)
        pT_r = Rot(nc, sf, "pT", [128, 2, 128], BF16, 3)
        sgo_r = Rot(nc, sf, "sgo", [128, D], F32, 2)
        o_r = Rot(nc, sf, "o", [128, D], F32, 2)
        ptp_r = Rot(nc, sf, "ptp", [128, 8, 128], BF16, 2, psum=True)
        ptq_r = Rot(nc, sf, "ptq", [128, 8, 128], BF16, 1, psum=True)
        pgp_r = Rot(nc, sf, "pgp", [128, 2, 512], F32, 1, psum=True)
        ppp_r = Rot(nc, sf, "ppp", [128, 2, 512], F32, 1, psum=True)
        for kc in range(8):
            P.dma("pool", lambda e, kc=kc: e.dma_start(out=wg[:, kc, :], in_=env["ple_gate"][kc * 128:(kc + 1) * 128, :]), writes=[("wg", kc)])
        for kc in range(2):
            P.dma("pool", lambda e, kc=kc: e.dma_start(out=wp[:, kc, :], in_=env["ple_proj"][kc * 128:(kc + 1) * 128, :]), writes=[("wp", kc)])
        C = [dict() for _ in range(HT)]

        def F0(tt):
            c = C[tt]
            if pre_tile is not None:
                pre_tile(tt)
            ptile, ptk = pt_r.next()
            P.dma("sp", lambda e: e.dma_start(out=ptile[:], in_=env["p_own"][(t0 + tt) * 128:(t0 + tt + 1) * 128, :]), writes=[ptk])
            c.update(ptile=ptile, ptk=ptk)

        def F1(tt):
            c = C[tt]
            hb, hbk = hb_r.next()
            P.act(lambda e: e.activation(out=hb[:], in_=acc[:, tt, :], func=AF.Copy), reads=[("acc", tt)], writes=[hbk])
            pb, pbk = pb_r.next()
            P.act(lambda e: e.activation(out=pb[:], in_=c["ptile"][:], func=AF.Copy), reads=[c["ptk"]], writes=[pbk])
            c.update(hb=hb, hbk=hbk, pb=pb, pbk=pbk)

        def F2(tt):
            c = C[tt]
            hb, hbk, pb, pbk = c["hb"], c["hbk"], c["pb"], c["pbk"]
            ptp, ptpk = ptp_r.next()
            for kc in range(8):
                P.pe(lambda e, kc=kc: e.transpose(out=ptp[:, kc, :], in_=hb[:, kc * 128:(kc + 1) * 128], identity=ident_bf[:]), reads=[hbk, "ident_bf"], writes=[ptpk])
            ptq, ptqk = ptq_r.next()
            for kc in range(2):
                P.pe(lambda e, kc=kc: e.transpose(out=ptq[:, kc, :], in_=pb[:, kc * 128:(kc + 1) * 128], identity=ident_bf[:]), reads=[pbk, "ident_bf"], writes=[ptqk])
            hT, hTk = hT_r.next()
            P.dve(lambda e: e.tensor_copy(out=hT[:], in_=ptp[:]), reads=[ptpk], writes=[hTk])
            pT, pTk = pT_r.next()
            P.dve(lambda e: e.tensor_copy(out=pT[:], in_=ptq[:, 0:2, :]), reads=[ptqk], writes=[pTk])
            c.update(hT=hT, hTk=hTk, pT=pT, pTk=pTk)

        def F3(tt):
            c = C[tt]
            hT, hTk, pT, pTk = c["hT"], c["hTk"], c["pT"], c["pTk"]
            pgp, pgpk = pgp_r.next()
            ppp, pppk = ppp_r.next()
            for dh in range(2):
                for kc in range(8):
                    P.pe(lambda e, kc=kc, dh=dh: e.matmul(pgp[:, dh, :], hT[:, kc, :], wg[:, kc, dh * 512:(dh + 1) * 512], start=(kc == 0), stop=(kc == 7)),
                         reads=[hTk, ("wg", kc)], writes=[pgpk])
                for kc in range(2):
                    P.pe(lambda e, kc=kc, dh=dh: e.matmul(ppp[:, dh, :], pT[:, kc, :], wp[:, kc, dh * 512:(dh + 1) * 512], start=(kc == 0), stop=(kc == 1)),
                         reads=[pTk, ("wp", kc)], writes=[pppk])
            sgo, sgok = sgo_r.next()
            P.act(lambda e: e.activation(out=sgo[:], in_=pgp[:].rearrange("p a b -> p (a b)"), func=AF.Sigmoid), reads=[pgpk], writes=[sgok])
            P.dve(lambda e: e.tensor_tensor(out=sgo[:], in0=ppp[:].rearrange("p a b -> p (a b)"), in1=sgo[:], op=ALU.mult), reads=[pppk, sgok], writes=[sgok])
            o, ok = o_r.next()
            P.pool(lambda e: e.tensor_tensor(out=o[:], in0=sgo[:], in1=acc[:, tt, :], op=ALU.add), reads=[sgok, ("acc", tt)], writes=[ok])
            P.dma("sp", lambda e: e.dma_start(out=out[(t0 + tt) * 128:(t0 + tt + 1) * 128, :], in_=o[:]), reads=[ok], writes=[("out", t0 + tt)])

        run_skewed(HT, [F0, F1, F2, F3])
        P.add("sp", lambda e: e.nop(), reads=[("out", t0 + tt) for tt in range(HT)])
        P.emit()


def _consts():
    c = {}
    c["c_ident_bf"] = np.eye(128, dtype=np.float32).astype(ml_dtypes.bfloat16)
    c["c_ident_f"] = np.eye(128, dtype=np.float32)
    k = np.arange(128)[:, None]
    q = np.arange(128)[None, :]
    band = np.concatenate([(q <= k), (q >= k)], axis=1).astype(np.float32)
    c["c_band"] = band.astype(ml_dtypes.bfloat16)
    s = np.arange(64)[:, None]
    t = np.arange(64)[None, :]
    hm = (s <= t).astype(np.float32)
    c["c_hmask"] = np.concatenate([hm, hm], axis=0).astype(np.float32)
    bo = np.zeros((128, 128), np.float32)
    bo[:64, :64] = 1
    bo[64:, 64:] = 1
    c["c_blockones"] = bo.astype(ml_dtypes.bfloat16)
    c["c_ones_f"] = np.ones((128, 128), np.float32)
    pm = np.zeros((128, 128), np.float32)
    for hh in range(2):
        for m in range(8):
            pm[hh * 64 + m + 8, hh * 64 + m] = -1.0
            pm[hh * 64 + m, hh * 64 + m + 8] = 1.0
    c["c_pm"] = pm.astype(ml_dtypes.bfloat16)
    invf = np.zeros((128, 1), np.float64)
    for p in range(128):
        cc = p % 64
        if cc < 16:
            invf[p, 0] = (500000.0 ** (-(cc % 8) / 8.0)) / (2 * math.pi)
    c["c_invf"] = invf.astype(np.float32)
    rs = np.ones((128, 512), np.float32)
    rs[:, 0::64] = 0
    c["c_reset"] = rs
    c["c_tri"] = (np.arange(128)[:, None] <= np.arange(128)[None, :]).astype(np.float32).astype(ml_dtypes.bfloat16)
    c["c_iota"] = np.tile(np.arange(NE, dtype=np.float32)[None, :], (128, 1))
    p = np.arange(128, dtype=np.float32)[:, None, None]
    e_ = np.arange(NE, dtype=np.float32)[None, :, None]
    j2 = np.arange(2, dtype=np.float32)[None, None, :]
    c["c_zbase"] = np.ascontiguousarray((e_ * TOK + j2 * 128 + p).reshape(128, 2 * NE).astype(np.float32))
    c["c_ztrash"] = np.ascontiguousarray(np.broadcast_to(XROWS + p, (128, NE, 2)).reshape(128, 2 * NE).astype(np.float32))
    c["c_zlim"] = np.ascontiguousarray(np.broadcast_to((e_ + 1) * TOK, (128, NE, 2)).reshape(128, 2 * NE).astype(np.float32))
    return c


def _fm(v, n):
    return np.ascontiguousarray(np.asarray(v, np.float32).reshape(n, 128).T)


def make_in_maps(inp, dbg=None, extra=None):
    x = np.asarray(inp["x"], np.float32)
    p = np.asarray(inp["p"], np.float32)[0]
    positions = np.asarray(inp["positions"]).astype(np.int32)
    consts = _consts()
    shared = dict(consts)
    shared["g_mix"] = _fm(inp["mix_norm_g"][0], 8)
    shared["w_in"] = np.ascontiguousarray(inp["w_in"][0], np.float32)
    shared["gq"] = np.tile(np.asarray(inp["q_norm_g"][0], np.float32), 2).reshape(128, 1)
    shared["gk"] = np.tile(np.asarray(inp["k_norm_g"][0], np.float32), 2).reshape(128, 1)
    shared["lb0"] = _fm(inp["hgrn_lb_logits"][0], 4)
    shared["lb1"] = _fm(inp["hgrn_lb_logits"][1], 4)
    shared["gnorm"] = np.asarray(inp["hgrn_norm_g"][0], np.float32).reshape(128, 1)
    shared["w_out"] = np.ascontiguousarray(inp["w_out"][0], np.float32)
    shared["g_ffn"] = _fm(inp["ffn_norm_g"][0], 8)
    shared["router_w"] = np.ascontiguousarray(inp["router_w"][0], np.float32)
    shared["router_b"] = np.asarray(inp["router_b"][0], np.float32).reshape(1, NE)
    shared["gffn_row"] = np.asarray(inp["ffn_norm_g"][0], np.float32).reshape(1, D)
    if SPARSE:
        shared["w_gu_nat"] = np.ascontiguousarray(inp["expert_w_gate_up"][0], np.float32)
        shared["b_gu_nat"] = np.ascontiguousarray(inp["expert_b_gate_up"][0], np.float32)
    wg = np.asarray(inp["expert_w_gate_up"][0], np.float32)
    wg5 = wg.reshape(NE, 8, 128, 2, 4, 256)
    if not SPARSE:
        shared["wgu"] = np.ascontiguousarray(wg5.transpose(0, 4, 2, 1, 3, 5)).reshape(NE, 4, 128, 4096)
        bgu = np.asarray(inp["expert_b_gate_up"][0], np.float32)
        shared["bgu"] = np.ascontiguousarray(bgu.reshape(NE, 16, 128).transpose(2, 0, 1))
    shared["w_down"] = np.ascontiguousarray(inp["expert_w_down"][0], np.float32)
    shared["b_down"] = np.ascontiguousarray(inp["expert_b_down"][0], np.float32)
    shared["ple_proj"] = np.ascontiguousarray(inp["ple_proj"][0], np.float32)
    shared["ple_gate"] = np.ascontiguousarray(inp["ple_gate"][0], np.float32)
    maps = []
    for c in range(NCORES):
        b, h = divmod(c, 2)
        m = dict(shared)
        m["xo"] = np.ascontiguousarray(x[b, h * TOK:(h + 1) * TOK])
        m["p_own"] = np.ascontiguousarray(p[b, h * TOK:(h + 1) * TOK])
        if h == 1:
            m["xc"] = np.ascontiguousarray(x[b, 0:TOK])
            pc = positions[b, 0:TOK]
            m["onesctx"] = np.ones((128, 64), np.float32)
        else:
            m["xc"] = np.zeros((TOK, D), np.float32)
            pc = np.zeros((TOK,), np.int32)
            m["onesctx"] = np.zeros((128, 64), np.float32)
        m["pos"] = np.concatenate([pc, positions[b, h * TOK:(h + 1) * TOK]]).reshape(1, 4096).astype(np.int32)
        if extra is not None:
            m.update(extra(c))
        maps.append(m)
    return maps


_NC_CACHE = {}


def kernel(**inputs):
    if "nc" not in _NC_CACHE:
        _NC_CACHE["nc"] = build()
    nc = _NC_CACHE["nc"]
    maps = make_in_maps(inputs)
    res = run_bass_kernel_spmd(nc, maps, core_ids=list(range(NCORES)))
    outp = np.zeros((4, 4096, D), np.float32)
    for c in range(NCORES):
        b, h = divmod(c, 2)
        outp[b, h * TOK:(h + 1) * TOK] = res.results[c]["out"]
    return outp
```

```python
import math
from contextlib import ExitStack

import numpy as np
import ml_dtypes
import concourse.bass as bass
import concourse.mybir as mybir
from concourse.bass_utils import run_bass_kernel_spmd

F32 = mybir.dt.float32
BF16 = mybir.dt.bfloat16
I32 = mybir.dt.int32
AF = mybir.ActivationFunctionType
ALU = mybir.AluOpType
AX = mybir.AxisListType

D = 1024
TOK = 2048
NE = 32
EPS = 1e-6
NCORES = 8
SPARSE = True
XROWS = NE * TOK


class _Op:
    __slots__ = ("sig_aux", "eng", "fn", "deps", "signal", "sig", "dma", "emitted", "idx", "region")


class Prog:
    COMPUTE = ("pe", "act", "dve", "pool")

    def __init__(self, nc, n_dma_sems=36):
        self.nc = nc
        self.ops = []
        self.last_w = {}
        self.readers = {}
        self.sems = {}
        self.cnt = {e: 0 for e in self.COMPUTE}
        self.dma_sems = []
        self.dma_tot = []
        self.dma_rr = 0
        self.n_dma_sems = n_dma_sems
        self.waited = {}
        self.stack = None
        self.cur_region = None
        self.regs = {}
        self.wstream_ops = set()

    def setup(self, stack):
        nc = self.nc
        for e in self.COMPUTE:
            self.sems[e] = stack.enter_context(nc.semaphore("s_" + e))
        for i in range(self.n_dma_sems):
            self.dma_sems.append(stack.enter_context(nc.semaphore("s_dma%d" % i)))
            self.dma_tot.append(0)
        self.wsems = [stack.enter_context(nc.semaphore("s_wdma%d" % i)) for i in range(32)]
        self.wtot = [0] * 32
        self.wrr = 0
        self.gsems = [stack.enter_context(nc.semaphore("s_gdma%d" % i)) for i in range(20)]
        self.gtot = [0] * 20
        self.grr = 0

    def add(self, eng, fn, reads=(), writes=(), dma=False, region=None):
        op = _Op()
        op.region = self.cur_region if region is None else (region or None)
        op.eng = eng
        op.fn = fn
        op.dma = dma
        op.signal = dma
        op.sig = None
        op.emitted = False
        op.idx = len(self.ops)
        deps = []
        for k in reads:
            w = self.last_w.get(k)
            if w is not None:
                deps.append(w)
        for k in writes:
            w = self.last_w.get(k)
            if w is not None:
                deps.append(w)
            for r in self.readers.get(k, ()):
                deps.append(r)
        dd = []
        seen = set()
        for d in deps:
            if d is op or id(d) in seen:
                continue
            seen.add(id(d))
            if d.eng == "pe" and eng == "pe" and not d.dma and not dma:
                continue
            if d.emitted and not d.dma:
                continue
            if d.eng == "sp" and not d.dma:
                continue
            dd.append(d)
            d.signal = True
        op.deps = dd
        for k in reads:
            self.readers.setdefault(k, []).append(op)
        for k in writes:
            self.last_w[k] = op
            self.readers[k] = []
        self.ops.append(op)
        return op

    def pe(self, fn, reads=(), writes=()):
        return self.add("pe", fn, reads, writes)

    def act(self, fn, reads=(), writes=()):
        return self.add("act", fn, reads, writes)

    def dve(self, fn, reads=(), writes=()):
        return self.add("dve", fn, reads, writes)

    def pool(self, fn, reads=(), writes=()):
        return self.add("pool", fn, reads, writes)

    def dma(self, q, fn, reads=(), writes=(), wstream=False):
        op = self.add(q, fn, reads, writes, dma=True)
        if wstream:
            self.wstream_ops.add(id(op))
        return op

    def regload(self, engname, key, ap, reads=()):
        op = self.add(engname, "regload", reads, (), region=False)
        op.region = None
        op.sig_aux = (key, ap)
        return op

    def emit(self):
        nc = self.nc
        todo = [o for o in self.ops if not o.emitted]
        for o in todo:
            if o.dma and id(o) in self.wstream_ops:
                j = self.wrr
                self.wrr = (self.wrr + 1) % len(self.wsems)
                prev = self.wtot[j]
                self.wtot[j] += 16
                o.sig = (self.wsems[j], self.wtot[j], ("w", j), prev)
            elif o.dma and o.eng == "pool":
                j = self.grr
                self.grr = (self.grr + 1) % len(self.gsems)
                prev = self.gtot[j]
                self.gtot[j] += 16
                o.sig = (self.gsems[j], self.gtot[j], ("g", j), prev)
            elif o.dma:
                j = self.dma_rr
                self.dma_rr = (self.dma_rr + 1) % self.n_dma_sems
                prev = self.dma_tot[j]
                self.dma_tot[j] += 16
                o.sig = (self.dma_sems[j], self.dma_tot[j], ("d", j), prev)
            elif o.signal:
                self.cnt[o.eng] += 1
                o.sig = (self.sems[o.eng], self.cnt[o.eng], ("c", o.eng), None)
        by = {}
        for o in todo:
            by.setdefault(o.eng, []).append(o)
        qmap = {"pe": "tensor", "act": "scalar", "dve": "vector", "pool": "gpsimd", "sp": "sync"}

        def run(engname, ops):
            def body(eng):
                waited = self.waited.setdefault(engname, {})
                regs = self.regs.setdefault(engname, {})
                rstack = ExitStack()

                def getreg(key):
                    if key not in regs:
                        regs[key] = rstack.enter_context(eng.register("r_%s_%s" % (engname, key)))
                    return regs[key]

                def emit_op(o):
                    ws = {}
                    for d in o.deps:
                        if d.sig[2] not in ws or ws[d.sig[2]][1] < d.sig[1]:
                            ws[d.sig[2]] = (d.sig[0], d.sig[1])
                    if o.dma and o.sig[3] > 0:
                        if o.sig[2] not in ws or ws[o.sig[2]][1] < o.sig[3]:
                            ws[o.sig[2]] = (o.sig[0], o.sig[3])
                    for key, (sem, val) in ws.items():
                        if waited.get(key, 0) >= val:
                            continue
                        waited[key] = val
                        eng.wait_ge(sem, val)
                    if o.fn == "regload":
                        eng.reg_load(getreg(o.sig_aux[0]), o.sig_aux[1])
                    else:
                        inst = o.fn(eng)
                        if o.sig is not None:
                            inst.then_inc(o.sig[0], 16 if o.dma else 1)
                    o.emitted = True

                def comp_for(groups):
                    ncomp = sum(1 for grp in groups for g in grp if g.sig is not None and not g.dma)
                    dmas = [g for grp in groups for g in grp if g.dma]
                    if ncomp:
                        eng.drain()
                        eng.sem_inc(self.sems[engname], ncomp)
                    for g in dmas:
                        if g.sig[3] > 0:
                            eng.wait_ge(g.sig[0], g.sig[3])
                        eng.sem_inc(g.sig[0], 16)
                    if not ncomp and not dmas:
                        eng.nop()

                def emit_chain(groups, gi):
                    grp = groups[gi]
                    with eng.If_lt(getreg(grp[0].region[0]), grp[0].region[1] + 1):
                        comp_for(groups[gi:])
                    with eng.Else():
                        for g in grp:
                            emit_op(g)
                        if gi + 1 < len(groups):
                            emit_chain(groups, gi + 1)

                i = 0
                while i < len(ops):
                    o = ops[i]
                    if o.region is None:
                        emit_op(o)
                        i += 1
                        continue
                    groups = []
                    j = i
                    while j < len(ops) and ops[j].region is not None and ops[j].region[0] == o.region[0] and \
                            (not groups or ops[j].region[1] >= groups[-1][0].region[1]):
                        if groups and ops[j].region == groups[-1][0].region:
                            groups[-1].append(ops[j])
                        else:
                            groups.append([ops[j]])
                        j += 1
                    saved = dict(waited)
                    emit_chain(groups, 0)
                    waited.clear()
                    waited.update(saved)
                    i = j
                rstack.close()
            return body

        with nc.Block() as block:
            for engname, ops in by.items():
                getattr(block, qmap[engname])(run(engname, ops))
        for o in todo:
            o.fn = None


_UID = [0]


def U(name):
    _UID[0] += 1
    return "%s_%d" % (name, _UID[0])


class Rot:
    def __init__(self, nc, stack, name, shape, dtype, n, psum=False):
        self.bufs = []
        for i in range(n):
            alloc = nc.psum_tensor if psum else nc.sbuf_tensor
            self.bufs.append(stack.enter_context(alloc(U("%s%d" % (name, i)), list(shape), dtype)))
        self.name = name
        self.i = 0
        self.gen = 0

    @classmethod
    def from_aps(cls, name, aps):
        r = cls.__new__(cls)
        r.bufs = list(aps)
        r.name = name
        r.i = 0
        r.gen = 0
        return r

    def next(self):
        b = self.bufs[self.i]
        key = (self.name, self.i)
        self.i = (self.i + 1) % len(self.bufs)
        return b, key


def build(dbg=None):
    nc = bass.Bass("TRN2", target_bir_lowering=False)

    def din(name, shape, dt=F32):
        return nc.dram_tensor(name, list(shape), dt, kind="ExternalInput").ap()

    xo = din("xo", [TOK, D])
    xc = din("xc", [TOK, D])
    pos = din("pos", [1, 4096], I32)
    onesctx = din("onesctx", [128, 64])
    g_mix = din("g_mix", [128, 8])
    w_in = din("w_in", [D, 3584])
    gq = din("gq", [128, 1])
    gk = din("gk", [128, 1])
    lb0 = din("lb0", [128, 4])
    lb1 = din("lb1", [128, 4])
    gnorm = din("gnorm", [128, 1])
    w_out = din("w_out", [D, D])
    g_ffn = din("g_ffn", [128, 8])
    router_w = din("router_w", [D, NE])
    router_b = din("router_b", [1, NE])
    if SPARSE:
        w_gu_nat = din("w_gu_nat", [NE, D, 2 * D])
        b_gu_nat = din("b_gu_nat", [NE, 2 * D])
    else:
        wgu = din("wgu", [NE, 4, 128, 4096])
        bgu = din("bgu", [128, NE, 16])
    w_down = din("w_down", [NE, D, D])
    b_down = din("b_down", [NE, D])
    ple_proj = din("ple_proj", [256, D])
    ple_gate = din("ple_gate", [D, D])
    p_own = din("p_own", [TOK, 256])
    c_ident_bf = din("c_ident_bf", [128, 128], BF16)
    c_ident_f = din("c_ident_f", [128, 128])
    c_band = din("c_band", [128, 256], BF16)
    c_hmask = din("c_hmask", [128, 64])
    c_blockones = din("c_blockones", [128, 128], BF16)
    c_ones_f = din("c_ones_f", [128, 128])
    c_pm = din("c_pm", [128, 128], BF16)
    c_invf = din("c_invf", [128, 1])
    c_reset = din("c_reset", [128, 512])
    if dbg == "moe":
        d_mixT = din("d_mixT", [128, 8, TOK], BF16)
    out = nc.dram_tensor("out", [TOK, D], F32, kind="ExternalOutput").ap()
    gffn_row = din("gffn_row", [1, D])
    c_tri = din("c_tri", [128, 128], BF16)
    c_iota = din("c_iota", [128, NE])
    c_zbase = din("c_zbase", [128, 2 * NE])
    c_zlim = din("c_zlim", [128, 2 * NE])
    c_ztrash = din("c_ztrash", [128, 2 * NE])
    Xd = nc.dram_tensor("Xd", [NE * TOK + 128, D], BF16, kind="Internal").ap()
    Yd = nc.dram_tensor("Yd", [NE * TOK, D], F32, kind="Internal").ap()
    if dbg and dbg.startswith("mix"):
        d_out_mixT = nc.dram_tensor("d_out_mixT", [128, 8, TOK], BF16, kind="ExternalOutput").ap()

    P = Prog(nc)
    with ExitStack() as top:
        P.setup(top)
        sb = lambda name, shape, dt, st=top: st.enter_context(nc.sbuf_tensor(U(name), list(shape), dt))
        ident_bf = sb("ident_bf", [128, 128], BF16)
        ident_f = sb("ident_f", [128, 128], F32)
        acc = sb("acc", [128, 16, D], F32)
        accb = acc[:].rearrange("p a b -> p (a b)").bitcast(BF16)
        gw = sb("gw", [128, 16, 4], F32)
        ridx = sb("ridx", [128, 16, 4], I32)
        cnt_i = sb("cnt_i", [1, NE], I32)
        zi = sb("zi", [128, 2 * NE], I32)
        smix = ExitStack()
        mixT = smix.enter_context(nc.sbuf_tensor(U("mixT"), [128, 8, TOK], BF16))
        P.dma("sp", lambda e: e.dma_start(out=ident_bf[:], in_=c_ident_bf), writes=["ident_bf"])
        P.dma("sp", lambda e: e.dma_start(out=ident_f[:], in_=c_ident_f), writes=["ident_f"])

        if dbg == "moe":
            P.dma("sp", lambda e: e.dma_start(out=mixT[:], in_=d_mixT), writes=["mixT"])
        else:
            emit_mixer(nc, P, locals(), upto=(dbg[3:] if dbg and dbg.startswith("mix") and len(dbg) > 3 else "T"))
        if dbg and dbg.startswith("mix"):
            P.dma("sp", lambda e: e.dma_start(out=d_out_mixT, in_=mixT[:]), reads=["mixT"], writes=["d_out"])
            P.add("sp", lambda e: e.nop(), reads=["d_out"])
            P.emit()
            smix.close()
            return nc

        if SPARSE:
            emit_sparse(nc, P, locals(), smix)
        else:
            for half in range(2):
                emit_half(nc, P, locals(), half)
            smix.close()
    return nc


def a1_block(nc, P, env, pools, blk, gmix):
    xt_r, junk_r, xn_r, st_r, tp_r, aT_r = pools
    ident_bf = env["ident_bf"]
    aT, aTk = aT_r.next()
    for t in range(4):
        gt = blk * 4 + t
        src = env["xc"][gt * 128:(gt + 1) * 128, :] if gt < 16 else env["xo"][(gt - 16) * 128:(gt - 15) * 128, :]
        xt, xk = xt_r.next()
        P.dma("sp", lambda e, xt=xt, src=src: e.dma_start(out=xt[:], in_=src), writes=[xk])
        junk, jk = junk_r.next()
        st, stk = st_r.next()
        P.act(lambda e, junk=junk, xt=xt, st=st: e.activation(out=junk[:], in_=xt[:], func=AF.Square, accum_out=st[:, 0:1]),
              reads=[xk], writes=[jk, stk])
        P.act(lambda e, st=st: e.activation(out=st[:, 1:2], in_=st[:, 0:1], func=AF.Sqrt, scale=1.0 / D, bias=EPS), reads=[stk], writes=[stk])
        P.dve(lambda e, st=st: e.reciprocal(out=st[:, 2:3], in_=st[:, 1:2]), reads=[stk], writes=[stk])
        xn, xnk = xn_r.next()
        P.dve(lambda e, xn=xn, xt=xt, st=st: e.tensor_scalar(out=xn[:], in0=xt[:], scalar1=st[:, 2:3], scalar2=None, op0=ALU.mult),
              reads=[xk, stk], writes=[xnk])
        tp, tpk = tp_r.next()
        for kc in range(8):
            P.pe(lambda e, tp=tp, xn=xn, kc=kc: e.transpose(out=tp[:, kc, :], in_=xn[:, kc * 128:(kc + 1) * 128], identity=ident_bf[:]),
                 reads=[xnk, "ident_bf"], writes=[tpk])
        P.dve(lambda e, aT=aT, tp=tp, t=t: e.tensor_tensor(out=aT[:, :, t * 128:(t + 1) * 128], in0=tp[:],
                                                            in1=gmix[:].unsqueeze(2).broadcast_to([128, 8, 128]), op=ALU.mult),
              reads=[tpk, "gmix"], writes=[(aTk, t)])
    return aT, [(aTk, t) for t in range(4)]


import os as _os


def emit_mixer(nc, P, env, upto="T"):
    mixT = env["mixT"]
    ident_bf = env["ident_bf"]
    with ExitStack() as sm_:
        sbm = lambda name, shape, dt: sm_.enter_context(nc.sbuf_tensor(U(name), list(shape), dt))
        gmix = sbm("gmix", [128, 8], F32)
        P.dma("sp", lambda e: e.dma_start(out=gmix[:], in_=env["g_mix"]), writes=["gmix"])

        with ExitStack() as sh:
            sbh = lambda name, shape, dt: sh.enter_context(nc.sbuf_tensor(U(name), list(shape), dt))
            accb = env["accb"]
            w_h = accb[:, 0:16384].rearrange("p (k c) -> p k c", k=8)
            for kc in range(8):
                P.dma("pool", lambda e, kc=kc: e.dma_start(out=w_h[:, kc, :], in_=env["w_in"][kc * 128:(kc + 1) * 128, 1536:3584]),
                      writes=[("w_h", kc)])
            hmask = sbh("hmask", [128, 64], F32)
            ones_f = sbh("ones_f", [128, 128], F32)
            reset = sbh("reset", [128, 512], F32)
            gnorm = sbh("gnorm", [128, 1], F32)
            l0 = sbh("l0", [128, 4], F32)
            l1 = sbh("l1", [128, 4], F32)
            oml = sbh("oml", [128, 4], F32)
            noml = sbh("noml", [128, 4], F32)
            for (t_, src, key) in ((hmask, "c_hmask", "hmask"), (ones_f, "c_ones_f", "ones_f"), (reset, "c_reset", "reset"),
                                   (gnorm, "gnorm", "gnorm"), (l0, "lb0", "l0"), (l1, "lb1", "l1")):
                P.dma("sp", lambda e, t_=t_, src=src: e.dma_start(out=t_[:], in_=env[src]), writes=[key])
            P.dve(lambda e: e.tensor_tensor(out=oml[:], in0=l1[:], in1=l0[:], op=ALU.subtract), reads=["l0", "l1"], writes=["oml"])
            P.act(lambda e: e.activation(out=oml[:], in_=oml[:], func=AF.Sigmoid), reads=["oml"], writes=["oml"])
            P.dve(lambda e: e.tensor_scalar(out=noml[:], in0=oml[:], scalar1=-1.0, scalar2=None, op0=ALU.mult), reads=["oml"], writes=["oml2"])
            tpb_r = Rot(nc, sh, "tpb", [128, 8, 128], BF16, 2, psum=True)
            pools = (Rot(nc, sh, "xt", [128, D], F32, 2), Rot(nc, sh, "junk", [128, D], BF16, 1), Rot(nc, sh, "xn", [128, D], BF16, 2),
                     Rot(nc, sh, "st", [128, 4], F32, 4), tpb_r,
                     Rot.from_aps("aTh", [accb[:, 16384 + i * 4096:16384 + (i + 1) * 4096].rearrange("p (k c) -> p k c", k=8) for i in range(2)]))
            vt_r = Rot.from_aps("vth", [accb[0:64, 24576 + i * 4096:24576 + (i + 1) * 4096].rearrange("p (k c) -> p k c", k=8) for i in range(2)])
            At_r = Rot(nc, sh, "At", [128, 64], BF16, 4)
            eb = [sbh("eb%d" % i, [128, 512], F32) for i in range(4)]
            kt = [sbh("kt%d" % i, [128, 512], BF16) for i in range(4)]
            qt = [sbh("qt%d" % i, [128, 512], BF16) for i in range(4)]
            ktok = [sbh("ktok%d" % i, [64, 8, 128], BF16) for i in range(4)]
            sgate = [sbh("sgate%d" % i, [128, 512], F32) for i in range(4)]
            oT = [sbh("oT%d" % i, [128, 512], F32) for i in range(4)]
            S = sbh("S", [128, 4, 128], F32)
            S_bf = sbh("S_bf", [128, 4, 128], BF16)
            pb_r = Rot(nc, sh, "pb", [128, 512], F32, 6, psum=True)
            mm_r = pb_r
            P.dve(lambda e: e.memset(S[:], 0.0), writes=[("S", i) for i in range(4)])
            P.dve(lambda e: e.memset(S_bf[:], 0.0), writes=[("S_bf", i) for i in range(4)])

            def mmgroup(col0, aT, aTkeys):
                mm, mmk = mm_r.next()
                for kc in range(8):
                    P.pe(lambda e, mm=mm, kc=kc, col0=col0, aT=aT: e.matmul(mm[:], w_h[:, kc, col0:col0 + 128], aT[:, kc, :],
                                                                          start=(kc == 0), stop=(kc == 7)),
                         reads=[("w_h", kc)] + aTkeys, writes=[mmk])
                return mm, mmk

            snegs = [sbh("snegh%d" % i, [128, 512], F32) for i in range(4)]
            ffs = [sbh("ffh%d" % i, [128, 512], F32) for i in range(4)]
            bbs = [sbh("bbh%d" % i, [128, 512], F32) for i in range(4)]
            enbs = [sbh("enbh%d" % i, [128, 512], F32) for i in range(4)]
            khs = [sbh("khh%d" % i, [128, 512], BF16) for i in range(4)]
            qss = [sbh("qsh%d" % i, [128, 512], F32) for i in range(4)]
            H4 = range(4)
            USE_STT = False
            def vgroup(aT, aTkeys, vt, vtk, n):
                mm, mmk = pb_r.next()
                for kc in range(8):
                    P.pe(lambda e, mm=mm, kc=kc: e.matmul(mm[0:64, :], aT[:, kc, n * 64:(n + 1) * 64], w_h[:, kc, 1024:1536],
                                                          start=(kc == 0), stop=(kc == 7)),
                         reads=[("w_h", kc), aTkeys[n // 2]], writes=[mmk])
                P.act(lambda e, mm=mm: e.activation(out=vt[0:64, n, :], in_=mm[0:64, :], func=AF.Copy), reads=[mmk], writes=[(vtk, n)])

            nxtA = a1_block(nc, P, env, pools, 0, gmix)
            nxtV = vt_r.next()
            for n in range(8):
                vgroup(nxtA[0], nxtA[1], nxtV[0], nxtV[1], n)
            for blk in range(8):
                own = blk >= 4
                ob = blk - 4
                aT, aTkeys = nxtA
                vt, vtk = nxtV
                mf = [mmgroup(512 + hd * 128, aT, aTkeys) for hd in H4]
                for hd in H4:
                    P.act(lambda e, hd=hd, mf=mf: e.activation(out=snegs[hd][:], in_=mf[hd][0][:], func=AF.Sigmoid, scale=-1.0), reads=[mf[hd][1]], writes=[("sneg", hd)])
                if own:
                    mq = [mmgroup(hd * 128, aT, aTkeys) for hd in H4]
                    for hd in H4:
                        P.act(lambda e, hd=hd, mq=mq: e.activation(out=qss[hd][:], in_=mq[hd][0][:], func=AF.Silu), reads=[mq[hd][1]], writes=[("qs", hd)])
                for hd in H4:
                    P.dve(lambda e, hd=hd: e.tensor_scalar(out=ffs[hd][:], in0=snegs[hd][:], scalar1=noml[:, hd:hd + 1], scalar2=1.0, op0=ALU.mult, op1=ALU.add),
                          reads=[("sneg", hd), "oml2"], writes=[("ff", hd)])
                for hd in H4:
                    P.act(lambda e, hd=hd: e.activation(out=ffs[hd][:], in_=ffs[hd][:], func=AF.Ln), reads=[("ff", hd)], writes=[("ff", hd)])
                if own:
                    mg = [mmgroup(1536 + hd * 128, aT, aTkeys) for hd in H4]
                    for hd in H4:
                        P.act(lambda e, hd=hd, mg=mg: e.activation(out=sgate[hd][:], in_=mg[hd][0][:], func=AF.Silu), reads=[mg[hd][1]], writes=[("sgate", hd)])
                for hd in H4:
                    P.dve(lambda e, hd=hd: e.tensor_tensor_scan(out=bbs[hd][:], data0=reset[:], data1=ffs[hd][:], initial=0.0, op0=ALU.mult, op1=ALU.add),
                          reads=[("ff", hd), "reset"], writes=[("bb", hd)])
                for hd in H4:
                    P.act(lambda e, hd=hd: e.activation(out=eb[hd][:], in_=bbs[hd][:], func=AF.Exp), reads=[("bb", hd)], writes=[("eb", hd)])
                    P.act(lambda e, hd=hd: e.activation(out=enbs[hd][:], in_=bbs[hd][:], func=AF.Exp, scale=-1.0), reads=[("bb", hd)], writes=[("enb", hd)])
                for hd in H4:
                    P.dve(lambda e, hd=hd: e.scalar_tensor_tensor(out=kt[hd][:], in0=snegs[hd][:], scalar=oml[:, hd:hd + 1], in1=enbs[hd][:],
                                                                  op0=ALU.mult, op1=ALU.mult), reads=[("sneg", hd), ("enb", hd), "oml"], writes=[("kt", hd)])
                    if own:
                        P.pool(lambda e, hd=hd: e.tensor_tensor(out=qt[hd][:], in0=qss[hd][:], in1=eb[hd][:], op=ALU.mult),
                               reads=[("qs", hd), ("eb", hd)], writes=[("qt", hd)])
                for hd in H4:
                    P.pool(lambda e, hd=hd: e.tensor_tensor(
                        out=khs[hd][:].rearrange("p (n c) -> p n c", c=64), in0=kt[hd][:].rearrange("p (n c) -> p n c", c=64),
                        in1=eb[hd][:].rearrange("p (n c) -> p n c", c=64)[:, :, 63:64].broadcast_to([128, 8, 64]), op=ALU.mult),
                        reads=[("kt", hd), ("eb", hd)], writes=[("kh", hd)])
                for hd in H4:
                    tpk_, tpkk = tpb_r.next()
                    for n in range(8):
                        P.pe(lambda e, hd=hd, n=n, tpk_=tpk_: e.transpose(out=tpk_[0:64, n, :], in_=khs[hd][:, n * 64:(n + 1) * 64], identity=ident_bf[:]),
                             reads=[("kh", hd), "ident_bf"], writes=[tpkk])
                    P.dve(lambda e, hd=hd, tpk_=tpk_: e.tensor_copy(out=ktok[hd][:], in_=tpk_[0:64, :, :]), reads=[tpkk], writes=[("ktok", hd)])
                if blk + 1 < 8:
                    nxtA = a1_block(nc, P, env, pools, blk + 1, gmix)
                    nxtV = vt_r.next()
                for n in range(8):
                    t = n
                    cs = slice(n * 64, n * 64 + 64)
                    if blk + 1 < 8:
                        vgroup(nxtA[0], nxtA[1], nxtV[0], nxtV[1], n)
                    for hd in H4:
                        if own:
                            pA, pAk = pb_r.next()
                            P.pe(lambda e, hd=hd, cs=cs, pA=pA: e.matmul(pA[0:64, 0:64], kt[hd][:, cs], qt[hd][:, cs], start=True, stop=True),
                                 reads=[("kt", hd), ("qt", hd)], writes=[pAk])
                            At, Atk = At_r.next()
                            P.dve(lambda e, At=At, pA=pA: e.tensor_tensor(out=At[0:64, :], in0=pA[0:64, 0:64], in1=hmask[0:64, :], op=ALU.mult),
                                  reads=[pAk, "hmask"], writes=[Atk])
                            pO, pOk = pb_r.next()
                            P.pe(lambda e, hd=hd, cs=cs, pO=pO: e.matmul(pO[:, 0:64], S_bf[:, hd, :], qt[hd][:, cs], start=True, stop=False),
                                 reads=[("S_bf", hd), ("qt", hd)], writes=[pOk])
                            P.pe(lambda e, hd=hd, t=t, At=At, vt=vt, pO=pO: e.matmul(pO[:, 0:64], vt[0:64, t, hd * 128:(hd + 1) * 128], At[0:64, :],
                                                                                 start=False, stop=True),
                                 reads=[(vtk, t), Atk], writes=[pOk])
                            P.act(lambda e, hd=hd, cs=cs, pO=pO: e.activation(out=oT[hd][:, cs], in_=pO[:, 0:64], func=AF.Copy),
                                  reads=[pOk], writes=[("oT", hd, n)])
                        pS, pSk = pb_r.next()
                        P.pe(lambda e, hd=hd, t=t, vt=vt, pS=pS: e.matmul(pS[:, 0:128], ktok[hd][0:64, t, :], vt[0:64, t, hd * 128:(hd + 1) * 128],
                                                                        start=True, stop=True),
                             reads=[("ktok", hd), (vtk, t)], writes=[pSk])
                        if USE_STT:
                            P.dve(lambda e, hd=hd, n=n, pS=pS: e.scalar_tensor_tensor(out=S[:, hd, :], in0=S[:, hd, :], scalar=eb[hd][:, n * 64 + 63:n * 64 + 64],
                                                                                    in1=pS[:, 0:128], op0=ALU.mult, op1=ALU.add),
                                  reads=[("S", hd), ("eb", hd), pSk], writes=[("S", hd)])
                        else:
                            if own and hd < 2:
                                P.pool(lambda e, hd=hd, n=n: e.tensor_scalar(out=S[:, hd, :], in0=S[:, hd, :], scalar1=eb[hd][:, n * 64 + 63:n * 64 + 64], scalar2=None, op0=ALU.mult),
                                       reads=[("S", hd), ("eb", hd)], writes=[("S", hd)])
                            else:
                                P.act(lambda e, hd=hd, n=n: e.activation(out=S[:, hd, :], in_=S[:, hd, :], func=AF.Copy, scale=eb[hd][:, n * 64 + 63:n * 64 + 64]),
                                      reads=[("S", hd), ("eb", hd)], writes=[("S", hd)])
                            P.dve(lambda e, hd=hd, pS=pS: e.tensor_tensor(out=S[:, hd, :], in0=pS[:, 0:128], in1=S[:, hd, :], op=ALU.add),
                                  reads=[("S", hd), pSk], writes=[("S", hd)])
                        if own or (blk == 3 and n == 7):
                            P.act(lambda e, hd=hd: e.activation(out=S_bf[:, hd, :], in_=S[:, hd, :], func=AF.Copy), reads=[("S", hd)], writes=[("S_bf", hd)])
                if own:
                    okeys = [[("oT", hd, n) for n in range(8)] for hd in H4]
                    for hd in H4:
                        P.act(lambda e, hd=hd: e.activation(out=snegs[hd][:], in_=oT[hd][:], func=AF.Square), reads=okeys[hd], writes=[("sneg", hd)])
                    mr = []
                    for hd in H4:
                        mm, mmk = pb_r.next()
                        P.pe(lambda e, mm=mm, hd=hd: e.matmul(mm[:], ones_f[:], snegs[hd][:], start=True, stop=True), reads=[("sneg", hd), "ones_f"], writes=[mmk])
                        mr.append((mm, mmk))
                    for hd in H4:
                        P.act(lambda e, hd=hd, mr=mr: e.activation(out=ffs[hd][:], in_=mr[hd][0][:], func=AF.Sqrt, scale=1.0 / 128, bias=EPS), reads=[mr[hd][1]], writes=[("ff", hd)])
                    for hd in H4:
                        P.dve(lambda e, hd=hd: e.reciprocal(out=ffs[hd][:], in_=ffs[hd][:]), reads=[("ff", hd)], writes=[("ff", hd)])
                    for hd in H4:
                        P.dve(lambda e, hd=hd: e.scalar_tensor_tensor(out=bbs[hd][:], in0=oT[hd][:], scalar=gnorm[:, 0:1], in1=ffs[hd][:],
                                                                      op0=ALU.mult, op1=ALU.mult), reads=okeys[hd] + [("ff", hd), "gnorm"], writes=[("bb", hd)])
                    for hd in H4:
                        P.pool(lambda e, hd=hd, ob=ob: e.tensor_tensor(out=mixT[:, 4 + hd, ob * 512:(ob + 1) * 512], in0=bbs[hd][:], in1=sgate[hd][:], op=ALU.mult),
                               reads=[("bb", hd), ("sgate", hd)], writes=[("mixT", 4 + hd, ob)])
            P.emit()

        if upto == "H":
            return
        accb = env["accb"]
        KT = [accb[:, i * 4096:(i + 1) * 4096] for i in range(4)]
        VT = [accb[:, 16384 + i * 4096:16384 + (i + 1) * 4096] for i in range(4)]
        QT = [sbm("QT%d" % i, [128, TOK], BF16) for i in range(4)]
        with ExitStack() as sp_:
            sbp = lambda name, shape, dt: sp_.enter_context(nc.sbuf_tensor(U(name), list(shape), dt))
            w_a = sbp("w_a", [128, 8, 1536], BF16)
            for kc in range(8):
                P.dma("pool", lambda e, kc=kc: e.dma_start(out=w_a[:, kc, :], in_=env["w_in"][kc * 128:(kc + 1) * 128, 0:1536]),
                      writes=[("w_a", kc)])
            Ct = sbp("Ct", [128, 4096], BF16)
            St = sbp("St", [128, 4096], BF16)
            blockones = sbp("blockones", [128, 128], BF16)
            pm = sbp("pm", [128, 128], BF16)
            invf = sbp("invf", [128, 1], F32)
            gq = sbp("gq", [128, 1], F32)
            gk = sbp("gk", [128, 1], F32)
            for (t_, src, key) in ((blockones, "c_blockones", "blockones"), (pm, "c_pm", "pm"), (invf, "c_invf", "invf"),
                                   (gq, "gq", "gq"), (gk, "gk", "gk")):
                P.dma("sp", lambda e, t_=t_, src=src: e.dma_start(out=t_[:], in_=env[src]), writes=[key])
            srope = ExitStack()
            posi_r = Rot(nc, srope, "posi", [128, 1024], I32, 2)
            rf_r = Rot(nc, srope, "rf", [128, 1024], F32, 4)
            ri_r = Rot(nc, srope, "ri", [128, 1024], I32, 2)
            for ch in range(4):
                csl = slice(ch * 1024, (ch + 1) * 1024)
                posi, pik = posi_r.next()
                P.dma("sp", lambda e, posi=posi, csl=csl: e.dma_start(out=posi[:], in_=env["pos"][0:1, csl].partition_broadcast(128)), writes=[pik])
                posf, pfk = rf_r.next()
                P.dve(lambda e, posf=posf, posi=posi: e.tensor_copy(out=posf[:], in_=posi[:]), reads=[pik], writes=[pfk])
                for (off, tab, tkey) in ((0.5, St, "St"), (0.75, Ct, "Ct")):
                    y, yk = rf_r.next()
                    P.dve(lambda e, y=y, posf=posf, off=off: e.tensor_scalar(out=y[:], in0=posf[:], scalar1=invf[:, 0:1], scalar2=off, op0=ALU.mult, op1=ALU.add),
                          reads=[pfk, "invf"], writes=[yk])
                    ni, nik = ri_r.next()
                    P.dve(lambda e, ni=ni, y=y: e.tensor_copy(out=ni[:], in_=y[:]), reads=[yk], writes=[nik])
                    nf, nfk = rf_r.next()
                    P.dve(lambda e, nf=nf, ni=ni: e.tensor_copy(out=nf[:], in_=ni[:]), reads=[nik], writes=[nfk])
                    P.dve(lambda e, y=y, nf=nf: e.tensor_tensor(out=y[:], in0=y[:], in1=nf[:], op=ALU.subtract), reads=[yk, nfk], writes=[yk])
                    P.dve(lambda e, y=y, nf=nf: e.tensor_single_scalar(out=nf[:], in_=y[:], scalar=0.0, op=ALU.is_lt), reads=[yk], writes=[nfk])
                    P.dve(lambda e, y=y, nf=nf: e.tensor_tensor(out=y[:], in0=y[:], in1=nf[:], op=ALU.add), reads=[yk, nfk], writes=[yk])
                    P.act(lambda e, y=y, tab=tab, csl=csl: e.activation(out=tab[:, csl], in_=y[:], func=AF.Sin, scale=6.2831845, bias=-3.1415922),
                          reads=[yk], writes=[(tkey, ch)])
            P.emit()
            srope.close()
            pools = (Rot(nc, sp_, "xt", [128, D], F32, 2), Rot(nc, sp_, "junk", [128, D], BF16, 1), Rot(nc, sp_, "xn", [128, D], BF16, 2),
                     Rot(nc, sp_, "st", [128, 4], F32, 4), Rot(nc, sp_, "tp", [128, 8, 128], BF16, 1, psum=True),
                     Rot(nc, sp_, "aT", [128, 8, 512], BF16, 2))
            mm_r = Rot(nc, sp_, "mm", [128, 512], F32, 3, psum=True)
            pn_r = Rot(nc, sp_, "pn", [128, 512], F32, 2, psum=True)
            pq_r = Rot(nc, sp_, "pq", [128, 512], F32, 2, psum=True)
            sq_r = Rot(nc, sp_, "sq", [128, 512], BF16, 2)
            rt_r = Rot(nc, sp_, "rt", [128, 512], F32, 2)
            qn_r = Rot(nc, sp_, "qn", [128, 512], BF16, 2)
            t1_r = Rot(nc, sp_, "t1", [128, 512], F32, 2)
            t2_r = Rot(nc, sp_, "t2", [128, 512], F32, 2)
            q1 = []
            q2 = []

            def qk_stage0(nm, coff, dest, dsl, gg, sc, bi, hp, blk, aT, aTkeys, tsl, tch):
                mm, mmk = mm_r.next()
                for kc in range(8):
                    P.pe(lambda e, mm=mm, kc=kc: e.matmul(mm[:], w_a[:, kc, coff + hp * 128:coff + (hp + 1) * 128], aT[:, kc, :],
                                                          start=(kc == 0), stop=(kc == 7)),
                         reads=[("w_a", kc)] + aTkeys, writes=[mmk])
                sq, sqk = sq_r.next()
                P.act(lambda e: e.activation(out=sq[:], in_=mm[:], func=AF.Square), reads=[mmk], writes=[sqk])

                def stage1():
                    pn, pnk = pn_r.next()
                    P.pe(lambda e: e.matmul(pn[:], blockones[:], sq[:], start=True, stop=True), reads=[sqk, "blockones"], writes=[pnk])
                    rt, rtk = rt_r.next()
                    P.act(lambda e: e.activation(out=rt[:], in_=pn[:], func=AF.Sqrt, scale=sc, bias=bi), reads=[pnk], writes=[rtk])
                    P.dve(lambda e: e.reciprocal(out=rt[:], in_=rt[:]), reads=[rtk], writes=[rtk])
                    qn, qnk = qn_r.next()
                    P.dve(lambda e: e.scalar_tensor_tensor(out=qn[:], in0=mm[:], scalar=gg[:, 0:1], in1=rt[:], op0=ALU.mult, op1=ALU.mult),
                          reads=[mmk, rtk, nm == "k" and "gk" or "gq"], writes=[qnk])

                    def stage2():
                        pq, pqk = pq_r.next()
                        P.pe(lambda e: e.matmul(pq[:], pm[:], qn[:], start=True, stop=True), reads=[qnk, "pm"], writes=[pqk])
                        t1, t1k = t1_r.next()
                        P.pool(lambda e: e.tensor_tensor(out=t1[:], in0=qn[:], in1=Ct[:, tsl], op=ALU.mult), reads=[qnk, ("Ct", tch)], writes=[t1k])
                        t2, t2k = t2_r.next()
                        P.dve(lambda e: e.tensor_tensor(out=t2[:], in0=pq[:], in1=St[:, tsl], op=ALU.mult), reads=[pqk, ("St", tch)], writes=[t2k])
                        P.pool(lambda e: e.tensor_tensor(out=dest[:, dsl], in0=t1[:], in1=t2[:], op=ALU.add), reads=[t1k, t2k], writes=[(nm + "T", hp, blk)])
                    return stage2
                return stage1

            def v_stage0(hp, blk, aT, aTkeys, tsl):
                mm, mmk = mm_r.next()
                for kc in range(8):
                    P.pe(lambda e, mm=mm, kc=kc: e.matmul(mm[:], w_a[:, kc, 1024 + hp * 128:1024 + (hp + 1) * 128], aT[:, kc, :],
                                                          start=(kc == 0), stop=(kc == 7)),
                         reads=[("w_a", kc)] + aTkeys, writes=[mmk])
                P.act(lambda e: e.activation(out=VT[hp][:, tsl], in_=mm[:], func=AF.Copy), reads=[mmk], writes=[("VT", hp, blk)])
                return None

            def step(s1):
                if q2:
                    q2.pop(0)()
                if q1:
                    q2.append(q1.pop(0)())
                if s1 is not None:
                    q1.append(s1)

            nxt = a1_block(nc, P, env, pools, 0, gmix)
            for blk in range(8):
                own = blk >= 4
                aT, aTkeys = nxt
                tsl = slice(blk * 512, (blk + 1) * 512)
                tch = blk // 2
                for hp in range(4):
                    step(qk_stage0("k", 512, KT[hp], tsl, gk, 1.0 / 64, EPS, hp, blk, aT, aTkeys, tsl, tch))
                    if own:
                        step(qk_stage0("q", 0, QT[hp], slice((blk - 4) * 512, (blk - 3) * 512), gq, 1.0, 64 * EPS, hp, blk, aT, aTkeys, tsl, tch))
                    v_stage0(hp, blk, aT, aTkeys, tsl)
                    if hp == 1 and blk + 1 < 8:
                        nxt = a1_block(nc, P, env, pools, blk + 1, gmix)
            step(None)
            step(None)
            step(None)
            P.emit()

        if upto == "P":
            return
        with ExitStack() as st_:
            sbt = lambda name, shape, dt: st_.enter_context(nc.sbuf_tensor(U(name), list(shape), dt))
            band = sbt("band", [128, 256], BF16)
            onesb = sbt("onesb", [128, 64], BF16)
            onesc32 = sbt("onesc32", [128, 64], F32)
            onesc = sbt("onesc", [128, 64], BF16)
            P.dma("sp", lambda e: e.dma_start(out=band[:], in_=env["c_band"]), writes=["band"])
            P.dma("sp", lambda e: e.dma_start(out=onesc32[:], in_=env["onesctx"]), writes=["onesc32"])
            P.dve(lambda e: e.tensor_copy(out=onesc[:], in_=onesc32[:]), reads=["onesc32"], writes=["onesc"])
            P.dve(lambda e: e.memset(onesb[:], 1.0), writes=["onesb"])
            Vt_r = Rot(nc, st_, "Vt", [128, 32, 192], BF16, 2)
            acc = sbt("acca", [128, 2, TOK], F32)
            den = sbt("den", [128, TOK], F32)
            E_r = Rot(nc, st_, "E", [128, 256], BF16, 3)
            Pm_r = Rot(nc, st_, "Pm", [128, 256], BF16, 3)
            tpv_r = Rot(nc, st_, "tpv", [128, 8, 128], BF16, 2, psum=True)
            ps_r = Rot(nc, st_, "psS", [128, 512], F32, 3, psum=True)
            po_r = Rot(nc, st_, "psO", [128, 512], F32, 3, psum=True)
            for hp in range(4):
                for bi_, d in enumerate((1, 4, 16)):
                    n_lo = 2048 // (128 * d)
                    n_hi = 4096 // (128 * d) - 1
                    Vt, Vtk = Vt_r.next()
                    tiles = {}
                    for r in range(d):
                        for m in range(n_lo - 1, n_hi + 1):
                            idx = len(tiles)
                            tiles[(r, m)] = idx
                            u0 = r + d * 128 * m
                            ksl = slice(u0, u0 + d * 127 + 1, d)
                            tpv, tpvk = tpv_r.next()
                            P.pe(lambda e, tpv=tpv, hp=hp, ksl=ksl: e.transpose(out=tpv[:, 0, :], in_=VT[hp][:, ksl], identity=ident_bf[:]),
                                 reads=[("VT", hp), "ident_bf"], writes=[tpvk])
                            P.act(lambda e, Vt=Vt, idx=idx, tpv=tpv: e.activation(
                                out=Vt[:, idx, :].rearrange("p (s c) -> p s c", c=64)[:, 0:3:2, :],
                                in_=tpv[:, 0, :].rearrange("p (s c) -> p s c", c=64), func=AF.Copy),
                                reads=[tpvk], writes=[(Vtk, idx)])
                            osrc = onesc if m < n_lo else onesb
                            P.pool(lambda e, Vt=Vt, idx=idx, osrc=osrc: e.tensor_copy(out=Vt[:, idx, 64:128], in_=osrc[:]),
                                   reads=["onesc", "onesb"], writes=[(Vtk, idx, "o")])
                    pend = []
                    LA = 2
                    for hh in range(2):
                        prow = slice(hh * 64, hh * 64 + 64)
                        vsl = slice(hh * 64, hh * 64 + 128)
                        for r in range(d):
                            for n in range(n_lo, n_hi + 1):
                                q0 = r + d * 128 * n - 2048
                                qsl = slice(q0, q0 + d * 127 + 1, d)
                                ps, psk = ps_r.next()
                                for w_, m in enumerate((n - 1, n)):
                                    u0 = r + d * 128 * m
                                    ksl = slice(u0, u0 + d * 127 + 1, d)
                                    P.pe(lambda e, ps=ps, w_=w_, hp=hp, prow=prow, ksl=ksl, qsl=qsl: e.matmul(
                                        ps[:, w_ * 128:(w_ + 1) * 128], KT[hp][prow, ksl], QT[hp][prow, qsl], start=True, stop=True),
                                        reads=[("KT", hp), ("QT", hp)], writes=[psk])
                                E, Ek = E_r.next()
                                P.act(lambda e, E=E, ps=ps: e.activation(out=E[:], in_=ps[:, 0:256], func=AF.Exp), reads=[psk], writes=[Ek])
                                Pm_, Pmk = Pm_r.next()
                                P.pool(lambda e, Pm_=Pm_, E=E: e.tensor_tensor(out=Pm_[:], in0=E[:], in1=band[:], op=ALU.mult), reads=[Ek, "band"], writes=[Pmk])
                                ai = r * (n_hi - n_lo + 1) + (n - n_lo)

                                def pv(Pm_=Pm_, Pmk=Pmk, r=r, n=n, hh=hh, qsl=qsl, vsl=vsl, ai=ai, Vt=Vt, Vtk=Vtk, tiles=tiles, bi_=bi_):
                                    po, pok = po_r.next()
                                    for w_, m in enumerate((n - 1, n)):
                                        idx = tiles[(r, m)]
                                        P.pe(lambda e, po=po, Vt=Vt, idx=idx, vsl=vsl, Pm_=Pm_, w_=w_: e.matmul(
                                            po[:, 0:128], Vt[:, idx, vsl], Pm_[:, w_ * 128:(w_ + 1) * 128], start=(w_ == 0), stop=(w_ == 1)),
                                            reads=[(Vtk, idx), (Vtk, idx, "o"), Pmk], writes=[pok])
                                    if bi_ == 0:
                                        P.dve(lambda e, po=po, hh=hh, qsl=qsl: e.tensor_copy(out=acc[:, hh, qsl], in_=po[:, 0:128]),
                                              reads=[pok], writes=[("acca", hh, 0, ai)])
                                    else:
                                        P.dve(lambda e, po=po, hh=hh, qsl=qsl: e.tensor_tensor(out=acc[:, hh, qsl], in0=po[:, 0:128], in1=acc[:, hh, qsl], op=ALU.add),
                                              reads=[pok] + [("acca", hh, bi_ - 1, i_) for i_ in range(16)], writes=[("acca", hh, bi_, ai)])
                                pend.append(pv)
                                if len(pend) > LA:
                                    pend.pop(0)()
                    while pend:
                        pend.pop(0)()
                for hh in range(2):
                    nrow = slice(hh * 64, hh * 64 + 64)
                    drow = slice(64 - hh * 64, 128 - hh * 64)
                    akeys = [("acca", hh, b_, i_) for b_ in range(3) for i_ in range(16)]
                    P.act(lambda e, hh=hh, nrow=nrow, drow=drow: e.activation(out=den[nrow, :], in_=acc[drow, hh, :], func=AF.Copy),
                          reads=akeys, writes=[("den", hh)])
                    P.dve(lambda e, nrow=nrow: e.reciprocal(out=den[nrow, :], in_=den[nrow, :]), reads=[("den", hh)], writes=[("den", hh)])
                    P.dve(lambda e, hh=hh, hp=hp, nrow=nrow: e.tensor_tensor(out=mixT[nrow, hp, :], in0=acc[nrow, hh, :], in1=den[nrow, :], op=ALU.mult),
                          reads=akeys + [("den", hh)], writes=[("mixT", hp, hh)])
            P.emit()


def run_skewed(n, stages):
    K = len(stages)
    for step in range(n + K - 1):
        for k, f in enumerate(stages):
            t = step - k
            if 0 <= t < n:
                f(t)


def emit_sparse(nc, P, env, smix):
    xo = env["xo"]; out = env["out"]; mixT = env["mixT"]
    ident_bf = env["ident_bf"]; ident_f = env["ident_f"]
    Xd = env["Xd"]; Yd = env["Yd"]
    NT = 16
    with ExitStack() as st:
        sb = lambda name, shape, dt: st.enter_context(nc.sbuf_tensor(U(name), list(shape), dt))
        acc = env["acc"]; gw = env["gw"]; ridx = env["ridx"]; cnt_i = env["cnt_i"]
        with ExitStack() as sw:
            sbw = lambda name, shape, dt: sw.enter_context(nc.sbuf_tensor(U(name), list(shape), dt))
            wo = sbw("wo", [128, 8, D], BF16)
            rw = sbw("rw", [128, 8, NE], F32)
            rb = sbw("rb", [128, NE], F32)
            gffn = sbw("gffn", [128, 8], F32)
            grow = sbw("grow", [128, D], F32)
            tri = sbw("tri", [128, 128], BF16)
            ones_bf = sbw("ones_bf", [128, 128], BF16)
            iota = sbw("iota", [128, NE], F32)
            run = sbw("run", [128, NE], F32)
            xt_r = Rot(nc, sw, "xt", [128, D], F32, 2)
            junk_r = Rot(nc, sw, "junk", [128, D], BF16, 1)
            hn_r = Rot(nc, sw, "hn", [128, D], F32, 3)
            mrow_r = Rot(nc, sw, "mrow", [128, D], BF16, 5)
            m32_r = Rot(nc, sw, "m32", [128, 8, 128], F32, 3)
            sm_r = Rot(nc, sw, "sm", [128, 8], F32, 12)
            lg_r = Rot(nc, sw, "lg", [128, 4, NE], F32, 3)
            selb_r = Rot(nc, sw, "selb", [128, NE], BF16, 3)
            ix_r = Rot(nc, sw, "ix", [128, 8], mybir.dt.uint32, 3)
            ef_r = Rot(nc, sw, "ef", [128, 12], F32, 3)
            pw_r = Rot(nc, sw, "pw", [128, 2, 512], F32, 2, psum=True)
            pt_r = Rot(nc, sw, "pt", [128, 8, 128], F32, 1, psum=True)
            pl_r = Rot(nc, sw, "pl", [128, 512], F32, 2, psum=True)
            for kc in range(8):
                P.dma("pool", lambda e, kc=kc: e.dma_start(out=wo[:, kc, :], in_=env["w_out"][kc * 128:(kc + 1) * 128, :]), writes=[("wo", kc)])
            P.dma("sp", lambda e: e.dma_start(out=rw[:], in_=env["router_w"].rearrange("(kc p) n -> p kc n", p=128)), writes=["rw"])
            P.dma("sp", lambda e: e.dma_start(out=rb[:], in_=env["router_b"].partition_broadcast(128)), writes=["rb"])
            P.dma("sp", lambda e: e.dma_start(out=gffn[:], in_=env["g_ffn"]), writes=["gffn"])
            P.dma("sp", lambda e: e.dma_start(out=grow[:], in_=env["gffn_row"].partition_broadcast(128)), writes=["grow"])
            P.dma("sp", lambda e: e.dma_start(out=tri[:], in_=env["c_tri"]), writes=["tri"])
            P.dma("sp", lambda e: e.dma_start(out=iota[:], in_=env["c_iota"]), writes=["iota"])
            P.dve(lambda e: e.memset(ones_bf[:], 1.0), writes=["ones_bf"])
            P.dve(lambda e: e.memset(run[:], 0.0), writes=["run"])
            C = [dict() for _ in range(NT)]

            def W0(tt):
                c = C[tt]
                xt, xk = xt_r.next()
                P.dma("sp", lambda e: e.dma_start(out=xt[:], in_=xo[tt * 128:(tt + 1) * 128, :]), writes=[xk])
                pw, pwk = pw_r.next()
                for dh in range(2):
                    for kc in range(8):
                        P.pe(lambda e, dh=dh, kc=kc: e.matmul(pw[:, dh, :], mixT[:, kc, tt * 128:(tt + 1) * 128], wo[:, kc, dh * 512:(dh + 1) * 512],
                                                              start=(kc == 0), stop=(kc == 7)), reads=["mixT", ("wo", kc)], writes=[pwk])
                c.update(xt=xt, xk=xk, pw=pw, pwk=pwk)

            def W1(tt):
                c = C[tt]
                xt, xk, pw, pwk = c["xt"], c["xk"], c["pw"], c["pwk"]
                hk = ("acc", tt)
                P.dve(lambda e: e.tensor_tensor(out=acc[:, tt, :], in0=pw[:].rearrange("p a b -> p (a b)"), in1=xt[:], op=ALU.add), reads=[pwk, xk], writes=[hk])
                junk, jk = junk_r.next()
                sm, smk = sm_r.next()
                P.act(lambda e: e.activation(out=junk[:], in_=acc[:, tt, :], func=AF.Square, accum_out=sm[:, 0:1]), reads=[hk], writes=[jk, smk])
                P.act(lambda e: e.activation(out=sm[:, 1:2], in_=sm[:, 0:1], func=AF.Sqrt, scale=1.0 / D, bias=EPS), reads=[smk], writes=[smk])
                P.dve(lambda e: e.reciprocal(out=sm[:, 2:3], in_=sm[:, 1:2]), reads=[smk], writes=[smk])
                hn, hnk = hn_r.next()
                P.dve(lambda e: e.tensor_scalar(out=hn[:], in0=acc[:, tt, :], scalar1=sm[:, 2:3], scalar2=None, op0=ALU.mult), reads=[hk, smk], writes=[hnk])
                mrow, mrk = mrow_r.next()
                P.pool(lambda e: e.tensor_tensor(out=mrow[:], in0=hn[:], in1=grow[:], op=ALU.mult), reads=[hnk, "grow"], writes=[mrk])
                c.update(hn=hn, hnk=hnk, mrow=mrow, mrk=mrk)

            def W2(tt):
                c = C[tt]
                hn, hnk = c["hn"], c["hnk"]
                pt, ptk = pt_r.next()
                for kc in range(8):
                    P.pe(lambda e, kc=kc: e.transpose(out=pt[:, kc, :], in_=hn[:, kc * 128:(kc + 1) * 128], identity=ident_f[:]), reads=[hnk, "ident_f"], writes=[ptk])
                m32, m32k = m32_r.next()
                P.dve(lambda e: e.tensor_tensor(out=m32[:], in0=pt[:], in1=gffn[:].unsqueeze(2).broadcast_to([128, 8, 128]), op=ALU.mult), reads=[ptk, "gffn"], writes=[m32k])
                c.update(m32=m32, m32k=m32k)

            def W3(tt):
                c = C[tt]
                m32, m32k = c["m32"], c["m32k"]
                pl, plk = pl_r.next()
                for kc in range(8):
                    P.pe(lambda e, kc=kc: e.matmul(pl[:, 0:NE], m32[:, kc, :], rw[:, kc, :], start=(kc == 0), stop=(kc == 7)), reads=[m32k, "rw"], writes=[plk])
                lg, lgk = lg_r.next()
                P.dve(lambda e: e.tensor_tensor(out=lg[:, 0, :], in0=pl[:, 0:NE], in1=rb[:], op=ALU.add), reads=[plk, "rb"], writes=[lgk])
                sm2, sm2k = sm_r.next()
                P.dve(lambda e: e.max(out=sm2[:, 0:8], in_=lg[:, 0, :]), reads=[lgk], writes=[sm2k])
                ix, ixk = ix_r.next()
                P.dve(lambda e: e.max_index(out=ix[:], in_max=sm2[:, 0:8], in_values=lg[:, 0, :]), reads=[lgk, sm2k], writes=[ixk])
                ef, efk = ef_r.next()
                P.dve(lambda e: e.tensor_copy(out=ef[:, 0:4], in_=ix[:, 0:4]), reads=[ixk], writes=[efk])
                selb, selk = selb_r.next()
                P.dve(lambda e: e.tensor_scalar(out=selb[:], in0=lg[:, 0, :], scalar1=sm2[:, 3:4], scalar2=None, op0=ALU.is_ge), reads=[lgk, sm2k], writes=[selk])
                sm3, sm3k = sm_r.next()
                P.dve(lambda e: e.tensor_scalar(out=sm3[:, 0:1], in0=sm2[:, 0:1], scalar1=-1.0, scalar2=None, op0=ALU.mult), reads=[sm2k], writes=[sm3k])
                P.act(lambda e: e.activation(out=sm3[:, 4:8], in_=sm2[:, 0:4], func=AF.Exp, bias=sm3[:, 0:1], scale=1.0), reads=[sm2k, sm3k], writes=[sm3k])
                P.dve(lambda e: e.tensor_reduce(out=sm3[:, 1:2], in_=sm3[:, 4:8], axis=AX.X, op=ALU.add), reads=[sm3k], writes=[sm3k])
                P.dve(lambda e: e.reciprocal(out=sm3[:, 2:3], in_=sm3[:, 1:2]), reads=[sm3k], writes=[sm3k])
                P.dve(lambda e: e.tensor_scalar(out=gw[:, tt, :], in0=sm3[:, 4:8], scalar1=sm3[:, 2:3], scalar2=None, op0=ALU.mult), reads=[sm3k], writes=[("gw", tt)])
                c.update(lg=lg, lgk=lgk, ef=ef, efk=efk, selb=selb, selk=selk)

            def W4(tt):
                c = C[tt]
                lg, lgk, ef, efk, selb, selk, mrow, mrk = c["lg"], c["lgk"], c["ef"], c["efk"], c["selb"], c["selk"], c["mrow"], c["mrk"]
                pl2, pl2k = pl_r.next()
                P.pe(lambda e: e.matmul(pl2[:, 0:NE], tri[:], selb[:], start=True, stop=True), reads=[selk, "tri"], writes=[pl2k])
                P.pe(lambda e: e.matmul(pl2[:, NE:2 * NE], ones_bf[:], selb[:], start=True, stop=True), reads=[selk, "ones_bf"], writes=[pl2k])
                P.dve(lambda e: e.tensor_tensor(out=lg[:, 1, :], in0=pl2[:, 0:NE], in1=run[:], op=ALU.add), reads=[pl2k, "run", lgk], writes=[lgk])
                P.dve(lambda e: e.tensor_tensor(out=run[:], in0=pl2[:, NE:2 * NE], in1=run[:], op=ALU.add), reads=[pl2k, "run", lgk], writes=["run"])
                for k in range(4):
                    P.dve(lambda e, k=k: e.scalar_tensor_tensor(out=lg[:, 2, :], in0=iota[:], scalar=ef[:, k:k + 1], in1=lg[:, 1, :],
                                                                 op0=ALU.is_equal, op1=ALU.mult, accum_out=ef[:, 4 + k:5 + k]),
                          reads=[lgk, efk, "iota"], writes=[lgk, efk])
                P.dve(lambda e: e.scalar_tensor_tensor(out=ef[:, 8:12], in0=ef[:, 0:4], scalar=float(TOK), in1=ef[:, 4:8], op0=ALU.mult, op1=ALU.add),
                      reads=[efk], writes=[efk])
                P.dve(lambda e: e.tensor_scalar(out=ridx[:, tt, :], in0=ef[:, 8:12], scalar1=-1.0, scalar2=None, op0=ALU.add), reads=[efk], writes=[("ridx", tt)])
                for k in range(4):
                    P.dma("pool", lambda e, k=k: e.indirect_dma_start(
                        out=Xd[:, :], out_offset=bass.IndirectOffsetOnAxis(ap=ridx[:, tt, k:k + 1], axis=0), in_=mrow[:, :], in_offset=None),
                        reads=[mrk, ("ridx", tt)], writes=[("Xd", tt, k)])

            run_skewed(NT, [W0, W1, W2, W3, W4])
            P.dve(lambda e: e.tensor_copy(out=cnt_i[:], in_=run[0:1, :]), reads=["run"], writes=["cnt_i"])
            zb = sbw("zb", [128, 2 * NE], F32)
            zl = sbw("zl", [128, 2 * NE], F32)
            zf = sbw("zf", [128, 2 * NE], F32)
            zm = sbw("zm", [128, 2 * NE], F32)
            zi = env["zi"]
            P.dma("sp", lambda e: e.dma_start(out=zb[:], in_=env["c_zbase"]), writes=["zb"])
            P.dma("sp", lambda e: e.dma_start(out=zl[:], in_=env["c_zlim"]), writes=["zl"])
            P.dve(lambda e: e.tensor_tensor(out=zf[:].rearrange("p (a b) -> p a b", b=2), in0=zb[:].rearrange("p (a b) -> p a b", b=2),
                                            in1=run[:].unsqueeze(2).broadcast_to([128, NE, 2]), op=ALU.add), reads=["zb", "run"], writes=["zf"])
            P.dve(lambda e: e.tensor_tensor(out=zm[:], in0=zf[:], in1=zl[:], op=ALU.is_ge), reads=["zf", "zl"], writes=["zm"])
            zt = sbw("zt", [128, 2 * NE], F32)
            P.dma("sp", lambda e: e.dma_start(out=zt[:], in_=env["c_ztrash"]), writes=["zt"])
            P.dve(lambda e: e.tensor_tensor(out=zt[:], in0=zt[:], in1=zf[:], op=ALU.subtract), reads=["zt", "zf"], writes=["zt"])
            P.dve(lambda e: e.tensor_tensor(out=zt[:], in0=zt[:], in1=zm[:], op=ALU.mult), reads=["zt", "zm"], writes=["zt"])
            P.dve(lambda e: e.tensor_tensor(out=zf[:], in0=zf[:], in1=zt[:], op=ALU.add), reads=["zt", "zf"], writes=["zf"])
            P.dve(lambda e: e.tensor_copy(out=zi[:], in_=zf[:]), reads=["zf"], writes=["zi"])
            P.emit()
        smix.close()

        with ExitStack() as se:
            sbe = lambda name, shape, dt: se.enter_context(nc.sbuf_tensor(U(name), list(shape), dt))
            wg = [sbe("wg%d" % i, [128, 8, 2 * D], BF16) for i in range(2)]
            wd = [sbe("wd%d" % i, [128, 8, D], BF16) for i in range(2)]
            bgr = sbe("bgr", [33, 2 * D], BF16)
            bdr = sbe("bdr", [33, D], BF16)
            ones_row = sbe("ones_row", [33, 128], BF16)
            xb_r = Rot(nc, se, "xb", [128, D], BF16, 2)
            xT_pre = [sbe("xTp%d" % i, [128, 8, 128], BF16) for i in range(2)]
            xT_r = Rot(nc, se, "xT", [128, 8, 128], BF16, 2)
            g_r = Rot(nc, se, "g", [128, 512], BF16, 2)
            sg_r = Rot(nc, se, "sg", [128, 512], BF16, 2)
            u_r = Rot(nc, se, "u", [128, 512], BF16, 2)
            actb_r = Rot(nc, se, "actb", [128, D], BF16, 1)
            actT_r = Rot(nc, se, "actT", [128, 8, 128], BF16, 1)
            ysb_r = Rot(nc, se, "ysb", [128, D], F32, 1)
            tpx_r = Rot(nc, se, "tpx", [128, 8, 128], BF16, 1, psum=True)
            pg_r = Rot(nc, se, "pgg", [128, 512], F32, 2, psum=True)
            pu_r = Rot(nc, se, "pgu", [128, 512], F32, 2, psum=True)
            tpa_r = Rot(nc, se, "tpa", [128, 8, 128], BF16, 1, psum=True)
            pd_r = Rot(nc, se, "pd", [128, 2, 512], F32, 1, psum=True)
            P.dve(lambda e: e.memset(ones_row[:], 1.0), writes=["ones_row"])
            xd_keys = [("Xd", tt, k) for tt in range(NT) for k in range(4)]
            zrow = sbe("zrow", [128, D], BF16)
            P.dve(lambda e: e.memset(zrow[:], 0.0), writes=["zrow"])

            def zero_tail(e_):
                for c_ in (2 * e_, 2 * e_ + 1):
                    P.dma("pool", lambda e, c_=c_: e.indirect_dma_start(
                        out=Xd[:, :], out_offset=bass.IndirectOffsetOnAxis(ap=env["zi"][:, c_:c_ + 1], axis=0), in_=zrow[:, :], in_offset=None),
                        reads=["zi", "zrow"], writes=[("Xz", e_)])
            P.add("sp", lambda e: e.nop(), reads=xd_keys, writes=["Xall"])
            engs = ("pe", "act", "dve", "sp")

            def load_weights(e_):
                par = e_ % 2
                pp = 32 * par
                for kc in range(8):
                    P.dma("pool", lambda e, kc=kc, par=par, e_=e_: e.dma_start(out=wg[par][:, kc, :], in_=env["w_gu_nat"][e_, kc * 128:(kc + 1) * 128, :]),
                          writes=[("wg", par, kc)], wstream=True)
                for fc in range(8):
                    P.dma("pool", lambda e, fc=fc, par=par, e_=e_: e.dma_start(out=wd[par][:, fc, :], in_=env["w_down"][e_, fc * 128:(fc + 1) * 128, :]),
                          writes=[("wd", par, fc)], wstream=True)
                P.dma("pool", lambda e, pp=pp, e_=e_: e.dma_start(out=bgr[pp:pp + 1, :], in_=env["b_gu_nat"][e_:e_ + 1, :]), writes=[("bgr", par)], wstream=True)
                P.dma("pool", lambda e, pp=pp, e_=e_: e.dma_start(out=bdr[pp:pp + 1, :], in_=env["b_down"][e_:e_ + 1, :]), writes=[("bdr", par)], wstream=True)

            xb_pre = [sbe("xbp%d" % i, [128, D], BF16) for i in range(2)]

            def xload(e_, k, xb, xbk):
                r0 = e_ * TOK + k * 128
                P.dma("sp", lambda e, xb=xb, r0=r0: e.dma_start(out=xb[:], in_=Xd[r0:r0 + 128, :]), reads=[("Xz", e_)], writes=[xbk])

            def xtrans(xb, xbk, xT, xTk):
                tpx, tpxk = tpx_r.next()
                for kc in range(8):
                    P.pe(lambda e, tpx=tpx, xb=xb, kc=kc: e.transpose(out=tpx[:, kc, :], in_=xb[:, kc * 128:(kc + 1) * 128], identity=ident_bf[:]),
                         reads=[xbk, "ident_bf"], writes=[tpxk])
                P.dve(lambda e, xT=xT, tpx=tpx: e.tensor_copy(out=xT[:], in_=tpx[:]), reads=[tpxk], writes=[xTk])

            def xprep(e_, k, xT, xTk):
                xb, xbk = xb_r.next()
                xload(e_, k, xb, xbk)
                xtrans(xb, xbk, xT, xTk)

            zero_tail(0)
            zero_tail(1)
            load_weights(0)
            xload(0, 0, xb_pre[0], ("xbp", 0))
            ykeys = []
            for e_ in range(NE):
                par = e_ % 2
                pp = 32 * par
                if e_ + 1 < NE:
                    load_weights(e_ + 1)
                    if e_ + 2 < NE:
                        zero_tail(e_ + 2)
                    xload(e_ + 1, 0, xb_pre[1 - par], ("xbp", 1 - par))
                xtrans(xb_pre[par], ("xbp", par), xT_pre[par], ("xTp", par))
                for en in engs:
                    P.regload(en, "n", cnt_i[0:1, e_:e_ + 1], reads=["cnt_i"])
                cur = (xT_pre[par], ("xTp", par))
                for k in range(TOK // 128):
                    P.cur_region = ("n", 128 * k)
                    r0 = e_ * TOK + k * 128
                    xT, xTk = cur
                    actb, actbk = actb_r.next()
                    for hf in range(2):
                        pg, pgk = pg_r.next()
                        pu, puk = pu_r.next()
                        for (pp_, ppk, c0) in ((pg, pgk, hf * 512), (pu, puk, D + hf * 512)):
                            for kc in range(8):
                                P.pe(lambda e, pp_=pp_, xT=xT, kc=kc, c0=c0, par=par: e.matmul(pp_[:], xT[:, kc, :], wg[par][:, kc, c0:c0 + 512],
                                                                                             start=(kc == 0), stop=False),
                                     reads=[xTk, ("wg", par, kc)], writes=[ppk])
                            P.pe(lambda e, pp_=pp_, c0=c0, pp=pp: e.matmul(pp_[:], ones_row[pp:pp + 1, :], bgr[pp:pp + 1, c0:c0 + 512], start=False, stop=True),
                                 reads=["ones_row", ("bgr", par)], writes=[ppk])
                        g, gk_ = g_r.next()
                        sg, sgk = sg_r.next()
                        u, uk = u_r.next()
                        P.dve(lambda e, g=g, pg=pg: e.tensor_scalar(out=g[:], in0=pg[:], scalar1=7.0, scalar2=None, op0=ALU.min), reads=[pgk], writes=[gk_])
                        P.act(lambda e, g=g, sg=sg: e.activation(out=sg[:], in_=g[:], func=AF.Sigmoid, scale=1.702), reads=[gk_], writes=[sgk])
                        P.dve(lambda e, u=u, pu=pu: e.tensor_scalar(out=u[:], in0=pu[:], scalar1=7.0, scalar2=-7.0, op0=ALU.min, op1=ALU.max), reads=[puk], writes=[uk])
                        P.dve(lambda e, g=g, sg=sg: e.tensor_tensor(out=g[:], in0=g[:], in1=sg[:], op=ALU.mult), reads=[gk_, sgk], writes=[gk_])
                        P.dve(lambda e, g=g, u=u, actb=actb, hf=hf: e.scalar_tensor_tensor(out=actb[:, hf * 512:(hf + 1) * 512], in0=u[:], scalar=1.0, in1=g[:],
                                                                                          op0=ALU.add, op1=ALU.mult), reads=[gk_, uk], writes=[(actbk, hf)])
                    if k + 1 < TOK // 128:
                        nxt = xT_r.next()
                        xprep(e_, k + 1, nxt[0], nxt[1])
                        cur = nxt
                    tpa, tpak = tpa_r.next()
                    for fc in range(8):
                        P.pe(lambda e, tpa=tpa, actb=actb, fc=fc: e.transpose(out=tpa[:, fc, :], in_=actb[:, fc * 128:(fc + 1) * 128], identity=ident_bf[:]),
                             reads=[(actbk, fc // 4), "ident_bf"], writes=[tpak])
                    actT, actTk = actT_r.next()
                    P.dve(lambda e, actT=actT, tpa=tpa: e.tensor_copy(out=actT[:], in_=tpa[:]), reads=[tpak], writes=[actTk])
                    pd, pdk = pd_r.next()
                    for dh in range(2):
                        for fc in range(8):
                            P.pe(lambda e, pd=pd, actT=actT, fc=fc, dh=dh, par=par: e.matmul(pd[:, dh, :], actT[:, fc, :], wd[par][:, fc, dh * 512:(dh + 1) * 512],
                                                                                            start=(fc == 0), stop=False),
                                 reads=[actTk, ("wd", par, fc)], writes=[pdk])
                        P.pe(lambda e, pd=pd, dh=dh, pp=pp: e.matmul(pd[:, dh, :], ones_row[pp:pp + 1, :], bdr[pp:pp + 1, dh * 512:(dh + 1) * 512], start=False, stop=True),
                             reads=["ones_row", ("bdr", par)], writes=[pdk])
                    ysb, ysbk = ysb_r.next()
                    P.dve(lambda e, ysb=ysb, pd=pd: e.tensor_copy(out=ysb[:], in_=pd[:].rearrange("p a b -> p (a b)")), reads=[pdk], writes=[ysbk])
                    yk = ("Yd", e_, k)
                    P.dma("sp", lambda e, ysb=ysb, r0=r0: e.dma_start(out=Yd[r0:r0 + 128, :], in_=ysb[:]), reads=[ysbk], writes=[yk])
                    ykeys.append(yk)
                    P.cur_region = None
            P.add("pool", lambda e: e.nop(), reads=ykeys, writes=["Yall"])
            P.emit()

        def combine(sf):
            yg_r = Rot(nc, sf, "yg", [128, D], F32, 8)

            def tile(tt):
                for k in range(4):
                    yg, ygk = yg_r.next()
                    P.dma("pool", lambda e, yg=yg, k=k: e.indirect_dma_start(
                        out=yg[:, :], out_offset=None, in_=Yd[:, :], in_offset=bass.IndirectOffsetOnAxis(ap=ridx[:, tt, k:k + 1], axis=0)),
                        reads=["Yall", ("ridx", tt)], writes=[ygk])
                    P.dve(lambda e, yg=yg, k=k: e.scalar_tensor_tensor(out=acc[:, tt, :], in0=yg[:], scalar=gw[:, tt, k:k + 1], in1=acc[:, tt, :],
                                                                       op0=ALU.mult, op1=ALU.add), reads=[ygk, ("gw", tt), ("acc", tt)], writes=[("acc", tt)])
            return tile
        emit_ple(nc, P, env, acc, 0, NT, pre=combine)


def emit_half(nc, P, env, half):
    xo = env["xo"]; out = env["out"]; mixT = env["mixT"]
    ident_bf = env["ident_bf"]; ident_f = env["ident_f"]
    HT = 8
    t0 = half * HT
    with ExitStack() as st:
        sb = lambda name, shape, dt: st.enter_context(nc.sbuf_tensor(U(name), list(shape), dt))
        acc = sb("acc", [128, HT, D], F32)
        mT = sb("mT", [128, 8, 1024], BF16)
        gates = sb("gates", [128, HT, NE], F32)
        gT = sb("gT", [NE, 1024], F32)
        bd = sb("bd", [NE, D], F32)
        bg = sb("bg", [128, NE, 16], F32)
        rb = sb("rb", [128, NE], F32)
        gffn = sb("gffn", [128, 8], F32)
        with ExitStack() as sw:
            sbw = lambda name, shape, dt: sw.enter_context(nc.sbuf_tensor(U(name), list(shape), dt))
            wo = sbw("wo", [128, 8, D], BF16)
            rw = sbw("rw", [128, 8, NE], F32)
            xt_r = Rot(nc, sw, "xt", [128, D], F32, 2)
            junk_r = Rot(nc, sw, "junk", [128, D], BF16, 2)
            hn_r = Rot(nc, sw, "hn", [128, D], F32, 2)
            m32_r = Rot(nc, sw, "m32", [128, 8, 128], F32, 2)
            sm_r = Rot(nc, sw, "sm", [128, 8], F32, 4)
            lg_r = Rot(nc, sw, "lg", [128, 3, NE], F32, 2)
            pw_r = Rot(nc, sw, "pw", [128, 2, 512], F32, 2, psum=True)
            pt_r = Rot(nc, sw, "pt", [128, 8, 128], F32, 1, psum=True)
            pl_r = Rot(nc, sw, "pl", [128, 512], F32, 2, psum=True)
            for kc in range(8):
                P.dma("pool", lambda e, kc=kc: e.dma_start(out=wo[:, kc, :], in_=env["w_out"][kc * 128:(kc + 1) * 128, :]),
                      writes=[("wo", kc)])
            P.dma("sp", lambda e: e.dma_start(out=rw[:], in_=env["router_w"].rearrange("(kc p) n -> p kc n", p=128)), writes=["rw"])
            P.dma("sp", lambda e: e.dma_start(out=rb[:], in_=env["router_b"].partition_broadcast(128)), writes=["rb"])
            P.dma("sp", lambda e: e.dma_start(out=gffn[:], in_=env["g_ffn"]), writes=["gffn"])
            P.dma("sp", lambda e: e.dma_start(out=bd[:], in_=env["b_down"]), writes=["bd"])
            P.dma("sp", lambda e: e.dma_start(out=bg[:], in_=env["bgu"]), writes=["bg"])
            P.dve(lambda e: e.tensor_scalar(out=bg[:, :, 8:16], in0=bg[:, :, 8:16], scalar1=1.0, scalar2=None, op0=ALU.add),
                  reads=["bg"], writes=["bg"])
            for tt in range(HT):
                gt = t0 + tt
                xt, xk = xt_r.next()
                P.dma("sp", lambda e, xt=xt, gt=gt: e.dma_start(out=xt[:], in_=xo[gt * 128:(gt + 1) * 128, :]), writes=[xk])
                pw, pwk = pw_r.next()
                for dh in range(2):
                    for kc in range(8):
                        P.pe(lambda e, pw=pw, dh=dh, kc=kc, gt=gt: e.matmul(
                            pw[:, dh, :], mixT[:, kc, gt * 128:(gt + 1) * 128], wo[:, kc, dh * 512:(dh + 1) * 512],
                            start=(kc == 0), stop=(kc == 7)), reads=["mixT", ("wo", kc)], writes=[pwk])
                hk = ("acc", tt)
                P.dve(lambda e, pw=pw, xt=xt, tt=tt: e.tensor_tensor(
                    out=acc[:, tt, :], in0=pw[:].rearrange("p a b -> p (a b)"), in1=xt[:], op=ALU.add),
                    reads=[pwk, xk], writes=[hk])
                junk, jk = junk_r.next()
                sm, smk = sm_r.next()
                P.act(lambda e, junk=junk, sm=sm, tt=tt: e.activation(out=junk[:], in_=acc[:, tt, :], func=AF.Square, accum_out=sm[:, 0:1]),
                      reads=[hk], writes=[jk, smk])
                P.act(lambda e, sm=sm: e.activation(out=sm[:, 1:2], in_=sm[:, 0:1], func=AF.Sqrt, scale=1.0 / D, bias=EPS),
                      reads=[smk], writes=[smk])
                P.dve(lambda e, sm=sm: e.reciprocal(out=sm[:, 2:3], in_=sm[:, 1:2]), reads=[smk], writes=[smk])
                hn, hnk = hn_r.next()
                P.dve(lambda e, hn=hn, sm=sm, tt=tt: e.tensor_scalar(out=hn[:], in0=acc[:, tt, :], scalar1=sm[:, 2:3], scalar2=None, op0=ALU.mult),
                      reads=[hk, smk], writes=[hnk])
                pt, ptk = pt_r.next()
                for kc in range(8):
                    P.pe(lambda e, pt=pt, hn=hn, kc=kc: e.transpose(out=pt[:, kc, :], in_=hn[:, kc * 128:(kc + 1) * 128], identity=ident_f[:]),
                         reads=[hnk, "ident_f"], writes=[ptk])
                m32, m32k = m32_r.next()
                P.dve(lambda e, pt=pt, m32=m32: e.tensor_tensor(out=m32[:], in0=pt[:], in1=gffn[:].unsqueeze(2).broadcast_to([128, 8, 128]), op=ALU.mult),
                      reads=[ptk, "gffn"], writes=[m32k])
                P.act(lambda e, m32=m32, tt=tt: e.activation(out=mT[:, :, tt * 128:(tt + 1) * 128], in_=m32[:], func=AF.Copy),
                      reads=[m32k], writes=[("mT", tt)])
                pl, plk = pl_r.next()
                for kc in range(8):
                    P.pe(lambda e, pl=pl, m32=m32, kc=kc: e.matmul(pl[:, 0:NE], m32[:, kc, :], rw[:, kc, :], start=(kc == 0), stop=(kc == 7)),
                         reads=[m32k, "rw"], writes=[plk])
                lg, lgk = lg_r.next()
                P.dve(lambda e, pl=pl, lg=lg: e.tensor_tensor(out=lg[:, 0, :], in0=pl[:, 0:NE], in1=rb[:], op=ALU.add),
                      reads=[plk, "rb"], writes=[lgk])
                sm2, sm2k = sm_r.next()
                P.dve(lambda e, lg=lg, sm2=sm2: e.max(out=sm2[:, 0:8], in_=lg[:, 0, :]), reads=[lgk], writes=[sm2k])
                P.dve(lambda e, lg=lg, sm2=sm2: e.tensor_scalar(out=lg[:, 1, :], in0=lg[:, 0, :], scalar1=sm2[:, 3:4], scalar2=None, op0=ALU.is_ge),
                      reads=[lgk, sm2k], writes=[lgk])
                sm3, sm3k = sm_r.next()
                P.dve(lambda e, sm2=sm2, sm3=sm3: e.tensor_scalar(out=sm3[:, 0:1], in0=sm2[:, 0:1], scalar1=-1.0, scalar2=None, op0=ALU.mult),
                      reads=[sm2k], writes=[sm3k])
                P.act(lambda e, lg=lg, sm3=sm3: e.activation(out=lg[:, 2, :], in_=lg[:, 0, :], func=AF.Exp, bias=sm3[:, 0:1], scale=1.0),
                      reads=[lgk, sm3k], writes=[lgk])
                P.dve(lambda e, lg=lg: e.tensor_tensor(out=lg[:, 2, :], in0=lg[:, 2, :], in1=lg[:, 1, :], op=ALU.mult), reads=[lgk], writes=[lgk])
                P.dve(lambda e, lg=lg, sm3=sm3: e.tensor_reduce(out=sm3[:, 1:2], in_=lg[:, 2, :], axis=AX.X, op=ALU.add), reads=[lgk, sm3k], writes=[sm3k])
                P.dve(lambda e, sm3=sm3: e.reciprocal(out=sm3[:, 2:3], in_=sm3[:, 1:2]), reads=[sm3k], writes=[sm3k])
                P.dve(lambda e, lg=lg, sm3=sm3, tt=tt: e.tensor_scalar(out=gates[:, tt, :], in0=lg[:, 2, :], scalar1=sm3[:, 2:3], scalar2=None, op0=ALU.mult),
                      reads=[lgk, sm3k], writes=[("gates", tt)])
                pl2, pl2k = pl_r.next()
                P.pe(lambda e, pl2=pl2, tt=tt: e.transpose(out=pl2[0:NE, 0:128], in_=gates[:, tt, :], identity=ident_f[:]),
                     reads=[("gates", tt), "ident_f"], writes=[pl2k])
                P.act(lambda e, pl2=pl2, tt=tt: e.activation(out=gT[:, tt * 128:(tt + 1) * 128], in_=pl2[0:NE, 0:128], func=AF.Copy),
                      reads=[pl2k], writes=[("gT", tt)])
                pw2, pw2k = pw_r.next()
                for dh in range(2):
                    P.pe(lambda e, pw2=pw2, dh=dh, tt=tt: e.matmul(pw2[:, dh, :], gT[:, tt * 128:(tt + 1) * 128], bd[:, dh * 512:(dh + 1) * 512],
                                                                     start=True, stop=True), reads=[("gT", tt), "bd"], writes=[pw2k])
                P.dve(lambda e, pw2=pw2, tt=tt: e.tensor_tensor(out=acc[:, tt, :], in0=pw2[:].rearrange("p a b -> p (a b)"), in1=acc[:, tt, :], op=ALU.add),
                      reads=[pw2k, hk], writes=[hk])
            P.emit()

        with ExitStack() as se:
            sbe = lambda name, shape, dt: se.enter_context(nc.sbuf_tensor(U(name), list(shape), dt))
            NSLOT = 9
            ring = [sbe("ring%d" % i, [128, 4096], BF16) for i in range(NSLOT)]
            actT = sbe("actT", [128, 8, 1024], BF16)
            g_r = Rot(nc, se, "g", [128, 512], F32, 2)
            sg_r = Rot(nc, se, "sg", [128, 512], F32, 2)
            u_r = Rot(nc, se, "u", [128, 512], F32, 2)
            pg_r = Rot(nc, se, "pg", [128, 512], F32, 2, psum=True)
            pu_r = Rot(nc, se, "pu", [128, 512], F32, 2, psum=True)
            pd_r = Rot(nc, se, "pd", [128, 2, 512], F32, 2, psum=True)
            pieces = []
            for e_ in range(NE):
                for j in range(4):
                    pieces.append(("gu", e_, j))
                for j in range(2):
                    pieces.append(("dn", e_, j))
            state = {"next": 0, "mark": -1}
            last_reader = {}
            finished = set()

            def issue_loads():
                while state["next"] < len(pieces):
                    i = state["next"]
                    if i >= NSLOT:
                        prev = i - NSLOT
                        if prev not in finished or last_reader[prev] > state["mark"]:
                            return
                    kind, e_, j = pieces[i]
                    slot = ring[i % NSLOT]
                    key = ("ring", i % NSLOT)
                    if kind == "gu":
                        src = env["wgu"][e_, j]
                        P.dma("pool", lambda e, slot=slot, src=src: e.dma_start(out=slot[:], in_=src, max_dma_last_dim=8192), writes=[key])
                    else:
                        src = env["w_down"][e_].rearrange("(fc p) d -> p fc d", p=128)[:, 4 * j:4 * j + 4, :]
                        P.dma("pool", lambda e, slot=slot, src=src: e.dma_start(out=slot[:].rearrange("p (fc d) -> p fc d", fc=4), in_=src), writes=[key])
                    state["next"] += 1

            issue_loads()
            for e_ in range(NE):
                base = e_ * 6
                for fc in range(8):
                    j = fc // 2
                    pi = base + j
                    assert pi < state["next"]
                    slot = ring[pi % NSLOT]
                    skey = ("ring", pi % NSLOT)
                    goff = (fc % 2) * 128
                    uoff = 256 + (fc % 2) * 128
                    for tb in range(2):
                        pg, pgk = pg_r.next()
                        pu, puk = pu_r.next()
                        lastop = None
                        for (pp, ppk, off) in ((pg, pgk, goff), (pu, puk, uoff)):
                            for kc in range(8):
                                lastop = P.pe(lambda e, pp=pp, slot=slot, kc=kc, off=off, tb=tb: e.matmul(
                                    pp[:], slot[:, kc * 512 + off: kc * 512 + off + 128], mT[:, kc, tb * 512:(tb + 1) * 512],
                                    start=(kc == 0), stop=(kc == 7)),
                                    reads=[skey] + [("mT", tb * 4 + q) for q in range(4)], writes=[ppk])
                        last_reader[pi] = lastop.idx
                        if fc % 2 == 1 and tb == 1:
                            finished.add(pi)
                        g, gk_ = g_r.next()
                        sg, sgk = sg_r.next()
                        u, uk = u_r.next()
                        P.dve(lambda e, g=g, pg=pg, e_=e_, fc=fc: e.tensor_scalar(out=g[:], in0=pg[:], scalar1=bg[:, e_, fc:fc + 1], scalar2=7.0,
                                                                                    op0=ALU.add, op1=ALU.min), reads=[pgk, "bg"], writes=[gk_])
                        P.act(lambda e, g=g, sg=sg: e.activation(out=sg[:], in_=g[:], func=AF.Sigmoid, scale=1.702), reads=[gk_], writes=[sgk])
                        P.act(lambda e, u=u, pu=pu, e_=e_, fc=fc: e.activation(out=u[:], in_=pu[:], func=AF.Identity, bias=bg[:, e_, 8 + fc:9 + fc], scale=1.0),
                              reads=[puk, "bg"], writes=[uk])
                        P.pool(lambda e, u=u: e.tensor_scalar(out=u[:], in0=u[:], scalar1=8.0, scalar2=-6.0, op0=ALU.min, op1=ALU.max),
                               reads=[uk], writes=[uk])
                        P.pool(lambda e, g=g, sg=sg: e.tensor_tensor(out=g[:], in0=g[:], in1=sg[:], op=ALU.mult), reads=[gk_, sgk], writes=[gk_])
                        P.pool(lambda e, g=g, u=u, fc=fc, tb=tb: e.tensor_tensor(out=actT[:, fc, tb * 512:(tb + 1) * 512], in0=g[:], in1=u[:], op=ALU.mult),
                               reads=[gk_, uk], writes=[("actT", fc, tb)])
                        state["mark"] = lastop.idx
                        issue_loads()
                p0 = base + 4
                for tt in range(HT):
                    pd, pdk = pd_r.next()
                    lastop = None
                    for dh in range(2):
                        for fc in range(8):
                            pj = p0 + fc // 4
                            slot = ring[pj % NSLOT]
                            lastop = P.pe(lambda e, pd=pd, dh=dh, fc=fc, tt=tt, slot=slot: e.matmul(
                                pd[:, dh, :], actT[:, fc, tt * 128:(tt + 1) * 128],
                                slot[:, (fc % 4) * 1024 + dh * 512:(fc % 4) * 1024 + (dh + 1) * 512],
                                start=(fc == 0), stop=(fc == 7)),
                                reads=[("ring", pj % NSLOT), ("actT", fc, tt // 4)], writes=[pdk])
                            last_reader[pj] = lastop.idx
                    P.dve(lambda e, pd=pd, tt=tt, e_=e_: e.scalar_tensor_tensor(
                        out=acc[:, tt, :], in0=pd[:].rearrange("p a b -> p (a b)"), scalar=gates[:, tt, e_:e_ + 1], in1=acc[:, tt, :],
                        op0=ALU.mult, op1=ALU.add), reads=[pdk, ("gates", tt), ("acc", tt)], writes=[("acc", tt)])
                finished.add(p0)
                finished.add(p0 + 1)
            P.emit()

        emit_ple(nc, P, env, acc, t0, HT)


def emit_ple(nc, P, env, acc, t0, HT, pre=None):
    out = env["out"]; ident_bf = env["ident_bf"]
    with ExitStack() as sf:
        pre_tile = pre(sf) if pre is not None else None
        sbf = lambda name, shape, dt: sf.enter_context(nc.sbuf_tensor(U(name), list(shape), dt))
        wg = sbf("wg", [128, 8, D], BF16)
        wp = sbf("wp", [128, 2, D], BF16)
        hb_r = Rot(nc, sf, "hb", [128, D], BF16, 3)
        hT_r = Rot(nc, sf, "hT", [128, 8, 128], BF16, 3)
        pt_r = Rot(nc, sf, "ptile", [128, 256], F32, 3)
        pb_r = Rot(nc, sf, "pb", [128, 256], BF16, 3)
        pT_r = Rot(nc, sf, "pT", [128, 2, 128], BF16, 3)
        sgo_r = Rot(nc, sf, "sgo", [128, D], F32, 2)
        o_r = Rot(nc, sf, "o", [128, D], F32, 2)
        ptp_r = Rot(nc, sf, "ptp", [128, 8, 128], BF16, 2, psum=True)
        ptq_r = Rot(nc, sf, "ptq", [128, 8, 128], BF16, 1, psum=True)
        pgp_r = Rot(nc, sf, "pgp", [128, 2, 512], F32, 1, psum=True)
        ppp_r = Rot(nc, sf, "ppp", [128, 2, 512], F32, 1, psum=True)
        for kc in range(8):
            P.dma("pool", lambda e, kc=kc: e.dma_start(out=wg[:, kc, :], in_=env["ple_gate"][kc * 128:(kc + 1) * 128, :]), writes=[("wg", kc)])
        for kc in range(2):
            P.dma("pool", lambda e, kc=kc: e.dma_start(out=wp[:, kc, :], in_=env["ple_proj"][kc * 128:(kc + 1) * 128, :]), writes=[("wp", kc)])
        C = [dict() for _ in range(HT)]

        def F0(tt):
            c = C[tt]
            if pre_tile is not None:
                pre_tile(tt)
            ptile, ptk = pt_r.next()
            P.dma("sp", lambda e: e.dma_start(out=ptile[:], in_=env["p_own"][(t0 + tt) * 128:(t0 + tt + 1) * 128, :]), writes=[ptk])
            c.update(ptile=ptile, ptk=ptk)

        def F1(tt):
            c = C[tt]
            hb, hbk = hb_r.next()
            P.act(lambda e: e.activation(out=hb[:], in_=acc[:, tt, :], func=AF.Copy), reads=[("acc", tt)], writes=[hbk])
            pb, pbk = pb_r.next()
            P.act(lambda e: e.activation(out=pb[:], in_=c["ptile"][:], func=AF.Copy), reads=[c["ptk"]], writes=[pbk])
            c.update(hb=hb, hbk=hbk, pb=pb, pbk=pbk)

        def F2(tt):
            c = C[tt]
            hb, hbk, pb, pbk = c["hb"], c["hbk"], c["pb"], c["pbk"]
            ptp, ptpk = ptp_r.next()
            for kc in range(8):
                P.pe(lambda e, kc=kc: e.transpose(out=ptp[:, kc, :], in_=hb[:, kc * 128:(kc + 1) * 128], identity=ident_bf[:]), reads=[hbk, "ident_bf"], writes=[ptpk])
            ptq, ptqk = ptq_r.next()
            for kc in range(2):
                P.pe(lambda e, kc=kc: e.transpose(out=ptq[:, kc, :], in_=pb[:, kc * 128:(kc + 1) * 128], identity=ident_bf[:]), reads=[pbk, "ident_bf"], writes=[ptqk])
            hT, hTk = hT_r.next()
            P.dve(lambda e: e.tensor_copy(out=hT[:], in_=ptp[:]), reads=[ptpk], writes=[hTk])
            pT, pTk = pT_r.next()
            P.dve(lambda e: e.tensor_copy(out=pT[:], in_=ptq[:, 0:2, :]), reads=[ptqk], writes=[pTk])
            c.update(hT=hT, hTk=hTk, pT=pT, pTk=pTk)

        def F3(tt):
            c = C[tt]
            hT, hTk, pT, pTk = c["hT"], c["hTk"], c["pT"], c["pTk"]
            pgp, pgpk = pgp_r.next()
            ppp, pppk = ppp_r.next()
            for dh in range(2):
                for kc in range(8):
                    P.pe(lambda e, kc=kc, dh=dh: e.matmul(pgp[:, dh, :], hT[:, kc, :], wg[:, kc, dh * 512:(dh + 1) * 512], start=(kc == 0), stop=(kc == 7)),
                         reads=[hTk, ("wg", kc)], writes=[pgpk])
                for kc in range(2):
                    P.pe(lambda e, kc=kc, dh=dh: e.matmul(ppp[:, dh, :], pT[:, kc, :], wp[:, kc, dh * 512:(dh + 1) * 512], start=(kc == 0), stop=(kc == 1)),
                         reads=[pTk, ("wp", kc)], writes=[pppk])
            sgo, sgok = sgo_r.next()
            P.act(lambda e: e.activation(out=sgo[:], in_=pgp[:].rearrange("p a b -> p (a b)"), func=AF.Sigmoid), reads=[pgpk], writes=[sgok])
            P.dve(lambda e: e.tensor_tensor(out=sgo[:], in0=ppp[:].rearrange("p a b -> p (a b)"), in1=sgo[:], op=ALU.mult), reads=[pppk, sgok], writes=[sgok])
            o, ok = o_r.next()
            P.pool(lambda e: e.tensor_tensor(out=o[:], in0=sgo[:], in1=acc[:, tt, :], op=ALU.add), reads=[sgok, ("acc", tt)], writes=[ok])
            P.dma("sp", lambda e: e.dma_start(out=out[(t0 + tt) * 128:(t0 + tt + 1) * 128, :], in_=o[:]), reads=[ok], writes=[("out", t0 + tt)])

        run_skewed(HT, [F0, F1, F2, F3])
        P.add("sp", lambda e: e.nop(), reads=[("out", t0 + tt) for tt in range(HT)])
        P.emit()


def _consts():
    c = {}
    c["c_ident_bf"] = np.eye(128, dtype=np.float32).astype(ml_dtypes.bfloat16)
    c["c_ident_f"] = np.eye(128, dtype=np.float32)
    k = np.arange(128)[:, None]
    q = np.arange(128)[None, :]
    band = np.concatenate([(q <= k), (q >= k)], axis=1).astype(np.float32)
    c["c_band"] = band.astype(ml_dtypes.bfloat16)
    s = np.arange(64)[:, None]
    t = np.arange(64)[None, :]
    hm = (s <= t).astype(np.float32)
    c["c_hmask"] = np.concatenate([hm, hm], axis=0).astype(np.float32)
    bo = np.zeros((128, 128), np.float32)
    bo[:64, :64] = 1
    bo[64:, 64:] = 1
    c["c_blockones"] = bo.astype(ml_dtypes.bfloat16)
    c["c_ones_f"] = np.ones((128, 128), np.float32)
    pm = np.zeros((128, 128), np.float32)
    for hh in range(2):
        for m in range(8):
            pm[hh * 64 + m + 8, hh * 64 + m] = -1.0
            pm[hh * 64 + m, hh * 64 + m + 8] = 1.0
    c["c_pm"] = pm.astype(ml_dtypes.bfloat16)
    invf = np.zeros((128, 1), np.float64)
    for p in range(128):
        cc = p % 64
        if cc < 16:
            invf[p, 0] = (500000.0 ** (-(cc % 8) / 8.0)) / (2 * math.pi)
    c["c_invf"] = invf.astype(np.float32)
    rs = np.ones((128, 512), np.float32)
    rs[:, 0::64] = 0
    c["c_reset"] = rs
    c["c_tri"] = (np.arange(128)[:, None] <= np.arange(128)[None, :]).astype(np.float32).astype(ml_dtypes.bfloat16)
    c["c_iota"] = np.tile(np.arange(NE, dtype=np.float32)[None, :], (128, 1))
    p = np.arange(128, dtype=np.float32)[:, None, None]
    e_ = np.arange(NE, dtype=np.float32)[None, :, None]
    j2 = np.arange(2, dtype=np.float32)[None, None, :]
    c["c_zbase"] = np.ascontiguousarray((e_ * TOK + j2 * 128 + p).reshape(128, 2 * NE).astype(np.float32))
    c["c_ztrash"] = np.ascontiguousarray(np.broadcast_to(XROWS + p, (128, NE, 2)).reshape(128, 2 * NE).astype(np.float32))
    c["c_zlim"] = np.ascontiguousarray(np.broadcast_to((e_ + 1) * TOK, (128, NE, 2)).reshape(128, 2 * NE).astype(np.float32))
    return c


def _fm(v, n):
    return np.ascontiguousarray(np.asarray(v, np.float32).reshape(n, 128).T)


def make_in_maps(inp, dbg=None, extra=None):
    x = np.asarray(inp["x"], np.float32)
    p = np.asarray(inp["p"], np.float32)[0]
    positions = np.asarray(inp["positions"]).astype(np.int32)
    consts = _consts()
    shared = dict(consts)
    shared["g_mix"] = _fm(inp["mix_norm_g"][0], 8)
    shared["w_in"] = np.ascontiguousarray(inp["w_in"][0], np.float32)
    shared["gq"] = np.tile(np.asarray(inp["q_norm_g"][0], np.float32), 2).reshape(128, 1)
    shared["gk"] = np.tile(np.asarray(inp["k_norm_g"][0], np.float32), 2).reshape(128, 1)
    shared["lb0"] = _fm(inp["hgrn_lb_logits"][0], 4)
    shared["lb1"] = _fm(inp["hgrn_lb_logits"][1], 4)
    shared["gnorm"] = np.asarray(inp["hgrn_norm_g"][0], np.float32).reshape(128, 1)
    shared["w_out"] = np.ascontiguousarray(inp["w_out"][0], np.float32)
    shared["g_ffn"] = _fm(inp["ffn_norm_g"][0], 8)
    shared["router_w"] = np.ascontiguousarray(inp["router_w"][0], np.float32)
    shared["router_b"] = np.asarray(inp["router_b"][0], np.float32).reshape(1, NE)
    shared["gffn_row"] = np.asarray(inp["ffn_norm_g"][0], np.float32).reshape(1, D)
    if SPARSE:
        shared["w_gu_nat"] = np.ascontiguousarray(inp["expert_w_gate_up"][0], np.float32)
        shared["b_gu_nat"] = np.ascontiguousarray(inp["expert_b_gate_up"][0], np.float32)
    wg = np.asarray(inp["expert_w_gate_up"][0], np.float32)
    wg5 = wg.reshape(NE, 8, 128, 2, 4, 256)
    if not SPARSE:
        shared["wgu"] = np.ascontiguousarray(wg5.transpose(0, 4, 2, 1, 3, 5)).reshape(NE, 4, 128, 4096)
        bgu = np.asarray(inp["expert_b_gate_up"][0], np.float32)
        shared["bgu"] = np.ascontiguousarray(bgu.reshape(NE, 16, 128).transpose(2, 0, 1))
    shared["w_down"] = np.ascontiguousarray(inp["expert_w_down"][0], np.float32)
    shared["b_down"] = np.ascontiguousarray(inp["expert_b_down"][0], np.float32)
    shared["ple_proj"] = np.ascontiguousarray(inp["ple_proj"][0], np.float32)
    shared["ple_gate"] = np.ascontiguousarray(inp["ple_gate"][0], np.float32)
    maps = []
    for c in range(NCORES):
        b, h = divmod(c, 2)
        m = dict(shared)
        m["xo"] = np.ascontiguousarray(x[b, h * TOK:(h + 1) * TOK])
        m["p_own"] = np.ascontiguousarray(p[b, h * TOK:(h + 1) * TOK])
        if h == 1:
            m["xc"] = np.ascontiguousarray(x[b, 0:TOK])
            pc = positions[b, 0:TOK]
            m["onesctx"] = np.ones((128, 64), np.float32)
        else:
            m["xc"] = np.zeros((TOK, D), np.float32)
            pc = np.zeros((TOK,), np.int32)
            m["onesctx"] = np.zeros((128, 64), np.float32)
        m["pos"] = np.concatenate([pc, positions[b, h * TOK:(h + 1) * TOK]]).reshape(1, 4096).astype(np.int32)
        if extra is not None:
            m.update(extra(c))
        maps.append(m)
    return maps


_NC_CACHE = {}


def kernel(**inputs):
    if "nc" not in _NC_CACHE:
        _NC_CACHE["nc"] = build()
    nc = _NC_CACHE["nc"]
    maps = make_in_maps(inputs)
    res = run_bass_kernel_spmd(nc, maps, core_ids=list(range(NCORES)))
    outp = np.zeros((4, 4096, D), np.float32)
    for c in range(NCORES):
        b, h = divmod(c, 2)
        outp[b, h * TOK:(h + 1) * TOK] = res.results[c]["out"]
    return outp
```
